# Optimizing a Trainium2 kernel written in Bass

```python
import math
import jax, jax.numpy as jnp
from jax import lax
import numpy as np

D_MODEL = 1024
BATCH = 4
SEQ = 4096
DEPTH = 2

GRID_W = 64
CTX_LEN = 256
HEAD_DIM = 64
N_EVEN = (DEPTH + 1) // 2
N_ODD = DEPTH // 2
ROPE_THETA = 10000.0
EPS = 1e-6
ATTN_SCALE = HEAD_DIM ** -0.5

A_HEADS = 8
A_KV_HEADS = 2
A_WINDOW = 128
A_BLOCK = 128
B_HEADS = 8
B_WIN_H = 8
B_WIN_W = 16
C_HEADS = 8
C_KV_HEADS = 2
C_BLOCK = 128
D_CH = 512
HY_ORDER = 2
HY_SHORT = 3
HY_BANDS = 16
HY_EMB = 1 + 2 * HY_BANDS
HY_FFN = 64
HY_MAX_DECAY = math.log(1e-2) / 0.3
HY_MIN_DECAY = math.log(1e-2) / 1.5

A_Q = A_HEADS * HEAD_DIM
A_KV = A_KV_HEADS * HEAD_DIM
B_W = B_HEADS * HEAD_DIM
C_Q = C_HEADS * HEAD_DIM
C_KV = C_KV_HEADS * HEAD_DIM
EVEN_SPLIT = (A_Q, A_KV, A_KV, B_W, B_W, B_W)
ODD_SPLIT = (C_Q, C_KV, C_KV, 3 * D_CH)
EVEN_IN = A_Q + 2 * A_KV + 3 * B_W
ODD_IN = C_Q + 2 * C_KV + 3 * D_CH
EVEN_OUT = A_Q + B_W
ODD_OUT = C_Q + D_CH

N_EXPERTS = 16
N_GROUPS = 4
EXPERTS_PER_GROUP = N_EXPERTS // N_GROUPS
TOP_K = 2
D_EXPERT = 512

kernel_name = 'hybrid_flow_backbone_block'

F32 = jnp.float32


def rmsnorm(x, g):
    xf = x.astype(F32)
    y = xf * lax.rsqrt(jnp.mean(xf * xf, axis=-1, keepdims=True) + EPS)
    return (y * g.astype(F32)).astype(x.dtype)


def split_cols(p, sizes):
    return jnp.split(p, [int(v) for v in np.cumsum(sizes)[:-1]], axis=-1)


def heads(t, n):
    return t.reshape(t.shape[0], t.shape[1], n, HEAD_DIM)


def joint_softmax(parts):
    sizes = [p.shape[-1] for p in parts]
    probs = jax.nn.softmax(jnp.concatenate(parts, axis=-1), axis=-1)
    return jnp.split(probs, [int(v) for v in np.cumsum(sizes)[:-1]], axis=-1)


def rope_tables(S):
    t = jnp.arange(S)
    row = (t // GRID_W).astype(F32)
    col = (t % GRID_W).astype(F32)
    half = HEAD_DIM // 2
    inv = ROPE_THETA ** (-jnp.arange(0, half, 2, dtype=F32) / half)
    ar = row[:, None] * inv[None]
    ac = col[:, None] * inv[None]
    return (jnp.cos(ar)[:, None, :], jnp.sin(ar)[:, None, :], jnp.cos(ac)[:, None, :], jnp.sin(ac)[:, None, :])


def _rot(x, cos, sin):
    m = x.shape[-1] // 2
    x1, x2 = x[..., :m], x[..., m:]
    return jnp.concatenate([x1 * cos - x2 * sin, x2 * cos + x1 * sin], axis=-1)


def rope2d(x, rope):
    cr, sr, cc, sc = rope
    xf = x.astype(F32)
    half = HEAD_DIM // 2
    return jnp.concatenate([_rot(xf[..., :half], cr, sr), _rot(xf[..., half:], cc, sc)], axis=-1).astype(x.dtype)


def ctx_self_attention(q, k, v, sink=None):
    B, L, H, d = q.shape
    HKV = k.shape[2]
    G = H // HKV
    qg = q.reshape(B, L, HKV, G, d)
    s = jnp.einsum('bqkgd,bjkd->bkgqj', qg, k).astype(F32) * ATTN_SCALE
    parts = [s]
    if sink is not None:
        parts.append(jnp.broadcast_to(sink.astype(F32).reshape(1, HKV, G, 1, 1), s.shape[:-1] + (1,)))
    p = joint_softmax(parts)[0]
    o = jnp.einsum('bkgqj,bjkd->bqkgd', p.astype(v.dtype), v)
    return o.reshape(B, L, H * d)


def window_attention(q, k, v, kc, vc, sink):
    B, S, H, d = q.shape
    HKV = k.shape[2]
    G = H // HKV
    nb = S // A_BLOCK
    n_side = -(-A_WINDOW // A_BLOCK)
    pad = n_side * A_BLOCK
    nw = 2 * n_side + 1
    qb = q.reshape(B, nb, A_BLOCK, HKV, G, d)
    kp = jnp.pad(k, ((0, 0), (pad, pad), (0, 0), (0, 0))).reshape(B, nb + 2 * n_side, A_BLOCK, HKV, d)
    vp = jnp.pad(v, ((0, 0), (pad, pad), (0, 0), (0, 0))).reshape(B, nb + 2 * n_side, A_BLOCK, HKV, d)
    kw = jnp.concatenate([kp[:, i:i + nb] for i in range(nw)], axis=2)
    vw = jnp.concatenate([vp[:, i:i + nb] for i in range(nw)], axis=2)
    qpos = (jnp.arange(nb) * A_BLOCK)[:, None] + jnp.arange(A_BLOCK)[None]
    kpos = (jnp.arange(nb) * A_BLOCK - pad)[:, None] + jnp.arange(nw * A_BLOCK)[None]
    mask = ((jnp.abs(qpos[:, :, None] - kpos[:, None, :]) <= A_WINDOW)
            & (kpos[:, None, :] >= 0) & (kpos[:, None, :] < S))
    s_loc = jnp.einsum('bnqkgd,bnjkd->bnkgqj', qb, kw).astype(F32) * ATTN_SCALE
    s_loc = jnp.where(mask[None, :, None, None], s_loc, -jnp.inf)
    s_ctx = jnp.einsum('bnqkgd,bjkd->bnkgqj', qb, kc).astype(F32) * ATTN_SCALE
    s_sink = jnp.broadcast_to(sink.astype(F32).reshape(1, 1, HKV, G, 1, 1), s_loc.shape[:-1] + (1,))
    p_loc, p_ctx, _ = joint_softmax([s_loc, s_ctx, s_sink])
    o = (jnp.einsum('bnkgqj,bnjkd->bnqkgd', p_loc.astype(v.dtype), vw)
         + jnp.einsum('bnkgqj,bjkd->bnqkgd', p_ctx.astype(vc.dtype), vc))
    return o.reshape(B, S, H * d)


def neighbourhood_attention(q, k, v, kc, vc, rpb):
    B, S, H, d = q.shape
    R = S // GRID_W
    kh = min(B_WIN_H, R)
    qg = q.reshape(B, R, GRID_W, H, d)
    kg = k.reshape(B, R, GRID_W, H, d)
    vg = v.reshape(B, R, GRID_W, H, d)
    r = jnp.arange(R)
    rs = jnp.clip(r - kh // 2, 0, R - kh)
    row_idx = rs[:, None] + jnp.arange(kh)[None]
    k_rows = kg[:, row_idx]
    v_rows = vg[:, row_idx]
    col = jnp.arange(GRID_W)
    cs = jnp.clip(col - B_WIN_W // 2, 0, GRID_W - B_WIN_W)
    colmask = (col[None, :] >= cs[:, None]) & (col[None, :] < cs[:, None] + B_WIN_W)
    dr = row_idx - r[:, None]
    dc = jnp.clip(col[None, :] - col[:, None], -(B_WIN_W - 1), B_WIN_W - 1)
    bias = rpb[:, dr[:, :, None, None] + B_WIN_H - 1, dc[None, None] + B_WIN_W - 1]
    bias = bias.transpose(1, 0, 3, 2, 4).astype(F32)
    s = jnp.einsum('brqhd,brkwhd->brhqkw', qg, k_rows).astype(F32) * ATTN_SCALE + bias[None]
    s = jnp.where(colmask[:, None, :], s, -jnp.inf).reshape(B, R, H, GRID_W, kh * GRID_W)
    s_ctx = jnp.einsum('brqhd,bjhd->brhqj', qg, kc).astype(F32) * ATTN_SCALE
    p_loc, p_ctx = joint_softmax([s, s_ctx])
    p_loc = p_loc.reshape(B, R, H, GRID_W, kh, GRID_W)
    o = (jnp.einsum('brhqkw,brkwhd->brqhd', p_loc.astype(v.dtype), v_rows)
         + jnp.einsum('brhqj,bjhd->brqhd', p_ctx.astype(vc.dtype), vc))
    return o.reshape(B, S, H * d)


def block_attention(q, k, v):
    B, S, H, d = q.shape
    HKV = k.shape[2]
    G = H // HKV
    nb = S // C_BLOCK
    qb = q.reshape(B, nb, C_BLOCK, HKV, G, d).transpose(1, 0, 2, 3, 4, 5)

    def one(qi):
        s = jnp.einsum('bqkgd,bjkd->bkgqj', qi, k).astype(F32) * ATTN_SCALE
        p = jax.nn.softmax(s, axis=-1).astype(v.dtype)
        return jnp.einsum('bkgqj,bjkd->bqkgd', p, v)

    o = lax.map(one, qb)
    return o.transpose(1, 0, 2, 3, 4, 5).reshape(B, S, H * d)


def short_conv(u, w, b):
    K, C = w.shape
    pad = K // 2
    y = lax.conv_general_dilated(u, w[:, None, :].astype(u.dtype), window_strides=(1,),
                                 padding=[(pad, K - 1 - pad)], dimension_numbers=('NWC', 'WIO', 'NWC'),
                                 feature_group_count=C)
    return y + b


def implicit_filters(L, w1, b1, f1, w2, b2, f2, w3, b3):
    t = jnp.arange(L, dtype=F32)
    tn = t / max(L - 1, 1)
    bands = jnp.linspace(1e-4, HY_BANDS - 1, HY_BANDS, dtype=F32)
    ang = 2.0 * math.pi * t[:, None] * bands[None] / L
    feats = jnp.concatenate([tn[:, None], jnp.cos(ang), jnp.sin(ang)], axis=-1)
    h = jnp.sin(f1 * (feats @ w1 + b1))
    h = jnp.sin(f2 * (h @ w2 + b2))
    h = (h @ w3 + b3).astype(F32).reshape(L, 2, HY_ORDER, D_CH)
    deltas = jnp.abs(jnp.linspace(HY_MIN_DECAY, HY_MAX_DECAY, D_CH, dtype=F32))
    decay = jnp.exp(-tn[:, None] * deltas[None])
    h = h * decay[:, None, None, :]
    return h / (jnp.sum(jnp.abs(h), axis=(0, 1), keepdims=True) + EPS)


def long_conv(z, hf, hb):
    L, C = hf.shape
    kf = jnp.concatenate([hf, jnp.zeros((1, C), F32), hb[1:][::-1]], axis=0)
    Z = jnp.fft.rfft(z.astype(F32), n=2 * L, axis=1)
    Kf = jnp.fft.rfft(kf, axis=0)
    y = jnp.fft.irfft(Z * Kf[None], n=2 * L, axis=1)[:, :L]
    return y.astype(z.dtype)


def hyena(u, sw, sb, w1, b1, f1, w2, b2, f2, w3, b3, hbias):
    L = u.shape[1]
    u = short_conv(u, sw, sb)
    v, g1, g2 = split_cols(u, (D_CH, D_CH, D_CH))
    h = implicit_filters(L, w1, b1, f1, w2, b2, f2, w3, b3)
    z = v
    for o, g in enumerate((g1, g2)):
        z = g * (long_conv(z, h[:, 0, o], h[:, 1, o]) + z * hbias[o])
    return z


def even_mixer(hx, hc, w_in, w_out, sink, rpb, rope, need_ctx):
    aq, ak, av, bq, bk, bv = split_cols(hx @ w_in, EVEN_SPLIT)
    caq, cak, cav, cbq, cbk, cbv = split_cols(hc @ w_in, EVEN_SPLIT)
    aq, ak, av = heads(aq, A_HEADS), heads(ak, A_KV_HEADS), heads(av, A_KV_HEADS)
    bq, bk, bv = heads(bq, B_HEADS), heads(bk, B_HEADS), heads(bv, B_HEADS)
    caq, cak, cav = heads(caq, A_HEADS), heads(cak, A_KV_HEADS), heads(cav, A_KV_HEADS)
    cbq, cbk, cbv = heads(cbq, B_HEADS), heads(cbk, B_HEADS), heads(cbv, B_HEADS)
    ya = window_attention(rope2d(aq, rope), rope2d(ak, rope), av, cak, cav, sink)
    yb = neighbourhood_attention(bq, bk, bv, cbk, cbv, rpb)
    yx = jnp.concatenate([ya, yb], axis=-1) @ w_out
    if not need_ctx:
        return yx, None
    yc = jnp.concatenate([ctx_self_attention(caq, cak, cav, sink), ctx_self_attention(cbq, cbk, cbv)], axis=-1) @ w_out
    return yx, yc


def odd_mixer(hx, hc, w_in, w_out, qn, kn, hy, rope, need_ctx):
    qx, kx, vx, ux = split_cols(hx @ w_in, ODD_SPLIT)
    qc, kc, vc, uc = split_cols(hc @ w_in, ODD_SPLIT)
    qx = rope2d(rmsnorm(heads(qx, C_HEADS), qn), rope)
    kx = rope2d(rmsnorm(heads(kx, C_KV_HEADS), kn), rope)
    vx = heads(vx, C_KV_HEADS)
    qc = rmsnorm(heads(qc, C_HEADS), qn)
    kc = rmsnorm(heads(kc, C_KV_HEADS), kn)
    vc = heads(vc, C_KV_HEADS)
    y_attn = block_attention(qx, jnp.concatenate([kc, kx], axis=1), jnp.concatenate([vc, vx], axis=1))
    y_hy = hyena(ux, *hy)
    yx = jnp.concatenate([y_attn, y_hy], axis=-1) @ w_out
    if not need_ctx:
        return yx, None
    yc = jnp.concatenate([ctx_self_attention(qc, kc, vc), hyena(uc, *hy)], axis=-1) @ w_out
    return yx, yc


def moe(h, w_router, b_router, wg, wu, wd):
    Bsz, L, D = h.shape
    t = h.reshape(Bsz * L, D)
    T = t.shape[0]
    s = jax.nn.sigmoid((t @ w_router).astype(F32))
    sel = s + b_router.astype(F32)
    gscore = lax.top_k(sel.reshape(T, N_GROUPS, EXPERTS_PER_GROUP), TOP_K)[0].sum(-1)
    gbest = jnp.argmax(gscore, axis=-1)
    in_group = (jnp.arange(N_EXPERTS) // EXPERTS_PER_GROUP)[None, :] == gbest[:, None]
    _, idx = lax.top_k(jnp.where(in_group, sel, -jnp.inf), TOP_K)
    wsel = jnp.take_along_axis(s, idx, axis=-1)
    wsel = wsel / jnp.sum(wsel, axis=-1, keepdims=True)
    combine = jnp.sum(jax.nn.one_hot(idx, N_EXPERTS, dtype=F32) * wsel[..., None], axis=1).astype(h.dtype)
    y = jnp.zeros_like(t)
    for e in range(N_EXPERTS):
        he = jax.nn.silu(t @ wg[e]) * (t @ wu[e])
        y = y + combine[:, e:e + 1] * (he @ wd[e])
    return y.reshape(Bsz, L, D)


def setup_inputs(seed: int = 0) -> dict:
    key = jax.random.key(seed)
    ks = iter(jax.random.split(key, 40))

    def nrm(shape, scale):
        return jax.random.normal(next(ks), shape, F32) * scale

    D = D_MODEL
    return {
        'x': nrm((BATCH, SEQ, D), 1.0),
        'c': nrm((BATCH, D), 1.0),
        'ctx': nrm((BATCH, CTX_LEN, D), 1.0),
        'c_ctx': nrm((D,), 1.0),
        'w_ada': nrm((DEPTH, D, 6 * D), 0.5 * D ** -0.5),
        'b_ada': nrm((DEPTH, 6 * D), 0.02),
        'norm_g': 1.0 + nrm((DEPTH, 2, D), 0.02),
        'final_g': 1.0 + nrm((D,), 0.02),
        'w_in_even': nrm((N_EVEN, D, EVEN_IN), D ** -0.5),
        'w_out_even': nrm((N_EVEN, EVEN_OUT, D), EVEN_OUT ** -0.5),
        'a_sink': nrm((N_EVEN, A_HEADS), 0.5),
        'b_rpb': nrm((N_EVEN, B_HEADS, 2 * B_WIN_H - 1, 2 * B_WIN_W - 1), 0.1),
        'w_in_odd': nrm((N_ODD, D, ODD_IN), D ** -0.5),
        'w_out_odd': nrm((N_ODD, ODD_OUT, D), ODD_OUT ** -0.5),
        'c_qnorm': 1.0 + nrm((N_ODD, HEAD_DIM), 0.02),
        'c_knorm': 1.0 + nrm((N_ODD, HEAD_DIM), 0.02),
        'hy_short_w': nrm((N_ODD, HY_SHORT, 3 * D_CH), HY_SHORT ** -0.5),
        'hy_short_b': nrm((N_ODD, 3 * D_CH), 0.02),
        'hy_w1': nrm((N_ODD, HY_EMB, HY_FFN), HY_EMB ** -0.5),
        'hy_b1': nrm((N_ODD, HY_FFN), 0.02),
        'hy_f1': 1.0 + nrm((N_ODD, HY_FFN), 0.1),
        'hy_w2': nrm((N_ODD, HY_FFN, HY_FFN), HY_FFN ** -0.5),
        'hy_b2': nrm((N_ODD, HY_FFN), 0.02),
        'hy_f2': 1.0 + nrm((N_ODD, HY_FFN), 0.1),
        'hy_w3': nrm((N_ODD, HY_FFN, 2 * HY_ORDER * D_CH), HY_FFN ** -0.5),
        'hy_b3': nrm((N_ODD, 2 * HY_ORDER * D_CH), 0.02),
        'hy_bias': nrm((N_ODD, HY_ORDER, D_CH), 0.5),
        'w_router': nrm((D, N_EXPERTS), D ** -0.5),
        'b_router': nrm((N_EXPERTS,), 0.01),
        'moe_wg': nrm((DEPTH, N_EXPERTS, D, D_EXPERT), D ** -0.5),
        'moe_wu': nrm((DEPTH, N_EXPERTS, D, D_EXPERT), D ** -0.5),
        'moe_wd': nrm((DEPTH, N_EXPERTS, D_EXPERT, D), D_EXPERT ** -0.5),
    }


def reference(x, c, ctx, c_ctx, w_ada, b_ada, norm_g, final_g,
              w_in_even, w_out_even, a_sink, b_rpb,
              w_in_odd, w_out_odd, c_qnorm, c_knorm,
              hy_short_w, hy_short_b, hy_w1, hy_b1, hy_f1, hy_w2, hy_b2, hy_f2, hy_w3, hy_b3, hy_bias,
              w_router, b_router, moe_wg, moe_wu, moe_wd):
    B, S, D = x.shape
    rope = rope_tables(S)
    sc = jax.nn.silu(c)
    scc = jax.nn.silu(c_ctx)
    xc = ctx
    for l in range(DEPTH):
        need_ctx = l < DEPTH - 1
        mx = (sc @ w_ada[l] + b_ada[l]).reshape(B, 6, 1, D)
        mc = (scc @ w_ada[l] + b_ada[l]).reshape(6, 1, 1, D)
        hx = rmsnorm(x, norm_g[l, 0]) * (1.0 + mx[:, 1]) + mx[:, 0]
        hc = rmsnorm(xc, norm_g[l, 0]) * (1.0 + mc[1]) + mc[0]
        i = l // 2
        if l % 2 == 0:
            yx, yc = even_mixer(hx, hc, w_in_even[i], w_out_even[i], a_sink[i], b_rpb[i], rope, need_ctx)
        else:
            hy = (hy_short_w[i], hy_short_b[i], hy_w1[i], hy_b1[i], hy_f1[i], hy_w2[i], hy_b2[i], hy_f2[i],
                  hy_w3[i], hy_b3[i], hy_bias[i])
            yx, yc = odd_mixer(hx, hc, w_in_odd[i], w_out_odd[i], c_qnorm[i], c_knorm[i], hy, rope, need_ctx)
        x = x + mx[:, 2] * yx
        hx = rmsnorm(x, norm_g[l, 1]) * (1.0 + mx[:, 4]) + mx[:, 3]
        if need_ctx:
            xc = xc + mc[2] * yc
            hc = rmsnorm(xc, norm_g[l, 1]) * (1.0 + mc[4]) + mc[3]
            y_all = moe(jnp.concatenate([hc, hx], axis=1), w_router, b_router, moe_wg[l], moe_wu[l], moe_wd[l])
            xc = xc + mc[5] * y_all[:, :CTX_LEN]
            x = x + mx[:, 5] * y_all[:, CTX_LEN:]
        else:
            x = x + mx[:, 5] * moe(hx, w_router, b_router, moe_wg[l], moe_wu[l], moe_wd[l])
    return rmsnorm(x, final_g)
```

```python
import contextlib
import math
import numpy as np
import concourse.bass as bass
import concourse.mybir as mybir
from concourse.bass_utils import run_bass_kernel_spmd

F32 = mybir.dt.float32
BF16 = mybir.dt.bfloat16
AF = mybir.ActivationFunctionType
ALU = mybir.AluOpType
AX = mybir.AxisListType

EPOCH = 3000
N_DMA_SEMS = 24
NCORES = 8

D = 1024
SEQ = 4096
NB = 4
CTX = 256
GW = 64
HD = 64
EPS = 1e-6
SCALE = HD ** -0.5
OWN = 2048
HALO = 256
NLAT = OWN + 2 * HALO
NTOK = CTX + NLAT
OWN0 = CTX + HALO
NRES = CTX + OWN
NE = 16
DE = 512


class Sched:
    ENGS = ("pe", "act", "dve", "pool", "sp")

    def __init__(self, nc):
        self.nc = nc
        self.stack = contextlib.ExitStack()
        self.eng = {"pe": nc.tensor, "act": nc.scalar, "dve": nc.vector, "pool": nc.gpsimd, "sp": nc.sync}
        self.seq = {e: 0 for e in self.ENGS}
        self.sems = {}
        self.dma_sems = []
        self.dma_uses = []
        self.dma_rr = 0
        self.dma_q = {}
        self.dead = False
        self.waited = {e: {} for e in self.ENGS}
        self.state = {}
        self.out_deps = []
        self.uid = 0

    def sbuf(self, name, shape, dtype, stack=None):
        self.uid += 1
        return (stack or self.stack).enter_context(self.nc.sbuf_tensor(f"sb{self.uid}_{name}", list(shape), dtype))

    def psum(self, name, shape, dtype, stack=None):
        return (stack or self.stack).enter_context(self.nc.psum_tensor(name, list(shape), dtype))

    def _new_sem(self, name):
        return self.stack.enter_context(self.nc.semaphore(name))

    def _sem(self, semkey):
        if semkey[0] == "c":
            k = (semkey[1], semkey[2])
            if k not in self.sems:
                self.sems[k] = self._new_sem(f"s_{semkey[1]}_{semkey[2]}")
            return self.sems[k]
        return self.dma_sems[semkey[1]]

    def _deps(self, eng, reads, writes):
        deps = []
        for k in reads:
            st = self.state.get(k)
            if st and st[0] is not None:
                deps.append(st[0])
        for k in writes:
            st = self.state.get(k)
            if st:
                if st[0] is not None:
                    deps.append(st[0])
                deps.extend(st[1].values())
        best = {}
        for semkey, val, deng in deps:
            if deng == eng and eng == "pe":
                continue
            if self.waited[eng].get(semkey, 0) >= val:
                continue
            best[semkey] = max(best.get(semkey, 0), val)
        for sk, v in best.items():
            self.waited[eng][sk] = v
        return list(best.items())

    def _commit(self, who, dep, reads, writes):
        for k in reads:
            st = self.state.setdefault(k, [None, {}])
            st[1][(who, dep[0])] = dep
        for k in writes:
            self.state[k] = [dep, {}]

    def _emit_waits(self, eng, waits):
        e = self.eng[eng]
        for sk, v in waits:
            e.wait_ge(self._sem(sk), v)

    def kill(self):
        self.barrier()
        self.dead = True

    def op(self, eng, fn, reads=(), writes=()):
        if self.dead:
            return
        waits = self._deps(eng, reads, writes)
        self.seq[eng] += 1
        epoch, val = divmod(self.seq[eng] - 1, EPOCH)
        val += 1
        semkey = ("c", eng, epoch)
        self._emit_waits(eng, waits)
        ins = fn(self.eng[eng])
        ins.then_inc(self._sem(semkey), 1)
        self._commit(eng, (semkey, val, eng), reads, writes)

    def dma(self, q, fn, reads=(), writes=(), is_out=False):
        if self.dead:
            return None
        if q not in self.dma_q:
            base = len(self.dma_sems)
            nq = 16 if q == "sp" else 8
            for i in range(nq):
                self.dma_sems.append(self._new_sem(f"s_dma_{q}_{i}"))
                self.dma_uses.append(0)
            self.dma_q[q] = [base, nq, 0]
        base, nq, rr = self.dma_q[q]
        j = base + rr
        self.dma_q[q][2] = (rr + 1) % nq
        semkey = ("d", j)
        waits = dict(self._deps(q, reads, writes))
        prev = self.dma_uses[j] * 16
        if prev > 0 and self.waited[q].get(semkey, 0) < prev:
            self.waited[q][semkey] = prev
            waits[semkey] = max(waits.get(semkey, 0), prev)
        self.dma_uses[j] += 1
        val = self.dma_uses[j] * 16
        self._emit_waits(q, list(waits.items()))
        ins = fn(self.eng[q])
        ins.then_inc(self.dma_sems[j], 16)
        dep = (semkey, val, "dma")
        self._commit("dma", dep, reads, writes)
        if is_out:
            self.out_deps.append(dep)
        return dep

    def barrier(self):
        if self.dead:
            return
        targets = []
        for e in self.ENGS:
            if self.seq[e] > 0:
                epoch, val = divmod(self.seq[e] - 1, EPOCH)
                targets.append((("c", e, epoch), val + 1, e))
        for j, u in enumerate(self.dma_uses):
            if u > 0:
                targets.append((("d", j), u * 16, "dma"))
        for e in self.ENGS:
            waits = []
            for sk, v, de in targets:
                if de == e:
                    continue
                if self.waited[e].get(sk, 0) >= v:
                    continue
                self.waited[e][sk] = v
                waits.append((sk, v))
            self._emit_waits(e, waits)
        self.state = {}

    def finish(self):
        self.dead = False
        self.barrier()
        self.stack.close()


class IO:
    def __init__(self, nc, ov=None, prefix=""):
        self.nc, self.ov, self.prefix = nc, dict(ov or {}), prefix

    def inp(self, name, shape, dt=F32):
        if name in self.ov:
            return self.ov[name]
        return din(self.nc, self.prefix + name, shape, dt)

    def out(self, name, shape, dt=F32):
        if name in self.ov:
            return self.ov[name]
        return dout(self.nc, self.prefix + name, shape, dt)


def din(nc, name, shape, dt=F32):
    return nc.dram_tensor(name, list(shape), dt, kind="ExternalInput").ap()


def dout(nc, name, shape, dt=F32):
    return nc.dram_tensor(name, list(shape), dt, kind="ExternalOutput").ap()


class Ctx:
    def __init__(self, nc):
        self.nc = nc
        self.S = Sched(nc)
        S = self.S
        self.ps = S.psum("psall", [128, 8, 512], F32)
        self.banks = [self.ps[:, i, :] for i in range(8)]
        self.ones_f = S.sbuf("ones_f", [128, 128], F32)
        self.ones_b = S.sbuf("ones_b", [128, 128], BF16)
        S.op("dve", lambda e: e.memset(self.ones_f[:], 1.0), writes=["ones_f"])
        S.op("dve", lambda e: e.memset(self.ones_b[:], 1.0), writes=["ones_b"])
        self.k = 0

    def key(self, base):
        self.k += 1
        return f"{base}#{self.k}"


def norm_mod_tile(C, xt, xkey, ntok, out_fn, gs_fn, sh_fn, tmp, bank, ranges, f32_out=None):
    S = C.S
    sq, rs = tmp["sq"], tmp["rs"]
    kq = C.key("sq")
    S.op("act", lambda e: e.activation(out=sq[:, :, :ntok], in_=xt, func=AF.Square), reads=[xkey], writes=[kq])
    bk = f"bank{bank}"
    ps = C.banks[bank]
    for k in range(8):
        S.op("pe", lambda e, k=k: e.matmul(ps[:, :ntok], C.ones_f[:], sq[:, k, :ntok], start=(k == 0), stop=(k == 7)),
             reads=[kq, "ones_f"], writes=[bk])
    kr = C.key("rs")
    S.op("act", lambda e: e.activation(out=rs[:, :ntok], in_=ps[:, :ntok], func=AF.Sqrt, bias=EPS, scale=1.0 / D),
         reads=[bk], writes=[kr])
    kr2 = C.key("rs2")
    S.op("dve", lambda e: e.reciprocal(out=rs[:, :ntok], in_=rs[:, :ntok]), reads=[kr], writes=[kr, kr2])
    for k in range(8):
        t = tmp["t"][k % 2]
        kt = f"normt{k % 2}"
        for (a, b, r) in ranges:
            gs, sh = gs_fn(r), sh_fn(r)
            S.op("dve", lambda e, k=k, a=a, b=b, gs=gs, t=t: e.scalar_tensor_tensor(
                out=t[:, a:b], in0=xt[:, k, a:b], scalar=gs[:, k:k + 1], in1=rs[:, a:b], op0=ALU.mult, op1=ALU.mult),
                reads=[xkey, kr2], writes=[kt])
            S.op("act", lambda e, k=k, a=a, b=b, sh=sh, t=t: e.activation(
                out=out_fn(k, a, b), in_=t[:, a:b], func=AF.Identity, bias=sh[:, k:k + 1], scale=1.0),
                reads=[kt], writes=[tmp["outkey"]])
            if f32_out is not None:
                S.op("pool", lambda e, k=k, a=a, b=b, sh=sh, t=t: e.tensor_scalar(
                    out=f32_out(k, a, b), in0=t[:, a:b], scalar1=sh[:, k:k + 1], scalar2=None, op0=ALU.add),
                    reads=[kt], writes=[tmp["f32key"]])


def build_mod():
    nc = bass.Bass("TRN2", target_bir_lowering=False)
    cT = din(nc, "cT", [128, 8, 5])
    w = din(nc, "w", [D, 1536])
    b = din(nc, "b", [1536])
    o = dout(nc, "o", [5, 1536])
    C = Ctx(nc)
    S = C.S
    ct = S.sbuf("ct", [128, 8, 5], F32)
    sg = S.sbuf("sg", [128, 8, 5], F32)
    wt = S.sbuf("wt", [128, 8, 1536], F32)
    bt = S.sbuf("bt", [5, 1536], F32)
    ot = S.sbuf("ot", [5, 1536], F32)
    S.dma("sp", lambda e: e.dma_start(out=ct[:], in_=cT), writes=["ct"])
    for k in range(8):
        S.dma("sp", lambda e, k=k: e.dma_start(out=wt[:, k, :], in_=w[k * 128:(k + 1) * 128, :]), writes=[f"wt{k}"])
    S.dma("sp", lambda e: e.dma_start(out=bt[:], in_=b.partition_broadcast(5)), writes=["bt"])
    S.op("act", lambda e: e.activation(out=sg[:], in_=ct[:], func=AF.Silu), reads=["ct"], writes=["sg"])
    for j in range(3):
        ps = C.banks[j]
        for k in range(8):
            S.op("pe", lambda e, k=k, j=j, ps=ps: e.matmul(ps[0:5, :], sg[:, k, :], wt[:, k, j * 512:(j + 1) * 512],
                                                           start=(k == 0), stop=(k == 7)),
                 reads=["sg", f"wt{k}"], writes=[f"bank{j}"])
        S.op("dve", lambda e, j=j, ps=ps: e.tensor_tensor(out=ot[:, j * 512:(j + 1) * 512], in0=ps[0:5, :],
                                                        in1=bt[:, j * 512:(j + 1) * 512], op=ALU.add),
             reads=[f"bank{j}", "bt"], writes=["ot"])
    S.dma("sp", lambda e: e.dma_start(out=o, in_=ot[:]), reads=["ot"], is_out=True)
    S.finish()
    return nc


def run_mod(inp):
    cond = np.concatenate([inp["c"], inp["c_ctx"][None]], axis=0)
    cT = np.ascontiguousarray(cond.T.reshape(8, 128, 5).transpose(1, 0, 2))
    maps = []
    for i in range(NCORES):
        l, q = divmod(i, 4)
        maps.append({"cT": cT,
                     "w": np.ascontiguousarray(inp["w_ada"][l][:, q * 1536:(q + 1) * 1536]),
                     "b": np.ascontiguousarray(inp["b_ada"][l][q * 1536:(q + 1) * 1536])})
    res = run_bass_kernel_spmd(build_mod(), maps, core_ids=list(range(NCORES)))
    mod = np.zeros((2, 5, 6144), np.float32)
    for i in range(NCORES):
        l, q = divmod(i, 4)
        mod[l][:, q * 1536:(q + 1) * 1536] = res.results[i]["o"]
    return mod


def rope_np(pos):
    pos = np.asarray(pos)
    row = (pos // GW).astype(np.float32)
    col = (pos % GW).astype(np.float32)
    inv = (10000.0 ** (-np.arange(0, 32, 2, dtype=np.float32) / 32)).astype(np.float32)
    ar = row[None, :] * inv[:, None]
    ac = col[None, :] * inv[:, None]
    Ct = np.concatenate([np.cos(ar), np.cos(ar), np.cos(ac), np.cos(ac)], 0)
    St = np.concatenate([-np.sin(ar), np.sin(ar), -np.sin(ac), np.sin(ac)], 0)
    return (np.ascontiguousarray(np.concatenate([Ct, Ct], 0), dtype=np.float32),
            np.ascontiguousarray(np.concatenate([St, St], 0), dtype=np.float32))


def rope_perm():
    return np.concatenate([np.arange(16, 32), np.arange(0, 16), np.arange(48, 64), np.arange(32, 48)])


def b_offsets(jl):
    if jl in (0, 1):
        return list(range(-2, 4))
    if jl in (14, 15):
        return list(range(-3, 3))
    return list(range(-2, 3))


def b_valid(j, o):
    kt = j + o
    m = np.zeros((128, 128), np.float32)
    if kt < 0 or kt >= 32:
        return m
    k = np.arange(128)
    krow = 2 * kt + k // 64
    kcol = k % 64
    qrow = 2 * j + k // 64
    qcol = k % 64
    rs = np.clip(qrow - 4, 0, 56)
    cs = np.clip(qcol - 8, 0, 48)
    ok_r = (krow[:, None] >= rs[None, :]) & (krow[:, None] < rs[None, :] + 8)
    ok_c = (kcol[:, None] >= cs[None, :]) & (kcol[:, None] < cs[None, :] + 16)
    return (ok_r & ok_c).astype(np.float32)


HORD = [0, 2, 4, 6, 1, 3, 5, 7]


def rpb_gather(rpb):
    k = np.arange(128)
    a, kc = k // 64, k % 64
    out = np.zeros((128, 7, 8, 128), np.float32)
    for oi, o in enumerate(range(-3, 4)):
        dr = 2 * o + a[:, None] - a[None, :]
        dc = kc[:, None] - kc[None, :]
        ok = (np.abs(dr) <= 7) & (np.abs(dc) <= 15)
        g = rpb[HORD][:, np.clip(dr + 7, 0, 14), np.clip(dc + 15, 0, 30)]
        g = np.where(ok[None], g, 0.0)
        out[:, oi] = g.transpose(1, 0, 2)
    return out


def l0_w_in_cols():
    P = rope_perm()
    aq = np.arange(0, 512)
    aqP = (aq.reshape(8, 64)[:, P]).reshape(-1)
    ak0 = 512 + np.arange(64)
    ak1 = 576 + np.arange(64)
    av0 = 640 + np.arange(64)
    av1 = 704 + np.arange(64)
    cols = [aq, aqP,
            np.concatenate([ak0, ak0]), np.concatenate([ak0[P], ak0[P]]),
            np.concatenate([ak1, ak1]), np.concatenate([ak1[P], ak1[P]]),
            768 + np.arange(512), 1280 + np.arange(512), 1792 + np.arange(512),
            np.concatenate([av0, av0, av1, av1])]
    return np.concatenate(cols)


def fm(v):
    v = np.asarray(v, np.float32)
    lead = v.shape[:-1]
    r = v.reshape(lead + (8, 128))
    return np.ascontiguousarray(np.moveaxis(r, -1, 0))


def l0_inputs(inp, mod, core):
    b, half = divmod(core, 2)
    pos = half * OWN - HALO + np.arange(NLAT)
    ok = (pos >= 0) & (pos < SEQ)
    xl = np.zeros((NTOK, D), np.float32)
    xl[:CTX] = inp["ctx"][b]
    xl[CTX:][ok] = inp["x"][b][pos[ok]]
    xT = np.ascontiguousarray(xl.T.reshape(8, 128, NTOK))
    rc, rs = rope_np(np.clip(pos, 0, SEQ - 1))
    modt = np.stack([fm(mod[0, b].reshape(6, D)), fm(mod[0, 4].reshape(6, D))], axis=1)
    k = np.arange(128)
    tri_lo = (k[:, None] >= k[None, :]).astype(np.float32)
    tri_hi = (k[:, None] <= k[None, :]).astype(np.float32)
    z = np.zeros_like(tri_lo)
    amask = np.stack([tri_lo, tri_hi, tri_lo if half == 1 else z, tri_hi if half == 0 else z], axis=1)
    vint = np.stack([b_valid(10, o) for o in range(-2, 3)], axis=1)
    vb = np.zeros((128, 4, 6, 128), np.float32)
    for ci, jl in enumerate((0, 1, 14, 15)):
        for oi, o in enumerate(b_offsets(jl)):
            vb[:, ci, oi] = b_valid(half * 16 + jl, o)
    selE = np.zeros((16, 16, 128), np.float32)
    for e in range(16):
        selE[e, e, :] = 1.0
    return {
        "xT": xT, "mod": np.ascontiguousarray(modt), "ng": fm(inp["norm_g"][0]),
        "w_in": np.ascontiguousarray(inp["w_in_even"][0][:, l0_w_in_cols()]),
        "ropeC": rc, "ropeS": rs, "w_out": np.ascontiguousarray(inp["w_out_even"][0]),
        "sink": np.ascontiguousarray(inp["a_sink"][0]), "rpbT": rpb_gather(inp["b_rpb"][0]),
        "amask": np.ascontiguousarray(amask), "vint": np.ascontiguousarray(vint), "vb": vb,
        "w_r": np.ascontiguousarray(inp["w_router"]), "b_r": np.ascontiguousarray(inp["b_router"]),
        "wg": np.ascontiguousarray(inp["moe_wg"][0]), "wu": np.ascontiguousarray(inp["moe_wu"][0]),
        "wd": np.ascontiguousarray(inp["moe_wd"][0]),
        "ident": np.eye(128, dtype=np.float32), "selE": selE,
    }


def load_mod(C, modt, ng, st):
    S = C.S
    mod_sb = S.sbuf("mod_sb", [128, 2, 6, 8], F32, st)
    ng_sb = S.sbuf("ng_sb", [128, 2, 8], F32, st)
    gs_sb = S.sbuf("gs_sb", [128, 2, 2, 8], F32, st)
    S.dma("sp", lambda e: e.dma_start(out=mod_sb[:], in_=modt), writes=["mod_sb"])
    S.dma("sp", lambda e: e.dma_start(out=ng_sb[:], in_=ng), writes=["ng_sb"])
    for i in range(2):
        for r in range(2):
            S.op("dve", lambda e, i=i, r=r: e.scalar_tensor_tensor(
                out=gs_sb[:, i, r, :], in0=mod_sb[:, r, 1 + 3 * i, :], scalar=1.0, in1=ng_sb[:, i, :],
                op0=ALU.add, op1=ALU.mult), reads=["mod_sb", "ng_sb"], writes=["gs_sb"])
    M = {"gs": [[gs_sb[:, i, r, :] for r in range(2)] for i in range(2)],
         "sh": [[mod_sb[:, r, 3 * i, :] for r in range(2)] for i in range(2)],
         "gate": [[mod_sb[:, r, 2 + 3 * i, :] for r in range(2)] for i in range(2)]}
    return M


def norm_phase(C, M, i, src_fn, src_key_fn, ntok_total, ctx_len, out_t, out_key, st, f32_cb=None):
    S = C.S
    tmp = {"sq": S.sbuf("n_sq", [128, 8, 512], F32, st), "rs": S.sbuf("n_rs", [128, 512], F32, st),
           "t": [S.sbuf("n_t0", [128, 512], F32, st), S.sbuf("n_t1", [128, 512], F32, st)],
           "outkey": out_key, "f32key": "h2f"}
    a = 0
    ti = 0
    while a < ntok_total:
        b = min(a + 512, ntok_total)
        n = b - a
        xt, xkey = src_fn(a, b, ti)
        ranges = []
        if a < ctx_len:
            ranges.append((0, min(ctx_len, b) - a, 1))
            if b > ctx_len:
                ranges.append((ctx_len - a, n, 0))
        else:
            ranges.append((0, n, 0))
        f32o = None
        if f32_cb is not None:
            f32o = f32_cb(a, b, ti, "pre")
        norm_mod_tile(C, xt, xkey, n, lambda k, aa, bb, a=a: out_t[:, k, a + aa:a + bb],
                      lambda r: M["gs"][i][r], lambda r: M["sh"][i][r], tmp, 7, ranges, f32_out=f32o)
        if f32_cb is not None:
            f32_cb(a, b, ti, "post")
        a = b
        ti += 1


RES_TILES = [(0, 256)] + [(256 + 512 * i, 256 + 512 * (i + 1)) for i in range(4)]


def moe_phase(C, xres, h2, combT, selE_sb, gate_fn, wg, wu, wd, st, nres=NRES, tiles=RES_TILES, ctx_len=CTX):
    S = C.S
    wgs = [S.sbuf(f"wg_sb{i}", [128, 8, DE], BF16, st) for i in range(2)]
    wus = [S.sbuf(f"wu_sb{i}", [128, 8, DE], BF16, st) for i in range(2)]
    wds = [S.sbuf(f"wd_sb{i}", [128, 4, D], BF16, st) for i in range(2)]
    he = [S.sbuf(f"he{i}", [128, 4, 512], BF16, st) for i in range(2)]
    sg = [S.sbuf(f"sg{i}", [128, 512], BF16, st) for i in range(2)]
    tt = [S.sbuf(f"tt{i}", [128, 512], BF16, st) for i in range(2)]
    bc = [S.sbuf(f"bc{i}", [128, 512], BF16, st) for i in range(2)]
    stg = [S.sbuf(f"wstg{i}", [128, 1024], F32, st) for i in range(2)]
    stg_i = [0]
    cnt = 0
    for ex in range(NE):
        wi = ex % 2
        wgv = wg[ex].rearrange("(k p) o -> p k o", p=128)
        wuv = wu[ex].rearrange("(k p) o -> p k o", p=128)
        wdv = wd[ex].rearrange("(k p) o -> p k o", p=128)
        for (src, dst, dkey, npiece, per) in ((wgv, wgs[wi], f"wg{wi}", 4, 2), (wuv, wus[wi], f"wu{wi}", 4, 2), (wdv, wds[wi], f"wd{wi}", 4, 1)):
            for pc in range(npiece):
                si = stg_i[0] % len(stg)
                stg_i[0] += 1
                sview = stg[si][:].rearrange("p (k o) -> p k o", k=per)
                S.dma("sp", lambda e, src=src, pc=pc, per=per, sview=sview: e.dma_start(out=sview, in_=src[:, pc * per:(pc + 1) * per, :]), writes=[f"stg{si}"])
                S.op("pool", lambda e, dst=dst, pc=pc, per=per, sview=sview: e.tensor_copy(out=dst[:, pc * per:(pc + 1) * per, :], in_=sview),
                     reads=[f"stg{si}"], writes=[dkey])
        for (a, b) in tiles:
            n = b - a
            r = 1 if a < ctx_len else 0
            hi = cnt % 2
            cnt += 1
            S.op("pe", lambda e, ex=ex, a=a, b=b, n=n: e.matmul(C.banks[6][:, :n], selE_sb[:, ex, :], combT[:, a:b],
                                                                start=True, stop=True),
                 reads=["combT", "selE"], writes=["bank6"])
            S.op("act", lambda e, hi=hi, n=n: e.activation(out=bc[hi][:, :n], in_=C.banks[6][:, :n], func=AF.Copy),
                 reads=["bank6"], writes=[f"bc{hi}"])
            for hc in range(4):
                gb = (hc % 2) * 2
                gk, uk = f"bank{gb}", f"bank{gb + 1}"
                for k in range(8):
                    S.op("pe", lambda e, k=k, hc=hc, gb=gb, a=a, b=b, n=n, wi=wi: e.matmul(
                        C.banks[gb][:, :n], wgs[wi][:, k, hc * 128:(hc + 1) * 128], h2[:, k, a:b],
                        start=(k == 0), stop=(k == 7)), reads=[f"wg{wi}", "h2"], writes=[gk])
                for k in range(8):
                    S.op("pe", lambda e, k=k, hc=hc, gb=gb, a=a, b=b, n=n, wi=wi: e.matmul(
                        C.banks[gb + 1][:, :n], wus[wi][:, k, hc * 128:(hc + 1) * 128], h2[:, k, a:b],
                        start=(k == 0), stop=(k == 7)), reads=[f"wu{wi}", "h2"], writes=[uk])
                si = hc % 2
                S.op("act", lambda e, gb=gb, si=si, n=n: e.activation(out=sg[si][:, :n], in_=C.banks[gb][:, :n], func=AF.Silu),
                     reads=[gk], writes=[f"sg{si}"])
                S.op("dve", lambda e, gb=gb, si=si, n=n: e.tensor_tensor(out=tt[si][:, :n], in0=sg[si][:, :n],
                                                                        in1=C.banks[gb + 1][:, :n], op=ALU.mult),
                     reads=[f"sg{si}", uk], writes=[f"tt{si}"])
                S.op("pool", lambda e, si=si, hi=hi, hc=hc, n=n: e.tensor_tensor(out=he[hi][:, hc, :n], in0=tt[si][:, :n],
                                                                               in1=bc[hi][:, :n], op=ALU.mult),
                     reads=[f"tt{si}", f"bc{hi}"], writes=[f"he{hi}"])
            for oc in range(8):
                yb = 4 + (oc % 2)
                for hc in range(4):
                    S.op("pe", lambda e, oc=oc, hc=hc, yb=yb, n=n, hi=hi, wi=wi: e.matmul(
                        C.banks[yb][:, :n], wds[wi][:, hc, oc * 128:(oc + 1) * 128], he[hi][:, hc, :n],
                        start=(hc == 0), stop=(hc == 3)), reads=[f"wd{wi}", f"he{hi}"], writes=[f"bank{yb}"])
                g = gate_fn(r)
                S.op("dve", lambda e, oc=oc, yb=yb, a=a, b=b, n=n, g=g: e.scalar_tensor_tensor(
                    out=xres[:, oc, a:b], in0=C.banks[yb][:, :n], scalar=g[:, oc:oc + 1], in1=xres[:, oc, a:b],
                    op0=ALU.mult, op1=ALU.add), reads=[f"bank{yb}", f"xres{oc}"], writes=[f"xres{oc}"])


def router_phase(C, lg_all, ntile, b_r, st):
    S = C.S
    n = ntile
    def T(name, shape):
        return S.sbuf(name, shape, F32, st)
    br = T("r_br", [128, 16])
    s = T("r_s", [128, n, 16]); sel = T("r_sel", [128, n, 16])
    S.dma("sp", lambda e: e.dma_start(out=br[:], in_=b_r.partition_broadcast(128)), writes=["r_br"])
    S.op("act", lambda e: e.activation(out=s[:], in_=lg_all[:], func=AF.Sigmoid), reads=["lg_all"], writes=["r_s"])
    S.op("dve", lambda e: e.tensor_tensor(out=sel[:], in0=s[:], in1=br[:].unsqueeze(1).to_broadcast([128, n, 16]), op=ALU.add),
         reads=["r_s", "r_br"], writes=["r_sel"])
    sv = sel[:].rearrange("p t (g j) -> p t g j", j=4)
    pr = T("r_pr", [128, n, 4]); gsc = T("r_gsc", [128, n, 4])
    first = True
    for i in range(4):
        for j in range(i + 1, 4):
            S.op("dve", lambda e, i=i, j=j: e.tensor_tensor(out=pr[:], in0=sv[:, :, :, i], in1=sv[:, :, :, j], op=ALU.add),
                 reads=["r_sel"], writes=["r_pr"])
            if first:
                S.op("dve", lambda e: e.tensor_copy(out=gsc[:], in_=pr[:]), reads=["r_pr"], writes=["r_gsc"])
                first = False
            else:
                S.op("dve", lambda e: e.tensor_tensor(out=gsc[:], in0=gsc[:], in1=pr[:], op=ALU.max),
                     reads=["r_pr", "r_gsc"], writes=["r_gsc"])
    gmax = T("r_gmax", [128, n]); ing = T("r_ing", [128, n, 4])
    S.op("dve", lambda e: e.tensor_reduce(out=gmax[:], in_=gsc[:], axis=AX.X, op=ALU.max), reads=["r_gsc"], writes=["r_gmax"])
    S.op("dve", lambda e: e.tensor_tensor(out=ing[:], in0=gsc[:], in1=gmax[:].unsqueeze(2).to_broadcast([128, n, 4]), op=ALU.is_ge),
         reads=["r_gsc", "r_gmax"], writes=["r_ing"])
    cg = T("r_cg", [128, n, 4, 4]); cm = T("r_cm", [128, n, 4])
    cgv = cg[:]
    for i in range(4):
        firstj = True
        for j in range(4):
            if j == i:
                continue
            if firstj:
                S.op("dve", lambda e, i=i, j=j: e.tensor_tensor(out=cgv[:, :, :, i], in0=sv[:, :, :, j], in1=sv[:, :, :, i], op=ALU.is_gt),
                     reads=["r_sel"], writes=["r_cg"])
                firstj = False
            else:
                S.op("dve", lambda e, i=i, j=j: e.tensor_tensor(out=cm[:], in0=sv[:, :, :, j], in1=sv[:, :, :, i], op=ALU.is_gt),
                     reads=["r_sel"], writes=["r_cm"])
                S.op("dve", lambda e, i=i: e.tensor_tensor(out=cgv[:, :, :, i], in0=cgv[:, :, :, i], in1=cm[:], op=ALU.add),
                     reads=["r_cm", "r_cg"], writes=["r_cg"])
    selm = T("r_selm", [128, n, 4, 4])
    S.op("dve", lambda e: e.tensor_single_scalar(out=selm[:], in_=cg[:], scalar=1.5, op=ALU.is_lt), reads=["r_cg"], writes=["r_selm"])
    S.op("dve", lambda e: e.tensor_tensor(out=selm[:], in0=selm[:], in1=ing[:].unsqueeze(3).to_broadcast([128, n, 4, 4]), op=ALU.mult),
         reads=["r_selm", "r_ing"], writes=["r_selm"])
    comb = T("r_comb", [128, n, 16]); den = T("r_den", [128, n])
    S.op("dve", lambda e: e.tensor_tensor(out=comb[:], in0=s[:], in1=selm[:].rearrange("p t g j -> p t (g j)"), op=ALU.mult),
         reads=["r_s", "r_selm"], writes=["r_comb"])
    S.op("dve", lambda e: e.tensor_reduce(out=den[:], in_=comb[:], axis=AX.X, op=ALU.add), reads=["r_comb"], writes=["r_den"])
    S.op("dve", lambda e: e.reciprocal(out=den[:], in_=den[:]), reads=["r_den"], writes=["r_den"])
    S.op("dve", lambda e: e.tensor_tensor(out=comb[:], in0=comb[:], in1=den[:].unsqueeze(2).to_broadcast([128, n, 16]), op=ALU.mult),
         reads=["r_comb", "r_den"], writes=["r_comb"])
    return comb


def tail_phase(C, M, ybuf, w_out_d, x_src, xres_out_d, w_r, b_r, ident_d, selE_d, wg, wu, wd, st,
               final_g_d=None, nres=NRES, tiles=RES_TILES, ctx_len=CTX):
    S = C.S
    ntile = nres // 128
    xres = S.sbuf("xres", [128, 8, nres], F32, st)
    lg_all = S.sbuf("lg_all", [128, ntile, 16], F32, st)
    with contextlib.ExitStack() as st2:
        wo = S.sbuf("wo_sb", [128, 8, D], BF16, st2)
        xts = [S.sbuf(f"xt_b{i}", [128, 8, 512], F32, st2) for i in range(2)]
        S.dma("pool", lambda e: e.dma_start(out=wo[:], in_=w_out_d.rearrange("(k p) o -> p k o", p=128)), writes=["wo"])
        for ti, (a, b) in enumerate(tiles):
            n = b - a
            r = 1 if a < ctx_len else 0
            xt = xts[ti % 2]
            S.dma("sp", lambda e, a=a, b=b, n=n, xt=xt: e.dma_start(out=xt[:, :, :n], in_=x_src(a, b)), writes=[f"xtb{ti % 2}"])
            for oc in range(8):
                bk = oc % 4
                for k in range(8):
                    S.op("pe", lambda e, oc=oc, k=k, bk=bk, a=a, b=b, n=n: e.matmul(
                        C.banks[bk][:, :n], wo[:, k, oc * 128:(oc + 1) * 128], ybuf[:, k, a:b], start=(k == 0), stop=(k == 7)),
                        reads=["wo", "Y"], writes=[f"bank{bk}"])
                g = M["gate"][0][r]
                S.op("dve", lambda e, oc=oc, bk=bk, a=a, b=b, n=n, g=g, xt=xt: e.scalar_tensor_tensor(
                    out=xres[:, oc, a:b], in0=C.banks[bk][:, :n], scalar=g[:, oc:oc + 1], in1=xt[:, oc, :n],
                    op0=ALU.mult, op1=ALU.add), reads=[f"bank{bk}", f"xtb{ti % 2}"], writes=[f"xres{oc}"])
    S.barrier()
    h2 = ybuf
    with contextlib.ExitStack() as st2:
        wr = S.sbuf("wr_sb", [128, 8, 16], F32, st2)
        h2f = S.sbuf("h2f", [128, 8, 512], F32, st2)
        S.dma("sp", lambda e: e.dma_start(out=wr[:], in_=w_r.rearrange("(k p) o -> p k o", p=128)), writes=["wr"])

        def src_fn(a, b, ti):
            return xres[:, :, a:b], None

        def f32_cb(a, b, ti, when):
            if when == "pre":
                return lambda k, aa, bb: h2f[:, k, aa:bb]
            n = b - a
            for t in range(n // 128):
                tile_i = a // 128 + t
                for k in range(8):
                    S.op("pe", lambda e, k=k, t=t: e.matmul(C.banks[5][:, 0:16], h2f[:, k, t * 128:(t + 1) * 128], wr[:, k, :],
                                                            start=(k == 0), stop=(k == 7)),
                         reads=["h2f", "wr"], writes=["bank5"])
                S.op("dve", lambda e, tile_i=tile_i: e.tensor_copy(out=lg_all[:, tile_i, :], in_=C.banks[5][:, 0:16]),
                     reads=["bank5"], writes=["lg_all"])
            return None

        tmp_key = [f"xres{oc}" for oc in range(8)]

        def src_fn2(a, b, ti):
            return xres[:, :, a:b], "xres_all"
        norm_phase(C, M, 1, src_fn2, None, nres, ctx_len, h2, "h2", st2, f32_cb=f32_cb)
    S.barrier()
    ident = S.sbuf("ident", [128, 128], F32, st)
    selE = S.sbuf("selE_sb", [16, 16, 128], F32, st)
    combT = S.sbuf("combT", [16, nres], F32, st)
    with contextlib.ExitStack() as st2:
        comb = router_phase(C, lg_all, ntile, b_r, st2)
        S.dma("sp", lambda e: e.dma_start(out=ident[:], in_=ident_d), writes=["ident"])
        S.dma("sp", lambda e: e.dma_start(out=selE[:], in_=selE_d), writes=["selE"])
        for t in range(ntile):
            S.op("pe", lambda e, t=t: e.transpose(C.banks[t % 2][0:16, 0:128], comb[:, t, :], ident[:]),
                 reads=["r_comb", "ident"], writes=[f"bank{t % 2}"])
            S.op("dve", lambda e, t=t: e.tensor_copy(out=combT[:, t * 128:(t + 1) * 128], in_=C.banks[t % 2][0:16, 0:128]),
                 reads=[f"bank{t % 2}"], writes=["combT"])
    S.barrier()
    with contextlib.ExitStack() as st2:
        moe_phase(C, xres, h2, combT, selE, lambda r: M["gate"][1][r], wg, wu, wd, st2, nres=nres, tiles=tiles, ctx_len=ctx_len)
    S.barrier()
    if final_g_d is None and isinstance(xres_out_d, tuple):
        x1nat, pp = xres_out_d
        for k in range(8):
            if pp == 0:
                S.dma("sp", lambda e, k=k: e.dma_start(out=x1nat[k][:, 0:CTX], in_=xres[:, k, 0:CTX]), reads=[f"xres{k}"], writes=["x1nat"])
            S.dma("sp", lambda e, k=k: e.dma_start(out=x1nat[k][:, CTX + pp * OWN:CTX + (pp + 1) * OWN], in_=xres[:, k, CTX:]),
                  reads=[f"xres{k}"], writes=["x1nat"])
    elif final_g_d is None:
        for k in range(8):
            S.dma("sp", lambda e, k=k: e.dma_start(out=xres_out_d[k], in_=xres[:, k, :]), reads=[f"xres{k}"], is_out=True)
    else:
        with contextlib.ExitStack() as st2:
            fg = S.sbuf("fg", [128, 8], F32, st2)
            zsh = S.sbuf("zsh", [128, 8], F32, st2)
            dummy = S.sbuf("fdummy", [128, 8, 512], BF16, st2)
            fo = [S.sbuf(f"fo{i}", [128, 8, 512], F32, st2) for i in range(2)]
            S.dma("sp", lambda e: e.dma_start(out=fg[:], in_=final_g_d), writes=["fg"])
            S.op("dve", lambda e: e.memset(zsh[:], 0.0), writes=["zsh"])
            S.barrier()
            Mf = {"gs": [[fg[:], fg[:]]], "sh": [[zsh[:], zsh[:]]]}
            xo = xres_out_d.rearrange("k p t -> p k t")

            def src_fn3(a, b, ti):
                return xres[:, :, a:b], "xres_all"

            def f32_cb3(a, b, ti, when):
                if when == "pre":
                    return lambda k, aa, bb: fo[ti % 2][:, k, aa:bb]
                S.dma("sp", lambda e: e.dma_start(out=xo[:, :, a:b], in_=fo[ti % 2][:, :, :b - a]), reads=[f"fo{ti % 2}"], is_out=True)
                return None
            tmpn = {"sq": S.sbuf("f_sq", [128, 8, 512], F32, st2), "rs": S.sbuf("f_rs", [128, 512], F32, st2),
                    "t": [S.sbuf("f_t0", [128, 512], F32, st2), S.sbuf("f_t1", [128, 512], F32, st2)]}
            a = 0
            ti = 0
            while a < nres:
                b = min(a + 512, nres)
                tmpn["outkey"] = "fdummy"
                tmpn["f32key"] = f"fo{ti % 2}"
                f32o = f32_cb3(a, b, ti, "pre")
                norm_mod_tile(C, xres[:, :, a:b], "xres_all", b - a, lambda k, aa, bb: dummy[:, k, aa:bb],
                              lambda r: fg[:], lambda r: zsh[:], tmpn, 7, [(0, b - a, 0)], f32_out=f32o)
                f32_cb3(a, b, ti, "post")
                a = b
                ti += 1
    return xres


class _Stop(Exception):
    pass


def build_l0(debug=False, stop=None):
    nc = bass.Bass("TRN2", target_bir_lowering=False)
    C = Ctx(nc)
    _build_l0_body(nc, C, debug, stop, IO(nc))
    C.S.finish()
    return nc


def _build_l0_body(nc, C, debug, stop, io, write_ctx=True):
    xT = io.inp("xT", [8, 128, NTOK])
    modt = io.inp("mod", [128, 2, 6, 8])
    ng = io.inp("ng", [128, 2, 8])
    w_in = io.inp("w_in", [D, 3328])
    ropeC = io.inp("ropeC", [128, NLAT])
    ropeS = io.inp("ropeS", [128, NLAT])
    w_out = io.inp("w_out", [D, D])
    sink = io.inp("sink", [8])
    rpbT = io.inp("rpbT", [128, 7, 8, 128])
    amask_d = io.inp("amask", [128, 4, 128])
    vint_d = io.inp("vint", [128, 5, 128])
    vb_d = io.inp("vb", [128, 4, 6, 128])
    w_r = io.inp("w_r", [D, 16])
    b_r = io.inp("b_r", [16])
    if stop is None or stop == "full":
        wg = io.inp("wg", [NE, D, DE])
        wu = io.inp("wu", [NE, D, DE])
        wd = io.inp("wd", [NE, DE, D])
    else:
        wg = wu = wd = None
    ident_d = io.inp("ident", [128, 128])
    selE_d = io.inp("selE", [16, 16, 128])
    x1T = io.out("x1T", [8, 128, NRES])
    dbg = {}
    if debug:
        dbg["hx"] = io.out("d_hx", [128, 8, NTOK], BF16)
        dbg["y"] = io.out("d_y", [128, 8, NRES], BF16)

    S = C.S
    st0 = contextlib.ExitStack()
    M = load_mod(C, modt, ng, st0)
    xv = xT.rearrange("k p t -> p k t")
    hy = S.sbuf("hy", [128, 8, NTOK], BF16, st0)
    hx = hy
    ybuf = hy[:, :, 0:NRES]
    with contextlib.ExitStack() as stA:
        xts = [S.sbuf(f"xa{i}", [128, 8, 512], F32, stA) for i in range(2)]

        def src_fn(a, b, ti):
            xt = xts[ti % 2]
            S.dma("sp", lambda e: e.dma_start(out=xt[:, :, :b - a], in_=xv[:, :, a:b]), writes=[f"xa{ti % 2}"])
            return xt[:, :, :b - a], f"xa{ti % 2}"
        norm_phase(C, M, 0, src_fn, None, NTOK, CTX, hx, "hx", stA)
    S.barrier()
    if stop == "A0":
        S.kill()
    if debug:
        for k in range(8):
            S.dma("sp", lambda e, k=k: e.dma_start(out=dbg["hx"][:, k, :], in_=hx[:, k, :]), reads=["hx"], is_out=True)
        S.barrier()
    if stop == "A":
        S.kill()

    with contextlib.ExitStack() as stQ:
        QA = S.sbuf("QA", [128, 4, NRES], BF16, stQ)
        QB = S.sbuf("QB", [128, 4, NRES], BF16, stQ)
        KAB = S.sbuf("KAB", [128, 2, NTOK], BF16, stQ)
        BK = S.sbuf("BK", [128, 4, NTOK], BF16, stQ)
        VA2 = S.sbuf("VA2", [128, 22, 256], BF16, stQ)
        VB = S.sbuf("VB", [128, 22, 512], BF16, stQ)
        if True:
            with contextlib.ExitStack() as stB:
                wts = [S.sbuf(f"wt{i}", [128, 8, 512], BF16, stB) for i in range(2)]
                rc = S.sbuf("rc", [128, NLAT], F32, stB)
                rs_ = S.sbuf("rs", [128, NLAT], F32, stB)
                t1 = [S.sbuf(f"t1_{i}", [128, 512], F32, stB) for i in range(2)]
                t2 = [S.sbuf(f"t2_{i}", [128, 512], F32, stB) for i in range(2)]
                S.dma("sp", lambda e: e.dma_start(out=rc[:], in_=ropeC), writes=["rc"])
                S.dma("sp", lambda e: e.dma_start(out=rs_[:], in_=ropeS), writes=["rs"])
                wv = w_in.rearrange("(k p) o -> p k o", p=128)
                ngrp = [0]

                def load_w(c0, ncol):
                    wi = ngrp[0] % 2
                    ngrp[0] += 1
                    S.dma("pool", lambda e: e.dma_start(out=wts[wi][:, :, :ncol], in_=wv[:, :, c0:c0 + ncol]), writes=[f"wt{wi}"])
                    return wts[wi], f"wt{wi}"

                cnt = [0]

                def fm_block(wt, wkey, j, toks, out_fn, okey):
                    for (a, b, da) in toks:
                        n = b - a
                        bk = cnt[0] % 4
                        cnt[0] += 1
                        for k in range(8):
                            S.op("pe", lambda e, k=k: e.matmul(C.banks[bk][:, :n], wt[:, k, j * 128:(j + 1) * 128], hx[:, k, a:b],
                                                               start=(k == 0), stop=(k == 7)), reads=[wkey, "hx"], writes=[f"bank{bk}"])
                        S.op("act", lambda e: e.activation(out=out_fn(da, da + n), in_=C.banks[bk][:, :n], func=AF.Copy),
                             reads=[f"bank{bk}"], writes=[okey])

                def rope_block(wt, wkey, j, jp, toks, out_fn, okey):
                    for (a, b, da) in toks:
                        n = b - a
                        bk = (cnt[0] % 2) * 2
                        ti = cnt[0] % 2
                        cnt[0] += 1
                        la = a - CTX
                        for k in range(8):
                            S.op("pe", lambda e, k=k: e.matmul(C.banks[bk][:, :n], wt[:, k, j * 128:(j + 1) * 128], hx[:, k, a:b],
                                                               start=(k == 0), stop=(k == 7)), reads=[wkey, "hx"], writes=[f"bank{bk}"])
                        for k in range(8):
                            S.op("pe", lambda e, k=k: e.matmul(C.banks[bk + 1][:, :n], wt[:, k, jp * 128:(jp + 1) * 128], hx[:, k, a:b],
                                                               start=(k == 0), stop=(k == 7)), reads=[wkey, "hx"], writes=[f"bank{bk + 1}"])
                        S.op("dve", lambda e: e.tensor_tensor(out=t1[ti][:, :n], in0=C.banks[bk][:, :n], in1=rc[:, la:la + n], op=ALU.mult),
                             reads=[f"bank{bk}", "rc"], writes=[f"t1_{ti}"])
                        S.op("dve", lambda e: e.tensor_tensor(out=t2[ti][:, :n], in0=C.banks[bk + 1][:, :n], in1=rs_[:, la:la + n], op=ALU.mult),
                             reads=[f"bank{bk + 1}", "rs"], writes=[f"t2_{ti}"])
                        S.op("pool", lambda e: e.tensor_tensor(out=out_fn(da, da + n), in0=t1[ti][:, :n], in1=t2[ti][:, :n], op=ALU.add),
                             reads=[f"t1_{ti}", f"t2_{ti}"], writes=[okey])

                own_toks = [(OWN0 + 512 * i, OWN0 + 512 * (i + 1), CTX + 512 * i) for i in range(4)]
                ctx_toks = [(0, CTX, 0)]
                lat_toks = [(CTX + 512 * i, CTX + 512 * (i + 1), CTX + 512 * i) for i in range(5)]
                wA, kA = load_w(0, 512)
                wP, kP = load_w(512, 512)
                for j in range(4):
                    fm_block(wA, kA, j, ctx_toks, lambda a, b, j=j: QA[:, j, a:b], "QA")
                    for (a, b, da) in own_toks:
                        n = b - a
                        bk = (cnt[0] % 2) * 2
                        ti = cnt[0] % 2
                        cnt[0] += 1
                        la = a - CTX
                        for k in range(8):
                            S.op("pe", lambda e, k=k: e.matmul(C.banks[bk][:, :n], wA[:, k, j * 128:(j + 1) * 128], hx[:, k, a:b],
                                                               start=(k == 0), stop=(k == 7)), reads=[kA, "hx"], writes=[f"bank{bk}"])
                        for k in range(8):
                            S.op("pe", lambda e, k=k: e.matmul(C.banks[bk + 1][:, :n], wP[:, k, j * 128:(j + 1) * 128], hx[:, k, a:b],
                                                               start=(k == 0), stop=(k == 7)), reads=[kP, "hx"], writes=[f"bank{bk + 1}"])
                        S.op("dve", lambda e: e.tensor_tensor(out=t1[ti][:, :n], in0=C.banks[bk][:, :n], in1=rc[:, la:la + n], op=ALU.mult),
                             reads=[f"bank{bk}", "rc"], writes=[f"t1_{ti}"])
                        S.op("dve", lambda e: e.tensor_tensor(out=t2[ti][:, :n], in0=C.banks[bk + 1][:, :n], in1=rs_[:, la:la + n], op=ALU.mult),
                             reads=[f"bank{bk + 1}", "rs"], writes=[f"t2_{ti}"])
                        S.op("pool", lambda e, da=da, n=n: e.tensor_tensor(out=QA[:, j, da:da + n], in0=t1[ti][:, :n], in1=t2[ti][:, :n], op=ALU.add),
                             reads=[f"t1_{ti}", f"t2_{ti}"], writes=["QA"])
                wK, kK = load_w(1024, 512)
                for g in range(2):
                    fm_block(wK, kK, 2 * g, ctx_toks, lambda a, b, g=g: KAB[:, g, a:b], "KAB")
                    rope_block(wK, kK, 2 * g, 2 * g + 1, lat_toks, lambda a, b, g=g: KAB[:, g, a:b], "KAB")
                wq, kq = load_w(1536, 512)
                for j in range(4):
                    fm_block(wq, kq, j, ctx_toks + own_toks, lambda a, b, j=j: QB[:, j, a:b], "QB")
                wk_, kk_ = load_w(2048, 512)
                all_toks = ctx_toks + lat_toks
                for j in range(4):
                    fm_block(wk_, kk_, j, all_toks, lambda a, b, j=j: BK[:, j, a:b], "BK")
                wvb, kvb = load_w(2560, 512)
                wva, kva = load_w(3072, 256)
                for t in range(22):
                    bk = cnt[0] % 4
                    cnt[0] += 1
                    for k in range(8):
                        S.op("pe", lambda e, k=k: e.matmul(C.banks[bk][:, :], hx[:, k, t * 128:(t + 1) * 128], wvb[:, k, :],
                                                           start=(k == 0), stop=(k == 7)), reads=[kvb, "hx"], writes=[f"bank{bk}"])
                    S.op("act", lambda e: e.activation(out=VB[:, t, :], in_=C.banks[bk][:, :], func=AF.Copy),
                         reads=[f"bank{bk}"], writes=["VB"])
                    bk = cnt[0] % 4
                    cnt[0] += 1
                    for k in range(8):
                        S.op("pe", lambda e, k=k: e.matmul(C.banks[bk][:, :256], hx[:, k, t * 128:(t + 1) * 128], wva[:, k, :256],
                                                           start=(k == 0), stop=(k == 7)), reads=[kva, "hx"], writes=[f"bank{bk}"])
                    S.op("dve", lambda e: e.tensor_copy(out=VA2[:, t, :], in_=C.banks[bk][:, :256]),
                         reads=[f"bank{bk}"], writes=["VA2"])
            S.barrier()
        if stop == "B":
            S.kill()
        with contextlib.ExitStack() as stC:
            esk = S.sbuf("esk", [128, 8], F32, stC)
            am = S.sbuf("am", [128, 4, 128], BF16, stC)
            Tm = S.sbuf("Tm", [128, 7, 8, 128], BF16, stC)
            TV = S.sbuf("TV", [128, 5, 8, 128], BF16, stC)
            vint = S.sbuf("vint", [128, 5, 128], BF16, stC)
            vbm = S.sbuf("vbm", [128, 4, 6, 128], BF16, stC)
            pts = [S.sbuf(f"pt{i}", [128, 1024], BF16, stC) for i in range(2)]
            dn = S.sbuf("dn", [128, 1024], F32, stC)
            S.dma("sp", lambda e: e.dma_start(out=esk[:], in_=sink.partition_broadcast(128)), writes=["esk"])
            S.op("act", lambda e: e.activation(out=esk[:], in_=esk[:], func=AF.Exp), reads=["esk"], writes=["esk"])
            S.dma("pool", lambda e: e.dma_start(out=am[:], in_=amask_d), writes=["am"])
            S.dma("pool", lambda e: e.dma_start(out=vint[:], in_=vint_d), writes=["vint"])
            S.dma("pool", lambda e: e.dma_start(out=vbm[:], in_=vb_d), writes=["vbm"])
            with contextlib.ExitStack() as stT:
                stg = S.sbuf("rp_stage", [128, 8, 128], F32, stT)
                for oi in range(7):
                    S.dma("sp", lambda e, oi=oi: e.dma_start(out=stg[:], in_=rpbT[:, oi]), writes=["rp_stage"])
                    S.op("act", lambda e, oi=oi: e.activation(out=Tm[:, oi], in_=stg[:], func=AF.Exp), reads=["rp_stage"], writes=["Tm"])
                for oi in range(5):
                    S.op("dve", lambda e, oi=oi: e.tensor_tensor(out=TV[:, oi], in0=Tm[:, oi + 1],
                                                                 in1=vint[:, oi, :].unsqueeze(1).to_broadcast([128, 8, 128]), op=ALU.mult),
                         reads=["Tm", "vint"], writes=["TV"])
            S.barrier()
            if stop == "C0":
                S.kill()
            ac = [0]

            import os
            ATT_STAGE = int(os.environ.get("ATT_STAGE", "9"))
            ATT_N = int(os.environ.get("ATT_N", "999"))

            def attnA(q0, klist):
                for g in range(2):
                    if ac[0] >= ATT_N:
                        return
                    it = ac[0]
                    ac[0] += 1
                    nb, db = 4 + it % 2, 6 + it % 2
                    for idx, (tk, mk) in enumerate(klist):
                        sb = idx % 2
                        pt = pts[idx % 2]
                        for s_ in range(4):
                            hh = (0, 2, 1, 3)[s_]
                            h = 4 * g + hh
                            c, off = h // 2, (h % 2) * 64
                            bnk = 2 * sb + s_ // 2
                            S.op("pe", lambda e, s_=s_, c=c, off=off, tk=tk, bnk=bnk: e.matmul(
                                C.banks[bnk][:, (s_ % 2) * 128:(s_ % 2 + 1) * 128], KAB[off:off + 64, g, tk * 128:(tk + 1) * 128],
                                QA[off:off + 64, c, q0:q0 + 128], start=True, stop=True),
                                reads=["KAB", "QA"], writes=[f"SA{sb}"])
                        S.op("act", lambda e, sb=sb, pt=pt: e.activation(
                            out=pt[:, 0:512].rearrange("p (a b) -> p a b", a=2), in_=C.ps[:, 2 * sb:2 * sb + 2, 0:256], func=AF.Exp, scale=SCALE),
                            reads=[f"SA{sb}"], writes=[f"pt{idx % 2}"])
                        if mk is not None:
                            S.op("pool", lambda e, pt=pt, mk=mk: e.tensor_tensor(
                                out=pt[:, 0:512].rearrange("p (h q) -> p h q", h=4), in0=pt[:, 0:512].rearrange("p (h q) -> p h q", h=4),
                                in1=mk.unsqueeze(1).to_broadcast([128, 4, 128]), op=ALU.mult),
                                reads=[f"pt{idx % 2}", "am"], writes=[f"pt{idx % 2}"])
                        last = idx == len(klist) - 1
                        if ATT_STAGE < 2:
                            continue
                        S.op("pe", lambda e, pt=pt, tk=tk, idx=idx, last=last, nb=nb: e.matmul(
                            C.banks[nb][:, :], VA2[:, tk, g * 128:(g + 1) * 128], pt[:, 0:512], start=(idx == 0), stop=last),
                            reads=["VA2", f"pt{idx % 2}"], writes=[f"bank{nb}"])
                        S.op("pe", lambda e, pt=pt, idx=idx, last=last, db=db: e.matmul(
                            C.banks[db][:, :], C.ones_b[:], pt[:, 0:512], start=(idx == 0), stop=last),
                            reads=["ones_b", f"pt{idx % 2}"], writes=[f"bank{db}"])
                    if ATT_STAGE < 3:
                        continue
                    for s_ in range(4):
                        hh = s_
                        h = 4 * g + (0, 2, 1, 3)[s_]
                        S.op("dve", lambda e, hh=hh, h=h, db=db: e.tensor_scalar(
                            out=dn[:, hh * 128:(hh + 1) * 128], in0=C.banks[db][:, hh * 128:(hh + 1) * 128],
                            scalar1=esk[:, h:h + 1], scalar2=None, op0=ALU.add), reads=[f"bank{db}", "esk"], writes=["dn"])
                    S.op("dve", lambda e: e.reciprocal(out=dn[:, 0:512], in_=dn[:, 0:512]), reads=["dn"], writes=["dn"])
                    for s_ in range(4):
                        hh = s_
                        h = 4 * g + (0, 2, 1, 3)[s_]
                        c, off = h // 2, (h % 2) * 64
                        S.op("dve", lambda e, hh=hh, c=c, off=off, nb=nb: e.tensor_tensor(
                            out=ybuf[off:off + 64, c, q0:q0 + 128], in0=C.banks[nb][off:off + 64, hh * 128:(hh + 1) * 128],
                            in1=dn[off:off + 64, hh * 128:(hh + 1) * 128], op=ALU.mult),
                            reads=[f"bank{nb}", "dn"], writes=["Y"])

            for qt in range(2):
                attnA(qt * 128, [(0, None), (1, None)])
            for jl in range(16):
                kl = [(0, None), (1, None)]
                for o in (-1, 0, 1):
                    tk = 4 + jl + o
                    mk = None
                    if o == -1:
                        mk = am[:, 2, :] if jl == 0 else am[:, 0, :]
                    elif o == 1:
                        mk = am[:, 3, :] if jl == 15 else am[:, 1, :]
                    kl.append((tk, mk))
                attnA(CTX + jl * 128, kl)
            S.barrier()
            if stop == "C":
                S.kill()
            if debug:
                pass

            bc_ = [0]

            def attnB(q0, klist):
                S2 = [C.ps[:, 0:2, :].rearrange("p a b -> p (a b)"), C.ps[:, 2:4, :].rearrange("p a b -> p (a b)")]
                NUM = C.ps[:, 4:6, :].rearrange("p a b -> p (a b)")
                DEN = C.ps[:, 6:8, :].rearrange("p a b -> p (a b)")
                BST = int(os.environ.get("ATTB_STAGE", "9"))
                bc_[0] += 1
                if bc_[0] > int(os.environ.get("ATTB_N", "999")):
                    return
                for idx, (tk, mks) in enumerate(klist):
                    sb = idx % 2
                    pt = pts[idx % 2]
                    sk = f"S2_{sb}"
                    for s_ in range(8):
                        h = s_
                        hd = HORD[s_]
                        c, off = hd // 2, (hd % 2) * 64
                        S.op("pe", lambda e, h=h, c=c, off=off, tk=tk, sb=sb: e.matmul(
                            S2[sb][:, h * 128:(h + 1) * 128], BK[off:off + 64, c, tk * 128:(tk + 1) * 128],
                            QB[off:off + 64, c, q0:q0 + 128], start=True, stop=True), reads=["BK", "QB"], writes=[sk])
                    for hf in range(2):
                        S.op("act", lambda e, hf=hf, sb=sb, pt=pt: e.activation(
                            out=pt[:, hf * 512:(hf + 1) * 512], in_=S2[sb][:, hf * 512:(hf + 1) * 512], func=AF.Exp, scale=SCALE),
                            reads=[sk], writes=[f"pt{idx % 2}"])
                    for mk, full in (mks if BST >= 2 else []):
                        in1 = mk if full else mk.unsqueeze(1).to_broadcast([128, 8, 128])
                        S.op("pool", lambda e, pt=pt, in1=in1: e.tensor_tensor(
                            out=pt[:].rearrange("p (h q) -> p h q", h=8), in0=pt[:].rearrange("p (h q) -> p h q", h=8),
                            in1=in1, op=ALU.mult), reads=[f"pt{idx % 2}", "TV", "Tm", "vbm"], writes=[f"pt{idx % 2}"])
                    last = idx == len(klist) - 1
                    if BST < 3:
                        continue
                    for h in range(8):
                        c = HORD[h] // 2
                        S.op("pe", lambda e, h=h, c=c, pt=pt, tk=tk, idx=idx, last=last: e.matmul(
                            NUM[:, h * 128:(h + 1) * 128], VB[:, tk, c * 128:(c + 1) * 128], pt[:, h * 128:(h + 1) * 128],
                            start=(idx == 0 and h % 4 == 0), stop=(last and h % 4 == 3), skip_group_check=True),
                            reads=["VB", f"pt{idx % 2}"], writes=["NUM"])
                    for hf in range(2):
                        S.op("pe", lambda e, hf=hf, pt=pt, idx=idx, last=last: e.matmul(
                            DEN[:, hf * 512:(hf + 1) * 512], C.ones_b[:], pt[:, hf * 512:(hf + 1) * 512],
                            start=(idx == 0), stop=last), reads=["ones_b", f"pt{idx % 2}"], writes=["DEN"])
                if BST < 4:
                    return
                S.op("dve", lambda e: e.reciprocal(out=dn[:], in_=DEN), reads=["DEN"], writes=["dn"])
                for h in range(8):
                    c, off = HORD[h] // 2, (HORD[h] % 2) * 64
                    S.op("dve", lambda e, h=h, c=c, off=off: e.tensor_tensor(
                        out=ybuf[off:off + 64, 4 + c, q0:q0 + 128], in0=NUM[off:off + 64, h * 128:(h + 1) * 128],
                        in1=dn[off:off + 64, h * 128:(h + 1) * 128], op=ALU.mult), reads=["NUM", "dn"], writes=["Y"])

            for qt in range(2):
                attnB(qt * 128, [(0, []), (1, [])])
            for jl in range(16):
                kl = [(0, []), (1, [])]
                offs = b_offsets(jl)
                for oi, o in enumerate(offs):
                    tk = 4 + jl + o
                    if jl in (0, 1, 14, 15):
                        ci = (0, 1, 14, 15).index(jl)
                        mks = [(Tm[:, o + 3], True), (vbm[:, ci, oi, :], False)]
                    else:
                        mks = [(TV[:, o + 2], True)]
                    kl.append((tk, mks))
                attnB(CTX + jl * 128, kl)
            S.barrier()
    if debug:
        for k in range(8):
            S.dma("sp", lambda e, k=k: e.dma_start(out=dbg["y"][:, k, :], in_=ybuf[:, k, :]), reads=["Y"], is_out=True)
        S.barrier()
    if stop == "Y":
        S.kill()

    def x_src(a, b):
        if a < CTX:
            return xv[:, :, a:b]
        return xv[:, :, a + HALO:b + HALO]
    tail_phase(C, M, ybuf, w_out, x_src, x1T, w_r, b_r, ident_d, selE_d, wg, wu, wd, st0)
    S.barrier()
    st0.close()


NFFT = 2 * SEQ
_HC = {}


def hy_consts():
    if _HC:
        return _HC
    import ml_dtypes
    bf = ml_dtypes.bfloat16
    L = SEQ
    t = np.arange(L, dtype=np.float32)
    tn = t / np.float32(L - 1)
    bands = np.linspace(1e-4, 15, 16, dtype=np.float32)
    ang = (np.float32(2.0 * math.pi) * t[:, None] * bands[None] / np.float32(L)).astype(np.float32)
    feats = np.concatenate([tn[:, None], np.cos(ang), np.sin(ang)], axis=-1).astype(np.float32)
    _HC["featsT"] = np.ascontiguousarray(feats.T)
    deltas = np.abs(np.linspace(math.log(1e-2) / 1.5, math.log(1e-2) / 0.3, 512, dtype=np.float32))
    decay = np.exp(-tn[:, None] * deltas[None]).astype(np.float32)
    _HC["decay"] = np.ascontiguousarray(decay.reshape(32, 128, 512).transpose(1, 0, 2))
    k = np.arange(NFFT, dtype=np.float64)
    ctab = np.cos(2 * np.pi * k / NFFT).astype(np.float32)
    stab = np.sin(2 * np.pi * k / NFFT).astype(np.float32)
    a = np.arange(L, dtype=np.int64)
    idx = (a[:, None] * a[None, :]) % NFFT
    Fc = ctab[idx]
    Fs = stab[idx]
    sgn = np.where(a % 2 == 0, 1.0, -1.0).astype(np.float32)
    Fs_f = Fs.copy()
    Fs_f[:, 0] = sgn

    def blk(Mx):
        return np.ascontiguousarray(Mx.reshape(32, 128, 32, 128).transpose(2, 1, 0, 3)).astype(bf)
    _HC["FcB"] = blk(Fc)
    _HC["FsB"] = blk(Fs_f)
    _HC["FsBi"] = blk(np.ascontiguousarray(Fs_f.T))
    cv = np.ones((128, 4), np.float32)
    cv[0, 0] = 0.5
    cv[0, 1] = 0.0
    cv[:, 2] = 0.0
    cv[0, 2] = 0.5
    _HC["cv"] = cv
    return _HC


PI = math.pi


def sin_rr(C, out_ap, x, xkey, out_key, ti_, tf_, tc_, tag):
    S = C.S
    ki, kf, kc = f"rri{tag}", f"rrf{tag}", f"rrc{tag}"
    S.op("dve", lambda e: e.tensor_scalar(out=ti_, in0=x, scalar1=1.0 / (2.0 * PI), scalar2=None, op0=ALU.mult), reads=[xkey], writes=[ki])
    S.op("dve", lambda e: e.tensor_copy(out=tf_, in_=ti_), reads=[ki], writes=[kf])
    S.op("dve", lambda e: e.scalar_tensor_tensor(out=x, in0=tf_, scalar=-2.0 * PI, in1=x, op0=ALU.mult, op1=ALU.add), reads=[kf, xkey], writes=[xkey])
    S.op("dve", lambda e: e.tensor_single_scalar(out=tc_, in_=x, scalar=PI, op=ALU.is_gt), reads=[xkey], writes=[kc])
    S.op("dve", lambda e: e.scalar_tensor_tensor(out=x, in0=tc_, scalar=-2.0 * PI, in1=x, op0=ALU.mult, op1=ALU.add), reads=[kc, xkey], writes=[xkey])
    S.op("dve", lambda e: e.tensor_single_scalar(out=tc_, in_=x, scalar=-PI, op=ALU.is_lt), reads=[xkey], writes=[kc])
    S.op("dve", lambda e: e.scalar_tensor_tensor(out=x, in0=tc_, scalar=2.0 * PI, in1=x, op0=ALU.mult, op1=ALU.add), reads=[kc, xkey], writes=[xkey])
    S.op("act", lambda e: e.activation(out=out_ap, in_=x, func=AF.Sin), reads=[xkey], writes=[out_key])


def build_l1k():
    nc = bass.Bass("TRN2", target_bir_lowering=False)
    C = Ctx(nc)
    l1k_body(nc, C, IO(nc))
    C.S.finish()
    return nc


def l1k_body(nc, C, io, NCH=64):
    W4, W2 = 4 * NCH, 2 * NCH
    featsT = io.inp("featsT", [33, SEQ])
    w1 = io.inp("w1", [33, 64]); w2 = io.inp("w2", [64, 64]); w3s = io.inp("w3s", [64, W4])
    pvec = io.inp("pvec", [64, 4])
    b3s = io.inp("b3s", [W4])
    decay = io.inp("decay", [128, 32, NCH])
    FcB = io.inp("FcB", [32, 128, 32, 128], BF16)
    FsB = io.inp("FsB", [32, 128, 32, 128], BF16)
    cv_d = io.inp("cv", [128, 4])
    ktab = io.out("ktab", [32, 128, 3, W2])
    S = C.S
    st = contextlib.ExitStack()
    st2 = contextlib.ExitStack()
    ksum = S.sbuf("ksum", [128, 32, W2], BF16, st); kdif = S.sbuf("kdif", [128, 32, W2], BF16, st)
    cv = S.sbuf("cvs", [128, 4], F32, st)
    C.negpi = S.sbuf("negpi", [128, 1], F32, st2)
    S.op("dve", lambda e: e.memset(C.negpi[:], -PI), writes=["negpi"])
    ft = S.sbuf("ft", [33, SEQ], F32, st2)
    w1t = S.sbuf("w1t", [33, 64], F32, st2); w2t = S.sbuf("w2t", [64, 64], F32, st2); w3t = S.sbuf("w3t", [64, W4], F32, st2)
    pv = S.sbuf("pv", [64, 4], F32, st2); b3bc = S.sbuf("b3bc", [128, W4], F32, st2)
    dec = S.sbuf("dec", [128, 32, NCH], F32, st2)
    h1 = S.sbuf("h1", [64, SEQ], F32, st2); h2 = S.sbuf("h2", [64, SEQ], F32, st2)
    hraw = S.sbuf("hraw", [128, 32, W4], F32, st2)
    for (dst, src, key) in ((ft, featsT, "ft"), (w1t, w1, "w1t"), (w2t, w2, "w2t"), (w3t, w3s, "w3t"), (pv, pvec, "pv"),
                            (dec, decay, "dec"), (cv, cv_d, "cv")):
        S.dma("sp", lambda e, dst=dst, src=src: e.dma_start(out=dst[:], in_=src), writes=[key])
    S.dma("sp", lambda e: e.dma_start(out=b3bc[:], in_=b3s.partition_broadcast(128)), writes=["b3bc"])
    pre = [S.sbuf(f"pre{i}", [64, 512], F32, st2) for i in range(2)]
    rr_i = [S.sbuf(f"rr_i{i}", [64, 512], mybir.dt.int32, st2) for i in range(2)]
    rr_f = [S.sbuf(f"rr_f{i}", [64, 512], F32, st2) for i in range(2)]
    rr_c = [S.sbuf(f"rr_c{i}", [64, 512], F32, st2) for i in range(2)]
    for layer, (wt, wk, src, skey, dst, dkey, K_) in enumerate(((w1t, "w1t", ft, "ft", h1, "h1", 33), (w2t, "w2t", h1, "h1", h2, "h2", 64))):
        for tt in range(8):
            bk = tt % 2
            S.op("pe", lambda e, tt=tt, bk=bk: e.matmul(C.banks[bk][0:64, :], wt[0:K_, :], src[0:K_, tt * 512:(tt + 1) * 512], start=True, stop=True),
                 reads=[wk, skey], writes=[f"bank{bk}"])
            S.op("dve", lambda e, bk=bk: e.tensor_scalar(out=pre[bk][:], in0=C.banks[bk][0:64, :], scalar1=pv[:, 2 * layer:2 * layer + 1],
                                                        scalar2=pv[:, 2 * layer + 1:2 * layer + 2], op0=ALU.add, op1=ALU.mult),
                 reads=[f"bank{bk}", "pv"], writes=[f"pre{bk}"])
            sin_rr(C, dst[:, tt * 512:(tt + 1) * 512], pre[bk][:], f"pre{bk}", dkey, rr_i[bk][:], rr_f[bk][:], rr_c[bk][:], bk)
    ab = [S.sbuf(f"habs{i}", [128, W4], F32, st2) for i in range(2)]
    for ti in range(32):
        bk = ti % 2
        S.op("pe", lambda e, ti=ti, bk=bk: e.matmul(C.banks[bk][:, 0:W4], h2[0:64, ti * 128:(ti + 1) * 128], w3t[0:64, :], start=True, stop=True),
             reads=["h2", "w3t"], writes=[f"bank{bk}"])
        S.op("dve", lambda e, ti=ti, bk=bk: e.tensor_tensor(out=hraw[:, ti, :], in0=C.banks[bk][:, 0:W4], in1=b3bc[:], op=ALU.add),
             reads=[f"bank{bk}", "b3bc"], writes=["hraw"])
        S.op("dve", lambda e, ti=ti: e.tensor_tensor(out=hraw[:, ti, :].rearrange("p (a c) -> p a c", a=4), in0=hraw[:, ti, :].rearrange("p (a c) -> p a c", a=4),
                                                  in1=dec[:, ti, :].unsqueeze(1).to_broadcast([128, 4, NCH]), op=ALU.mult),
             reads=["hraw", "dec"], writes=["hraw"])
        S.op("act", lambda e, ti=ti, bk=bk: e.activation(out=ab[bk][:], in_=hraw[:, ti, :], func=AF.Abs),
             reads=["hraw"], writes=[f"habs{bk}"])
        S.op("pe", lambda e, ti=ti, bk=bk: e.matmul(C.banks[2][:, 0:W4], C.ones_f[:], ab[bk][:], start=(ti == 0), stop=(ti == 31)),
             reads=[f"habs{bk}", "ones_f"], writes=["bank2"])
    scl = S.sbuf("scl", [128, W2], F32, st2)
    S.op("dve", lambda e: e.tensor_copy(out=scl[:], in_=C.banks[2][:, 0:W2]), reads=["bank2"], writes=["scl"])
    S.op("dve", lambda e: e.tensor_tensor(out=scl[:], in0=scl[:], in1=C.banks[2][:, W2:W4], op=ALU.add), reads=["bank2", "scl"], writes=["scl"])
    S.op("dve", lambda e: e.tensor_scalar(out=scl[:], in0=scl[:], scalar1=EPS, scalar2=None, op0=ALU.add), reads=["scl"], writes=["scl"])
    S.op("dve", lambda e: e.reciprocal(out=scl[:], in_=scl[:]), reads=["scl"], writes=["scl"])
    hn = S.sbuf("hn", [128, W4], F32, st2)
    for ti in range(32):
        S.op("dve", lambda e, ti=ti: e.tensor_tensor(out=hn[:].rearrange("p (a c) -> p a c", a=2), in0=hraw[:, ti, :].rearrange("p (a c) -> p a c", a=2),
                                                  in1=scl[:].unsqueeze(1).to_broadcast([128, 2, W2]), op=ALU.mult),
             reads=["hraw", "scl"], writes=["hn"])
        if ti == 0:
            S.op("dve", lambda e: e.tensor_scalar(out=hn[:, W2:W4], in0=hn[:, W2:W4], scalar1=cv[:, 1:2], scalar2=None, op0=ALU.mult),
                 reads=["hn", "cv"], writes=["hn"])
        S.op("dve", lambda e, ti=ti: e.tensor_tensor(out=ksum[:, ti, :], in0=hn[:, 0:W2], in1=hn[:, W2:W4], op=ALU.add), reads=["hn"], writes=["ksum"])
        S.op("dve", lambda e, ti=ti: e.tensor_tensor(out=kdif[:, ti, :], in0=hn[:, 0:W2], in1=hn[:, W2:W4], op=ALU.subtract), reads=["hn"], writes=["kdif"])
    S.barrier()
    st2.close()
    fcs = [S.sbuf(f"fcb{i}", [128, 32, 128], BF16, st) for i in range(2)]
    fss = [S.sbuf(f"fsb{i}", [128, 32, 128], BF16, st) for i in range(2)]
    kt = [S.sbuf(f"kt{i}", [128, 3, W2], F32, st) for i in range(2)]
    for j in range(32):
        bi = j % 2
        S.dma("sp", lambda e, j=j, bi=bi: e.dma_start(out=fcs[bi][:], in_=FcB[j]), writes=[f"fcb{bi}"])
        S.dma("sp", lambda e, j=j, bi=bi: e.dma_start(out=fss[bi][:], in_=FsB[j]), writes=[f"fsb{bi}"])
        pb = 3 + 2 * bi
        for ti in range(32):
            S.op("pe", lambda e, ti=ti, bi=bi, pb=pb: e.matmul(C.banks[pb][:, 0:W2], fcs[bi][:, ti, :], ksum[:, ti, :], start=(ti == 0), stop=(ti == 31)),
                 reads=[f"fcb{bi}", "ksum"], writes=[f"bank{pb}"])
        for ti in range(32):
            S.op("pe", lambda e, ti=ti, bi=bi, pb=pb: e.matmul(C.banks[pb + 1][:, 0:W2], fss[bi][:, ti, :], kdif[:, ti, :], start=(ti == 0), stop=(ti == 31)),
                 reads=[f"fsb{bi}", "kdif"], writes=[f"bank{pb + 1}"])
        if j == 0:
            for ti in range(32):
                S.op("pe", lambda e, ti=ti, bi=bi: e.matmul(C.banks[7][:, 0:W2], fss[bi][:, ti, :], ksum[:, ti, :], start=(ti == 0), stop=(ti == 31)),
                     reads=[f"fsb{bi}", "ksum"], writes=["bank7"])
            S.op("dve", lambda e, bi=bi, pb=pb: e.tensor_scalar(out=kt[bi][:, 0, :], in0=C.banks[pb][:, 0:W2], scalar1=cv[:, 0:1], scalar2=None, op0=ALU.mult),
                 reads=[f"bank{pb}", "cv"], writes=[f"kt{bi}"])
            S.op("dve", lambda e, bi=bi, pb=pb: e.tensor_scalar(out=kt[bi][:, 1, :], in0=C.banks[pb + 1][:, 0:W2], scalar1=cv[:, 1:2], scalar2=None, op0=ALU.mult),
                 reads=[f"bank{pb + 1}", "cv"], writes=[f"kt{bi}"])
            S.op("dve", lambda e, bi=bi, pb=pb: e.tensor_scalar(out=kt[bi][:, 2, :], in0=C.banks[pb][:, 0:W2], scalar1=cv[:, 1:2], scalar2=None, op0=ALU.mult),
                 reads=[f"bank{pb}", "cv"], writes=[f"kt{bi}"])
            S.op("dve", lambda e, bi=bi: e.scalar_tensor_tensor(out=kt[bi][:, 2, :], in0=C.banks[7][:, 0:W2], scalar=cv[:, 2:3], in1=kt[bi][:, 2, :],
                                                               op0=ALU.mult, op1=ALU.add), reads=["bank7", "cv", f"kt{bi}"], writes=[f"kt{bi}"])
        else:
            S.op("act", lambda e, bi=bi, pb=pb: e.activation(out=kt[bi][:, 0, :], in_=C.banks[pb][:, 0:W2], func=AF.Copy), reads=[f"bank{pb}"], writes=[f"kt{bi}"])
            S.op("dve", lambda e, bi=bi, pb=pb: e.tensor_copy(out=kt[bi][:, 1, :], in_=C.banks[pb + 1][:, 0:W2]), reads=[f"bank{pb + 1}"], writes=[f"kt{bi}"])
            S.op("act", lambda e, bi=bi, pb=pb: e.activation(out=kt[bi][:, 2, :], in_=C.banks[pb][:, 0:W2], func=AF.Copy), reads=[f"bank{pb}"], writes=[f"kt{bi}"])
        S.dma("sp", lambda e, j=j, bi=bi: e.dma_start(out=ktab[j], in_=kt[bi][:]), reads=[f"kt{bi}"], writes=["ktblk"], is_out=True)
    S.barrier()
    st.close()


def l1k_inputs(inp, core):
    hc = hy_consts()
    ch = 64 * core + np.arange(64)
    cols = np.concatenate([d * 1024 + o * 512 + ch for d in range(2) for o in range(2)])
    pvec = np.stack([inp["hy_b1"][0], inp["hy_f1"][0], inp["hy_b2"][0], inp["hy_f2"][0]], axis=1).astype(np.float32)
    return {"featsT": hc["featsT"], "w1": np.ascontiguousarray(inp["hy_w1"][0]), "w2": np.ascontiguousarray(inp["hy_w2"][0]),
            "w3s": np.ascontiguousarray(inp["hy_w3"][0][:, cols]), "pvec": np.ascontiguousarray(pvec),
            "b3s": np.ascontiguousarray(inp["hy_b3"][0][cols]), "decay": np.ascontiguousarray(hc["decay"][:, :, ch]),
            "FcB": hc["FcB"], "FsB": hc["FsB"], "cv": hc["cv"]}


NT1 = CTX + SEQ


def l1_w_in_cols(half):
    P = rope_perm()
    q = np.arange(0, 512)
    qP = (q.reshape(8, 64)[:, P]).reshape(-1)
    k0 = 512 + np.arange(64)
    k1 = 576 + np.arange(64)
    v0 = 640 + np.arange(64)
    v1 = 704 + np.arange(64)
    c0 = half * 256
    hy = np.concatenate([768 + o * 512 + c0 + np.arange(256) for o in range(3)])
    return np.concatenate([q, qP, np.concatenate([k0, k0]), np.concatenate([k0[P], k0[P]]),
                           np.concatenate([k1, k1]), np.concatenate([k1[P], k1[P]]),
                           np.concatenate([v0, v0, v1, v1]), hy])


def build_l1a(stop=None):
    nc = bass.Bass("TRN2", target_bir_lowering=False)
    C = Ctx(nc)
    l1a_body(nc, C, IO(nc))
    C.S.finish()
    return nc


def l1a_body(nc, C, io, hy_halves=(None,)):
    xT = io.inp("xT", [8, 128, NT1])
    xTo = io.inp("xTo", [8, 128, OWN])
    ropeCq = io.inp("ropeCq", [128, OWN])
    ropeSq = io.inp("ropeSq", [128, OWN])
    modt = io.inp("mod", [128, 2, 6, 8])
    ng = io.inp("ng", [128, 2, 8])
    w_in = io.inp("w_in", [D, 2560])
    ropeC = io.inp("ropeC", [128, SEQ])
    ropeS = io.inp("ropeS", [128, SEQ])
    gvec_d = io.inp("gvec", [128, 4])
    bones_d = io.inp("bones", [128, 128])
    ident_d = io.inp("ident", [128, 128])
    if hy_halves == (None,):
        swT_d = io.inp("swT", [128, 6, 4])
        hb_d = io.inp("hbias", [2, 256])
        Kt = io.inp("Kt", [32, 128, 2, 3, 256])
    else:
        swT2_d = io.inp("swT2", [128, 2, 6, 4])
        hb2_d = io.inp("hbias2", [2, 2, 256])
        kt_blk = io.inp("kt_blk", None)
        Kt2 = [kt_blk[0:2], kt_blk[2:4]]
        w_hy = io.inp("w_hy", [D, 2, 768])
        w_hy_v = w_hy.rearrange("(k p) c o -> p k c o", p=128)
    FcB = io.inp("FcB", [32, 128, 32, 128], BF16)
    FsB = io.inp("FsB", [32, 128, 32, 128], BF16)
    FsBi = io.inp("FsBi", [32, 128, 32, 128], BF16)
    yatt_o = io.out("yatt", [4, 128, OWN], BF16)
    yhy_o = io.out("yhy", [128, 32, 256], BF16) if hy_halves == (None,) else io.out("yhy2", [2, 128, 32, 256], BF16)
    S = C.S
    st0 = contextlib.ExitStack()
    M = load_mod(C, modt, ng, st0)
    xv = xT.rearrange("k p t -> p k t")
    gvec = S.sbuf("gvec", [128, 4], F32, st0)
    bones = S.sbuf("bones", [128, 128], F32, st0)
    ident = S.sbuf("ident1", [128, 128], F32, st0)
    swT = S.sbuf("swT", [128, 6, 4], F32, st0)
    hbb = S.sbuf("hbb", [128, 2, 256], F32, st0)
    for dst, src, key in ((gvec, gvec_d, "gvec"), (bones, bones_d, "bones"), (ident, ident_d, "ident")):
        S.dma("sp", lambda e, dst=dst, src=src: e.dma_start(out=dst[:], in_=src), writes=[key])
    wv = w_in.rearrange("(k p) o -> p k o", p=128)
    def make_qk_block(src, srckey, rc, rs_, sq, rstd, ta, tb_):
        def qk_block(wa, wak, ja, wp, wpk, jp, gi, toks, out_fn, okey, rope):
            for (a, b, da, la) in toks:
                n = b - a
                for k in range(8):
                    S.op("pe", lambda e, k=k: e.matmul(C.banks[0][:, :n], wa[:, k, ja * 128:(ja + 1) * 128], src[:, k, a:b],
                                                       start=(k == 0), stop=(k == 7)), reads=[wak, srckey], writes=["bank0"])
                if rope:
                    for k in range(8):
                        S.op("pe", lambda e, k=k: e.matmul(C.banks[1][:, :n], wp[:, k, jp * 128:(jp + 1) * 128], src[:, k, a:b],
                                                           start=(k == 0), stop=(k == 7)), reads=[wpk, srckey], writes=["bank1"])
                S.op("act", lambda e: e.activation(out=sq[:, :n], in_=C.banks[0][:, :n], func=AF.Square), reads=["bank0"], writes=["sq1"])
                S.op("pe", lambda e: e.matmul(C.banks[2][:, :n], bones[:], sq[:, :n], start=True, stop=True), reads=["bones", "sq1"], writes=["bank2"])
                S.op("act", lambda e: e.activation(out=rstd[:, :n], in_=C.banks[2][:, :n], func=AF.Sqrt, bias=EPS, scale=1.0 / HD),
                     reads=["bank2"], writes=["rstd1"])
                S.op("dve", lambda e: e.reciprocal(out=rstd[:, :n], in_=rstd[:, :n]), reads=["rstd1"], writes=["rstd1"])
                if rope:
                    S.op("dve", lambda e: e.scalar_tensor_tensor(out=ta[:, :n], in0=C.banks[0][:, :n], scalar=gvec[:, gi:gi + 1], in1=rc[:, la:la + n],
                                                                 op0=ALU.mult, op1=ALU.mult), reads=["bank0", "gvec", "rc"], writes=["ta1"])
                    S.op("dve", lambda e: e.scalar_tensor_tensor(out=tb_[:, :n], in0=C.banks[1][:, :n], scalar=gvec[:, gi + 1:gi + 2], in1=rs_[:, la:la + n],
                                                                 op0=ALU.mult, op1=ALU.mult), reads=["bank1", "gvec", "rs"], writes=["tb1"])
                    S.op("pool", lambda e: e.tensor_tensor(out=ta[:, :n], in0=ta[:, :n], in1=tb_[:, :n], op=ALU.add), reads=["ta1", "tb1"], writes=["ta1"])
                    S.op("pool", lambda e: e.tensor_tensor(out=out_fn(da, da + n), in0=ta[:, :n], in1=rstd[:, :n], op=ALU.mult),
                         reads=["ta1", "rstd1"], writes=[okey])
                else:
                    S.op("dve", lambda e: e.scalar_tensor_tensor(out=out_fn(da, da + n), in0=C.banks[0][:, :n], scalar=gvec[:, gi:gi + 1], in1=rstd[:, :n],
                                                                 op0=ALU.mult, op1=ALU.mult), reads=["bank0", "gvec", "rstd1"], writes=[okey])
        return qk_block

    with contextlib.ExitStack() as stH:
        hx = S.sbuf("hx1", [128, 8, NT1], BF16, stH)
        with contextlib.ExitStack() as stA:
            xts = [S.sbuf(f"xa{i}", [128, 8, 512], F32, stA) for i in range(2)]

            def src_fn(a, b, ti):
                xt = xts[ti % 2]
                S.dma("sp", lambda e: e.dma_start(out=xt[:, :, :b - a], in_=xv[:, :, a:b]), writes=[f"xa{ti % 2}"])
                return xt[:, :, :b - a], f"xa{ti % 2}"
            norm_phase(C, M, 0, src_fn, None, NT1, CTX, hx, "hx", stA)
        S.barrier()
        for chalf in hy_halves:
            if chalf is None:
                hy_w = lambda grp: wv[:, :, 1792 + grp * 256:1792 + (grp + 1) * 256]
                swT_src, hb_src, Kt_c, yhy_c = swT_d, hb_d, Kt, yhy_o
            else:
                hy_w = lambda grp, chalf=chalf: w_hy_v[:, :, chalf, grp * 256:(grp + 1) * 256]
                swT_src, hb_src, Kt_c, yhy_c = swT2_d[:, chalf], hb2_d[chalf], Kt2[chalf], yhy_o[chalf]
            with contextlib.ExitStack() as stZ:
                zg = [S.sbuf(f"zg{i}", [128, 32, 256], BF16, stZ) for i in range(3)]
                S.dma("sp", lambda e: e.dma_start(out=swT[:], in_=swT_src), writes=["swT"])
                for o in range(2):
                    S.dma("sp", lambda e, o=o: e.dma_start(out=hbb[:, o, :], in_=hb_src[o].partition_broadcast(128)), writes=["hbb"])
                with contextlib.ExitStack() as stU:
                    wu_ = [S.sbuf(f"wu{i}", [128, 8, 256], BF16, stU) for i in range(2)]
                    U = S.sbuf("U", [128, SEQ + 2], F32, stU)
                    cvt = [S.sbuf(f"cv{i}", [128, 512], F32, stU) for i in range(2)]
                    S.op("dve", lambda e: e.memset(U[:, 0:1], 0.0), writes=["Upad0"])
                    S.op("dve", lambda e: e.memset(U[:, SEQ + 1:SEQ + 2], 0.0), writes=["Upad1"])
                    tcount = [0]
                    for grp in range(3):
                        wb = wu_[grp % 2]
                        S.dma("pool", lambda e, grp=grp, wb=wb: e.dma_start(out=wb[:], in_=hy_w(grp)), writes=[f"wu{grp % 2}"])
                        for cc in range(2):
                            c = grp * 2 + cc
                            for tt in range(8):
                                bk = tt % 2
                                for k in range(8):
                                    S.op("pe", lambda e, k=k, tt=tt, bk=bk, cc=cc, wb=wb: e.matmul(
                                        C.banks[bk][:, :], wb[:, k, cc * 128:(cc + 1) * 128], hx[:, k, CTX + tt * 512:CTX + (tt + 1) * 512],
                                        start=(k == 0), stop=(k == 7)), reads=[f"wu{grp % 2}", "hx"], writes=[f"bank{bk}"])
                                S.op("act", lambda e, tt=tt, bk=bk: e.activation(out=U[:, 1 + tt * 512:1 + (tt + 1) * 512], in_=C.banks[bk][:, :], func=AF.Copy),
                                     reads=[f"bank{bk}"], writes=["U"])
                            for tt in range(8):
                                cv_ = cvt[tt % 2]
                                ck = f"cv{tt % 2}"
                                a = tt * 512
                                S.op("dve", lambda e, a=a, c=c, cv_=cv_: e.tensor_scalar(out=cv_[:], in0=U[:, 1 + a:1 + a + 512], scalar1=swT[:, c, 1:2],
                                                                                       scalar2=swT[:, c, 3:4], op0=ALU.mult, op1=ALU.add),
                                     reads=["U", "swT", "Upad0", "Upad1"], writes=[ck])
                                S.op("dve", lambda e, a=a, c=c, cv_=cv_: e.scalar_tensor_tensor(out=cv_[:], in0=U[:, a:a + 512], scalar=swT[:, c, 0:1], in1=cv_[:],
                                                                                              op0=ALU.mult, op1=ALU.add), reads=["U", "swT", ck], writes=[ck])
                                S.op("dve", lambda e, a=a, c=c, cv_=cv_: e.scalar_tensor_tensor(out=cv_[:], in0=U[:, 2 + a:2 + a + 512], scalar=swT[:, c, 2:3], in1=cv_[:],
                                                                                              op0=ALU.mult, op1=ALU.add), reads=["U", "swT", ck], writes=[ck])
                                for s_ in range(4):
                                    tb = 2 + tcount[0] % 4
                                    tcount[0] += 1
                                    S.op("pe", lambda e, s_=s_, tb=tb, cv_=cv_: e.transpose(C.banks[tb][:, 0:128], cv_[:, s_ * 128:(s_ + 1) * 128], ident[:]),
                                         reads=[ck, "ident"], writes=[f"bank{tb}"])
                                    dst = zg[grp][:, tt * 4 + s_, cc * 128:(cc + 1) * 128]
                                    eng = "act" if s_ % 2 == 0 else "dve"
                                    if eng == "act":
                                        S.op("act", lambda e, tb=tb, dst=dst: e.activation(out=dst, in_=C.banks[tb][:, 0:128], func=AF.Copy), reads=[f"bank{tb}"], writes=[f"zg{grp}"])
                                    else:
                                        S.op("dve", lambda e, tb=tb, dst=dst: e.tensor_copy(out=dst, in_=C.banks[tb][:, 0:128]), reads=[f"bank{tb}"], writes=[f"zg{grp}"])
                S.barrier()
                S.barrier()
                with contextlib.ExitStack() as stF:
                    YW = S.sbuf("YW", [128, 2, 32, 256], BF16, stF)
                    fa = [S.sbuf(f"fa{i}", [128, 32, 128], BF16, stF) for i in range(2)]
                    fb = [S.sbuf(f"fb{i}", [128, 32, 128], BF16, stF) for i in range(2)]
                    kts = [S.sbuf(f"ktb{i}", [128, 3, 256], F32, stF) for i in range(2)]
                    tmp = [S.sbuf(f"hyt{i}", [128, 256], F32, stF) for i in range(4)]
                    z = zg[0]
                    for o in range(2):
                        gate = zg[1 + o]
                        for j in range(32):
                            bi = j % 2
                            S.dma("sp", lambda e, j=j, bi=bi: e.dma_start(out=fa[bi][:], in_=FcB[j]), writes=[f"fa{bi}"])
                            S.dma("sp", lambda e, j=j, bi=bi: e.dma_start(out=fb[bi][:], in_=FsB[j]), writes=[f"fb{bi}"])
                            if chalf is None:
                                S.dma("sp", lambda e, j=j, bi=bi, o=o: e.dma_start(out=kts[bi][:], in_=Kt_c[j, :, o]), writes=[f"ktb{bi}"])
                            else:
                                for q4 in range(2):
                                    S.dma("sp", lambda e, j=j, bi=bi, o=o, q4=q4: e.dma_start(
                                        out=kts[bi][:, :, q4 * 128:(q4 + 1) * 128], in_=Kt_c[q4][j, :, :, o * 128:(o + 1) * 128]), reads=["ktblk"], writes=[f"ktb{bi}"])
                            zc, zs = 2 * bi, 2 * bi + 1
                            for ti in range(32):
                                S.op("pe", lambda e, ti=ti, bi=bi, zc=zc: e.matmul(C.banks[zc][:, 0:256], fa[bi][:, ti, :], z[:, ti, :], start=(ti == 0), stop=(ti == 31)),
                                     reads=[f"fa{bi}", "z"], writes=[f"bank{zc}"])
                            for ti in range(32):
                                S.op("pe", lambda e, ti=ti, bi=bi, zs=zs: e.matmul(C.banks[zs][:, 0:256], fb[bi][:, ti, :], z[:, ti, :], start=(ti == 0), stop=(ti == 31)),
                                     reads=[f"fb{bi}", "z"], writes=[f"bank{zs}"])
                            kt = kts[bi]
                            S.op("dve", lambda e, zc=zc, kt=kt: e.tensor_tensor(out=tmp[0][:], in0=C.banks[zc][:, 0:256], in1=kt[:, 0, :], op=ALU.mult),
                                 reads=[f"bank{zc}", f"ktb{bi}"], writes=["hyt0"])
                            S.op("dve", lambda e, zs=zs, kt=kt: e.tensor_tensor(out=tmp[1][:], in0=C.banks[zs][:, 0:256], in1=kt[:, 1, :], op=ALU.mult),
                                 reads=[f"bank{zs}", f"ktb{bi}"], writes=["hyt1"])
                            S.op("pool", lambda e, j=j: e.tensor_tensor(out=YW[:, 0, j, :], in0=tmp[0][:], in1=tmp[1][:], op=ALU.subtract),
                                 reads=["hyt0", "hyt1"], writes=["YW"])
                            S.op("dve", lambda e, zc=zc, kt=kt: e.tensor_tensor(out=tmp[2][:], in0=C.banks[zc][:, 0:256], in1=kt[:, 1, :], op=ALU.mult),
                                 reads=[f"bank{zc}", f"ktb{bi}"], writes=["hyt2"])
                            S.op("dve", lambda e, zs=zs, kt=kt: e.tensor_tensor(out=tmp[3][:], in0=C.banks[zs][:, 0:256], in1=kt[:, 2, :], op=ALU.mult),
                                 reads=[f"bank{zs}", f"ktb{bi}"], writes=["hyt3"])
                            S.op("pool", lambda e, j=j: e.tensor_tensor(out=YW[:, 1, j, :], in0=tmp[2][:], in1=tmp[3][:], op=ALU.add),
                                 reads=["hyt2", "hyt3"], writes=["YW"])
                        for ni in range(32):
                            bi = ni % 2
                            S.dma("sp", lambda e, ni=ni, bi=bi: e.dma_start(out=fa[bi][:], in_=FcB[ni]), writes=[f"fa{bi}"])
                            S.dma("sp", lambda e, ni=ni, bi=bi: e.dma_start(out=fb[bi][:], in_=FsBi[ni]), writes=[f"fb{bi}"])
                            yb_ = 4 + bi
                            for fj in range(32):
                                S.op("pe", lambda e, fj=fj, bi=bi, yb_=yb_: e.matmul(C.banks[yb_][:, 0:256], fa[bi][:, fj, :], YW[:, 0, fj, :], start=(fj == 0), stop=False),
                                     reads=[f"fa{bi}", "YW"], writes=[f"bank{yb_}"])
                            for fj in range(32):
                                S.op("pe", lambda e, fj=fj, bi=bi, yb_=yb_: e.matmul(C.banks[yb_][:, 0:256], fb[bi][:, fj, :], YW[:, 1, fj, :], start=False, stop=(fj == 31)),
                                     reads=[f"fb{bi}", "YW"], writes=[f"bank{yb_}"])
                            S.op("pool", lambda e, ni=ni, o=o: e.tensor_tensor(out=tmp[0][:], in0=z[:, ni, :], in1=hbb[:, o, :], op=ALU.mult), reads=["z", "hbb"], writes=["hyt0"])
                            S.op("dve", lambda e, yb_=yb_: e.scalar_tensor_tensor(out=tmp[1][:], in0=C.banks[yb_][:, 0:256], scalar=2.0 / NFFT, in1=tmp[0][:],
                                                                                 op0=ALU.mult, op1=ALU.add), reads=[f"bank{yb_}", "hyt0"], writes=["hyt1"])
                            S.op("pool", lambda e, ni=ni, gate=gate: e.tensor_tensor(out=z[:, ni, :], in0=gate[:, ni, :], in1=tmp[1][:], op=ALU.mult),
                                 reads=["hyt1", "zg"], writes=["z"])
                        S.barrier()
                    for q in range(4):
                        S.dma("sp", lambda e, q=q: e.dma_start(out=yhy_c[:, q * 8:(q + 1) * 8, :], in_=z[:, q * 8:(q + 1) * 8, :]), reads=["z"], is_out=True)
                S.barrier()
        with contextlib.ExitStack() as stQ:
            KAB = S.sbuf("KABc", [128, 2, NT1], BF16, stQ)
            V2 = S.sbuf("V2c", [128, 34, 256], BF16, stQ)
            with contextlib.ExitStack() as stB:
                wts = [S.sbuf(f"wt{i}", [128, 8, 256], BF16, stB) for i in range(2)]
                rc = S.sbuf("rc", [128, SEQ], BF16, stB)
                rs_ = S.sbuf("rs", [128, SEQ], BF16, stB)
                sq = S.sbuf("sq1", [128, 512], F32, stB)
                rstd = S.sbuf("rstd1", [128, 512], F32, stB)
                ta = S.sbuf("ta1", [128, 512], F32, stB)
                tb_ = S.sbuf("tb1", [128, 512], F32, stB)
                S.dma("pool", lambda e: e.dma_start(out=rc[:], in_=ropeC), writes=["rc"])
                S.dma("pool", lambda e: e.dma_start(out=rs_[:], in_=ropeS), writes=["rs"])

                qk_block = make_qk_block(hx, "hx", rc, rs_, sq, rstd, ta, tb_)

                def loadw(i, c0):
                    S.dma("pool", lambda e: e.dma_start(out=wts[i][:], in_=wv[:, :, c0:c0 + 256]), writes=[f"wt{i}"])
                    return wts[i], f"wt{i}"
                lat_toks = [(CTX + 512 * i, CTX + 512 * (i + 1), CTX + 512 * i, 512 * i) for i in range(8)]
                ctx_toks = [(0, CTX, 0, 0)]
                wk1, wk1k = loadw(0, 1024)
                wk2, wk2k = loadw(1, 1280)
                for g, (wk_, wkk) in enumerate(((wk1, wk1k), (wk2, wk2k))):
                    qk_block(wk_, wkk, 0, wk_, wkk, 1, 2, ctx_toks, lambda a, b, g=g: KAB[:, g, a:b], "KAB", False)
                    qk_block(wk_, wkk, 0, wk_, wkk, 1, 2, lat_toks, lambda a, b, g=g: KAB[:, g, a:b], "KAB", True)
                wv_, wvk = loadw(0, 1536)
                for t in range(34):
                    bk = 4 + t % 2
                    for k in range(8):
                        S.op("pe", lambda e, k=k, t=t, bk=bk: e.matmul(C.banks[bk][:, 0:256], hx[:, k, t * 128:(t + 1) * 128], wv_[:, k, :],
                                                                       start=(k == 0), stop=(k == 7)), reads=[wvk, "hx"], writes=[f"bank{bk}"])
                    S.op("act", lambda e, t=t, bk=bk: e.activation(out=V2[:, t, :], in_=C.banks[bk][:, 0:256], func=AF.Copy), reads=[f"bank{bk}"], writes=["V2"])
            S.barrier()
            Q = S.sbuf("Qc", [128, 4, OWN], BF16, stQ)
            xvo = xTo.rearrange("k p t -> p k t")
            with contextlib.ExitStack() as stO:
                hxo = S.sbuf("hxo", [128, 8, OWN], BF16, stO)
                with contextlib.ExitStack() as stA:
                    xts = [S.sbuf(f"xo{i}", [128, 8, 512], F32, stA) for i in range(1)]

                    def src_fno(a, b, ti):
                        xt = xts[0]
                        S.dma("sp", lambda e: e.dma_start(out=xt[:, :, :b - a], in_=xvo[:, :, a:b]), writes=["xo0"])
                        return xt[:, :, :b - a], "xo0"
                    norm_phase(C, M, 0, src_fno, None, OWN, 0, hxo, "hxo", stA)
                S.barrier()
                with contextlib.ExitStack() as stB:
                    wts = [S.sbuf(f"wq{i}", [128, 8, 256], BF16, stB) for i in range(2)]
                    rcq = S.sbuf("rcq", [128, OWN], BF16, stB)
                    rsq = S.sbuf("rsq", [128, OWN], BF16, stB)
                    sq = S.sbuf("sq1", [128, 512], F32, stB)
                    rstd = S.sbuf("rstd1", [128, 512], F32, stB)
                    ta = S.sbuf("ta1", [128, 512], F32, stB)
                    tb_ = S.sbuf("tb1", [128, 512], F32, stB)
                    S.dma("pool", lambda e: e.dma_start(out=rcq[:], in_=ropeCq), writes=["rc"])
                    S.dma("pool", lambda e: e.dma_start(out=rsq[:], in_=ropeSq), writes=["rs"])
                    qkb = make_qk_block(hxo, "hxo", rcq, rsq, sq, rstd, ta, tb_)
                    own_toks = [(512 * i, 512 * (i + 1), 512 * i, 512 * i) for i in range(4)]
                    for jj in range(2):
                        S.dma("pool", lambda e, jj=jj: e.dma_start(out=wts[0][:], in_=wv[:, :, jj * 256:(jj + 1) * 256]), writes=["wq0"])
                        S.dma("pool", lambda e, jj=jj: e.dma_start(out=wts[1][:], in_=wv[:, :, 512 + jj * 256:512 + (jj + 1) * 256]), writes=["wq1"])
                        for j2 in range(2):
                            j = jj * 2 + j2
                            qkb(wts[0], "wq0", j2, wts[1], "wq1", j2, 0, own_toks, lambda a, b, j=j: Q[:, j, a:b], "Q", True)
                S.barrier()
            yat = S.sbuf("yat", [128, 4, OWN], BF16, stQ)
            with contextlib.ExitStack() as stC:
                pts = [S.sbuf(f"ptc{i}", [128, 512], BF16, stC) for i in range(2)]
                dn = S.sbuf("dnc", [128, 512], F32, stC)
                it = 0
                for qg in range(4):
                    for h in range(8):
                        g = h // 4
                        c, off = h // 2, (h % 2) * 64
                        nb, db = 2 + it % 2, 4 + it % 2
                        it += 1
                        for tk in range(34):
                            sb = tk % 2
                            pt = pts[sb]
                            S.op("pe", lambda e, tk=tk, sb=sb: e.matmul(C.banks[sb][:, :], KAB[off:off + 64, g, tk * 128:(tk + 1) * 128],
                                                                        Q[off:off + 64, c, qg * 512:(qg + 1) * 512], start=True, stop=True),
                                 reads=["KAB", "Q"], writes=[f"bank{sb}"])
                            S.op("act", lambda e, sb=sb, pt=pt: e.activation(out=pt[:], in_=C.banks[sb][:, :], func=AF.Exp, scale=SCALE),
                                 reads=[f"bank{sb}"], writes=[f"ptc{sb}"])
                            S.op("pe", lambda e, tk=tk, pt=pt: e.matmul(C.banks[nb][:, :], V2[:, tk, g * 128:(g + 1) * 128], pt[:], start=(tk == 0), stop=(tk == 33)),
                                 reads=["V2", f"ptc{sb}"], writes=[f"bank{nb}"])
                            S.op("pe", lambda e, tk=tk, pt=pt: e.matmul(C.banks[db][:, :], C.ones_b[:], pt[:], start=(tk == 0), stop=(tk == 33)),
                                 reads=["ones_b", f"ptc{sb}"], writes=[f"bank{db}"])
                        S.op("dve", lambda e: e.reciprocal(out=dn[:], in_=C.banks[db][:, :]), reads=[f"bank{db}"], writes=["dnc"])
                        S.op("dve", lambda e: e.tensor_tensor(out=yat[off:off + 64, c, qg * 512:(qg + 1) * 512], in0=C.banks[nb][off:off + 64, :],
                                                              in1=dn[off:off + 64, :], op=ALU.mult), reads=[f"bank{nb}", "dnc"], writes=["yat"])
                for c in range(4):
                    S.dma("sp", lambda e, c=c: e.dma_start(out=yatt_o[c], in_=yat[:, c, :]), reads=["yat"], is_out=True)
            S.barrier()
    S.barrier()
    st0.close()

def l1a_inputs(inp, mod, x1_full, Ktabs, core):
    hc = hy_consts()
    b, half = divmod(core, 2)
    order = np.arange(SEQ)
    xt = x1_full[b]
    xT = np.ascontiguousarray(xt)
    xTo = np.ascontiguousarray(xt[:, :, CTX + half * OWN:CTX + (half + 1) * OWN])
    rc, rs = rope_np(order)
    rcq, rsq = rope_np(half * OWN + np.arange(OWN))
    modt = np.stack([fm(mod[1, b].reshape(6, D)), fm(mod[1, 4].reshape(6, D))], axis=1)
    P = rope_perm()
    qn, kn = inp["c_qnorm"][0], inp["c_knorm"][0]
    gvec = np.stack([np.tile(qn, 2), np.tile(qn[P], 2), np.tile(kn, 2), np.tile(kn[P], 2)], axis=1).astype(np.float32)
    bones = np.zeros((128, 128), np.float32)
    bones[:64, :64] = 1.0
    bones[64:, 64:] = 1.0
    c0 = half * 256
    chs = np.concatenate([o * 512 + c0 + np.arange(256) for o in range(3)])
    sw = inp["hy_short_w"][0][:, chs]
    sb = inp["hy_short_b"][0][chs]
    swT = np.concatenate([sw, sb[None]], 0).T.reshape(6, 128, 4).transpose(1, 0, 2)
    return {"xT": xT, "xTo": xTo, "ropeCq": rcq, "ropeSq": rsq, "mod": np.ascontiguousarray(modt), "ng": fm(inp["norm_g"][1]),
            "w_in": np.ascontiguousarray(inp["w_in_odd"][0][:, l1_w_in_cols(half)]),
            "ropeC": rc, "ropeS": rs, "gvec": np.ascontiguousarray(gvec), "bones": bones, "ident": np.eye(128, dtype=np.float32),
            "swT": np.ascontiguousarray(swT, dtype=np.float32), "hbias": np.ascontiguousarray(inp["hy_bias"][0][:, c0:c0 + 256]),
            "Kt": Ktabs[half], "FcB": hc["FcB"], "FsB": hc["FsB"], "FsBi": hc["FsBi"]}, order


def assemble_ktabs(kouts):
    res = []
    for half in range(2):
        t = np.zeros((32, 128, 2, 3, 256), np.float32)
        for q in range(4):
            k = kouts[half * 4 + q].reshape(32, 128, 3, 2, 64)
            t[:, :, :, :, q * 64:(q + 1) * 64] = k.transpose(0, 1, 3, 2, 4)
        res.append(t)
    return res


def assemble_x1(l0_out):
    res = []
    for b in range(NB):
        a, c = l0_out[2 * b], l0_out[2 * b + 1]
        res.append(np.ascontiguousarray(np.concatenate([a[:, :, :CTX], a[:, :, CTX:], c[:, :, CTX:]], axis=2)))
    return res


L1B_TILES = [(512 * i, 512 * (i + 1)) for i in range(4)]


def build_l1b():
    nc = bass.Bass("TRN2", target_bir_lowering=False)
    C = Ctx(nc)
    l1b_body(nc, C, IO(nc))
    C.S.finish()
    return nc


def l1b_body(nc, C, io, fill_y=None):
    yT = io.inp("yT", [8, 128, OWN], BF16) if fill_y is None else None
    xTo = io.inp("xTo", [8, 128, OWN])
    modt = io.inp("mod", [128, 2, 6, 8])
    ng = io.inp("ng", [128, 2, 8])
    w_out = io.inp("w_out", [D, D])
    w_r = io.inp("w_r", [D, 16])
    b_r = io.inp("b_r", [16])
    wg = io.inp("wg", [NE, D, DE])
    wu = io.inp("wu", [NE, D, DE])
    wd = io.inp("wd", [NE, DE, D])
    ident_d = io.inp("ident", [128, 128])
    selE_d = io.inp("selE", [16, 16, 128])
    fg_d = io.inp("fg", [128, 8])
    outT = io.out("outT", [8, 128, OWN])
    S = C.S
    st0 = contextlib.ExitStack()
    M = load_mod(C, modt, ng, st0)
    ybuf = S.sbuf("ybuf1", [128, 8, OWN], BF16, st0)
    if fill_y is None:
        for k in range(8):
            S.dma("sp", lambda e, k=k: e.dma_start(out=ybuf[:, k, :], in_=yT[k]), writes=["Y"])
    else:
        fill_y(ybuf)
    xv = xTo.rearrange("k p t -> p k t")
    tail_phase(C, M, ybuf, w_out, lambda a, b: xv[:, :, a:b], outT, w_r, b_r, ident_d, selE_d, wg, wu, wd, st0,
               final_g_d=fg_d, nres=OWN, tiles=L1B_TILES, ctx_len=0)
    S.barrier()
    st0.close()


def l1b_inputs(inp, mod, x1_full, yatt, yhy, core):
    import ml_dtypes
    b, half = divmod(core, 2)
    yh = np.concatenate([np.asarray(yhy[2 * b + h]).transpose(1, 0, 2).reshape(SEQ, 256) for h in range(2)], axis=1)
    yh_own = yh[half * OWN:(half + 1) * OWN]
    yhT = np.ascontiguousarray(yh_own.T.reshape(4, 128, OWN))
    yT = np.ascontiguousarray(np.concatenate([np.asarray(yatt[core]), yhT], axis=0)).astype(ml_dtypes.bfloat16)
    xt = x1_full[b]
    modt = np.stack([fm(mod[1, b].reshape(6, D)), fm(mod[1, 4].reshape(6, D))], axis=1)
    selE = np.zeros((16, 16, 128), np.float32)
    for e in range(16):
        selE[e, e, :] = 1.0
    return {"yT": yT, "xTo": np.ascontiguousarray(xt[:, :, CTX + half * OWN:CTX + (half + 1) * OWN]),
            "mod": np.ascontiguousarray(modt), "ng": fm(inp["norm_g"][1]),
            "w_out": np.ascontiguousarray(inp["w_out_odd"][0]),
            "w_r": np.ascontiguousarray(inp["w_router"]), "b_r": np.ascontiguousarray(inp["b_router"]),
            "wg": np.ascontiguousarray(inp["moe_wg"][1]), "wu": np.ascontiguousarray(inp["moe_wu"][1]),
            "wd": np.ascontiguousarray(inp["moe_wd"][1]),
            "ident": np.eye(128, dtype=np.float32), "selE": selE, "fg": fm(inp["final_g"])}


def kernel_unfused(**inp):
    inp = {k: np.asarray(v) for k, v in inp.items()}
    cores = list(range(NCORES))
    mod = run_mod(inp)
    r0 = run_bass_kernel_spmd(build_l0(), [l0_inputs(inp, mod, c) for c in cores], core_ids=cores)
    x1_full = assemble_x1([r0.results[c]["x1T"] for c in cores])
    rk = run_bass_kernel_spmd(build_l1k(), [l1k_inputs(inp, c) for c in cores], core_ids=cores)
    Kt = assemble_ktabs([rk.results[c]["ktab"] for c in cores])
    ra = run_bass_kernel_spmd(build_l1a(), [l1a_inputs(inp, mod, x1_full, Kt, c)[0] for c in cores], core_ids=cores)
    yatt = [ra.results[c]["yatt"] for c in cores]
    yhy = [ra.results[c]["yhy"] for c in cores]
    rb = run_bass_kernel_spmd(build_l1b(), [l1b_inputs(inp, mod, x1_full, yatt, yhy, c) for c in cores], core_ids=cores)
    out = np.zeros((NB, SEQ, D), np.float32)
    for c in cores:
        b, half = divmod(c, 2)
        o = rb.results[c]["outT"]
        out[b, half * OWN:(half + 1) * OWN] = o.transpose(2, 0, 1).reshape(OWN, D)
    return out


def mod_phase(nc, C, cT_d, w_ada_d, b_ada_fm_d):
    S = C.S
    st = S.stack
    mods = [S.sbuf(f"modL{l}", [128, 2, 6, 8], F32, st) for l in range(2)]
    with contextlib.ExitStack() as st2:
        ct = S.sbuf("m_ct", [128, 8, 2], F32, st2)
        sg = S.sbuf("m_sg", [128, 8, 2], F32, st2)
        bfm = S.sbuf("m_b", [128, 2, 6, 8], F32, st2)
        wts = [S.sbuf(f"m_w{i}", [128, 8, 512], F32, st2) for i in range(2)]
        S.dma("sp", lambda e: e.dma_start(out=ct[:], in_=cT_d), writes=["m_ct"])
        S.dma("sp", lambda e: e.dma_start(out=bfm[:], in_=b_ada_fm_d), writes=["m_b"])
        S.op("act", lambda e: e.activation(out=sg[:], in_=ct[:], func=AF.Silu), reads=["m_ct"], writes=["m_sg"])
        it = 0
        for l in range(2):
            wv = w_ada_d[l].rearrange("(k p) o -> p k o", p=128)
            for grp in range(12):
                wi = it % 2
                it += 1
                S.dma("sp", lambda e, grp=grp, wi=wi, wv=wv: e.dma_start(out=wts[wi][:], in_=wv[:, :, grp * 512:(grp + 1) * 512]), writes=[f"m_w{wi}"])
                for oc in range(4):
                    col = grp * 4 + oc
                    j, kk = divmod(col, 8)
                    bk = col % 4
                    for k in range(8):
                        S.op("pe", lambda e, k=k, oc=oc, wi=wi, bk=bk: e.matmul(C.banks[bk][:, 0:2], wts[wi][:, k, oc * 128:(oc + 1) * 128], sg[:, k, :],
                                                                               start=(k == 0), stop=(k == 7)), reads=[f"m_w{wi}", "m_sg"], writes=[f"bank{bk}"])
                    S.op("dve", lambda e, l=l, j=j, kk=kk, bk=bk: e.tensor_scalar(out=mods[l][:, :, j, kk], in0=C.banks[bk][:, 0:2], scalar1=bfm[:, l, j, kk:kk + 1],
                                                                                 scalar2=None, op0=ALU.add), reads=[f"bank{bk}", "m_b"], writes=[f"modL{l}"])
        S.barrier()
    return mods


def build_fused():
    nc = bass.Bass("TRN2", target_bir_lowering=False)
    C = Ctx(nc)
    S = C.S

    def scr(name, shape, dt=F32):
        return nc.dram_tensor(name, list(shape), dt, kind="Internal").ap()
    E = {}
    for name, shape, dt in (
            ("cT", [128, 8, 2], F32), ("w_ada", [2, D, 6 * D], F32), ("b_ada_fm", [128, 2, 6, 8], F32),
            ("ng0", [128, 2, 8], F32), ("ng1", [128, 2, 8], F32),
            ("w_in0", [D, 3328], F32), ("w_out0", [D, D], F32), ("sink", [8], F32), ("rpbT", [128, 7, 8, 128], F32),
            ("vint", [128, 5, 128], F32), ("w_r", [D, 16], F32), ("b_r", [16], F32),
            ("wg0", [NE, D, DE], F32), ("wu0", [NE, D, DE], F32), ("wd0", [NE, DE, D], F32),
            ("wg1", [NE, D, DE], F32), ("wu1", [NE, D, DE], F32), ("wd1", [NE, DE, D], F32),
            ("ident", [128, 128], F32), ("selE", [16, 16, 128], F32),
            ("featsT", [33, SEQ], F32), ("w1", [33, 64], F32), ("w2", [64, 64], F32), ("pvec", [64, 4], F32),
            ("FcB", [32, 128, 32, 128], BF16), ("FsB", [32, 128, 32, 128], BF16), ("FsBi", [32, 128, 32, 128], BF16), ("cv", [128, 4], F32),
            ("w_in1", [D, 2560], F32), ("w_out1", [D, D], F32), ("ropeCn", [128, SEQ], F32), ("ropeSn", [128, SEQ], F32),
            ("ropeCq", [128, OWN], F32), ("ropeSq", [128, OWN], F32), ("gvec", [128, 4], F32), ("bones", [128, 128], F32),
            ("swT2", [128, 2, 6, 4], F32), ("hbias2", [2, 2, 256], F32), ("w_hy", [D, 2, 768], F32),
            ("selv", [128, 2], F32), ("fg", [128, 8], F32)):
        E[name] = din(nc, name, shape, dt)
    outT = dout(nc, "outT", [8, 128, OWN])
    x1nat = scr("x1nat", [8, 128, NT1])
    xown = scr("xown", [8, 128, OWN])
    kt_blk = [scr(f"ktblk{i}", [32, 128, 3, 256]) for i in range(4)]
    yatt_s = scr("yatt_s", [4, 128, OWN], BF16)
    yhy_s = scr("yhy_s", [2, 128, 32, 256], BF16)

    mods = mod_phase(nc, C, E["cT"], E["w_ada"], E["b_ada_fm"])
    for p in range(2):
        ov = {"mod": mods[0][:], "ng": E["ng0"], "w_in": E["w_in0"], "w_out": E["w_out0"], "sink": E["sink"], "rpbT": E["rpbT"],
              "vint": E["vint"], "w_r": E["w_r"], "b_r": E["b_r"], "wg": E["wg0"], "wu": E["wu0"], "wd": E["wd0"],
              "ident": E["ident"], "selE": E["selE"], "x1T": (x1nat, p)}
        _build_l0_body(nc, C, False, "full", IO(nc, ov, prefix=f"p{p}_"))
        S.barrier()
    for cb in range(4):
        ov = {"featsT": E["featsT"], "w1": E["w1"], "w2": E["w2"], "pvec": E["pvec"], "FcB": E["FcB"], "FsB": E["FsB"], "cv": E["cv"],
              "ktab": kt_blk[cb]}
        l1k_body(nc, C, IO(nc, ov, prefix=f"k{cb}_"), NCH=128)
        S.barrier()
    with contextlib.ExitStack() as st:
        sel = S.sbuf("selv_sb", [128, 2], F32, st)
        ta = [S.sbuf(f"bl_a{i}", [128, 512], F32, st) for i in range(2)]
        tb = [S.sbuf(f"bl_b{i}", [128, 512], F32, st) for i in range(2)]
        S.dma("sp", lambda e: e.dma_start(out=sel[:], in_=E["selv"]), writes=["selv"])
        it = 0
        for k in range(8):
            for tt in range(4):
                bi = it % 2
                it += 1
                a0 = CTX + tt * 512
                S.dma("sp", lambda e, k=k, a0=a0, bi=bi: e.dma_start(out=ta[bi][:], in_=x1nat[k][:, a0:a0 + 512]), reads=["x1nat"], writes=[f"bl_a{bi}"])
                S.dma("sp", lambda e, k=k, a0=a0, bi=bi: e.dma_start(out=tb[bi][:], in_=x1nat[k][:, OWN + a0:OWN + a0 + 512]), reads=["x1nat"], writes=[f"bl_b{bi}"])
                S.op("dve", lambda e, bi=bi: e.tensor_scalar(out=ta[bi][:], in0=ta[bi][:], scalar1=sel[:, 0:1], scalar2=None, op0=ALU.mult),
                     reads=[f"bl_a{bi}", "selv"], writes=[f"bl_a{bi}"])
                S.op("dve", lambda e, bi=bi: e.scalar_tensor_tensor(out=ta[bi][:], in0=tb[bi][:], scalar=sel[:, 1:2], in1=ta[bi][:], op0=ALU.mult, op1=ALU.add),
                     reads=[f"bl_a{bi}", f"bl_b{bi}", "selv"], writes=[f"bl_a{bi}"])
                S.dma("sp", lambda e, k=k, tt=tt, bi=bi: e.dma_start(out=xown[k][:, tt * 512:(tt + 1) * 512], in_=ta[bi][:]), reads=[f"bl_a{bi}"], writes=["xown"])
        S.barrier()
    ov = {"xT": x1nat, "xTo": xown, "ropeCq": E["ropeCq"], "ropeSq": E["ropeSq"], "mod": mods[1][:], "ng": E["ng1"], "w_in": E["w_in1"],
          "ropeC": E["ropeCn"], "ropeS": E["ropeSn"], "gvec": E["gvec"], "bones": E["bones"], "ident": E["ident"],
          "swT2": E["swT2"], "hbias2": E["hbias2"], "kt_blk": kt_blk, "w_hy": E["w_hy"],
          "FcB": E["FcB"], "FsB": E["FsB"], "FsBi": E["FsBi"], "yatt": yatt_s, "yhy2": yhy_s}
    l1a_body(nc, C, IO(nc, ov), hy_halves=(0, 1))
    S.barrier()

    def fill_y(ybuf):
        for c in range(4):
            S.dma("sp", lambda e, c=c: e.dma_start(out=ybuf[:, c, :], in_=yatt_s[c]), writes=["Y"])
        with contextlib.ExitStack() as st:
            sel = S.sbuf("selv_sb2", [128, 2], F32, st)
            idn = S.sbuf("idn2", [128, 128], F32, st)
            t0 = [S.sbuf(f"fy_a{i}", [128, 256], BF16, st) for i in range(2)]
            t1 = [S.sbuf(f"fy_b{i}", [128, 256], BF16, st) for i in range(2)]
            tf = [S.sbuf(f"fy_f{i}", [128, 256], F32, st) for i in range(2)]
            S.dma("sp", lambda e: e.dma_start(out=sel[:], in_=E["selv"]), writes=["selv2"])
            S.dma("sp", lambda e: e.dma_start(out=idn[:], in_=E["ident"]), writes=["idn2"])
            it = 0
            for c in range(2):
                for i in range(16):
                    bi = it % 2
                    it += 1
                    S.dma("sp", lambda e, c=c, i=i, bi=bi: e.dma_start(out=t0[bi][:], in_=yhy_s[c][:, i, :]), writes=[f"fy_a{bi}"])
                    S.dma("sp", lambda e, c=c, i=i, bi=bi: e.dma_start(out=t1[bi][:], in_=yhy_s[c][:, 16 + i, :]), writes=[f"fy_b{bi}"])
                    S.op("dve", lambda e, bi=bi: e.tensor_scalar(out=tf[bi][:], in0=t0[bi][:], scalar1=sel[:, 0:1], scalar2=None, op0=ALU.mult),
                         reads=[f"fy_a{bi}", "selv2"], writes=[f"fy_f{bi}"])
                    S.op("dve", lambda e, bi=bi: e.scalar_tensor_tensor(out=tf[bi][:], in0=t1[bi][:], scalar=sel[:, 1:2], in1=tf[bi][:], op0=ALU.mult, op1=ALU.add),
                         reads=[f"fy_b{bi}", f"fy_f{bi}", "selv2"], writes=[f"fy_f{bi}"])
                    for cc in range(2):
                        bk = (2 * it + cc) % 4
                        S.op("pe", lambda e, bi=bi, cc=cc, bk=bk: e.transpose(C.banks[bk][:, 0:128], tf[bi][:, cc * 128:(cc + 1) * 128], idn[:]),
                             reads=[f"fy_f{bi}", "idn2"], writes=[f"bank{bk}"])
                        S.op("act", lambda e, c=c, cc=cc, i=i, bk=bk: e.activation(out=ybuf[:, 4 + 2 * c + cc, i * 128:(i + 1) * 128], in_=C.banks[bk][:, 0:128], func=AF.Copy),
                             reads=[f"bank{bk}"], writes=["Y"])
            S.barrier()
    ov = {"xTo": xown, "mod": mods[1][:], "ng": E["ng1"], "w_out": E["w_out1"], "w_r": E["w_r"], "b_r": E["b_r"],
          "wg": E["wg1"], "wu": E["wu1"], "wd": E["wd1"], "ident": E["ident"], "selE": E["selE"], "fg": E["fg"], "outT": outT}
    l1b_body(nc, C, IO(nc, ov), fill_y=fill_y)
    S.finish()
    return nc


def fused_inputs(inp, core):
    hc = hy_consts()
    b, half = divmod(core, 2)
    m = {}
    cond = np.stack([inp["c"][b], inp["c_ctx"]], axis=0)
    m["cT"] = np.ascontiguousarray(cond.T.reshape(8, 128, 2).transpose(1, 0, 2))
    m["w_ada"] = np.ascontiguousarray(inp["w_ada"])
    m["b_ada_fm"] = np.ascontiguousarray(np.stack([fm(inp["b_ada"][l].reshape(6, D)) for l in range(2)], axis=1))
    m["ng0"] = fm(inp["norm_g"][0]); m["ng1"] = fm(inp["norm_g"][1])
    m["w_in0"] = np.ascontiguousarray(inp["w_in_even"][0][:, l0_w_in_cols()])
    m["w_out0"] = np.ascontiguousarray(inp["w_out_even"][0])
    m["sink"] = np.ascontiguousarray(inp["a_sink"][0]); m["rpbT"] = rpb_gather(inp["b_rpb"][0])
    m["vint"] = np.ascontiguousarray(np.stack([b_valid(10, o) for o in range(-2, 3)], axis=1))
    m["w_r"] = np.ascontiguousarray(inp["w_router"]); m["b_r"] = np.ascontiguousarray(inp["b_router"])
    for l in range(2):
        m[f"wg{l}"] = np.ascontiguousarray(inp["moe_wg"][l]); m[f"wu{l}"] = np.ascontiguousarray(inp["moe_wu"][l])
        m[f"wd{l}"] = np.ascontiguousarray(inp["moe_wd"][l])
    m["ident"] = np.eye(128, dtype=np.float32)
    selE = np.zeros((16, 16, 128), np.float32)
    for e in range(16):
        selE[e, e, :] = 1.0
    m["selE"] = selE
    k = np.arange(128)
    tri_lo = (k[:, None] >= k[None, :]).astype(np.float32)
    tri_hi = (k[:, None] <= k[None, :]).astype(np.float32)
    z = np.zeros_like(tri_lo)
    for p in range(2):
        pos = p * OWN - HALO + np.arange(NLAT)
        ok = (pos >= 0) & (pos < SEQ)
        xl = np.zeros((NTOK, D), np.float32)
        xl[:CTX] = inp["ctx"][b]
        xl[CTX:][ok] = inp["x"][b][pos[ok]]
        m[f"p{p}_xT"] = np.ascontiguousarray(xl.T.reshape(8, 128, NTOK))
        rc, rs = rope_np(np.clip(pos, 0, SEQ - 1))
        m[f"p{p}_ropeC"], m[f"p{p}_ropeS"] = rc, rs
        m[f"p{p}_amask"] = np.ascontiguousarray(np.stack([tri_lo, tri_hi, tri_lo if p == 1 else z, tri_hi if p == 0 else z], axis=1))
        vb = np.zeros((128, 4, 6, 128), np.float32)
        for ci, jl in enumerate((0, 1, 14, 15)):
            for oi, o in enumerate(b_offsets(jl)):
                vb[:, ci, oi] = b_valid(p * 16 + jl, o)
        m[f"p{p}_vb"] = vb
    m["featsT"] = hc["featsT"]; m["w1"] = np.ascontiguousarray(inp["hy_w1"][0]); m["w2"] = np.ascontiguousarray(inp["hy_w2"][0])
    m["pvec"] = np.ascontiguousarray(np.stack([inp["hy_b1"][0], inp["hy_f1"][0], inp["hy_b2"][0], inp["hy_f2"][0]], axis=1).astype(np.float32))
    m["FcB"], m["FsB"], m["FsBi"], m["cv"] = hc["FcB"], hc["FsB"], hc["FsBi"], hc["cv"]
    for cb in range(4):
        ch = 128 * cb + np.arange(128)
        cols = np.concatenate([d * 1024 + o * 512 + ch for d in range(2) for o in range(2)])
        m[f"k{cb}_w3s"] = np.ascontiguousarray(inp["hy_w3"][0][:, cols])
        m[f"k{cb}_b3s"] = np.ascontiguousarray(inp["hy_b3"][0][cols])
        m[f"k{cb}_decay"] = np.ascontiguousarray(hc["decay"][:, :, ch])
    m["w_in1"] = np.ascontiguousarray(inp["w_in_odd"][0][:, l1_w_in_cols(0)])
    m["w_out1"] = np.ascontiguousarray(inp["w_out_odd"][0])
    m["ropeCn"], m["ropeSn"] = rope_np(np.arange(SEQ))
    m["ropeCq"], m["ropeSq"] = rope_np(half * OWN + np.arange(OWN))
    P = rope_perm()
    qn, kn = inp["c_qnorm"][0], inp["c_knorm"][0]
    m["gvec"] = np.ascontiguousarray(np.stack([np.tile(qn, 2), np.tile(qn[P], 2), np.tile(kn, 2), np.tile(kn[P], 2)], axis=1).astype(np.float32))
    bones = np.zeros((128, 128), np.float32)
    bones[:64, :64] = 1.0
    bones[64:, 64:] = 1.0
    m["bones"] = bones
    swT2 = np.zeros((128, 2, 6, 4), np.float32)
    w_hy = np.zeros((D, 2, 768), np.float32)
    for c in range(2):
        chs = np.concatenate([o * 512 + c * 256 + np.arange(256) for o in range(3)])
        sw = inp["hy_short_w"][0][:, chs]
        sb = inp["hy_short_b"][0][chs]
        swT2[:, c] = np.concatenate([sw, sb[None]], 0).T.reshape(6, 128, 4).transpose(1, 0, 2)
        w_hy[:, c] = inp["w_in_odd"][0][:, 768 + chs]
    m["swT2"] = swT2
    m["w_hy"] = w_hy
    m["hbias2"] = np.ascontiguousarray(inp["hy_bias"][0].reshape(2, 2, 256).transpose(1, 0, 2))
    selv = np.zeros((128, 2), np.float32)
    selv[:, half] = 1.0
    m["selv"] = selv
    m["fg"] = fm(inp["final_g"])
    return m


def kernel(**inp):
    inp = {k: np.asarray(v) for k, v in inp.items()}
    cores = list(range(NCORES))
    res = run_bass_kernel_spmd(build_fused(), [fused_inputs(inp, c) for c in cores], core_ids=cores)
    out = np.zeros((NB, SEQ, D), np.float32)
    for c in cores:
        b, half = divmod(c, 2)
        o = res.results[c]["outT"]
        out[b, half * OWN:(half + 1) * OWN] = o.transpose(2, 0, 1).reshape(OWN, D)
    return out
```

```python
import contextlib
import math
import numpy as np
import concourse.bass as bass
import concourse.mybir as mybir
from concourse.bass_utils import run_bass_kernel_spmd

F32 = mybir.dt.float32
BF16 = mybir.dt.bfloat16
AF = mybir.ActivationFunctionType
ALU = mybir.AluOpType
AX = mybir.AxisListType

EPOCH = 3000
N_DMA_SEMS = 24
NCORES = 8

D = 1024
SEQ = 4096
NB = 4
CTX = 256
GW = 64
HD = 64
EPS = 1e-6
SCALE = HD ** -0.5
OWN = 2048
HALO = 256
NLAT = OWN + 2 * HALO
NTOK = CTX + NLAT
OWN0 = CTX + HALO
NRES = CTX + OWN
NE = 16
DE = 512


class Sched:
    ENGS = ("pe", "act", "dve", "pool", "sp")

    def __init__(self, nc):
        self.nc = nc
        self.stack = contextlib.ExitStack()
        self.eng = {"pe": nc.tensor, "act": nc.scalar, "dve": nc.vector, "pool": nc.gpsimd, "sp": nc.sync}
        self.seq = {e: 0 for e in self.ENGS}
        self.sems = {}
        self.dma_sems = []
        self.dma_uses = []
        self.dma_rr = 0
        self.dma_q = {}
        self.dead = False
        self.waited = {e: {} for e in self.ENGS}
        self.state = {}
        self.out_deps = []
        self.uid = 0

    def sbuf(self, name, shape, dtype, stack=None):
        self.uid += 1
        return (stack or self.stack).enter_context(self.nc.sbuf_tensor(f"sb{self.uid}_{name}", list(shape), dtype))

    def psum(self, name, shape, dtype, stack=None):
        return (stack or self.stack).enter_context(self.nc.psum_tensor(name, list(shape), dtype))

    def _new_sem(self, name):
        return self.stack.enter_context(self.nc.semaphore(name))

    def _sem(self, semkey):
        if semkey[0] == "c":
            k = (semkey[1], semkey[2])
            if k not in self.sems:
                self.sems[k] = self._new_sem(f"s_{semkey[1]}_{semkey[2]}")
            return self.sems[k]
        return self.dma_sems[semkey[1]]

    def _deps(self, eng, reads, writes):
        deps = []
        for k in reads:
            st = self.state.get(k)
            if st and st[0] is not None:
                deps.append(st[0])
        for k in writes:
            st = self.state.get(k)
            if st:
                if st[0] is not None:
                    deps.append(st[0])
                deps.extend(st[1].values())
        best = {}
        for semkey, val, deng in deps:
            if deng == eng and eng == "pe":
                continue
            if self.waited[eng].get(semkey, 0) >= val:
                continue
            best[semkey] = max(best.get(semkey, 0), val)
        for sk, v in best.items():
            self.waited[eng][sk] = v
        return list(best.items())

    def _commit(self, who, dep, reads, writes):
        for k in reads:
            st = self.state.setdefault(k, [None, {}])
            st[1][(who, dep[0])] = dep
        for k in writes:
            self.state[k] = [dep, {}]

    def _emit_waits(self, eng, waits):
        e = self.eng[eng]
        for sk, v in waits:
            e.wait_ge(self._sem(sk), v)

    def kill(self):
        self.barrier()
        self.dead = True

    def op(self, eng, fn, reads=(), writes=()):
        if self.dead:
            return
        waits = self._deps(eng, reads, writes)
        self.seq[eng] += 1
        epoch, val = divmod(self.seq[eng] - 1, EPOCH)
        val += 1
        semkey = ("c", eng, epoch)
        self._emit_waits(eng, waits)
        ins = fn(self.eng[eng])
        ins.then_inc(self._sem(semkey), 1)
        self._commit(eng, (semkey, val, eng), reads, writes)

    def dma(self, q, fn, reads=(), writes=(), is_out=False):
        if self.dead:
            return None
        if q not in self.dma_q:
            base = len(self.dma_sems)
            nq = 16 if q == "sp" else 8
            for i in range(nq):
                self.dma_sems.append(self._new_sem(f"s_dma_{q}_{i}"))
                self.dma_uses.append(0)
            self.dma_q[q] = [base, nq, 0]
        base, nq, rr = self.dma_q[q]
        j = base + rr
        self.dma_q[q][2] = (rr + 1) % nq
        semkey = ("d", j)
        waits = dict(self._deps(q, reads, writes))
        prev = self.dma_uses[j] * 16
        if prev > 0 and self.waited[q].get(semkey, 0) < prev:
            self.waited[q][semkey] = prev
            waits[semkey] = max(waits.get(semkey, 0), prev)
        self.dma_uses[j] += 1
        val = self.dma_uses[j] * 16
        self._emit_waits(q, list(waits.items()))
        ins = fn(self.eng[q])
        ins.then_inc(self.dma_sems[j], 16)
        dep = (semkey, val, "dma")
        self._commit("dma", dep, reads, writes)
        if is_out:
            self.out_deps.append(dep)
        return dep

    def barrier(self):
        if self.dead:
            return
        targets = []
        for e in self.ENGS:
            if self.seq[e] > 0:
                epoch, val = divmod(self.seq[e] - 1, EPOCH)
                targets.append((("c", e, epoch), val + 1, e))
        for j, u in enumerate(self.dma_uses):
            if u > 0:
                targets.append((("d", j), u * 16, "dma"))
        for e in self.ENGS:
            waits = []
            for sk, v, de in targets:
                if de == e:
                    continue
                if self.waited[e].get(sk, 0) >= v:
                    continue
                self.waited[e][sk] = v
                waits.append((sk, v))
            self._emit_waits(e, waits)
        self.state = {}

    def finish(self):
        self.dead = False
        self.barrier()
        self.stack.close()


class IO:
    def __init__(self, nc, ov=None, prefix=""):
        self.nc, self.ov, self.prefix = nc, dict(ov or {}), prefix

    def inp(self, name, shape, dt=F32):
        if name in self.ov:
            return self.ov[name]
        return din(self.nc, self.prefix + name, shape, dt)

    def out(self, name, shape, dt=F32):
        if name in self.ov:
            return self.ov[name]
        return dout(self.nc, self.prefix + name, shape, dt)


def din(nc, name, shape, dt=F32):
    return nc.dram_tensor(name, list(shape), dt, kind="ExternalInput").ap()


def dout(nc, name, shape, dt=F32):
    return nc.dram_tensor(name, list(shape), dt, kind="ExternalOutput").ap()


class Ctx:
    def __init__(self, nc):
        self.nc = nc
        self.S = Sched(nc)
        S = self.S
        self.ps = S.psum("psall", [128, 8, 512], F32)
        self.banks = [self.ps[:, i, :] for i in range(8)]
        self.ones_f = S.sbuf("ones_f", [128, 128], F32)
        self.ones_b = S.sbuf("ones_b", [128, 128], BF16)
        S.op("dve", lambda e: e.memset(self.ones_f[:], 1.0), writes=["ones_f"])
        S.op("dve", lambda e: e.memset(self.ones_b[:], 1.0), writes=["ones_b"])
        self.k = 0

    def key(self, base):
        self.k += 1
        return f"{base}#{self.k}"


def norm_mod_tile(C, xt, xkey, ntok, out_fn, gs_fn, sh_fn, tmp, bank, ranges, f32_out=None):
    S = C.S
    sq, rs = tmp["sq"], tmp["rs"]
    kq = C.key("sq")
    S.op("act", lambda e: e.activation(out=sq[:, :, :ntok], in_=xt, func=AF.Square), reads=[xkey], writes=[kq])
    bk = f"bank{bank}"
    ps = C.banks[bank]
    for k in range(8):
        S.op("pe", lambda e, k=k: e.matmul(ps[:, :ntok], C.ones_f[:], sq[:, k, :ntok], start=(k == 0), stop=(k == 7)),
             reads=[kq, "ones_f"], writes=[bk])
    kr = C.key("rs")
    S.op("act", lambda e: e.activation(out=rs[:, :ntok], in_=ps[:, :ntok], func=AF.Sqrt, bias=EPS, scale=1.0 / D),
         reads=[bk], writes=[kr])
    kr2 = C.key("rs2")
    S.op("dve", lambda e: e.reciprocal(out=rs[:, :ntok], in_=rs[:, :ntok]), reads=[kr], writes=[kr, kr2])
    for k in range(8):
        t = tmp["t"][k % 2]
        kt = f"normt{k % 2}"
        for (a, b, r) in ranges:
            gs, sh = gs_fn(r), sh_fn(r)
            S.op("dve", lambda e, k=k, a=a, b=b, gs=gs, t=t: e.scalar_tensor_tensor(
                out=t[:, a:b], in0=xt[:, k, a:b], scalar=gs[:, k:k + 1], in1=rs[:, a:b], op0=ALU.mult, op1=ALU.mult),
                reads=[xkey, kr2], writes=[kt])
            S.op("act", lambda e, k=k, a=a, b=b, sh=sh, t=t: e.activation(
                out=out_fn(k, a, b), in_=t[:, a:b], func=AF.Identity, bias=sh[:, k:k + 1], scale=1.0),
                reads=[kt], writes=[tmp["outkey"]])
            if f32_out is not None:
                S.op("pool", lambda e, k=k, a=a, b=b, sh=sh, t=t: e.tensor_scalar(
                    out=f32_out(k, a, b), in0=t[:, a:b], scalar1=sh[:, k:k + 1], scalar2=None, op0=ALU.add),
                    reads=[kt], writes=[tmp["f32key"]])


def build_mod():
    nc = bass.Bass("TRN2", target_bir_lowering=False)
    cT = din(nc, "cT", [128, 8, 5])
    w = din(nc, "w", [D, 1536])
    b = din(nc, "b", [1536])
    o = dout(nc, "o", [5, 1536])
    C = Ctx(nc)
    S = C.S
    ct = S.sbuf("ct", [128, 8, 5], F32)
    sg = S.sbuf("sg", [128, 8, 5], F32)
    wt = S.sbuf("wt", [128, 8, 1536], F32)
    bt = S.sbuf("bt", [5, 1536], F32)
    ot = S.sbuf("ot", [5, 1536], F32)
    S.dma("sp", lambda e: e.dma_start(out=ct[:], in_=cT), writes=["ct"])
    for k in range(8):
        S.dma("sp", lambda e, k=k: e.dma_start(out=wt[:, k, :], in_=w[k * 128:(k + 1) * 128, :]), writes=[f"wt{k}"])
    S.dma("sp", lambda e: e.dma_start(out=bt[:], in_=b.partition_broadcast(5)), writes=["bt"])
    S.op("act", lambda e: e.activation(out=sg[:], in_=ct[:], func=AF.Silu), reads=["ct"], writes=["sg"])
    for j in range(3):
        ps = C.banks[j]
        for k in range(8):
            S.op("pe", lambda e, k=k, j=j, ps=ps: e.matmul(ps[0:5, :], sg[:, k, :], wt[:, k, j * 512:(j + 1) * 512],
                                                           start=(k == 0), stop=(k == 7)),
                 reads=["sg", f"wt{k}"], writes=[f"bank{j}"])
        S.op("dve", lambda e, j=j, ps=ps: e.tensor_tensor(out=ot[:, j * 512:(j + 1) * 512], in0=ps[0:5, :],
                                                        in1=bt[:, j * 512:(j + 1) * 512], op=ALU.add),
             reads=[f"bank{j}", "bt"], writes=["ot"])
    S.dma("sp", lambda e: e.dma_start(out=o, in_=ot[:]), reads=["ot"], is_out=True)
    S.finish()
    return nc


def run_mod(inp):
    cond = np.concatenate([inp["c"], inp["c_ctx"][None]], axis=0)
    cT = np.ascontiguousarray(cond.T.reshape(8, 128, 5).transpose(1, 0, 2))
    maps = []
    for i in range(NCORES):
        l, q = divmod(i, 4)
        maps.append({"cT": cT,
                     "w": np.ascontiguousarray(inp["w_ada"][l][:, q * 1536:(q + 1) * 1536]),
                     "b": np.ascontiguousarray(inp["b_ada"][l][q * 1536:(q + 1) * 1536])})
    res = run_bass_kernel_spmd(build_mod(), maps, core_ids=list(range(NCORES)))
    mod = np.zeros((2, 5, 6144), np.float32)
    for i in range(NCORES):
        l, q = divmod(i, 4)
        mod[l][:, q * 1536:(q + 1) * 1536] = res.results[i]["o"]
    return mod


def rope_np(pos):
    pos = np.asarray(pos)
    row = (pos // GW).astype(np.float32)
    col = (pos % GW).astype(np.float32)
    inv = (10000.0 ** (-np.arange(0, 32, 2, dtype=np.float32) / 32)).astype(np.float32)
    ar = row[None, :] * inv[:, None]
    ac = col[None, :] * inv[:, None]
    Ct = np.concatenate([np.cos(ar), np.cos(ar), np.cos(ac), np.cos(ac)], 0)
    St = np.concatenate([-np.sin(ar), np.sin(ar), -np.sin(ac), np.sin(ac)], 0)
    return (np.ascontiguousarray(np.concatenate([Ct, Ct], 0), dtype=np.float32),
            np.ascontiguousarray(np.concatenate([St, St], 0), dtype=np.float32))


def rope_perm():
    return np.concatenate([np.arange(16, 32), np.arange(0, 16), np.arange(48, 64), np.arange(32, 48)])


def b_offsets(jl):
    if jl in (0, 1):
        return list(range(-2, 4))
    if jl in (14, 15):
        return list(range(-3, 3))
    return list(range(-2, 3))


def b_valid(j, o):
    kt = j + o
    m = np.zeros((128, 128), np.float32)
    if kt < 0 or kt >= 32:
        return m
    k = np.arange(128)
    krow = 2 * kt + k // 64
    kcol = k % 64
    qrow = 2 * j + k // 64
    qcol = k % 64
    rs = np.clip(qrow - 4, 0, 56)
    cs = np.clip(qcol - 8, 0, 48)
    ok_r = (krow[:, None] >= rs[None, :]) & (krow[:, None] < rs[None, :] + 8)
    ok_c = (kcol[:, None] >= cs[None, :]) & (kcol[:, None] < cs[None, :] + 16)
    return (ok_r & ok_c).astype(np.float32)


HORD = [0, 2, 4, 6, 1, 3, 5, 7]


def rpb_gather(rpb):
    k = np.arange(128)
    a, kc = k // 64, k % 64
    out = np.zeros((128, 7, 8, 128), np.float32)
    for oi, o in enumerate(range(-3, 4)):
        dr = 2 * o + a[:, None] - a[None, :]
        dc = kc[:, None] - kc[None, :]
        ok = (np.abs(dr) <= 7) & (np.abs(dc) <= 15)
        g = rpb[HORD][:, np.clip(dr + 7, 0, 14), np.clip(dc + 15, 0, 30)]
        g = np.where(ok[None], g, 0.0)
        out[:, oi] = g.transpose(1, 0, 2)
    return out


def l0_w_in_cols():
    P = rope_perm()
    aq = np.arange(0, 512)
    aqP = (aq.reshape(8, 64)[:, P]).reshape(-1)
    ak0 = 512 + np.arange(64)
    ak1 = 576 + np.arange(64)
    av0 = 640 + np.arange(64)
    av1 = 704 + np.arange(64)
    cols = [aq, aqP,
            np.concatenate([ak0, ak0]), np.concatenate([ak0[P], ak0[P]]),
            np.concatenate([ak1, ak1]), np.concatenate([ak1[P], ak1[P]]),
            768 + np.arange(512), 1280 + np.arange(512), 1792 + np.arange(512),
            np.concatenate([av0, av0, av1, av1])]
    return np.concatenate(cols)


def fm(v):
    v = np.asarray(v, np.float32)
    lead = v.shape[:-1]
    r = v.reshape(lead + (8, 128))
    return np.ascontiguousarray(np.moveaxis(r, -1, 0))


def l0_inputs(inp, mod, core):
    b, half = divmod(core, 2)
    pos = half * OWN - HALO + np.arange(NLAT)
    ok = (pos >= 0) & (pos < SEQ)
    xl = np.zeros((NTOK, D), np.float32)
    xl[:CTX] = inp["ctx"][b]
    xl[CTX:][ok] = inp["x"][b][pos[ok]]
    xT = np.ascontiguousarray(xl.T.reshape(8, 128, NTOK))
    rc, rs = rope_np(np.clip(pos, 0, SEQ - 1))
    modt = np.stack([fm(mod[0, b].reshape(6, D)), fm(mod[0, 4].reshape(6, D))], axis=1)
    k = np.arange(128)
    tri_lo = (k[:, None] >= k[None, :]).astype(np.float32)
    tri_hi = (k[:, None] <= k[None, :]).astype(np.float32)
    z = np.zeros_like(tri_lo)
    amask = np.stack([tri_lo, tri_hi, tri_lo if half == 1 else z, tri_hi if half == 0 else z], axis=1)
    vint = np.stack([b_valid(10, o) for o in range(-2, 3)], axis=1)
    vb = np.zeros((128, 4, 6, 128), np.float32)
    for ci, jl in enumerate((0, 1, 14, 15)):
        for oi, o in enumerate(b_offsets(jl)):
            vb[:, ci, oi] = b_valid(half * 16 + jl, o)
    selE = np.zeros((16, 16, 128), np.float32)
    for e in range(16):
        selE[e, e, :] = 1.0
    return {
        "xT": xT, "mod": np.ascontiguousarray(modt), "ng": fm(inp["norm_g"][0]),
        "w_in": np.ascontiguousarray(inp["w_in_even"][0][:, l0_w_in_cols()]),
        "ropeC": rc, "ropeS": rs, "w_out": np.ascontiguousarray(inp["w_out_even"][0]),
        "sink": np.ascontiguousarray(inp["a_sink"][0]), "rpbT": rpb_gather(inp["b_rpb"][0]),
        "amask": np.ascontiguousarray(amask), "vint": np.ascontiguousarray(vint), "vb": vb,
        "w_r": np.ascontiguousarray(inp["w_router"]), "b_r": np.ascontiguousarray(inp["b_router"]),
        "wg": np.ascontiguousarray(inp["moe_wg"][0]), "wu": np.ascontiguousarray(inp["moe_wu"][0]),
        "wd": np.ascontiguousarray(inp["moe_wd"][0]),
        "ident": np.eye(128, dtype=np.float32), "selE": selE,
    }


def load_mod(C, modt, ng, st):
    S = C.S
    mod_sb = S.sbuf("mod_sb", [128, 2, 6, 8], F32, st)
    ng_sb = S.sbuf("ng_sb", [128, 2, 8], F32, st)
    gs_sb = S.sbuf("gs_sb", [128, 2, 2, 8], F32, st)
    S.dma("sp", lambda e: e.dma_start(out=mod_sb[:], in_=modt), writes=["mod_sb"])
    S.dma("sp", lambda e: e.dma_start(out=ng_sb[:], in_=ng), writes=["ng_sb"])
    for i in range(2):
        for r in range(2):
            S.op("dve", lambda e, i=i, r=r: e.scalar_tensor_tensor(
                out=gs_sb[:, i, r, :], in0=mod_sb[:, r, 1 + 3 * i, :], scalar=1.0, in1=ng_sb[:, i, :],
                op0=ALU.add, op1=ALU.mult), reads=["mod_sb", "ng_sb"], writes=["gs_sb"])
    M = {"gs": [[gs_sb[:, i, r, :] for r in range(2)] for i in range(2)],
         "sh": [[mod_sb[:, r, 3 * i, :] for r in range(2)] for i in range(2)],
         "gate": [[mod_sb[:, r, 2 + 3 * i, :] for r in range(2)] for i in range(2)]}
    return M


def norm_phase(C, M, i, src_fn, src_key_fn, ntok_total, ctx_len, out_t, out_key, st, f32_cb=None):
    S = C.S
    tmp = {"sq": S.sbuf("n_sq", [128, 8, 512], F32, st), "rs": S.sbuf("n_rs", [128, 512], F32, st),
           "t": [S.sbuf("n_t0", [128, 512], F32, st), S.sbuf("n_t1", [128, 512], F32, st)],
           "outkey": out_key, "f32key": "h2f"}
    a = 0
    ti = 0
    while a < ntok_total:
        b = min(a + 512, ntok_total)
        n = b - a
        xt, xkey = src_fn(a, b, ti)
        ranges = []
        if a < ctx_len:
            ranges.append((0, min(ctx_len, b) - a, 1))
            if b > ctx_len:
                ranges.append((ctx_len - a, n, 0))
        else:
            ranges.append((0, n, 0))
        f32o = None
        if f32_cb is not None:
            f32o = f32_cb(a, b, ti, "pre")
        norm_mod_tile(C, xt, xkey, n, lambda k, aa, bb, a=a: out_t[:, k, a + aa:a + bb],
                      lambda r: M["gs"][i][r], lambda r: M["sh"][i][r], tmp, 7, ranges, f32_out=f32o)
        if f32_cb is not None:
            f32_cb(a, b, ti, "post")
        a = b
        ti += 1


RES_TILES = [(0, 256)] + [(256 + 512 * i, 256 + 512 * (i + 1)) for i in range(4)]


def moe_phase(C, xres, h2, combT, selE_sb, gate_fn, wg, wu, wd, st, nres=NRES, tiles=RES_TILES, ctx_len=CTX):
    S = C.S
    wgs = [S.sbuf(f"wg_sb{i}", [128, 8, DE], BF16, st) for i in range(2)]
    wus = [S.sbuf(f"wu_sb{i}", [128, 8, DE], BF16, st) for i in range(2)]
    wds = [S.sbuf(f"wd_sb{i}", [128, 4, D], BF16, st) for i in range(2)]
    he = [S.sbuf(f"he{i}", [128, 4, 512], BF16, st) for i in range(2)]
    sg = [S.sbuf(f"sg{i}", [128, 512], BF16, st) for i in range(2)]
    tt = [S.sbuf(f"tt{i}", [128, 512], BF16, st) for i in range(2)]
    bc = [S.sbuf(f"bc{i}", [128, 512], BF16, st) for i in range(2)]
    cnt = 0
    for ex in range(NE):
        wi = ex % 2
        S.dma("pool", lambda e, ex=ex, wi=wi: e.dma_start(out=wgs[wi][:], in_=wg[ex].rearrange("(k p) o -> p k o", p=128)),
              writes=[f"wg{wi}"])
        S.dma("pool", lambda e, ex=ex, wi=wi: e.dma_start(out=wus[wi][:], in_=wu[ex].rearrange("(k p) o -> p k o", p=128)),
              writes=[f"wu{wi}"])
        S.dma("pool", lambda e, ex=ex, wi=wi: e.dma_start(out=wds[wi][:], in_=wd[ex].rearrange("(k p) o -> p k o", p=128)),
              writes=[f"wd{wi}"])
        for (a, b) in tiles:
            n = b - a
            r = 1 if a < ctx_len else 0
            hi = cnt % 2
            cnt += 1
            S.op("pe", lambda e, ex=ex, a=a, b=b, n=n: e.matmul(C.banks[6][:, :n], selE_sb[:, ex, :], combT[:, a:b],
                                                                start=True, stop=True),
                 reads=["combT", "selE"], writes=["bank6"])
            S.op("act", lambda e, hi=hi, n=n: e.activation(out=bc[hi][:, :n], in_=C.banks[6][:, :n], func=AF.Copy),
                 reads=["bank6"], writes=[f"bc{hi}"])
            for hc in range(4):
                gb = (hc % 2) * 2
                gk, uk = f"bank{gb}", f"bank{gb + 1}"
                for k in range(8):
                    S.op("pe", lambda e, k=k, hc=hc, gb=gb, a=a, b=b, n=n, wi=wi: e.matmul(
                        C.banks[gb][:, :n], wgs[wi][:, k, hc * 128:(hc + 1) * 128], h2[:, k, a:b],
                        start=(k == 0), stop=(k == 7)), reads=[f"wg{wi}", "h2"], writes=[gk])
                for k in range(8):
                    S.op("pe", lambda e, k=k, hc=hc, gb=gb, a=a, b=b, n=n, wi=wi: e.matmul(
                        C.banks[gb + 1][:, :n], wus[wi][:, k, hc * 128:(hc + 1) * 128], h2[:, k, a:b],
                        start=(k == 0), stop=(k == 7)), reads=[f"wu{wi}", "h2"], writes=[uk])
                si = hc % 2
                S.op("act", lambda e, gb=gb, si=si, n=n: e.activation(out=sg[si][:, :n], in_=C.banks[gb][:, :n], func=AF.Silu),
                     reads=[gk], writes=[f"sg{si}"])
                S.op("dve", lambda e, gb=gb, si=si, n=n: e.tensor_tensor(out=tt[si][:, :n], in0=sg[si][:, :n],
                                                                        in1=C.banks[gb + 1][:, :n], op=ALU.mult),
                     reads=[f"sg{si}", uk], writes=[f"tt{si}"])
                S.op("pool", lambda e, si=si, hi=hi, hc=hc, n=n: e.tensor_tensor(out=he[hi][:, hc, :n], in0=tt[si][:, :n],
                                                                               in1=bc[hi][:, :n], op=ALU.mult),
                     reads=[f"tt{si}", f"bc{hi}"], writes=[f"he{hi}"])
            for oc in range(8):
                yb = 4 + (oc % 2)
                for hc in range(4):
                    S.op("pe", lambda e, oc=oc, hc=hc, yb=yb, n=n, hi=hi, wi=wi: e.matmul(
                        C.banks[yb][:, :n], wds[wi][:, hc, oc * 128:(oc + 1) * 128], he[hi][:, hc, :n],
                        start=(hc == 0), stop=(hc == 3)), reads=[f"wd{wi}", f"he{hi}"], writes=[f"bank{yb}"])
                g = gate_fn(r)
                S.op("dve", lambda e, oc=oc, yb=yb, a=a, b=b, n=n, g=g: e.scalar_tensor_tensor(
                    out=xres[:, oc, a:b], in0=C.banks[yb][:, :n], scalar=g[:, oc:oc + 1], in1=xres[:, oc, a:b],
                    op0=ALU.mult, op1=ALU.add), reads=[f"bank{yb}", f"xres{oc}"], writes=[f"xres{oc}"])


def router_phase(C, lg_all, ntile, b_r, st):
    S = C.S
    n = ntile
    def T(name, shape):
        return S.sbuf(name, shape, F32, st)
    br = T("r_br", [128, 16])
    s = T("r_s", [128, n, 16]); sel = T("r_sel", [128, n, 16])
    S.dma("sp", lambda e: e.dma_start(out=br[:], in_=b_r.partition_broadcast(128)), writes=["r_br"])
    S.op("act", lambda e: e.activation(out=s[:], in_=lg_all[:], func=AF.Sigmoid), reads=["lg_all"], writes=["r_s"])
    S.op("dve", lambda e: e.tensor_tensor(out=sel[:], in0=s[:], in1=br[:].unsqueeze(1).to_broadcast([128, n, 16]), op=ALU.add),
         reads=["r_s", "r_br"], writes=["r_sel"])
    sv = sel[:].rearrange("p t (g j) -> p t g j", j=4)
    pr = T("r_pr", [128, n, 4]); gsc = T("r_gsc", [128, n, 4])
    first = True
    for i in range(4):
        for j in range(i + 1, 4):
            S.op("dve", lambda e, i=i, j=j: e.tensor_tensor(out=pr[:], in0=sv[:, :, :, i], in1=sv[:, :, :, j], op=ALU.add),
                 reads=["r_sel"], writes=["r_pr"])
            if first:
                S.op("dve", lambda e: e.tensor_copy(out=gsc[:], in_=pr[:]), reads=["r_pr"], writes=["r_gsc"])
                first = False
            else:
                S.op("dve", lambda e: e.tensor_tensor(out=gsc[:], in0=gsc[:], in1=pr[:], op=ALU.max),
                     reads=["r_pr", "r_gsc"], writes=["r_gsc"])
    gmax = T("r_gmax", [128, n]); ing = T("r_ing", [128, n, 4])
    S.op("dve", lambda e: e.tensor_reduce(out=gmax[:], in_=gsc[:], axis=AX.X, op=ALU.max), reads=["r_gsc"], writes=["r_gmax"])
    S.op("dve", lambda e: e.tensor_tensor(out=ing[:], in0=gsc[:], in1=gmax[:].unsqueeze(2).to_broadcast([128, n, 4]), op=ALU.is_ge),
         reads=["r_gsc", "r_gmax"], writes=["r_ing"])
    cg = T("r_cg", [128, n, 4, 4]); cm = T("r_cm", [128, n, 4])
    cgv = cg[:]
    for i in range(4):
        firstj = True
        for j in range(4):
            if j == i:
                continue
            if firstj:
                S.op("dve", lambda e, i=i, j=j: e.tensor_tensor(out=cgv[:, :, :, i], in0=sv[:, :, :, j], in1=sv[:, :, :, i], op=ALU.is_gt),
                     reads=["r_sel"], writes=["r_cg"])
                firstj = False
            else:
                S.op("dve", lambda e, i=i, j=j: e.tensor_tensor(out=cm[:], in0=sv[:, :, :, j], in1=sv[:, :, :, i], op=ALU.is_gt),
                     reads=["r_sel"], writes=["r_cm"])
                S.op("dve", lambda e, i=i: e.tensor_tensor(out=cgv[:, :, :, i], in0=cgv[:, :, :, i], in1=cm[:], op=ALU.add),
                     reads=["r_cm", "r_cg"], writes=["r_cg"])
    selm = T("r_selm", [128, n, 4, 4])
    S.op("dve", lambda e: e.tensor_single_scalar(out=selm[:], in_=cg[:], scalar=1.5, op=ALU.is_lt), reads=["r_cg"], writes=["r_selm"])
    S.op("dve", lambda e: e.tensor_tensor(out=selm[:], in0=selm[:], in1=ing[:].unsqueeze(3).to_broadcast([128, n, 4, 4]), op=ALU.mult),
         reads=["r_selm", "r_ing"], writes=["r_selm"])
    comb = T("r_comb", [128, n, 16]); den = T("r_den", [128, n])
    S.op("dve", lambda e: e.tensor_tensor(out=comb[:], in0=s[:], in1=selm[:].rearrange("p t g j -> p t (g j)"), op=ALU.mult),
         reads=["r_s", "r_selm"], writes=["r_comb"])
    S.op("dve", lambda e: e.tensor_reduce(out=den[:], in_=comb[:], axis=AX.X, op=ALU.add), reads=["r_comb"], writes=["r_den"])
    S.op("dve", lambda e: e.reciprocal(out=den[:], in_=den[:]), reads=["r_den"], writes=["r_den"])
    S.op("dve", lambda e: e.tensor_tensor(out=comb[:], in0=comb[:], in1=den[:].unsqueeze(2).to_broadcast([128, n, 16]), op=ALU.mult),
         reads=["r_comb", "r_den"], writes=["r_comb"])
    return comb


def tail_phase(C, M, ybuf, w_out_d, x_src, xres_out_d, w_r, b_r, ident_d, selE_d, wg, wu, wd, st,
               final_g_d=None, nres=NRES, tiles=RES_TILES, ctx_len=CTX):
    S = C.S
    ntile = nres // 128
    xres = S.sbuf("xres", [128, 8, nres], F32, st)
    lg_all = S.sbuf("lg_all", [128, ntile, 16], F32, st)
    with contextlib.ExitStack() as st2:
        wo = S.sbuf("wo_sb", [128, 8, D], BF16, st2)
        xts = [S.sbuf(f"xt_b{i}", [128, 8, 512], F32, st2) for i in range(2)]
        S.dma("pool", lambda e: e.dma_start(out=wo[:], in_=w_out_d.rearrange("(k p) o -> p k o", p=128)), writes=["wo"])
        for ti, (a, b) in enumerate(tiles):
            n = b - a
            r = 1 if a < ctx_len else 0
            xt = xts[ti % 2]
            S.dma("sp", lambda e, a=a, b=b, n=n, xt=xt: e.dma_start(out=xt[:, :, :n], in_=x_src(a, b)), writes=[f"xtb{ti % 2}"])
            for oc in range(8):
                bk = oc % 4
                for k in range(8):
                    S.op("pe", lambda e, oc=oc, k=k, bk=bk, a=a, b=b, n=n: e.matmul(
                        C.banks[bk][:, :n], wo[:, k, oc * 128:(oc + 1) * 128], ybuf[:, k, a:b], start=(k == 0), stop=(k == 7)),
                        reads=["wo", "Y"], writes=[f"bank{bk}"])
                g = M["gate"][0][r]
                S.op("dve", lambda e, oc=oc, bk=bk, a=a, b=b, n=n, g=g, xt=xt: e.scalar_tensor_tensor(
                    out=xres[:, oc, a:b], in0=C.banks[bk][:, :n], scalar=g[:, oc:oc + 1], in1=xt[:, oc, :n],
                    op0=ALU.mult, op1=ALU.add), reads=[f"bank{bk}", f"xtb{ti % 2}"], writes=[f"xres{oc}"])
    S.barrier()
    h2 = ybuf
    with contextlib.ExitStack() as st2:
        wr = S.sbuf("wr_sb", [128, 8, 16], F32, st2)
        h2f = S.sbuf("h2f", [128, 8, 512], F32, st2)
        S.dma("sp", lambda e: e.dma_start(out=wr[:], in_=w_r.rearrange("(k p) o -> p k o", p=128)), writes=["wr"])

        def src_fn(a, b, ti):
            return xres[:, :, a:b], None

        def f32_cb(a, b, ti, when):
            if when == "pre":
                return lambda k, aa, bb: h2f[:, k, aa:bb]
            n = b - a
            for t in range(n // 128):
                tile_i = a // 128 + t
                for k in range(8):
                    S.op("pe", lambda e, k=k, t=t: e.matmul(C.banks[5][:, 0:16], h2f[:, k, t * 128:(t + 1) * 128], wr[:, k, :],
                                                            start=(k == 0), stop=(k == 7)),
                         reads=["h2f", "wr"], writes=["bank5"])
                S.op("dve", lambda e, tile_i=tile_i: e.tensor_copy(out=lg_all[:, tile_i, :], in_=C.banks[5][:, 0:16]),
                     reads=["bank5"], writes=["lg_all"])
            return None

        tmp_key = [f"xres{oc}" for oc in range(8)]

        def src_fn2(a, b, ti):
            return xres[:, :, a:b], "xres_all"
        norm_phase(C, M, 1, src_fn2, None, nres, ctx_len, h2, "h2", st2, f32_cb=f32_cb)
    S.barrier()
    ident = S.sbuf("ident", [128, 128], F32, st)
    selE = S.sbuf("selE_sb", [16, 16, 128], F32, st)
    combT = S.sbuf("combT", [16, nres], F32, st)
    with contextlib.ExitStack() as st2:
        comb = router_phase(C, lg_all, ntile, b_r, st2)
        S.dma("sp", lambda e: e.dma_start(out=ident[:], in_=ident_d), writes=["ident"])
        S.dma("sp", lambda e: e.dma_start(out=selE[:], in_=selE_d), writes=["selE"])
        for t in range(ntile):
            S.op("pe", lambda e, t=t: e.transpose(C.banks[t % 2][0:16, 0:128], comb[:, t, :], ident[:]),
                 reads=["r_comb", "ident"], writes=[f"bank{t % 2}"])
            S.op("dve", lambda e, t=t: e.tensor_copy(out=combT[:, t * 128:(t + 1) * 128], in_=C.banks[t % 2][0:16, 0:128]),
                 reads=[f"bank{t % 2}"], writes=["combT"])
    S.barrier()
    with contextlib.ExitStack() as st2:
        moe_phase(C, xres, h2, combT, selE, lambda r: M["gate"][1][r], wg, wu, wd, st2, nres=nres, tiles=tiles, ctx_len=ctx_len)
    S.barrier()
    if final_g_d is None and isinstance(xres_out_d, tuple):
        x1nat, pp = xres_out_d
        for k in range(8):
            if pp == 0:
                S.dma("sp", lambda e, k=k: e.dma_start(out=x1nat[k][:, 0:CTX], in_=xres[:, k, 0:CTX]), reads=[f"xres{k}"], writes=["x1nat"])
            S.dma("sp", lambda e, k=k: e.dma_start(out=x1nat[k][:, CTX + pp * OWN:CTX + (pp + 1) * OWN], in_=xres[:, k, CTX:]),
                  reads=[f"xres{k}"], writes=["x1nat"])
    elif final_g_d is None:
        for k in range(8):
            S.dma("sp", lambda e, k=k: e.dma_start(out=xres_out_d[k], in_=xres[:, k, :]), reads=[f"xres{k}"], is_out=True)
    else:
        with contextlib.ExitStack() as st2:
            fg = S.sbuf("fg", [128, 8], F32, st2)
            zsh = S.sbuf("zsh", [128, 8], F32, st2)
            dummy = S.sbuf("fdummy", [128, 8, 512], BF16, st2)
            fo = [S.sbuf(f"fo{i}", [128, 8, 512], F32, st2) for i in range(2)]
            S.dma("sp", lambda e: e.dma_start(out=fg[:], in_=final_g_d), writes=["fg"])
            S.op("dve", lambda e: e.memset(zsh[:], 0.0), writes=["zsh"])
            S.barrier()
            Mf = {"gs": [[fg[:], fg[:]]], "sh": [[zsh[:], zsh[:]]]}
            xo = xres_out_d.rearrange("k p t -> p k t")

            def src_fn3(a, b, ti):
                return xres[:, :, a:b], "xres_all"

            def f32_cb3(a, b, ti, when):
                if when == "pre":
                    return lambda k, aa, bb: fo[ti % 2][:, k, aa:bb]
                S.dma("sp", lambda e: e.dma_start(out=xo[:, :, a:b], in_=fo[ti % 2][:, :, :b - a]), reads=[f"fo{ti % 2}"], is_out=True)
                return None
            tmpn = {"sq": S.sbuf("f_sq", [128, 8, 512], F32, st2), "rs": S.sbuf("f_rs", [128, 512], F32, st2),
                    "t": [S.sbuf("f_t0", [128, 512], F32, st2), S.sbuf("f_t1", [128, 512], F32, st2)]}
            a = 0
            ti = 0
            while a < nres:
                b = min(a + 512, nres)
                tmpn["outkey"] = "fdummy"
                tmpn["f32key"] = f"fo{ti % 2}"
                f32o = f32_cb3(a, b, ti, "pre")
                norm_mod_tile(C, xres[:, :, a:b], "xres_all", b - a, lambda k, aa, bb: dummy[:, k, aa:bb],
                              lambda r: fg[:], lambda r: zsh[:], tmpn, 7, [(0, b - a, 0)], f32_out=f32o)
                f32_cb3(a, b, ti, "post")
                a = b
                ti += 1
    return xres


class _Stop(Exception):
    pass


def build_l0(debug=False, stop=None):
    nc = bass.Bass("TRN2", target_bir_lowering=False)
    C = Ctx(nc)
    _build_l0_body(nc, C, debug, stop, IO(nc))
    C.S.finish()
    return nc


def _build_l0_body(nc, C, debug, stop, io, write_ctx=True):
    xT = io.inp("xT", [8, 128, NTOK])
    modt = io.inp("mod", [128, 2, 6, 8])
    ng = io.inp("ng", [128, 2, 8])
    w_in = io.inp("w_in", [D, 3328])
    ropeC = io.inp("ropeC", [128, NLAT])
    ropeS = io.inp("ropeS", [128, NLAT])
    w_out = io.inp("w_out", [D, D])
    sink = io.inp("sink", [8])
    rpbT = io.inp("rpbT", [128, 7, 8, 128])
    amask_d = io.inp("amask", [128, 4, 128])
    vint_d = io.inp("vint", [128, 5, 128])
    vb_d = io.inp("vb", [128, 4, 6, 128])
    w_r = io.inp("w_r", [D, 16])
    b_r = io.inp("b_r", [16])
    if stop is None or stop == "full":
        wg = io.inp("wg", [NE, D, DE])
        wu = io.inp("wu", [NE, D, DE])
        wd = io.inp("wd", [NE, DE, D])
    else:
        wg = wu = wd = None
    ident_d = io.inp("ident", [128, 128])
    selE_d = io.inp("selE", [16, 16, 128])
    x1T = io.out("x1T", [8, 128, NRES])
    dbg = {}
    if debug:
        dbg["hx"] = io.out("d_hx", [128, 8, NTOK], BF16)
        dbg["y"] = io.out("d_y", [128, 8, NRES], BF16)

    S = C.S
    st0 = contextlib.ExitStack()
    M = load_mod(C, modt, ng, st0)
    xv = xT.rearrange("k p t -> p k t")
    hy = S.sbuf("hy", [128, 8, NTOK], BF16, st0)
    hx = hy
    ybuf = hy[:, :, 0:NRES]
    with contextlib.ExitStack() as stA:
        xts = [S.sbuf(f"xa{i}", [128, 8, 512], F32, stA) for i in range(2)]

        def src_fn(a, b, ti):
            xt = xts[ti % 2]
            S.dma("sp", lambda e: e.dma_start(out=xt[:, :, :b - a], in_=xv[:, :, a:b]), writes=[f"xa{ti % 2}"])
            return xt[:, :, :b - a], f"xa{ti % 2}"
        norm_phase(C, M, 0, src_fn, None, NTOK, CTX, hx, "hx", stA)
    S.barrier()
    if stop == "A0":
        S.kill()
    if debug:
        for k in range(8):
            S.dma("sp", lambda e, k=k: e.dma_start(out=dbg["hx"][:, k, :], in_=hx[:, k, :]), reads=["hx"], is_out=True)
        S.barrier()
    if stop == "A":
        S.kill()

    with contextlib.ExitStack() as stQ:
        QA = S.sbuf("QA", [128, 4, NRES], BF16, stQ)
        QB = S.sbuf("QB", [128, 4, NRES], BF16, stQ)
        KAB = S.sbuf("KAB", [128, 2, NTOK], BF16, stQ)
        BK = S.sbuf("BK", [128, 4, NTOK], BF16, stQ)
        VA2 = S.sbuf("VA2", [128, 22, 256], BF16, stQ)
        VB = S.sbuf("VB", [128, 22, 512], BF16, stQ)
        if True:
            with contextlib.ExitStack() as stB:
                wts = [S.sbuf(f"wt{i}", [128, 8, 512], BF16, stB) for i in range(2)]
                rc = S.sbuf("rc", [128, NLAT], F32, stB)
                rs_ = S.sbuf("rs", [128, NLAT], F32, stB)
                t1 = [S.sbuf(f"t1_{i}", [128, 512], F32, stB) for i in range(2)]
                t2 = [S.sbuf(f"t2_{i}", [128, 512], F32, stB) for i in range(2)]
                S.dma("sp", lambda e: e.dma_start(out=rc[:], in_=ropeC), writes=["rc"])
                S.dma("sp", lambda e: e.dma_start(out=rs_[:], in_=ropeS), writes=["rs"])
                wv = w_in.rearrange("(k p) o -> p k o", p=128)
                ngrp = [0]

                def load_w(c0, ncol):
                    wi = ngrp[0] % 2
                    ngrp[0] += 1
                    S.dma("pool", lambda e: e.dma_start(out=wts[wi][:, :, :ncol], in_=wv[:, :, c0:c0 + ncol]), writes=[f"wt{wi}"])
                    return wts[wi], f"wt{wi}"

                cnt = [0]

                def fm_block(wt, wkey, j, toks, out_fn, okey):
                    for (a, b, da) in toks:
                        n = b - a
                        bk = cnt[0] % 4
                        cnt[0] += 1
                        for k in range(8):
                            S.op("pe", lambda e, k=k: e.matmul(C.banks[bk][:, :n], wt[:, k, j * 128:(j + 1) * 128], hx[:, k, a:b],
                                                               start=(k == 0), stop=(k == 7)), reads=[wkey, "hx"], writes=[f"bank{bk}"])
                        S.op("act", lambda e: e.activation(out=out_fn(da, da + n), in_=C.banks[bk][:, :n], func=AF.Copy),
                             reads=[f"bank{bk}"], writes=[okey])

                def rope_block(wt, wkey, j, jp, toks, out_fn, okey):
                    for (a, b, da) in toks:
                        n = b - a
                        bk = (cnt[0] % 2) * 2
                        ti = cnt[0] % 2
                        cnt[0] += 1
                        la = a - CTX
                        for k in range(8):
                            S.op("pe", lambda e, k=k: e.matmul(C.banks[bk][:, :n], wt[:, k, j * 128:(j + 1) * 128], hx[:, k, a:b],
                                                               start=(k == 0), stop=(k == 7)), reads=[wkey, "hx"], writes=[f"bank{bk}"])
                        for k in range(8):
                            S.op("pe", lambda e, k=k: e.matmul(C.banks[bk + 1][:, :n], wt[:, k, jp * 128:(jp + 1) * 128], hx[:, k, a:b],
                                                               start=(k == 0), stop=(k == 7)), reads=[wkey, "hx"], writes=[f"bank{bk + 1}"])
                        S.op("dve", lambda e: e.tensor_tensor(out=t1[ti][:, :n], in0=C.banks[bk][:, :n], in1=rc[:, la:la + n], op=ALU.mult),
                             reads=[f"bank{bk}", "rc"], writes=[f"t1_{ti}"])
                        S.op("dve", lambda e: e.tensor_tensor(out=t2[ti][:, :n], in0=C.banks[bk + 1][:, :n], in1=rs_[:, la:la + n], op=ALU.mult),
                             reads=[f"bank{bk + 1}", "rs"], writes=[f"t2_{ti}"])
                        S.op("pool", lambda e: e.tensor_tensor(out=out_fn(da, da + n), in0=t1[ti][:, :n], in1=t2[ti][:, :n], op=ALU.add),
                             reads=[f"t1_{ti}", f"t2_{ti}"], writes=[okey])

                own_toks = [(OWN0 + 512 * i, OWN0 + 512 * (i + 1), CTX + 512 * i) for i in range(4)]
                ctx_toks = [(0, CTX, 0)]
                lat_toks = [(CTX + 512 * i, CTX + 512 * (i + 1), CTX + 512 * i) for i in range(5)]
                wA, kA = load_w(0, 512)
                wP, kP = load_w(512, 512)
                for j in range(4):
                    fm_block(wA, kA, j, ctx_toks, lambda a, b, j=j: QA[:, j, a:b], "QA")
                    for (a, b, da) in own_toks:
                        n = b - a
                        bk = (cnt[0] % 2) * 2
                        ti = cnt[0] % 2
                        cnt[0] += 1
                        la = a - CTX
                        for k in range(8):
                            S.op("pe", lambda e, k=k: e.matmul(C.banks[bk][:, :n], wA[:, k, j * 128:(j + 1) * 128], hx[:, k, a:b],
                                                               start=(k == 0), stop=(k == 7)), reads=[kA, "hx"], writes=[f"bank{bk}"])
                        for k in range(8):
                            S.op("pe", lambda e, k=k: e.matmul(C.banks[bk + 1][:, :n], wP[:, k, j * 128:(j + 1) * 128], hx[:, k, a:b],
                                                               start=(k == 0), stop=(k == 7)), reads=[kP, "hx"], writes=[f"bank{bk + 1}"])
                        S.op("dve", lambda e: e.tensor_tensor(out=t1[ti][:, :n], in0=C.banks[bk][:, :n], in1=rc[:, la:la + n], op=ALU.mult),
                             reads=[f"bank{bk}", "rc"], writes=[f"t1_{ti}"])
                        S.op("dve", lambda e: e.tensor_tensor(out=t2[ti][:, :n], in0=C.banks[bk + 1][:, :n], in1=rs_[:, la:la + n], op=ALU.mult),
                             reads=[f"bank{bk + 1}", "rs"], writes=[f"t2_{ti}"])
                        S.op("pool", lambda e, da=da, n=n: e.tensor_tensor(out=QA[:, j, da:da + n], in0=t1[ti][:, :n], in1=t2[ti][:, :n], op=ALU.add),
                             reads=[f"t1_{ti}", f"t2_{ti}"], writes=["QA"])
                wK, kK = load_w(1024, 512)
                for g in range(2):
                    fm_block(wK, kK, 2 * g, ctx_toks, lambda a, b, g=g: KAB[:, g, a:b], "KAB")
                    rope_block(wK, kK, 2 * g, 2 * g + 1, lat_toks, lambda a, b, g=g: KAB[:, g, a:b], "KAB")
                wq, kq = load_w(1536, 512)
                for j in range(4):
                    fm_block(wq, kq, j, ctx_toks + own_toks, lambda a, b, j=j: QB[:, j, a:b], "QB")
                wk_, kk_ = load_w(2048, 512)
                all_toks = ctx_toks + lat_toks
                for j in range(4):
                    fm_block(wk_, kk_, j, all_toks, lambda a, b, j=j: BK[:, j, a:b], "BK")
                wvb, kvb = load_w(2560, 512)
                wva, kva = load_w(3072, 256)
                for t in range(22):
                    bk = cnt[0] % 4
                    cnt[0] += 1
                    for k in range(8):
                        S.op("pe", lambda e, k=k: e.matmul(C.banks[bk][:, :], hx[:, k, t * 128:(t + 1) * 128], wvb[:, k, :],
                                                           start=(k == 0), stop=(k == 7)), reads=[kvb, "hx"], writes=[f"bank{bk}"])
                    S.op("act", lambda e: e.activation(out=VB[:, t, :], in_=C.banks[bk][:, :], func=AF.Copy),
                         reads=[f"bank{bk}"], writes=["VB"])
                    bk = cnt[0] % 4
                    cnt[0] += 1
                    for k in range(8):
                        S.op("pe", lambda e, k=k: e.matmul(C.banks[bk][:, :256], hx[:, k, t * 128:(t + 1) * 128], wva[:, k, :256],
                                                           start=(k == 0), stop=(k == 7)), reads=[kva, "hx"], writes=[f"bank{bk}"])
                    S.op("dve", lambda e: e.tensor_copy(out=VA2[:, t, :], in_=C.banks[bk][:, :256]),
                         reads=[f"bank{bk}"], writes=["VA2"])
            S.barrier()
        if stop == "B":
            S.kill()
        with contextlib.ExitStack() as stC:
            esk = S.sbuf("esk", [128, 8], F32, stC)
            am = S.sbuf("am", [128, 4, 128], BF16, stC)
            Tm = S.sbuf("Tm", [128, 7, 8, 128], BF16, stC)
            TV = S.sbuf("TV", [128, 5, 8, 128], BF16, stC)
            vint = S.sbuf("vint", [128, 5, 128], BF16, stC)
            vbm = S.sbuf("vbm", [128, 4, 6, 128], BF16, stC)
            pts = [S.sbuf(f"pt{i}", [128, 1024], BF16, stC) for i in range(2)]
            dn = S.sbuf("dn", [128, 1024], F32, stC)
            S.dma("sp", lambda e: e.dma_start(out=esk[:], in_=sink.partition_broadcast(128)), writes=["esk"])
            S.op("act", lambda e: e.activation(out=esk[:], in_=esk[:], func=AF.Exp), reads=["esk"], writes=["esk"])
            S.dma("pool", lambda e: e.dma_start(out=am[:], in_=amask_d), writes=["am"])
            S.dma("pool", lambda e: e.dma_start(out=vint[:], in_=vint_d), writes=["vint"])
            S.dma("pool", lambda e: e.dma_start(out=vbm[:], in_=vb_d), writes=["vbm"])
            with contextlib.ExitStack() as stT:
                stg = S.sbuf("rp_stage", [128, 8, 128], F32, stT)
                for oi in range(7):
                    S.dma("sp", lambda e, oi=oi: e.dma_start(out=stg[:], in_=rpbT[:, oi]), writes=["rp_stage"])
                    S.op("act", lambda e, oi=oi: e.activation(out=Tm[:, oi], in_=stg[:], func=AF.Exp), reads=["rp_stage"], writes=["Tm"])
                for oi in range(5):
                    S.op("dve", lambda e, oi=oi: e.tensor_tensor(out=TV[:, oi], in0=Tm[:, oi + 1],
                                                                 in1=vint[:, oi, :].unsqueeze(1).to_broadcast([128, 8, 128]), op=ALU.mult),
                         reads=["Tm", "vint"], writes=["TV"])
            S.barrier()
            if stop == "C0":
                S.kill()
            ac = [0]

            import os
            ATT_STAGE = int(os.environ.get("ATT_STAGE", "9"))
            ATT_N = int(os.environ.get("ATT_N", "999"))

            def attnA(q0, klist):
                for g in range(2):
                    if ac[0] >= ATT_N:
                        return
                    it = ac[0]
                    ac[0] += 1
                    nb, db = 4 + it % 2, 6 + it % 2
                    def emit_SA(idx):
                        tk = klist[idx][0]
                        sb = idx % 2
                        for s_ in range(4):
                            hh = (0, 2, 1, 3)[s_]
                            h = 4 * g + hh
                            c, off = h // 2, (h % 2) * 64
                            bnk = 2 * sb + s_ // 2
                            S.op("pe", lambda e, s_=s_, c=c, off=off, tk=tk, bnk=bnk: e.matmul(
                                C.banks[bnk][:, (s_ % 2) * 128:(s_ % 2 + 1) * 128], KAB[off:off + 64, g, tk * 128:(tk + 1) * 128],
                                QA[off:off + 64, c, q0:q0 + 128], start=True, stop=True),
                                reads=["KAB", "QA"], writes=[f"SA{sb}"])
                    emit_SA(0)
                    for idx, (tk, mk) in enumerate(klist):
                        sb = idx % 2
                        pt = pts[idx % 2]
                        if idx + 1 < len(klist):
                            emit_SA(idx + 1)
                        S.op("act", lambda e, sb=sb, pt=pt: e.activation(
                            out=pt[:, 0:512].rearrange("p (a b) -> p a b", a=2), in_=C.ps[:, 2 * sb:2 * sb + 2, 0:256], func=AF.Exp, scale=SCALE),
                            reads=[f"SA{sb}"], writes=[f"pt{idx % 2}"])
                        if mk is not None:
                            S.op("pool", lambda e, pt=pt, mk=mk: e.tensor_tensor(
                                out=pt[:, 0:512].rearrange("p (h q) -> p h q", h=4), in0=pt[:, 0:512].rearrange("p (h q) -> p h q", h=4),
                                in1=mk.unsqueeze(1).to_broadcast([128, 4, 128]), op=ALU.mult),
                                reads=[f"pt{idx % 2}", "am"], writes=[f"pt{idx % 2}"])
                        last = idx == len(klist) - 1
                        if ATT_STAGE < 2:
                            continue
                        S.op("pe", lambda e, pt=pt, tk=tk, idx=idx, last=last, nb=nb: e.matmul(
                            C.banks[nb][:, :], VA2[:, tk, g * 128:(g + 1) * 128], pt[:, 0:512], start=(idx == 0), stop=last),
                            reads=["VA2", f"pt{idx % 2}"], writes=[f"bank{nb}"])
                        S.op("pe", lambda e, pt=pt, idx=idx, last=last, db=db: e.matmul(
                            C.banks[db][:, :], C.ones_b[:], pt[:, 0:512], start=(idx == 0), stop=last),
                            reads=["ones_b", f"pt{idx % 2}"], writes=[f"bank{db}"])
                    if ATT_STAGE < 3:
                        continue
                    for s_ in range(4):
                        hh = s_
                        h = 4 * g + (0, 2, 1, 3)[s_]
                        S.op("dve", lambda e, hh=hh, h=h, db=db: e.tensor_scalar(
                            out=dn[:, hh * 128:(hh + 1) * 128], in0=C.banks[db][:, hh * 128:(hh + 1) * 128],
                            scalar1=esk[:, h:h + 1], scalar2=None, op0=ALU.add), reads=[f"bank{db}", "esk"], writes=["dn"])
                    S.op("dve", lambda e: e.reciprocal(out=dn[:, 0:512], in_=dn[:, 0:512]), reads=["dn"], writes=["dn"])
                    for s_ in range(4):
                        hh = s_
                        h = 4 * g + (0, 2, 1, 3)[s_]
                        c, off = h // 2, (h % 2) * 64
                        S.op("dve", lambda e, hh=hh, c=c, off=off, nb=nb: e.tensor_tensor(
                            out=ybuf[off:off + 64, c, q0:q0 + 128], in0=C.banks[nb][off:off + 64, hh * 128:(hh + 1) * 128],
                            in1=dn[off:off + 64, hh * 128:(hh + 1) * 128], op=ALU.mult),
                            reads=[f"bank{nb}", "dn"], writes=["Y"])

            for qt in range(2):
                attnA(qt * 128, [(0, None), (1, None)])
            for jl in range(16):
                kl = [(0, None), (1, None)]
                for o in (-1, 0, 1):
                    tk = 4 + jl + o
                    mk = None
                    if o == -1:
                        mk = am[:, 2, :] if jl == 0 else am[:, 0, :]
                    elif o == 1:
                        mk = am[:, 3, :] if jl == 15 else am[:, 1, :]
                    kl.append((tk, mk))
                attnA(CTX + jl * 128, kl)
            S.barrier()
            if stop == "C":
                S.kill()
            if debug:
                pass

            bc_ = [0]

            def attnB(q0, klist):
                S2 = [C.ps[:, 0:2, :].rearrange("p a b -> p (a b)"), C.ps[:, 2:4, :].rearrange("p a b -> p (a b)")]
                NUM = C.ps[:, 4:6, :].rearrange("p a b -> p (a b)")
                DEN = C.ps[:, 6:8, :].rearrange("p a b -> p (a b)")
                BST = int(os.environ.get("ATTB_STAGE", "9"))
                bc_[0] += 1
                if bc_[0] > int(os.environ.get("ATTB_N", "999")):
                    return
                def emit_SB(idx):
                    tk = klist[idx][0]
                    sb = idx % 2
                    for s_ in range(8):
                        h = s_
                        hd = HORD[s_]
                        c, off = hd // 2, (hd % 2) * 64
                        S.op("pe", lambda e, h=h, c=c, off=off, tk=tk, sb=sb: e.matmul(
                            S2[sb][:, h * 128:(h + 1) * 128], BK[off:off + 64, c, tk * 128:(tk + 1) * 128],
                            QB[off:off + 64, c, q0:q0 + 128], start=True, stop=True), reads=["BK", "QB"], writes=[f"S2_{sb}"])
                emit_SB(0)
                for idx, (tk, mks) in enumerate(klist):
                    sb = idx % 2
                    pt = pts[idx % 2]
                    sk = f"S2_{sb}"
                    if idx + 1 < len(klist):
                        emit_SB(idx + 1)
                    for hf in range(2):
                        S.op("act", lambda e, hf=hf, sb=sb, pt=pt: e.activation(
                            out=pt[:, hf * 512:(hf + 1) * 512], in_=S2[sb][:, hf * 512:(hf + 1) * 512], func=AF.Exp, scale=SCALE),
                            reads=[sk], writes=[f"pt{idx % 2}"])
                    for mk, full in (mks if BST >= 2 else []):
                        in1 = mk if full else mk.unsqueeze(1).to_broadcast([128, 8, 128])
                        S.op("pool", lambda e, pt=pt, in1=in1: e.tensor_tensor(
                            out=pt[:].rearrange("p (h q) -> p h q", h=8), in0=pt[:].rearrange("p (h q) -> p h q", h=8),
                            in1=in1, op=ALU.mult), reads=[f"pt{idx % 2}", "TV", "Tm", "vbm"], writes=[f"pt{idx % 2}"])
                    last = idx == len(klist) - 1
                    if BST < 3:
                        continue
                    for h in range(8):
                        c = HORD[h] // 2
                        S.op("pe", lambda e, h=h, c=c, pt=pt, tk=tk, idx=idx, last=last: e.matmul(
                            NUM[:, h * 128:(h + 1) * 128], VB[:, tk, c * 128:(c + 1) * 128], pt[:, h * 128:(h + 1) * 128],
                            start=(idx == 0 and h % 4 == 0), stop=(last and h % 4 == 3), skip_group_check=True),
                            reads=["VB", f"pt{idx % 2}"], writes=["NUM"])
                    for hf in range(2):
                        S.op("pe", lambda e, hf=hf, pt=pt, idx=idx, last=last: e.matmul(
                            DEN[:, hf * 512:(hf + 1) * 512], C.ones_b[:], pt[:, hf * 512:(hf + 1) * 512],
                            start=(idx == 0), stop=last), reads=["ones_b", f"pt{idx % 2}"], writes=["DEN"])
                if BST < 4:
                    return
                S.op("dve", lambda e: e.reciprocal(out=dn[:], in_=DEN), reads=["DEN"], writes=["dn"])
                for h in range(8):
                    c, off = HORD[h] // 2, (HORD[h] % 2) * 64
                    S.op("dve", lambda e, h=h, c=c, off=off: e.tensor_tensor(
                        out=ybuf[off:off + 64, 4 + c, q0:q0 + 128], in0=NUM[off:off + 64, h * 128:(h + 1) * 128],
                        in1=dn[off:off + 64, h * 128:(h + 1) * 128], op=ALU.mult), reads=["NUM", "dn"], writes=["Y"])

            for qt in range(2):
                attnB(qt * 128, [(0, []), (1, [])])
            for jl in range(16):
                kl = [(0, []), (1, [])]
                offs = b_offsets(jl)
                for oi, o in enumerate(offs):
                    tk = 4 + jl + o
                    if jl in (0, 1, 14, 15):
                        ci = (0, 1, 14, 15).index(jl)
                        mks = [(Tm[:, o + 3], True), (vbm[:, ci, oi, :], False)]
                    else:
                        mks = [(TV[:, o + 2], True)]
                    kl.append((tk, mks))
                attnB(CTX + jl * 128, kl)
            S.barrier()
    if debug:
        for k in range(8):
            S.dma("sp", lambda e, k=k: e.dma_start(out=dbg["y"][:, k, :], in_=ybuf[:, k, :]), reads=["Y"], is_out=True)
        S.barrier()
    if stop == "Y":
        S.kill()

    def x_src(a, b):
        if a < CTX:
            return xv[:, :, a:b]
        return xv[:, :, a + HALO:b + HALO]
    tail_phase(C, M, ybuf, w_out, x_src, x1T, w_r, b_r, ident_d, selE_d, wg, wu, wd, st0)
    S.barrier()
    st0.close()


NFFT = 2 * SEQ
_HC = {}


def hy_consts():
    if _HC:
        return _HC
    import ml_dtypes
    bf = ml_dtypes.bfloat16
    L = SEQ
    t = np.arange(L, dtype=np.float32)
    tn = t / np.float32(L - 1)
    bands = np.linspace(1e-4, 15, 16, dtype=np.float32)
    ang = (np.float32(2.0 * math.pi) * t[:, None] * bands[None] / np.float32(L)).astype(np.float32)
    feats = np.concatenate([tn[:, None], np.cos(ang), np.sin(ang)], axis=-1).astype(np.float32)
    _HC["featsT"] = np.ascontiguousarray(feats.T)
    deltas = np.abs(np.linspace(math.log(1e-2) / 1.5, math.log(1e-2) / 0.3, 512, dtype=np.float32))
    decay = np.exp(-tn[:, None] * deltas[None]).astype(np.float32)
    _HC["decay"] = np.ascontiguousarray(decay.reshape(32, 128, 512).transpose(1, 0, 2))
    k = np.arange(NFFT, dtype=np.float64)
    ctab = np.cos(2 * np.pi * k / NFFT).astype(np.float32)
    stab = np.sin(2 * np.pi * k / NFFT).astype(np.float32)
    a = np.arange(L, dtype=np.int64)
    idx = (a[:, None] * a[None, :]) % NFFT
    Fc = ctab[idx]
    Fs = stab[idx]
    sgn = np.where(a % 2 == 0, 1.0, -1.0).astype(np.float32)
    Fs_f = Fs.copy()
    Fs_f[:, 0] = sgn

    def blk(Mx):
        return np.ascontiguousarray(Mx.reshape(32, 128, 32, 128).transpose(2, 1, 0, 3)).astype(bf)
    _HC["FcB"] = blk(Fc)
    _HC["FsB"] = blk(Fs_f)
    _HC["FsBi"] = blk(np.ascontiguousarray(Fs_f.T))
    cv = np.ones((128, 4), np.float32)
    cv[0, 0] = 0.5
    cv[0, 1] = 0.0
    cv[:, 2] = 0.0
    cv[0, 2] = 0.5
    _HC["cv"] = cv
    return _HC


PI = math.pi


def sin_rr(C, out_ap, x, xkey, out_key, ti_, tf_, tc_, tag):
    S = C.S
    ki, kf, kc = f"rri{tag}", f"rrf{tag}", f"rrc{tag}"
    S.op("dve", lambda e: e.tensor_scalar(out=ti_, in0=x, scalar1=1.0 / (2.0 * PI), scalar2=None, op0=ALU.mult), reads=[xkey], writes=[ki])
    S.op("dve", lambda e: e.tensor_copy(out=tf_, in_=ti_), reads=[ki], writes=[kf])
    S.op("dve", lambda e: e.scalar_tensor_tensor(out=x, in0=tf_, scalar=-2.0 * PI, in1=x, op0=ALU.mult, op1=ALU.add), reads=[kf, xkey], writes=[xkey])
    S.op("dve", lambda e: e.tensor_single_scalar(out=tc_, in_=x, scalar=PI, op=ALU.is_gt), reads=[xkey], writes=[kc])
    S.op("dve", lambda e: e.scalar_tensor_tensor(out=x, in0=tc_, scalar=-2.0 * PI, in1=x, op0=ALU.mult, op1=ALU.add), reads=[kc, xkey], writes=[xkey])
    S.op("dve", lambda e: e.tensor_single_scalar(out=tc_, in_=x, scalar=-PI, op=ALU.is_lt), reads=[xkey], writes=[kc])
    S.op("dve", lambda e: e.scalar_tensor_tensor(out=x, in0=tc_, scalar=2.0 * PI, in1=x, op0=ALU.mult, op1=ALU.add), reads=[kc, xkey], writes=[xkey])
    S.op("act", lambda e: e.activation(out=out_ap, in_=x, func=AF.Sin), reads=[xkey], writes=[out_key])


def build_l1k():
    nc = bass.Bass("TRN2", target_bir_lowering=False)
    C = Ctx(nc)
    l1k_body(nc, C, IO(nc))
    C.S.finish()
    return nc


def l1k_body(nc, C, io, NCH=64):
    W4, W2 = 4 * NCH, 2 * NCH
    featsT = io.inp("featsT", [33, SEQ])
    w1 = io.inp("w1", [33, 64]); w2 = io.inp("w2", [64, 64]); w3s = io.inp("w3s", [64, W4])
    pvec = io.inp("pvec", [64, 4])
    b3s = io.inp("b3s", [W4])
    decay = io.inp("decay", [128, 32, NCH])
    FcB = io.inp("FcB", [32, 128, 32, 128], BF16)
    FsB = io.inp("FsB", [32, 128, 32, 128], BF16)
    cv_d = io.inp("cv", [128, 4])
    ktab = io.out("ktab", [32, 128, 3, W2])
    S = C.S
    st = contextlib.ExitStack()
    st2 = contextlib.ExitStack()
    ksum = S.sbuf("ksum", [128, 32, W2], BF16, st); kdif = S.sbuf("kdif", [128, 32, W2], BF16, st)
    cv = S.sbuf("cvs", [128, 4], F32, st)
    C.negpi = S.sbuf("negpi", [128, 1], F32, st2)
    S.op("dve", lambda e: e.memset(C.negpi[:], -PI), writes=["negpi"])
    ft = S.sbuf("ft", [33, SEQ], F32, st2)
    w1t = S.sbuf("w1t", [33, 64], F32, st2); w2t = S.sbuf("w2t", [64, 64], F32, st2); w3t = S.sbuf("w3t", [64, W4], F32, st2)
    pv = S.sbuf("pv", [64, 4], F32, st2); b3bc = S.sbuf("b3bc", [128, W4], F32, st2)
    dec = S.sbuf("dec", [128, 32, NCH], F32, st2)
    h1 = S.sbuf("h1", [64, SEQ], F32, st2); h2 = S.sbuf("h2", [64, SEQ], F32, st2)
    hraw = S.sbuf("hraw", [128, 32, W4], F32, st2)
    for (dst, src, key) in ((ft, featsT, "ft"), (w1t, w1, "w1t"), (w2t, w2, "w2t"), (w3t, w3s, "w3t"), (pv, pvec, "pv"),
                            (dec, decay, "dec"), (cv, cv_d, "cv")):
        S.dma("sp", lambda e, dst=dst, src=src: e.dma_start(out=dst[:], in_=src), writes=[key])
    S.dma("sp", lambda e: e.dma_start(out=b3bc[:], in_=b3s.partition_broadcast(128)), writes=["b3bc"])
    pre = [S.sbuf(f"pre{i}", [64, 512], F32, st2) for i in range(2)]
    rr_i = [S.sbuf(f"rr_i{i}", [64, 512], mybir.dt.int32, st2) for i in range(2)]
    rr_f = [S.sbuf(f"rr_f{i}", [64, 512], F32, st2) for i in range(2)]
    rr_c = [S.sbuf(f"rr_c{i}", [64, 512], F32, st2) for i in range(2)]
    for layer, (wt, wk, src, skey, dst, dkey, K_) in enumerate(((w1t, "w1t", ft, "ft", h1, "h1", 33), (w2t, "w2t", h1, "h1", h2, "h2", 64))):
        for tt in range(8):
            bk = tt % 2
            S.op("pe", lambda e, tt=tt, bk=bk: e.matmul(C.banks[bk][0:64, :], wt[0:K_, :], src[0:K_, tt * 512:(tt + 1) * 512], start=True, stop=True),
                 reads=[wk, skey], writes=[f"bank{bk}"])
            S.op("dve", lambda e, bk=bk: e.tensor_scalar(out=pre[bk][:], in0=C.banks[bk][0:64, :], scalar1=pv[:, 2 * layer:2 * layer + 1],
                                                        scalar2=pv[:, 2 * layer + 1:2 * layer + 2], op0=ALU.add, op1=ALU.mult),
                 reads=[f"bank{bk}", "pv"], writes=[f"pre{bk}"])
            sin_rr(C, dst[:, tt * 512:(tt + 1) * 512], pre[bk][:], f"pre{bk}", dkey, rr_i[bk][:], rr_f[bk][:], rr_c[bk][:], bk)
    ab = [S.sbuf(f"habs{i}", [128, W4], F32, st2) for i in range(2)]
    for ti in range(32):
        bk = ti % 2
        S.op("pe", lambda e, ti=ti, bk=bk: e.matmul(C.banks[bk][:, 0:W4], h2[0:64, ti * 128:(ti + 1) * 128], w3t[0:64, :], start=True, stop=True),
             reads=["h2", "w3t"], writes=[f"bank{bk}"])
        S.op("dve", lambda e, ti=ti, bk=bk: e.tensor_tensor(out=hraw[:, ti, :], in0=C.banks[bk][:, 0:W4], in1=b3bc[:], op=ALU.add),
             reads=[f"bank{bk}", "b3bc"], writes=["hraw"])
        S.op("dve", lambda e, ti=ti: e.tensor_tensor(out=hraw[:, ti, :].rearrange("p (a c) -> p a c", a=4), in0=hraw[:, ti, :].rearrange("p (a c) -> p a c", a=4),
                                                  in1=dec[:, ti, :].unsqueeze(1).to_broadcast([128, 4, NCH]), op=ALU.mult),
             reads=["hraw", "dec"], writes=["hraw"])
        S.op("act", lambda e, ti=ti, bk=bk: e.activation(out=ab[bk][:], in_=hraw[:, ti, :], func=AF.Abs),
             reads=["hraw"], writes=[f"habs{bk}"])
        S.op("pe", lambda e, ti=ti, bk=bk: e.matmul(C.banks[2][:, 0:W4], C.ones_f[:], ab[bk][:], start=(ti == 0), stop=(ti == 31)),
             reads=[f"habs{bk}", "ones_f"], writes=["bank2"])
    scl = S.sbuf("scl", [128, W2], F32, st2)
    S.op("dve", lambda e: e.tensor_copy(out=scl[:], in_=C.banks[2][:, 0:W2]), reads=["bank2"], writes=["scl"])
    S.op("dve", lambda e: e.tensor_tensor(out=scl[:], in0=scl[:], in1=C.banks[2][:, W2:W4], op=ALU.add), reads=["bank2", "scl"], writes=["scl"])
    S.op("dve", lambda e: e.tensor_scalar(out=scl[:], in0=scl[:], scalar1=EPS, scalar2=None, op0=ALU.add), reads=["scl"], writes=["scl"])
    S.op("dve", lambda e: e.reciprocal(out=scl[:], in_=scl[:]), reads=["scl"], writes=["scl"])
    hn = S.sbuf("hn", [128, W4], F32, st2)
    for ti in range(32):
        S.op("dve", lambda e, ti=ti: e.tensor_tensor(out=hn[:].rearrange("p (a c) -> p a c", a=2), in0=hraw[:, ti, :].rearrange("p (a c) -> p a c", a=2),
                                                  in1=scl[:].unsqueeze(1).to_broadcast([128, 2, W2]), op=ALU.mult),
             reads=["hraw", "scl"], writes=["hn"])
        if ti == 0:
            S.op("dve", lambda e: e.tensor_scalar(out=hn[:, W2:W4], in0=hn[:, W2:W4], scalar1=cv[:, 1:2], scalar2=None, op0=ALU.mult),
                 reads=["hn", "cv"], writes=["hn"])
        S.op("dve", lambda e, ti=ti: e.tensor_tensor(out=ksum[:, ti, :], in0=hn[:, 0:W2], in1=hn[:, W2:W4], op=ALU.add), reads=["hn"], writes=["ksum"])
        S.op("dve", lambda e, ti=ti: e.tensor_tensor(out=kdif[:, ti, :], in0=hn[:, 0:W2], in1=hn[:, W2:W4], op=ALU.subtract), reads=["hn"], writes=["kdif"])
    S.barrier()
    st2.close()
    fcs = [S.sbuf(f"fcb{i}", [128, 32, 128], BF16, st) for i in range(2)]
    fss = [S.sbuf(f"fsb{i}", [128, 32, 128], BF16, st) for i in range(2)]
    kt = [S.sbuf(f"kt{i}", [128, 3, W2], F32, st) for i in range(2)]
    for j in range(32):
        bi = j % 2
        S.dma("sp", lambda e, j=j, bi=bi: e.dma_start(out=fcs[bi][:], in_=FcB[j]), writes=[f"fcb{bi}"])
        S.dma("sp", lambda e, j=j, bi=bi: e.dma_start(out=fss[bi][:], in_=FsB[j]), writes=[f"fsb{bi}"])
        pb = 3 + 2 * bi
        for ti in range(32):
            S.op("pe", lambda e, ti=ti, bi=bi, pb=pb: e.matmul(C.banks[pb][:, 0:W2], fcs[bi][:, ti, :], ksum[:, ti, :], start=(ti == 0), stop=(ti == 31)),
                 reads=[f"fcb{bi}", "ksum"], writes=[f"bank{pb}"])
        for ti in range(32):
            S.op("pe", lambda e, ti=ti, bi=bi, pb=pb: e.matmul(C.banks[pb + 1][:, 0:W2], fss[bi][:, ti, :], kdif[:, ti, :], start=(ti == 0), stop=(ti == 31)),
                 reads=[f"fsb{bi}", "kdif"], writes=[f"bank{pb + 1}"])
        if j == 0:
            for ti in range(32):
                S.op("pe", lambda e, ti=ti, bi=bi: e.matmul(C.banks[7][:, 0:W2], fss[bi][:, ti, :], ksum[:, ti, :], start=(ti == 0), stop=(ti == 31)),
                     reads=[f"fsb{bi}", "ksum"], writes=["bank7"])
            S.op("dve", lambda e, bi=bi, pb=pb: e.tensor_scalar(out=kt[bi][:, 0, :], in0=C.banks[pb][:, 0:W2], scalar1=cv[:, 0:1], scalar2=None, op0=ALU.mult),
                 reads=[f"bank{pb}", "cv"], writes=[f"kt{bi}"])
            S.op("dve", lambda e, bi=bi, pb=pb: e.tensor_scalar(out=kt[bi][:, 1, :], in0=C.banks[pb + 1][:, 0:W2], scalar1=cv[:, 1:2], scalar2=None, op0=ALU.mult),
                 reads=[f"bank{pb + 1}", "cv"], writes=[f"kt{bi}"])
            S.op("dve", lambda e, bi=bi, pb=pb: e.tensor_scalar(out=kt[bi][:, 2, :], in0=C.banks[pb][:, 0:W2], scalar1=cv[:, 1:2], scalar2=None, op0=ALU.mult),
                 reads=[f"bank{pb}", "cv"], writes=[f"kt{bi}"])
            S.op("dve", lambda e, bi=bi: e.scalar_tensor_tensor(out=kt[bi][:, 2, :], in0=C.banks[7][:, 0:W2], scalar=cv[:, 2:3], in1=kt[bi][:, 2, :],
                                                               op0=ALU.mult, op1=ALU.add), reads=["bank7", "cv", f"kt{bi}"], writes=[f"kt{bi}"])
        else:
            S.op("act", lambda e, bi=bi, pb=pb: e.activation(out=kt[bi][:, 0, :], in_=C.banks[pb][:, 0:W2], func=AF.Copy), reads=[f"bank{pb}"], writes=[f"kt{bi}"])
            S.op("dve", lambda e, bi=bi, pb=pb: e.tensor_copy(out=kt[bi][:, 1, :], in_=C.banks[pb + 1][:, 0:W2]), reads=[f"bank{pb + 1}"], writes=[f"kt{bi}"])
            S.op("act", lambda e, bi=bi, pb=pb: e.activation(out=kt[bi][:, 2, :], in_=C.banks[pb][:, 0:W2], func=AF.Copy), reads=[f"bank{pb}"], writes=[f"kt{bi}"])
        S.dma("sp", lambda e, j=j, bi=bi: e.dma_start(out=ktab[j], in_=kt[bi][:]), reads=[f"kt{bi}"], writes=["ktblk"], is_out=True)
    S.barrier()
    st.close()


def l1k_inputs(inp, core):
    hc = hy_consts()
    ch = 64 * core + np.arange(64)
    cols = np.concatenate([d * 1024 + o * 512 + ch for d in range(2) for o in range(2)])
    pvec = np.stack([inp["hy_b1"][0], inp["hy_f1"][0], inp["hy_b2"][0], inp["hy_f2"][0]], axis=1).astype(np.float32)
    return {"featsT": hc["featsT"], "w1": np.ascontiguousarray(inp["hy_w1"][0]), "w2": np.ascontiguousarray(inp["hy_w2"][0]),
            "w3s": np.ascontiguousarray(inp["hy_w3"][0][:, cols]), "pvec": np.ascontiguousarray(pvec),
            "b3s": np.ascontiguousarray(inp["hy_b3"][0][cols]), "decay": np.ascontiguousarray(hc["decay"][:, :, ch]),
            "FcB": hc["FcB"], "FsB": hc["FsB"], "cv": hc["cv"]}


NT1 = CTX + SEQ


def l1_w_in_cols(half):
    P = rope_perm()
    q = np.arange(0, 512)
    qP = (q.reshape(8, 64)[:, P]).reshape(-1)
    k0 = 512 + np.arange(64)
    k1 = 576 + np.arange(64)
    v0 = 640 + np.arange(64)
    v1 = 704 + np.arange(64)
    c0 = half * 256
    hy = np.concatenate([768 + o * 512 + c0 + np.arange(256) for o in range(3)])
    return np.concatenate([q, qP, np.concatenate([k0, k0]), np.concatenate([k0[P], k0[P]]),
                           np.concatenate([k1, k1]), np.concatenate([k1[P], k1[P]]),
                           np.concatenate([v0, v0, v1, v1]), hy])


def build_l1a(stop=None):
    nc = bass.Bass("TRN2", target_bir_lowering=False)
    C = Ctx(nc)
    l1a_body(nc, C, IO(nc))
    C.S.finish()
    return nc


def l1a_body(nc, C, io, hy_halves=(None,)):
    xT = io.inp("xT", [8, 128, NT1])
    xTo = io.inp("xTo", [8, 128, OWN])
    ropeCq = io.inp("ropeCq", [128, OWN])
    ropeSq = io.inp("ropeSq", [128, OWN])
    modt = io.inp("mod", [128, 2, 6, 8])
    ng = io.inp("ng", [128, 2, 8])
    w_in = io.inp("w_in", [D, 2560])
    ropeC = io.inp("ropeC", [128, SEQ])
    ropeS = io.inp("ropeS", [128, SEQ])
    gvec_d = io.inp("gvec", [128, 4])
    bones_d = io.inp("bones", [128, 128])
    ident_d = io.inp("ident", [128, 128])
    if hy_halves == (None,):
        swT_d = io.inp("swT", [128, 6, 4])
        hb_d = io.inp("hbias", [2, 256])
        Kt = io.inp("Kt", [32, 128, 2, 3, 256])
    else:
        swT2_d = io.inp("swT2", [128, 2, 6, 4])
        hb2_d = io.inp("hbias2", [2, 2, 256])
        kt_blk = io.inp("kt_blk", None)
        Kt2 = [kt_blk[0:2], kt_blk[2:4]]
        w_hy = io.inp("w_hy", [D, 2, 768])
        w_hy_v = w_hy.rearrange("(k p) c o -> p k c o", p=128)
    FcB = io.inp("FcB", [32, 128, 32, 128], BF16)
    FsB = io.inp("FsB", [32, 128, 32, 128], BF16)
    FsBi = io.inp("FsBi", [32, 128, 32, 128], BF16)
    yatt_o = io.out("yatt", [4, 128, OWN], BF16)
    yhy_o = io.out("yhy", [128, 32, 256], BF16) if hy_halves == (None,) else io.out("yhy2", [2, 128, 32, 256], BF16)
    S = C.S
    st0 = contextlib.ExitStack()
    M = load_mod(C, modt, ng, st0)
    xv = xT.rearrange("k p t -> p k t")
    gvec = S.sbuf("gvec", [128, 4], F32, st0)
    bones = S.sbuf("bones", [128, 128], F32, st0)
    ident = S.sbuf("ident1", [128, 128], F32, st0)
    swT = S.sbuf("swT", [128, 6, 4], F32, st0)
    hbb = S.sbuf("hbb", [128, 2, 256], F32, st0)
    for dst, src, key in ((gvec, gvec_d, "gvec"), (bones, bones_d, "bones"), (ident, ident_d, "ident")):
        S.dma("sp", lambda e, dst=dst, src=src: e.dma_start(out=dst[:], in_=src), writes=[key])
    wv = w_in.rearrange("(k p) o -> p k o", p=128)
    def make_qk_block(src, srckey, rc, rs_, sq, rstd, ta, tb_):
        def qk_block(wa, wak, ja, wp, wpk, jp, gi, toks, out_fn, okey, rope):
            for (a, b, da, la) in toks:
                n = b - a
                for k in range(8):
                    S.op("pe", lambda e, k=k: e.matmul(C.banks[0][:, :n], wa[:, k, ja * 128:(ja + 1) * 128], src[:, k, a:b],
                                                       start=(k == 0), stop=(k == 7)), reads=[wak, srckey], writes=["bank0"])
                if rope:
                    for k in range(8):
                        S.op("pe", lambda e, k=k: e.matmul(C.banks[1][:, :n], wp[:, k, jp * 128:(jp + 1) * 128], src[:, k, a:b],
                                                           start=(k == 0), stop=(k == 7)), reads=[wpk, srckey], writes=["bank1"])
                S.op("act", lambda e: e.activation(out=sq[:, :n], in_=C.banks[0][:, :n], func=AF.Square), reads=["bank0"], writes=["sq1"])
                S.op("pe", lambda e: e.matmul(C.banks[2][:, :n], bones[:], sq[:, :n], start=True, stop=True), reads=["bones", "sq1"], writes=["bank2"])
                S.op("act", lambda e: e.activation(out=rstd[:, :n], in_=C.banks[2][:, :n], func=AF.Sqrt, bias=EPS, scale=1.0 / HD),
                     reads=["bank2"], writes=["rstd1"])
                S.op("dve", lambda e: e.reciprocal(out=rstd[:, :n], in_=rstd[:, :n]), reads=["rstd1"], writes=["rstd1"])
                if rope:
                    S.op("dve", lambda e: e.scalar_tensor_tensor(out=ta[:, :n], in0=C.banks[0][:, :n], scalar=gvec[:, gi:gi + 1], in1=rc[:, la:la + n],
                                                                 op0=ALU.mult, op1=ALU.mult), reads=["bank0", "gvec", "rc"], writes=["ta1"])
                    S.op("dve", lambda e: e.scalar_tensor_tensor(out=tb_[:, :n], in0=C.banks[1][:, :n], scalar=gvec[:, gi + 1:gi + 2], in1=rs_[:, la:la + n],
                                                                 op0=ALU.mult, op1=ALU.mult), reads=["bank1", "gvec", "rs"], writes=["tb1"])
                    S.op("pool", lambda e: e.tensor_tensor(out=ta[:, :n], in0=ta[:, :n], in1=tb_[:, :n], op=ALU.add), reads=["ta1", "tb1"], writes=["ta1"])
                    S.op("pool", lambda e: e.tensor_tensor(out=out_fn(da, da + n), in0=ta[:, :n], in1=rstd[:, :n], op=ALU.mult),
                         reads=["ta1", "rstd1"], writes=[okey])
                else:
                    S.op("dve", lambda e: e.scalar_tensor_tensor(out=out_fn(da, da + n), in0=C.banks[0][:, :n], scalar=gvec[:, gi:gi + 1], in1=rstd[:, :n],
                                                                 op0=ALU.mult, op1=ALU.mult), reads=["bank0", "gvec", "rstd1"], writes=[okey])
        return qk_block

    with contextlib.ExitStack() as stH:
        hx = S.sbuf("hx1", [128, 8, NT1], BF16, stH)
        with contextlib.ExitStack() as stA:
            xts = [S.sbuf(f"xa{i}", [128, 8, 512], F32, stA) for i in range(2)]

            def src_fn(a, b, ti):
                xt = xts[ti % 2]
                S.dma("sp", lambda e: e.dma_start(out=xt[:, :, :b - a], in_=xv[:, :, a:b]), writes=[f"xa{ti % 2}"])
                return xt[:, :, :b - a], f"xa{ti % 2}"
            norm_phase(C, M, 0, src_fn, None, NT1, CTX, hx, "hx", stA)
        S.barrier()
        for chalf in hy_halves:
            if chalf is None:
                hy_w = lambda grp: wv[:, :, 1792 + grp * 256:1792 + (grp + 1) * 256]
                swT_src, hb_src, Kt_c, yhy_c = swT_d, hb_d, Kt, yhy_o
            else:
                hy_w = lambda grp, chalf=chalf: w_hy_v[:, :, chalf, grp * 256:(grp + 1) * 256]
                swT_src, hb_src, Kt_c, yhy_c = swT2_d[:, chalf], hb2_d[chalf], Kt2[chalf], yhy_o[chalf]
            with contextlib.ExitStack() as stZ:
                zg = [S.sbuf(f"zg{i}", [128, 32, 256], BF16, stZ) for i in range(3)]
                S.dma("sp", lambda e: e.dma_start(out=swT[:], in_=swT_src), writes=["swT"])
                for o in range(2):
                    S.dma("sp", lambda e, o=o: e.dma_start(out=hbb[:, o, :], in_=hb_src[o].partition_broadcast(128)), writes=["hbb"])
                with contextlib.ExitStack() as stU:
                    wu_ = [S.sbuf(f"wu{i}", [128, 8, 256], BF16, stU) for i in range(2)]
                    U = S.sbuf("U", [128, SEQ + 2], F32, stU)
                    cvt = [S.sbuf(f"cv{i}", [128, 512], F32, stU) for i in range(2)]
                    S.op("dve", lambda e: e.memset(U[:, 0:1], 0.0), writes=["Upad0"])
                    S.op("dve", lambda e: e.memset(U[:, SEQ + 1:SEQ + 2], 0.0), writes=["Upad1"])
                    tcount = [0]
                    for grp in range(3):
                        wb = wu_[grp % 2]
                        S.dma("pool", lambda e, grp=grp, wb=wb: e.dma_start(out=wb[:], in_=hy_w(grp)), writes=[f"wu{grp % 2}"])
                        for cc in range(2):
                            c = grp * 2 + cc
                            for tt in range(8):
                                bk = tt % 2
                                for k in range(8):
                                    S.op("pe", lambda e, k=k, tt=tt, bk=bk, cc=cc, wb=wb: e.matmul(
                                        C.banks[bk][:, :], wb[:, k, cc * 128:(cc + 1) * 128], hx[:, k, CTX + tt * 512:CTX + (tt + 1) * 512],
                                        start=(k == 0), stop=(k == 7)), reads=[f"wu{grp % 2}", "hx"], writes=[f"bank{bk}"])
                                S.op("act", lambda e, tt=tt, bk=bk: e.activation(out=U[:, 1 + tt * 512:1 + (tt + 1) * 512], in_=C.banks[bk][:, :], func=AF.Copy),
                                     reads=[f"bank{bk}"], writes=["U"])
                            for tt in range(8):
                                cv_ = cvt[tt % 2]
                                ck = f"cv{tt % 2}"
                                a = tt * 512
                                S.op("dve", lambda e, a=a, c=c, cv_=cv_: e.tensor_scalar(out=cv_[:], in0=U[:, 1 + a:1 + a + 512], scalar1=swT[:, c, 1:2],
                                                                                       scalar2=swT[:, c, 3:4], op0=ALU.mult, op1=ALU.add),
                                     reads=["U", "swT", "Upad0", "Upad1"], writes=[ck])
                                S.op("dve", lambda e, a=a, c=c, cv_=cv_: e.scalar_tensor_tensor(out=cv_[:], in0=U[:, a:a + 512], scalar=swT[:, c, 0:1], in1=cv_[:],
                                                                                              op0=ALU.mult, op1=ALU.add), reads=["U", "swT", ck], writes=[ck])
                                S.op("dve", lambda e, a=a, c=c, cv_=cv_: e.scalar_tensor_tensor(out=cv_[:], in0=U[:, 2 + a:2 + a + 512], scalar=swT[:, c, 2:3], in1=cv_[:],
                                                                                              op0=ALU.mult, op1=ALU.add), reads=["U", "swT", ck], writes=[ck])
                                for s_ in range(4):
                                    tb = 2 + tcount[0] % 4
                                    tcount[0] += 1
                                    S.op("pe", lambda e, s_=s_, tb=tb, cv_=cv_: e.transpose(C.banks[tb][:, 0:128], cv_[:, s_ * 128:(s_ + 1) * 128], ident[:]),
                                         reads=[ck, "ident"], writes=[f"bank{tb}"])
                                    dst = zg[grp][:, tt * 4 + s_, cc * 128:(cc + 1) * 128]
                                    eng = "act" if s_ % 2 == 0 else "dve"
                                    if eng == "act":
                                        S.op("act", lambda e, tb=tb, dst=dst: e.activation(out=dst, in_=C.banks[tb][:, 0:128], func=AF.Copy), reads=[f"bank{tb}"], writes=[f"zg{grp}"])
                                    else:
                                        S.op("dve", lambda e, tb=tb, dst=dst: e.tensor_copy(out=dst, in_=C.banks[tb][:, 0:128]), reads=[f"bank{tb}"], writes=[f"zg{grp}"])
                S.barrier()
                S.barrier()
                with contextlib.ExitStack() as stF:
                    YW = S.sbuf("YW", [128, 2, 32, 256], BF16, stF)
                    fa = [S.sbuf(f"fa{i}", [128, 32, 128], BF16, stF) for i in range(2)]
                    fb = [S.sbuf(f"fb{i}", [128, 32, 128], BF16, stF) for i in range(2)]
                    kts = [S.sbuf(f"ktb{i}", [128, 3, 256], F32, stF) for i in range(2)]
                    tmp = [S.sbuf(f"hyt{i}", [128, 256], F32, stF) for i in range(4)]
                    z = zg[0]
                    for o in range(2):
                        gate = zg[1 + o]
                        for j in range(32):
                            bi = j % 2
                            S.dma("sp", lambda e, j=j, bi=bi: e.dma_start(out=fa[bi][:], in_=FcB[j]), writes=[f"fa{bi}"])
                            S.dma("sp", lambda e, j=j, bi=bi: e.dma_start(out=fb[bi][:], in_=FsB[j]), writes=[f"fb{bi}"])
                            if chalf is None:
                                S.dma("sp", lambda e, j=j, bi=bi, o=o: e.dma_start(out=kts[bi][:], in_=Kt_c[j, :, o]), writes=[f"ktb{bi}"])
                            else:
                                for q4 in range(2):
                                    S.dma("sp", lambda e, j=j, bi=bi, o=o, q4=q4: e.dma_start(
                                        out=kts[bi][:, :, q4 * 128:(q4 + 1) * 128], in_=Kt_c[q4][j, :, :, o * 128:(o + 1) * 128]), reads=["ktblk"], writes=[f"ktb{bi}"])
                            zc, zs = 2 * bi, 2 * bi + 1
                            for ti in range(32):
                                S.op("pe", lambda e, ti=ti, bi=bi, zc=zc: e.matmul(C.banks[zc][:, 0:256], fa[bi][:, ti, :], z[:, ti, :], start=(ti == 0), stop=(ti == 31)),
                                     reads=[f"fa{bi}", "z"], writes=[f"bank{zc}"])
                            for ti in range(32):
                                S.op("pe", lambda e, ti=ti, bi=bi, zs=zs: e.matmul(C.banks[zs][:, 0:256], fb[bi][:, ti, :], z[:, ti, :], start=(ti == 0), stop=(ti == 31)),
                                     reads=[f"fb{bi}", "z"], writes=[f"bank{zs}"])
                            kt = kts[bi]
                            S.op("dve", lambda e, zc=zc, kt=kt: e.tensor_tensor(out=tmp[0][:], in0=C.banks[zc][:, 0:256], in1=kt[:, 0, :], op=ALU.mult),
                                 reads=[f"bank{zc}", f"ktb{bi}"], writes=["hyt0"])
                            S.op("dve", lambda e, zs=zs, kt=kt: e.tensor_tensor(out=tmp[1][:], in0=C.banks[zs][:, 0:256], in1=kt[:, 1, :], op=ALU.mult),
                                 reads=[f"bank{zs}", f"ktb{bi}"], writes=["hyt1"])
                            S.op("pool", lambda e, j=j: e.tensor_tensor(out=YW[:, 0, j, :], in0=tmp[0][:], in1=tmp[1][:], op=ALU.subtract),
                                 reads=["hyt0", "hyt1"], writes=["YW"])
                            S.op("dve", lambda e, zc=zc, kt=kt: e.tensor_tensor(out=tmp[2][:], in0=C.banks[zc][:, 0:256], in1=kt[:, 1, :], op=ALU.mult),
                                 reads=[f"bank{zc}", f"ktb{bi}"], writes=["hyt2"])
                            S.op("dve", lambda e, zs=zs, kt=kt: e.tensor_tensor(out=tmp[3][:], in0=C.banks[zs][:, 0:256], in1=kt[:, 2, :], op=ALU.mult),
                                 reads=[f"bank{zs}", f"ktb{bi}"], writes=["hyt3"])
                            S.op("pool", lambda e, j=j: e.tensor_tensor(out=YW[:, 1, j, :], in0=tmp[2][:], in1=tmp[3][:], op=ALU.add),
                                 reads=["hyt2", "hyt3"], writes=["YW"])
                        for ni in range(32):
                            bi = ni % 2
                            S.dma("sp", lambda e, ni=ni, bi=bi: e.dma_start(out=fa[bi][:], in_=FcB[ni]), writes=[f"fa{bi}"])
                            S.dma("sp", lambda e, ni=ni, bi=bi: e.dma_start(out=fb[bi][:], in_=FsBi[ni]), writes=[f"fb{bi}"])
                            yb_ = 4 + bi
                            for fj in range(32):
                                S.op("pe", lambda e, fj=fj, bi=bi, yb_=yb_: e.matmul(C.banks[yb_][:, 0:256], fa[bi][:, fj, :], YW[:, 0, fj, :], start=(fj == 0), stop=False),
                                     reads=[f"fa{bi}", "YW"], writes=[f"bank{yb_}"])
                            for fj in range(32):
                                S.op("pe", lambda e, fj=fj, bi=bi, yb_=yb_: e.matmul(C.banks[yb_][:, 0:256], fb[bi][:, fj, :], YW[:, 1, fj, :], start=False, stop=(fj == 31)),
                                     reads=[f"fb{bi}", "YW"], writes=[f"bank{yb_}"])
                            S.op("pool", lambda e, ni=ni, o=o: e.tensor_tensor(out=tmp[0][:], in0=z[:, ni, :], in1=hbb[:, o, :], op=ALU.mult), reads=["z", "hbb"], writes=["hyt0"])
                            S.op("dve", lambda e, yb_=yb_: e.scalar_tensor_tensor(out=tmp[1][:], in0=C.banks[yb_][:, 0:256], scalar=2.0 / NFFT, in1=tmp[0][:],
                                                                                 op0=ALU.mult, op1=ALU.add), reads=[f"bank{yb_}", "hyt0"], writes=["hyt1"])
                            S.op("pool", lambda e, ni=ni, gate=gate: e.tensor_tensor(out=z[:, ni, :], in0=gate[:, ni, :], in1=tmp[1][:], op=ALU.mult),
                                 reads=["hyt1", "zg"], writes=["z"])
                        S.barrier()
                    for q in range(4):
                        S.dma("sp", lambda e, q=q: e.dma_start(out=yhy_c[:, q * 8:(q + 1) * 8, :], in_=z[:, q * 8:(q + 1) * 8, :]), reads=["z"], is_out=True)
                S.barrier()
        with contextlib.ExitStack() as stQ:
            KAB = S.sbuf("KABc", [128, 2, NT1], BF16, stQ)
            V2 = S.sbuf("V2c", [128, 34, 256], BF16, stQ)
            with contextlib.ExitStack() as stB:
                wts = [S.sbuf(f"wt{i}", [128, 8, 256], BF16, stB) for i in range(2)]
                rc = S.sbuf("rc", [128, SEQ], BF16, stB)
                rs_ = S.sbuf("rs", [128, SEQ], BF16, stB)
                sq = S.sbuf("sq1", [128, 512], F32, stB)
                rstd = S.sbuf("rstd1", [128, 512], F32, stB)
                ta = S.sbuf("ta1", [128, 512], F32, stB)
                tb_ = S.sbuf("tb1", [128, 512], F32, stB)
                S.dma("pool", lambda e: e.dma_start(out=rc[:], in_=ropeC), writes=["rc"])
                S.dma("pool", lambda e: e.dma_start(out=rs_[:], in_=ropeS), writes=["rs"])

                qk_block = make_qk_block(hx, "hx", rc, rs_, sq, rstd, ta, tb_)

                def loadw(i, c0):
                    S.dma("pool", lambda e: e.dma_start(out=wts[i][:], in_=wv[:, :, c0:c0 + 256]), writes=[f"wt{i}"])
                    return wts[i], f"wt{i}"
                lat_toks = [(CTX + 512 * i, CTX + 512 * (i + 1), CTX + 512 * i, 512 * i) for i in range(8)]
                ctx_toks = [(0, CTX, 0, 0)]
                wk1, wk1k = loadw(0, 1024)
                wk2, wk2k = loadw(1, 1280)
                for g, (wk_, wkk) in enumerate(((wk1, wk1k), (wk2, wk2k))):
                    qk_block(wk_, wkk, 0, wk_, wkk, 1, 2, ctx_toks, lambda a, b, g=g: KAB[:, g, a:b], "KAB", False)
                    qk_block(wk_, wkk, 0, wk_, wkk, 1, 2, lat_toks, lambda a, b, g=g: KAB[:, g, a:b], "KAB", True)
                wv_, wvk = loadw(0, 1536)
                for t in range(34):
                    bk = 4 + t % 2
                    for k in range(8):
                        S.op("pe", lambda e, k=k, t=t, bk=bk: e.matmul(C.banks[bk][:, 0:256], hx[:, k, t * 128:(t + 1) * 128], wv_[:, k, :],
                                                                       start=(k == 0), stop=(k == 7)), reads=[wvk, "hx"], writes=[f"bank{bk}"])
                    S.op("act", lambda e, t=t, bk=bk: e.activation(out=V2[:, t, :], in_=C.banks[bk][:, 0:256], func=AF.Copy), reads=[f"bank{bk}"], writes=["V2"])
            S.barrier()
            Q = S.sbuf("Qc", [128, 4, OWN], BF16, stQ)
            xvo = xTo.rearrange("k p t -> p k t")
            with contextlib.ExitStack() as stO:
                hxo = S.sbuf("hxo", [128, 8, OWN], BF16, stO)
                with contextlib.ExitStack() as stA:
                    xts = [S.sbuf(f"xo{i}", [128, 8, 512], F32, stA) for i in range(1)]

                    def src_fno(a, b, ti):
                        xt = xts[0]
                        S.dma("sp", lambda e: e.dma_start(out=xt[:, :, :b - a], in_=xvo[:, :, a:b]), writes=["xo0"])
                        return xt[:, :, :b - a], "xo0"
                    norm_phase(C, M, 0, src_fno, None, OWN, 0, hxo, "hxo", stA)
                S.barrier()
                with contextlib.ExitStack() as stB:
                    wts = [S.sbuf(f"wq{i}", [128, 8, 256], BF16, stB) for i in range(2)]
                    rcq = S.sbuf("rcq", [128, OWN], BF16, stB)
                    rsq = S.sbuf("rsq", [128, OWN], BF16, stB)
                    sq = S.sbuf("sq1", [128, 512], F32, stB)
                    rstd = S.sbuf("rstd1", [128, 512], F32, stB)
                    ta = S.sbuf("ta1", [128, 512], F32, stB)
                    tb_ = S.sbuf("tb1", [128, 512], F32, stB)
                    S.dma("pool", lambda e: e.dma_start(out=rcq[:], in_=ropeCq), writes=["rc"])
                    S.dma("pool", lambda e: e.dma_start(out=rsq[:], in_=ropeSq), writes=["rs"])
                    qkb = make_qk_block(hxo, "hxo", rcq, rsq, sq, rstd, ta, tb_)
                    own_toks = [(512 * i, 512 * (i + 1), 512 * i, 512 * i) for i in range(4)]
                    for jj in range(2):
                        S.dma("pool", lambda e, jj=jj: e.dma_start(out=wts[0][:], in_=wv[:, :, jj * 256:(jj + 1) * 256]), writes=["wq0"])
                        S.dma("pool", lambda e, jj=jj: e.dma_start(out=wts[1][:], in_=wv[:, :, 512 + jj * 256:512 + (jj + 1) * 256]), writes=["wq1"])
                        for j2 in range(2):
                            j = jj * 2 + j2
                            qkb(wts[0], "wq0", j2, wts[1], "wq1", j2, 0, own_toks, lambda a, b, j=j: Q[:, j, a:b], "Q", True)
                S.barrier()
            yat = S.sbuf("yat", [128, 4, OWN], BF16, stQ)
            with contextlib.ExitStack() as stC:
                pts = [S.sbuf(f"ptc{i}", [128, 512], BF16, stC) for i in range(2)]
                dn = S.sbuf("dnc", [128, 512], F32, stC)
                it = 0
                for qg in range(4):
                    for h in range(8):
                        g = h // 4
                        c, off = h // 2, (h % 2) * 64
                        nb, db = 2 + it % 2, 4 + it % 2
                        it += 1
                        def emit_S(tk):
                            sb = tk % 2
                            S.op("pe", lambda e, tk=tk, sb=sb: e.matmul(C.banks[sb][:, :], KAB[off:off + 64, g, tk * 128:(tk + 1) * 128],
                                                                        Q[off:off + 64, c, qg * 512:(qg + 1) * 512], start=True, stop=True),
                                 reads=["KAB", "Q"], writes=[f"bank{sb}"])
                        emit_S(0)
                        for tk in range(34):
                            sb = tk % 2
                            pt = pts[sb]
                            if tk + 1 < 34:
                                emit_S(tk + 1)
                            S.op("act", lambda e, sb=sb, pt=pt: e.activation(out=pt[:], in_=C.banks[sb][:, :], func=AF.Exp, scale=SCALE),
                                 reads=[f"bank{sb}"], writes=[f"ptc{sb}"])
                            S.op("pe", lambda e, tk=tk, pt=pt: e.matmul(C.banks[nb][:, :], V2[:, tk, g * 128:(g + 1) * 128], pt[:], start=(tk == 0), stop=(tk == 33)),
                                 reads=["V2", f"ptc{sb}"], writes=[f"bank{nb}"])
                            S.op("pe", lambda e, tk=tk, pt=pt: e.matmul(C.banks[db][:, :], C.ones_b[:], pt[:], start=(tk == 0), stop=(tk == 33)),
                                 reads=["ones_b", f"ptc{sb}"], writes=[f"bank{db}"])
                        S.op("dve", lambda e: e.reciprocal(out=dn[:], in_=C.banks[db][:, :]), reads=[f"bank{db}"], writes=["dnc"])
                        S.op("dve", lambda e: e.tensor_tensor(out=yat[off:off + 64, c, qg * 512:(qg + 1) * 512], in0=C.banks[nb][off:off + 64, :],
                                                              in1=dn[off:off + 64, :], op=ALU.mult), reads=[f"bank{nb}", "dnc"], writes=["yat"])
                for c in range(4):
                    S.dma("sp", lambda e, c=c: e.dma_start(out=yatt_o[c], in_=yat[:, c, :]), reads=["yat"], is_out=True)
            S.barrier()
    S.barrier()
    st0.close()

def l1a_inputs(inp, mod, x1_full, Ktabs, core):
    hc = hy_consts()
    b, half = divmod(core, 2)
    order = np.arange(SEQ)
    xt = x1_full[b]
    xT = np.ascontiguousarray(xt)
    xTo = np.ascontiguousarray(xt[:, :, CTX + half * OWN:CTX + (half + 1) * OWN])
    rc, rs = rope_np(order)
    rcq, rsq = rope_np(half * OWN + np.arange(OWN))
    modt = np.stack([fm(mod[1, b].reshape(6, D)), fm(mod[1, 4].reshape(6, D))], axis=1)
    P = rope_perm()
    qn, kn = inp["c_qnorm"][0], inp["c_knorm"][0]
    gvec = np.stack([np.tile(qn, 2), np.tile(qn[P], 2), np.tile(kn, 2), np.tile(kn[P], 2)], axis=1).astype(np.float32)
    bones = np.zeros((128, 128), np.float32)
    bones[:64, :64] = 1.0
    bones[64:, 64:] = 1.0
    c0 = half * 256
    chs = np.concatenate([o * 512 + c0 + np.arange(256) for o in range(3)])
    sw = inp["hy_short_w"][0][:, chs]
    sb = inp["hy_short_b"][0][chs]
    swT = np.concatenate([sw, sb[None]], 0).T.reshape(6, 128, 4).transpose(1, 0, 2)
    return {"xT": xT, "xTo": xTo, "ropeCq": rcq, "ropeSq": rsq, "mod": np.ascontiguousarray(modt), "ng": fm(inp["norm_g"][1]),
            "w_in": np.ascontiguousarray(inp["w_in_odd"][0][:, l1_w_in_cols(half)]),
            "ropeC": rc, "ropeS": rs, "gvec": np.ascontiguousarray(gvec), "bones": bones, "ident": np.eye(128, dtype=np.float32),
            "swT": np.ascontiguousarray(swT, dtype=np.float32), "hbias": np.ascontiguousarray(inp["hy_bias"][0][:, c0:c0 + 256]),
            "Kt": Ktabs[half], "FcB": hc["FcB"], "FsB": hc["FsB"], "FsBi": hc["FsBi"]}, order


def assemble_ktabs(kouts):
    res = []
    for half in range(2):
        t = np.zeros((32, 128, 2, 3, 256), np.float32)
        for q in range(4):
            k = kouts[half * 4 + q].reshape(32, 128, 3, 2, 64)
            t[:, :, :, :, q * 64:(q + 1) * 64] = k.transpose(0, 1, 3, 2, 4)
        res.append(t)
    return res


def assemble_x1(l0_out):
    res = []
    for b in range(NB):
        a, c = l0_out[2 * b], l0_out[2 * b + 1]
        res.append(np.ascontiguousarray(np.concatenate([a[:, :, :CTX], a[:, :, CTX:], c[:, :, CTX:]], axis=2)))
    return res


L1B_TILES = [(512 * i, 512 * (i + 1)) for i in range(4)]


def build_l1b():
    nc = bass.Bass("TRN2", target_bir_lowering=False)
    C = Ctx(nc)
    l1b_body(nc, C, IO(nc))
    C.S.finish()
    return nc


def l1b_body(nc, C, io, fill_y=None):
    yT = io.inp("yT", [8, 128, OWN], BF16) if fill_y is None else None
    xTo = io.inp("xTo", [8, 128, OWN])
    modt = io.inp("mod", [128, 2, 6, 8])
    ng = io.inp("ng", [128, 2, 8])
    w_out = io.inp("w_out", [D, D])
    w_r = io.inp("w_r", [D, 16])
    b_r = io.inp("b_r", [16])
    wg = io.inp("wg", [NE, D, DE])
    wu = io.inp("wu", [NE, D, DE])
    wd = io.inp("wd", [NE, DE, D])
    ident_d = io.inp("ident", [128, 128])
    selE_d = io.inp("selE", [16, 16, 128])
    fg_d = io.inp("fg", [128, 8])
    outT = io.out("outT", [8, 128, OWN])
    S = C.S
    st0 = contextlib.ExitStack()
    M = load_mod(C, modt, ng, st0)
    ybuf = S.sbuf("ybuf1", [128, 8, OWN], BF16, st0)
    if fill_y is None:
        for k in range(8):
            S.dma("sp", lambda e, k=k: e.dma_start(out=ybuf[:, k, :], in_=yT[k]), writes=["Y"])
    else:
        fill_y(ybuf)
    xv = xTo.rearrange("k p t -> p k t")
    tail_phase(C, M, ybuf, w_out, lambda a, b: xv[:, :, a:b], outT, w_r, b_r, ident_d, selE_d, wg, wu, wd, st0,
               final_g_d=fg_d, nres=OWN, tiles=L1B_TILES, ctx_len=0)
    S.barrier()
    st0.close()


def l1b_inputs(inp, mod, x1_full, yatt, yhy, core):
    import ml_dtypes
    b, half = divmod(core, 2)
    yh = np.concatenate([np.asarray(yhy[2 * b + h]).transpose(1, 0, 2).reshape(SEQ, 256) for h in range(2)], axis=1)
    yh_own = yh[half * OWN:(half + 1) * OWN]
    yhT = np.ascontiguousarray(yh_own.T.reshape(4, 128, OWN))
    yT = np.ascontiguousarray(np.concatenate([np.asarray(yatt[core]), yhT], axis=0)).astype(ml_dtypes.bfloat16)
    xt = x1_full[b]
    modt = np.stack([fm(mod[1, b].reshape(6, D)), fm(mod[1, 4].reshape(6, D))], axis=1)
    selE = np.zeros((16, 16, 128), np.float32)
    for e in range(16):
        selE[e, e, :] = 1.0
    return {"yT": yT, "xTo": np.ascontiguousarray(xt[:, :, CTX + half * OWN:CTX + (half + 1) * OWN]),
            "mod": np.ascontiguousarray(modt), "ng": fm(inp["norm_g"][1]),
            "w_out": np.ascontiguousarray(inp["w_out_odd"][0]),
            "w_r": np.ascontiguousarray(inp["w_router"]), "b_r": np.ascontiguousarray(inp["b_router"]),
            "wg": np.ascontiguousarray(inp["moe_wg"][1]), "wu": np.ascontiguousarray(inp["moe_wu"][1]),
            "wd": np.ascontiguousarray(inp["moe_wd"][1]),
            "ident": np.eye(128, dtype=np.float32), "selE": selE, "fg": fm(inp["final_g"])}


def kernel_unfused(**inp):
    inp = {k: np.asarray(v) for k, v in inp.items()}
    cores = list(range(NCORES))
    mod = run_mod(inp)
    r0 = run_bass_kernel_spmd(build_l0(), [l0_inputs(inp, mod, c) for c in cores], core_ids=cores)
    x1_full = assemble_x1([r0.results[c]["x1T"] for c in cores])
    rk = run_bass_kernel_spmd(build_l1k(), [l1k_inputs(inp, c) for c in cores], core_ids=cores)
    Kt = assemble_ktabs([rk.results[c]["ktab"] for c in cores])
    ra = run_bass_kernel_spmd(build_l1a(), [l1a_inputs(inp, mod, x1_full, Kt, c)[0] for c in cores], core_ids=cores)
    yatt = [ra.results[c]["yatt"] for c in cores]
    yhy = [ra.results[c]["yhy"] for c in cores]
    rb = run_bass_kernel_spmd(build_l1b(), [l1b_inputs(inp, mod, x1_full, yatt, yhy, c) for c in cores], core_ids=cores)
    out = np.zeros((NB, SEQ, D), np.float32)
    for c in cores:
        b, half = divmod(c, 2)
        o = rb.results[c]["outT"]
        out[b, half * OWN:(half + 1) * OWN] = o.transpose(2, 0, 1).reshape(OWN, D)
    return out


def mod_phase(nc, C, cT_d, w_ada_d, b_ada_fm_d):
    S = C.S
    st = S.stack
    mods = [S.sbuf(f"modL{l}", [128, 2, 6, 8], F32, st) for l in range(2)]
    with contextlib.ExitStack() as st2:
        ct = S.sbuf("m_ct", [128, 8, 2], F32, st2)
        sg = S.sbuf("m_sg", [128, 8, 2], F32, st2)
        bfm = S.sbuf("m_b", [128, 2, 6, 8], F32, st2)
        wts = [S.sbuf(f"m_w{i}", [128, 8, 512], F32, st2) for i in range(2)]
        S.dma("sp", lambda e: e.dma_start(out=ct[:], in_=cT_d), writes=["m_ct"])
        S.dma("sp", lambda e: e.dma_start(out=bfm[:], in_=b_ada_fm_d), writes=["m_b"])
        S.op("act", lambda e: e.activation(out=sg[:], in_=ct[:], func=AF.Silu), reads=["m_ct"], writes=["m_sg"])
        it = 0
        for l in range(2):
            wv = w_ada_d[l].rearrange("(k p) o -> p k o", p=128)
            for grp in range(12):
                wi = it % 2
                it += 1
                S.dma("sp", lambda e, grp=grp, wi=wi, wv=wv: e.dma_start(out=wts[wi][:], in_=wv[:, :, grp * 512:(grp + 1) * 512]), writes=[f"m_w{wi}"])
                for oc in range(4):
                    col = grp * 4 + oc
                    j, kk = divmod(col, 8)
                    bk = col % 4
                    for k in range(8):
                        S.op("pe", lambda e, k=k, oc=oc, wi=wi, bk=bk: e.matmul(C.banks[bk][:, 0:2], wts[wi][:, k, oc * 128:(oc + 1) * 128], sg[:, k, :],
                                                                               start=(k == 0), stop=(k == 7)), reads=[f"m_w{wi}", "m_sg"], writes=[f"bank{bk}"])
                    S.op("dve", lambda e, l=l, j=j, kk=kk, bk=bk: e.tensor_scalar(out=mods[l][:, :, j, kk], in0=C.banks[bk][:, 0:2], scalar1=bfm[:, l, j, kk:kk + 1],
                                                                                 scalar2=None, op0=ALU.add), reads=[f"bank{bk}", "m_b"], writes=[f"modL{l}"])
        S.barrier()
    return mods


def build_fused():
    nc = bass.Bass("TRN2", target_bir_lowering=False)
    C = Ctx(nc)
    S = C.S

    def scr(name, shape, dt=F32):
        return nc.dram_tensor(name, list(shape), dt, kind="Internal").ap()
    E = {}
    for name, shape, dt in (
            ("cT", [128, 8, 2], F32), ("w_ada", [2, D, 6 * D], F32), ("b_ada_fm", [128, 2, 6, 8], F32),
            ("ng0", [128, 2, 8], F32), ("ng1", [128, 2, 8], F32),
            ("w_in0", [D, 3328], F32), ("w_out0", [D, D], F32), ("sink", [8], F32), ("rpbT", [128, 7, 8, 128], F32),
            ("vint", [128, 5, 128], F32), ("w_r", [D, 16], F32), ("b_r", [16], F32),
            ("wg0", [NE, D, DE], F32), ("wu0", [NE, D, DE], F32), ("wd0", [NE, DE, D], F32),
            ("wg1", [NE, D, DE], F32), ("wu1", [NE, D, DE], F32), ("wd1", [NE, DE, D], F32),
            ("ident", [128, 128], F32), ("selE", [16, 16, 128], F32),
            ("featsT", [33, SEQ], F32), ("w1", [33, 64], F32), ("w2", [64, 64], F32), ("pvec", [64, 4], F32),
            ("FcB", [32, 128, 32, 128], BF16), ("FsB", [32, 128, 32, 128], BF16), ("FsBi", [32, 128, 32, 128], BF16), ("cv", [128, 4], F32),
            ("w_in1", [D, 2560], F32), ("w_out1", [D, D], F32), ("ropeCn", [128, SEQ], F32), ("ropeSn", [128, SEQ], F32),
            ("ropeCq", [128, OWN], F32), ("ropeSq", [128, OWN], F32), ("gvec", [128, 4], F32), ("bones", [128, 128], F32),
            ("swT2", [128, 2, 6, 4], F32), ("hbias2", [2, 2, 256], F32), ("w_hy", [D, 2, 768], F32),
            ("selv", [128, 2], F32), ("fg", [128, 8], F32)):
        E[name] = din(nc, name, shape, dt)
    outT = dout(nc, "outT", [8, 128, OWN])
    x1nat = scr("x1nat", [8, 128, NT1])
    xown = scr("xown", [8, 128, OWN])
    kt_blk = [scr(f"ktblk{i}", [32, 128, 3, 256]) for i in range(4)]
    yatt_s = scr("yatt_s", [4, 128, OWN], BF16)
    yhy_s = scr("yhy_s", [2, 128, 32, 256], BF16)

    mods = mod_phase(nc, C, E["cT"], E["w_ada"], E["b_ada_fm"])
    for p in range(2):
        ov = {"mod": mods[0][:], "ng": E["ng0"], "w_in": E["w_in0"], "w_out": E["w_out0"], "sink": E["sink"], "rpbT": E["rpbT"],
              "vint": E["vint"], "w_r": E["w_r"], "b_r": E["b_r"], "wg": E["wg0"], "wu": E["wu0"], "wd": E["wd0"],
              "ident": E["ident"], "selE": E["selE"], "x1T": (x1nat, p)}
        _build_l0_body(nc, C, False, "full", IO(nc, ov, prefix=f"p{p}_"))
        S.barrier()
    for cb in range(4):
        ov = {"featsT": E["featsT"], "w1": E["w1"], "w2": E["w2"], "pvec": E["pvec"], "FcB": E["FcB"], "FsB": E["FsB"], "cv": E["cv"],
              "ktab": kt_blk[cb]}
        l1k_body(nc, C, IO(nc, ov, prefix=f"k{cb}_"), NCH=128)
        S.barrier()
    with contextlib.ExitStack() as st:
        sel = S.sbuf("selv_sb", [128, 2], F32, st)
        ta = [S.sbuf(f"bl_a{i}", [128, 512], F32, st) for i in range(2)]
        tb = [S.sbuf(f"bl_b{i}", [128, 512], F32, st) for i in range(2)]
        S.dma("sp", lambda e: e.dma_start(out=sel[:], in_=E["selv"]), writes=["selv"])
        it = 0
        for k in range(8):
            for tt in range(4):
                bi = it % 2
                it += 1
                a0 = CTX + tt * 512
                S.dma("sp", lambda e, k=k, a0=a0, bi=bi: e.dma_start(out=ta[bi][:], in_=x1nat[k][:, a0:a0 + 512]), reads=["x1nat"], writes=[f"bl_a{bi}"])
                S.dma("sp", lambda e, k=k, a0=a0, bi=bi: e.dma_start(out=tb[bi][:], in_=x1nat[k][:, OWN + a0:OWN + a0 + 512]), reads=["x1nat"], writes=[f"bl_b{bi}"])
                S.op("dve", lambda e, bi=bi: e.tensor_scalar(out=ta[bi][:], in0=ta[bi][:], scalar1=sel[:, 0:1], scalar2=None, op0=ALU.mult),
                     reads=[f"bl_a{bi}", "selv"], writes=[f"bl_a{bi}"])
                S.op("dve", lambda e, bi=bi: e.scalar_tensor_tensor(out=ta[bi][:], in0=tb[bi][:], scalar=sel[:, 1:2], in1=ta[bi][:], op0=ALU.mult, op1=ALU.add),
                     reads=[f"bl_a{bi}", f"bl_b{bi}", "selv"], writes=[f"bl_a{bi}"])
                S.dma("sp", lambda e, k=k, tt=tt, bi=bi: e.dma_start(out=xown[k][:, tt * 512:(tt + 1) * 512], in_=ta[bi][:]), reads=[f"bl_a{bi}"], writes=["xown"])
        S.barrier()
    ov = {"xT": x1nat, "xTo": xown, "ropeCq": E["ropeCq"], "ropeSq": E["ropeSq"], "mod": mods[1][:], "ng": E["ng1"], "w_in": E["w_in1"],
          "ropeC": E["ropeCn"], "ropeS": E["ropeSn"], "gvec": E["gvec"], "bones": E["bones"], "ident": E["ident"],
          "swT2": E["swT2"], "hbias2": E["hbias2"], "kt_blk": kt_blk, "w_hy": E["w_hy"],
          "FcB": E["FcB"], "FsB": E["FsB"], "FsBi": E["FsBi"], "yatt": yatt_s, "yhy2": yhy_s}
    l1a_body(nc, C, IO(nc, ov), hy_halves=(0, 1))
    S.barrier()

    def fill_y(ybuf):
        for c in range(4):
            S.dma("sp", lambda e, c=c: e.dma_start(out=ybuf[:, c, :], in_=yatt_s[c]), writes=["Y"])
        with contextlib.ExitStack() as st:
            sel = S.sbuf("selv_sb2", [128, 2], F32, st)
            idn = S.sbuf("idn2", [128, 128], F32, st)
            t0 = [S.sbuf(f"fy_a{i}", [128, 256], BF16, st) for i in range(2)]
            t1 = [S.sbuf(f"fy_b{i}", [128, 256], BF16, st) for i in range(2)]
            tf = [S.sbuf(f"fy_f{i}", [128, 256], F32, st) for i in range(2)]
            S.dma("sp", lambda e: e.dma_start(out=sel[:], in_=E["selv"]), writes=["selv2"])
            S.dma("sp", lambda e: e.dma_start(out=idn[:], in_=E["ident"]), writes=["idn2"])
            it = 0
            for c in range(2):
                for i in range(16):
                    bi = it % 2
                    it += 1
                    S.dma("sp", lambda e, c=c, i=i, bi=bi: e.dma_start(out=t0[bi][:], in_=yhy_s[c][:, i, :]), writes=[f"fy_a{bi}"])
                    S.dma("sp", lambda e, c=c, i=i, bi=bi: e.dma_start(out=t1[bi][:], in_=yhy_s[c][:, 16 + i, :]), writes=[f"fy_b{bi}"])
                    S.op("dve", lambda e, bi=bi: e.tensor_scalar(out=tf[bi][:], in0=t0[bi][:], scalar1=sel[:, 0:1], scalar2=None, op0=ALU.mult),
                         reads=[f"fy_a{bi}", "selv2"], writes=[f"fy_f{bi}"])
                    S.op("dve", lambda e, bi=bi: e.scalar_tensor_tensor(out=tf[bi][:], in0=t1[bi][:], scalar=sel[:, 1:2], in1=tf[bi][:], op0=ALU.mult, op1=ALU.add),
                         reads=[f"fy_b{bi}", f"fy_f{bi}", "selv2"], writes=[f"fy_f{bi}"])
                    for cc in range(2):
                        bk = (2 * it + cc) % 4
                        S.op("pe", lambda e, bi=bi, cc=cc, bk=bk: e.transpose(C.banks[bk][:, 0:128], tf[bi][:, cc * 128:(cc + 1) * 128], idn[:]),
                             reads=[f"fy_f{bi}", "idn2"], writes=[f"bank{bk}"])
                        S.op("act", lambda e, c=c, cc=cc, i=i, bk=bk: e.activation(out=ybuf[:, 4 + 2 * c + cc, i * 128:(i + 1) * 128], in_=C.banks[bk][:, 0:128], func=AF.Copy),
                             reads=[f"bank{bk}"], writes=["Y"])
            S.barrier()
    ov = {"xTo": xown, "mod": mods[1][:], "ng": E["ng1"], "w_out": E["w_out1"], "w_r": E["w_r"], "b_r": E["b_r"],
          "wg": E["wg1"], "wu": E["wu1"], "wd": E["wd1"], "ident": E["ident"], "selE": E["selE"], "fg": E["fg"], "outT": outT}
    l1b_body(nc, C, IO(nc, ov), fill_y=fill_y)
    S.finish()
    return nc


def fused_inputs(inp, core):
    hc = hy_consts()
    b, half = divmod(core, 2)
    m = {}
    cond = np.stack([inp["c"][b], inp["c_ctx"]], axis=0)
    m["cT"] = np.ascontiguousarray(cond.T.reshape(8, 128, 2).transpose(1, 0, 2))
    m["w_ada"] = np.ascontiguousarray(inp["w_ada"])
    m["b_ada_fm"] = np.ascontiguousarray(np.stack([fm(inp["b_ada"][l].reshape(6, D)) for l in range(2)], axis=1))
    m["ng0"] = fm(inp["norm_g"][0]); m["ng1"] = fm(inp["norm_g"][1])
    m["w_in0"] = np.ascontiguousarray(inp["w_in_even"][0][:, l0_w_in_cols()])
    m["w_out0"] = np.ascontiguousarray(inp["w_out_even"][0])
    m["sink"] = np.ascontiguousarray(inp["a_sink"][0]); m["rpbT"] = rpb_gather(inp["b_rpb"][0])
    m["vint"] = np.ascontiguousarray(np.stack([b_valid(10, o) for o in range(-2, 3)], axis=1))
    m["w_r"] = np.ascontiguousarray(inp["w_router"]); m["b_r"] = np.ascontiguousarray(inp["b_router"])
    for l in range(2):
        m[f"wg{l}"] = np.ascontiguousarray(inp["moe_wg"][l]); m[f"wu{l}"] = np.ascontiguousarray(inp["moe_wu"][l])
        m[f"wd{l}"] = np.ascontiguousarray(inp["moe_wd"][l])
    m["ident"] = np.eye(128, dtype=np.float32)
    selE = np.zeros((16, 16, 128), np.float32)
    for e in range(16):
        selE[e, e, :] = 1.0
    m["selE"] = selE
    k = np.arange(128)
    tri_lo = (k[:, None] >= k[None, :]).astype(np.float32)
    tri_hi = (k[:, None] <= k[None, :]).astype(np.float32)
    z = np.zeros_like(tri_lo)
    for p in range(2):
        pos = p * OWN - HALO + np.arange(NLAT)
        ok = (pos >= 0) & (pos < SEQ)
        xl = np.zeros((NTOK, D), np.float32)
        xl[:CTX] = inp["ctx"][b]
        xl[CTX:][ok] = inp["x"][b][pos[ok]]
        m[f"p{p}_xT"] = np.ascontiguousarray(xl.T.reshape(8, 128, NTOK))
        rc, rs = rope_np(np.clip(pos, 0, SEQ - 1))
        m[f"p{p}_ropeC"], m[f"p{p}_ropeS"] = rc, rs
        m[f"p{p}_amask"] = np.ascontiguousarray(np.stack([tri_lo, tri_hi, tri_lo if p == 1 else z, tri_hi if p == 0 else z], axis=1))
        vb = np.zeros((128, 4, 6, 128), np.float32)
        for ci, jl in enumerate((0, 1, 14, 15)):
            for oi, o in enumerate(b_offsets(jl)):
                vb[:, ci, oi] = b_valid(p * 16 + jl, o)
        m[f"p{p}_vb"] = vb
    m["featsT"] = hc["featsT"]; m["w1"] = np.ascontiguousarray(inp["hy_w1"][0]); m["w2"] = np.ascontiguousarray(inp["hy_w2"][0])
    m["pvec"] = np.ascontiguousarray(np.stack([inp["hy_b1"][0], inp["hy_f1"][0], inp["hy_b2"][0], inp["hy_f2"][0]], axis=1).astype(np.float32))
    m["FcB"], m["FsB"], m["FsBi"], m["cv"] = hc["FcB"], hc["FsB"], hc["FsBi"], hc["cv"]
    for cb in range(4):
        ch = 128 * cb + np.arange(128)
        cols = np.concatenate([d * 1024 + o * 512 + ch for d in range(2) for o in range(2)])
        m[f"k{cb}_w3s"] = np.ascontiguousarray(inp["hy_w3"][0][:, cols])
        m[f"k{cb}_b3s"] = np.ascontiguousarray(inp["hy_b3"][0][cols])
        m[f"k{cb}_decay"] = np.ascontiguousarray(hc["decay"][:, :, ch])
    m["w_in1"] = np.ascontiguousarray(inp["w_in_odd"][0][:, l1_w_in_cols(0)])
    m["w_out1"] = np.ascontiguousarray(inp["w_out_odd"][0])
    m["ropeCn"], m["ropeSn"] = rope_np(np.arange(SEQ))
    m["ropeCq"], m["ropeSq"] = rope_np(half * OWN + np.arange(OWN))
    P = rope_perm()
    qn, kn = inp["c_qnorm"][0], inp["c_knorm"][0]
    m["gvec"] = np.ascontiguousarray(np.stack([np.tile(qn, 2), np.tile(qn[P], 2), np.tile(kn, 2), np.tile(kn[P], 2)], axis=1).astype(np.float32))
    bones = np.zeros((128, 128), np.float32)
    bones[:64, :64] = 1.0
    bones[64:, 64:] = 1.0
    m["bones"] = bones
    swT2 = np.zeros((128, 2, 6, 4), np.float32)
    w_hy = np.zeros((D, 2, 768), np.float32)
    for c in range(2):
        chs = np.concatenate([o * 512 + c * 256 + np.arange(256) for o in range(3)])
        sw = inp["hy_short_w"][0][:, chs]
        sb = inp["hy_short_b"][0][chs]
        swT2[:, c] = np.concatenate([sw, sb[None]], 0).T.reshape(6, 128, 4).transpose(1, 0, 2)
        w_hy[:, c] = inp["w_in_odd"][0][:, 768 + chs]
    m["swT2"] = swT2
    m["w_hy"] = w_hy
    m["hbias2"] = np.ascontiguousarray(inp["hy_bias"][0].reshape(2, 2, 256).transpose(1, 0, 2))
    selv = np.zeros((128, 2), np.float32)
    selv[:, half] = 1.0
    m["selv"] = selv
    m["fg"] = fm(inp["final_g"])
    return m


def kernel(**inp):
    inp = {k: np.asarray(v) for k, v in inp.items()}
    cores = list(range(NCORES))
    res = run_bass_kernel_spmd(build_fused(), [fused_inputs(inp, c) for c in cores], core_ids=cores)
    out = np.zeros((NB, SEQ, D), np.float32)
    for c in cores:
        b, half = divmod(c, 2)
        o = res.results[c]["outT"]
        out[b, half * OWN:(half + 1) * OWN] = o.transpose(2, 0, 1).reshape(OWN, D)
    return out
```

```python
import contextlib
import math
import numpy as np
import concourse.bass as bass
import concourse.mybir as mybir
from concourse.bass_utils import run_bass_kernel_spmd

F32 = mybir.dt.float32
BF16 = mybir.dt.bfloat16
AF = mybir.ActivationFunctionType
ALU = mybir.AluOpType
AX = mybir.AxisListType

EPOCH = 3000
N_DMA_SEMS = 24
NCORES = 8

D = 1024
SEQ = 4096
NB = 4
CTX = 256
GW = 64
HD = 64
EPS = 1e-6
SCALE = HD ** -0.5
OWN = 2048
HALO = 256
NLAT = OWN + 2 * HALO
NTOK = CTX + NLAT
OWN0 = CTX + HALO
NRES = CTX + OWN
NE = 16
DE = 512


class Sched:
    ENGS = ("pe", "act", "dve", "pool", "sp")

    def __init__(self, nc):
        self.nc = nc
        self.stack = contextlib.ExitStack()
        self.eng = {"pe": nc.tensor, "act": nc.scalar, "dve": nc.vector, "pool": nc.gpsimd, "sp": nc.sync}
        self.seq = {e: 0 for e in self.ENGS}
        self.sems = {}
        self.dma_sems = []
        self.dma_uses = []
        self.dma_rr = 0
        self.dma_q = {}
        self.dead = False
        self.waited = {e: {} for e in self.ENGS}
        self.state = {}
        self.out_deps = []
        self.uid = 0

    def sbuf(self, name, shape, dtype, stack=None):
        self.uid += 1
        return (stack or self.stack).enter_context(self.nc.sbuf_tensor(f"sb{self.uid}_{name}", list(shape), dtype))

    def psum(self, name, shape, dtype, stack=None):
        return (stack or self.stack).enter_context(self.nc.psum_tensor(name, list(shape), dtype))

    def _new_sem(self, name):
        return self.stack.enter_context(self.nc.semaphore(name))

    def _sem(self, semkey):
        if semkey[0] == "c":
            k = (semkey[1], semkey[2])
            if k not in self.sems:
                self.sems[k] = self._new_sem(f"s_{semkey[1]}_{semkey[2]}")
            return self.sems[k]
        return self.dma_sems[semkey[1]]

    def _deps(self, eng, reads, writes):
        deps = []
        for k in reads:
            st = self.state.get(k)
            if st and st[0] is not None:
                deps.append(st[0])
        for k in writes:
            st = self.state.get(k)
            if st:
                if st[0] is not None:
                    deps.append(st[0])
                deps.extend(st[1].values())
        best = {}
        for semkey, val, deng in deps:
            if deng == eng and eng == "pe":
                continue
            if self.waited[eng].get(semkey, 0) >= val:
                continue
            best[semkey] = max(best.get(semkey, 0), val)
        for sk, v in best.items():
            self.waited[eng][sk] = v
        return list(best.items())

    def _commit(self, who, dep, reads, writes):
        for k in reads:
            st = self.state.setdefault(k, [None, {}])
            st[1][(who, dep[0])] = dep
        for k in writes:
            self.state[k] = [dep, {}]

    def _emit_waits(self, eng, waits):
        e = self.eng[eng]
        for sk, v in waits:
            e.wait_ge(self._sem(sk), v)

    def kill(self):
        self.barrier()
        self.dead = True

    def op(self, eng, fn, reads=(), writes=()):
        if self.dead:
            return
        waits = self._deps(eng, reads, writes)
        self.seq[eng] += 1
        epoch, val = divmod(self.seq[eng] - 1, EPOCH)
        val += 1
        semkey = ("c", eng, epoch)
        self._emit_waits(eng, waits)
        ins = fn(self.eng[eng])
        ins.then_inc(self._sem(semkey), 1)
        self._commit(eng, (semkey, val, eng), reads, writes)

    def dma(self, q, fn, reads=(), writes=(), is_out=False):
        if self.dead:
            return None
        if q not in self.dma_q:
            base = len(self.dma_sems)
            nq = 16 if q == "sp" else 8
            for i in range(nq):
                self.dma_sems.append(self._new_sem(f"s_dma_{q}_{i}"))
                self.dma_uses.append(0)
            self.dma_q[q] = [base, nq, 0]
        base, nq, rr = self.dma_q[q]
        j = base + rr
        self.dma_q[q][2] = (rr + 1) % nq
        semkey = ("d", j)
        waits = dict(self._deps(q, reads, writes))
        prev = self.dma_uses[j] * 16
        if prev > 0 and self.waited[q].get(semkey, 0) < prev:
            self.waited[q][semkey] = prev
            waits[semkey] = max(waits.get(semkey, 0), prev)
        self.dma_uses[j] += 1
        val = self.dma_uses[j] * 16
        self._emit_waits(q, list(waits.items()))
        ins = fn(self.eng[q])
        ins.then_inc(self.dma_sems[j], 16)
        dep = (semkey, val, "dma")
        self._commit("dma", dep, reads, writes)
        if is_out:
            self.out_deps.append(dep)
        return dep

    def barrier(self):
        if self.dead:
            return
        targets = []
        for e in self.ENGS:
            if self.seq[e] > 0:
                epoch, val = divmod(self.seq[e] - 1, EPOCH)
                targets.append((("c", e, epoch), val + 1, e))
        for j, u in enumerate(self.dma_uses):
            if u > 0:
                targets.append((("d", j), u * 16, "dma"))
        for e in self.ENGS:
            waits = []
            for sk, v, de in targets:
                if de == e:
                    continue
                if self.waited[e].get(sk, 0) >= v:
                    continue
                self.waited[e][sk] = v
                waits.append((sk, v))
            self._emit_waits(e, waits)
        self.state = {}

    def finish(self):
        self.dead = False
        self.barrier()
        self.stack.close()


class IO:
    def __init__(self, nc, ov=None, prefix=""):
        self.nc, self.ov, self.prefix = nc, dict(ov or {}), prefix

    def inp(self, name, shape, dt=F32):
        if name in self.ov:
            return self.ov[name]
        return din(self.nc, self.prefix + name, shape, dt)

    def out(self, name, shape, dt=F32):
        if name in self.ov:
            return self.ov[name]
        return dout(self.nc, self.prefix + name, shape, dt)


def din(nc, name, shape, dt=F32):
    return nc.dram_tensor(name, list(shape), dt, kind="ExternalInput").ap()


def dout(nc, name, shape, dt=F32):
    return nc.dram_tensor(name, list(shape), dt, kind="ExternalOutput").ap()


class Ctx:
    def __init__(self, nc):
        self.nc = nc
        self.S = Sched(nc)
        S = self.S
        self.ps = S.psum("psall", [128, 8, 512], F32)
        self.banks = [self.ps[:, i, :] for i in range(8)]
        self.ones_f = S.sbuf("ones_f", [128, 128], F32)
        self.ones_b = S.sbuf("ones_b", [128, 128], BF16)
        S.op("dve", lambda e: e.memset(self.ones_f[:], 1.0), writes=["ones_f"])
        S.op("dve", lambda e: e.memset(self.ones_b[:], 1.0), writes=["ones_b"])
        self.k = 0

    def key(self, base):
        self.k += 1
        return f"{base}#{self.k}"


def norm_mod_tile(C, xt, xkey, ntok, out_fn, gs_fn, sh_fn, tmp, bank, ranges, f32_out=None):
    S = C.S
    sq, rs = tmp["sq"], tmp["rs"]
    kq = C.key("sq")
    S.op("act", lambda e: e.activation(out=sq[:, :, :ntok], in_=xt, func=AF.Square), reads=[xkey], writes=[kq])
    bk = f"bank{bank}"
    ps = C.banks[bank]
    for k in range(8):
        S.op("pe", lambda e, k=k: e.matmul(ps[:, :ntok], C.ones_f[:], sq[:, k, :ntok], start=(k == 0), stop=(k == 7)),
             reads=[kq, "ones_f"], writes=[bk])
    kr = C.key("rs")
    S.op("act", lambda e: e.activation(out=rs[:, :ntok], in_=ps[:, :ntok], func=AF.Sqrt, bias=EPS, scale=1.0 / D),
         reads=[bk], writes=[kr])
    kr2 = C.key("rs2")
    S.op("dve", lambda e: e.reciprocal(out=rs[:, :ntok], in_=rs[:, :ntok]), reads=[kr], writes=[kr, kr2])
    for k in range(8):
        t = tmp["t"][k % 2]
        kt = f"normt{k % 2}"
        for (a, b, r) in ranges:
            gs, sh = gs_fn(r), sh_fn(r)
            S.op("dve", lambda e, k=k, a=a, b=b, gs=gs, t=t: e.scalar_tensor_tensor(
                out=t[:, a:b], in0=xt[:, k, a:b], scalar=gs[:, k:k + 1], in1=rs[:, a:b], op0=ALU.mult, op1=ALU.mult),
                reads=[xkey, kr2], writes=[kt])
            S.op("act", lambda e, k=k, a=a, b=b, sh=sh, t=t: e.activation(
                out=out_fn(k, a, b), in_=t[:, a:b], func=AF.Identity, bias=sh[:, k:k + 1], scale=1.0),
                reads=[kt], writes=[tmp["outkey"]])
            if f32_out is not None:
                S.op("pool", lambda e, k=k, a=a, b=b, sh=sh, t=t: e.tensor_scalar(
                    out=f32_out(k, a, b), in0=t[:, a:b], scalar1=sh[:, k:k + 1], scalar2=None, op0=ALU.add),
                    reads=[kt], writes=[tmp["f32key"]])


def build_mod():
    nc = bass.Bass("TRN2", target_bir_lowering=False)
    cT = din(nc, "cT", [128, 8, 5])
    w = din(nc, "w", [D, 1536])
    b = din(nc, "b", [1536])
    o = dout(nc, "o", [5, 1536])
    C = Ctx(nc)
    S = C.S
    ct = S.sbuf("ct", [128, 8, 5], F32)
    sg = S.sbuf("sg", [128, 8, 5], F32)
    wt = S.sbuf("wt", [128, 8, 1536], F32)
    bt = S.sbuf("bt", [5, 1536], F32)
    ot = S.sbuf("ot", [5, 1536], F32)
    S.dma("sp", lambda e: e.dma_start(out=ct[:], in_=cT), writes=["ct"])
    for k in range(8):
        S.dma("sp", lambda e, k=k: e.dma_start(out=wt[:, k, :], in_=w[k * 128:(k + 1) * 128, :]), writes=[f"wt{k}"])
    S.dma("sp", lambda e: e.dma_start(out=bt[:], in_=b.partition_broadcast(5)), writes=["bt"])
    S.op("act", lambda e: e.activation(out=sg[:], in_=ct[:], func=AF.Silu), reads=["ct"], writes=["sg"])
    for j in range(3):
        ps = C.banks[j]
        for k in range(8):
            S.op("pe", lambda e, k=k, j=j, ps=ps: e.matmul(ps[0:5, :], sg[:, k, :], wt[:, k, j * 512:(j + 1) * 512],
                                                           start=(k == 0), stop=(k == 7)),
                 reads=["sg", f"wt{k}"], writes=[f"bank{j}"])
        S.op("dve", lambda e, j=j, ps=ps: e.tensor_tensor(out=ot[:, j * 512:(j + 1) * 512], in0=ps[0:5, :],
                                                        in1=bt[:, j * 512:(j + 1) * 512], op=ALU.add),
             reads=[f"bank{j}", "bt"], writes=["ot"])
    S.dma("sp", lambda e: e.dma_start(out=o, in_=ot[:]), reads=["ot"], is_out=True)
    S.finish()
    return nc


def run_mod(inp):
    cond = np.concatenate([inp["c"], inp["c_ctx"][None]], axis=0)
    cT = np.ascontiguousarray(cond.T.reshape(8, 128, 5).transpose(1, 0, 2))
    maps = []
    for i in range(NCORES):
        l, q = divmod(i, 4)
        maps.append({"cT": cT,
                     "w": np.ascontiguousarray(inp["w_ada"][l][:, q * 1536:(q + 1) * 1536]),
                     "b": np.ascontiguousarray(inp["b_ada"][l][q * 1536:(q + 1) * 1536])})
    res = run_bass_kernel_spmd(build_mod(), maps, core_ids=list(range(NCORES)))
    mod = np.zeros((2, 5, 6144), np.float32)
    for i in range(NCORES):
        l, q = divmod(i, 4)
        mod[l][:, q * 1536:(q + 1) * 1536] = res.results[i]["o"]
    return mod


def rope_np(pos):
    pos = np.asarray(pos)
    row = (pos // GW).astype(np.float32)
    col = (pos % GW).astype(np.float32)
    inv = (10000.0 ** (-np.arange(0, 32, 2, dtype=np.float32) / 32)).astype(np.float32)
    ar = row[None, :] * inv[:, None]
    ac = col[None, :] * inv[:, None]
    Ct = np.concatenate([np.cos(ar), np.cos(ar), np.cos(ac), np.cos(ac)], 0)
    St = np.concatenate([-np.sin(ar), np.sin(ar), -np.sin(ac), np.sin(ac)], 0)
    return (np.ascontiguousarray(np.concatenate([Ct, Ct], 0), dtype=np.float32),
            np.ascontiguousarray(np.concatenate([St, St], 0), dtype=np.float32))


def rope_perm():
    return np.concatenate([np.arange(16, 32), np.arange(0, 16), np.arange(48, 64), np.arange(32, 48)])


def b_offsets(jl):
    if jl in (0, 1):
        return list(range(-2, 4))
    if jl in (14, 15):
        return list(range(-3, 3))
    return list(range(-2, 3))


def b_valid(j, o):
    kt = j + o
    m = np.zeros((128, 128), np.float32)
    if kt < 0 or kt >= 32:
        return m
    k = np.arange(128)
    krow = 2 * kt + k // 64
    kcol = k % 64
    qrow = 2 * j + k // 64
    qcol = k % 64
    rs = np.clip(qrow - 4, 0, 56)
    cs = np.clip(qcol - 8, 0, 48)
    ok_r = (krow[:, None] >= rs[None, :]) & (krow[:, None] < rs[None, :] + 8)
    ok_c = (kcol[:, None] >= cs[None, :]) & (kcol[:, None] < cs[None, :] + 16)
    return (ok_r & ok_c).astype(np.float32)


HORD = [0, 2, 4, 6, 1, 3, 5, 7]


def rpb_gather(rpb):
    k = np.arange(128)
    a, kc = k // 64, k % 64
    out = np.zeros((128, 7, 8, 128), np.float32)
    for oi, o in enumerate(range(-3, 4)):
        dr = 2 * o + a[:, None] - a[None, :]
        dc = kc[:, None] - kc[None, :]
        ok = (np.abs(dr) <= 7) & (np.abs(dc) <= 15)
        g = rpb[HORD][:, np.clip(dr + 7, 0, 14), np.clip(dc + 15, 0, 30)]
        g = np.where(ok[None], g, 0.0)
        out[:, oi] = g.transpose(1, 0, 2)
    return out


def l0_w_in_cols():
    P = rope_perm()
    aq = np.arange(0, 512)
    aqP = (aq.reshape(8, 64)[:, P]).reshape(-1)
    ak0 = 512 + np.arange(64)
    ak1 = 576 + np.arange(64)
    av0 = 640 + np.arange(64)
    av1 = 704 + np.arange(64)
    cols = [aq, aqP,
            np.concatenate([ak0, ak0]), np.concatenate([ak0[P], ak0[P]]),
            np.concatenate([ak1, ak1]), np.concatenate([ak1[P], ak1[P]]),
            768 + np.arange(512), 1280 + np.arange(512), 1792 + np.arange(512),
            np.concatenate([av0, av0, av1, av1])]
    return np.concatenate(cols)


def fm(v):
    v = np.asarray(v, np.float32)
    lead = v.shape[:-1]
    r = v.reshape(lead + (8, 128))
    return np.ascontiguousarray(np.moveaxis(r, -1, 0))


def l0_inputs(inp, mod, core):
    b, half = divmod(core, 2)
    pos = half * OWN - HALO + np.arange(NLAT)
    ok = (pos >= 0) & (pos < SEQ)
    xl = np.zeros((NTOK, D), np.float32)
    xl[:CTX] = inp["ctx"][b]
    xl[CTX:][ok] = inp["x"][b][pos[ok]]
    xT = np.ascontiguousarray(xl.T.reshape(8, 128, NTOK))
    rc, rs = rope_np(np.clip(pos, 0, SEQ - 1))
    modt = np.stack([fm(mod[0, b].reshape(6, D)), fm(mod[0, 4].reshape(6, D))], axis=1)
    k = np.arange(128)
    tri_lo = (k[:, None] >= k[None, :]).astype(np.float32)
    tri_hi = (k[:, None] <= k[None, :]).astype(np.float32)
    z = np.zeros_like(tri_lo)
    amask = np.stack([tri_lo, tri_hi, tri_lo if half == 1 else z, tri_hi if half == 0 else z], axis=1)
    vint = np.stack([b_valid(10, o) for o in range(-2, 3)], axis=1)
    vb = np.zeros((128, 4, 6, 128), np.float32)
    for ci, jl in enumerate((0, 1, 14, 15)):
        for oi, o in enumerate(b_offsets(jl)):
            vb[:, ci, oi] = b_valid(half * 16 + jl, o)
    selE = np.zeros((16, 16, 128), np.float32)
    for e in range(16):
        selE[e, e, :] = 1.0
    return {
        "xT": xT, "mod": np.ascontiguousarray(modt), "ng": fm(inp["norm_g"][0]),
        "w_in": np.ascontiguousarray(inp["w_in_even"][0][:, l0_w_in_cols()]),
        "ropeC": rc, "ropeS": rs, "w_out": np.ascontiguousarray(inp["w_out_even"][0]),
        "sink": np.ascontiguousarray(inp["a_sink"][0]), "rpbT": rpb_gather(inp["b_rpb"][0]),
        "amask": np.ascontiguousarray(amask), "vint": np.ascontiguousarray(vint), "vb": vb,
        "w_r": np.ascontiguousarray(inp["w_router"]), "b_r": np.ascontiguousarray(inp["b_router"]),
        "wg": np.ascontiguousarray(inp["moe_wg"][0]), "wu": np.ascontiguousarray(inp["moe_wu"][0]),
        "wd": np.ascontiguousarray(inp["moe_wd"][0]),
        "ident": np.eye(128, dtype=np.float32), "selE": selE,
    }


def load_mod(C, modt, ng, st):
    S = C.S
    mod_sb = S.sbuf("mod_sb", [128, 2, 6, 8], F32, st)
    ng_sb = S.sbuf("ng_sb", [128, 2, 8], F32, st)
    gs_sb = S.sbuf("gs_sb", [128, 2, 2, 8], F32, st)
    S.dma("sp", lambda e: e.dma_start(out=mod_sb[:], in_=modt), writes=["mod_sb"])
    S.dma("sp", lambda e: e.dma_start(out=ng_sb[:], in_=ng), writes=["ng_sb"])
    for i in range(2):
        for r in range(2):
            S.op("dve", lambda e, i=i, r=r: e.scalar_tensor_tensor(
                out=gs_sb[:, i, r, :], in0=mod_sb[:, r, 1 + 3 * i, :], scalar=1.0, in1=ng_sb[:, i, :],
                op0=ALU.add, op1=ALU.mult), reads=["mod_sb", "ng_sb"], writes=["gs_sb"])
    M = {"gs": [[gs_sb[:, i, r, :] for r in range(2)] for i in range(2)],
         "sh": [[mod_sb[:, r, 3 * i, :] for r in range(2)] for i in range(2)],
         "gate": [[mod_sb[:, r, 2 + 3 * i, :] for r in range(2)] for i in range(2)]}
    return M


def norm_phase(C, M, i, src_fn, src_key_fn, ntok_total, ctx_len, out_t, out_key, st, f32_cb=None):
    S = C.S
    tmp = {"sq": S.sbuf("n_sq", [128, 8, 512], F32, st), "rs": S.sbuf("n_rs", [128, 512], F32, st),
           "t": [S.sbuf("n_t0", [128, 512], F32, st), S.sbuf("n_t1", [128, 512], F32, st)],
           "outkey": out_key, "f32key": "h2f"}
    a = 0
    ti = 0
    while a < ntok_total:
        b = min(a + 512, ntok_total)
        n = b - a
        xt, xkey = src_fn(a, b, ti)
        ranges = []
        if a < ctx_len:
            ranges.append((0, min(ctx_len, b) - a, 1))
            if b > ctx_len:
                ranges.append((ctx_len - a, n, 0))
        else:
            ranges.append((0, n, 0))
        f32o = None
        if f32_cb is not None:
            f32o = f32_cb(a, b, ti, "pre")
        norm_mod_tile(C, xt, xkey, n, lambda k, aa, bb, a=a: out_t[:, k, a + aa:a + bb],
                      lambda r: M["gs"][i][r], lambda r: M["sh"][i][r], tmp, 7, ranges, f32_out=f32o)
        if f32_cb is not None:
            f32_cb(a, b, ti, "post")
        a = b
        ti += 1


RES_TILES = [(0, 256)] + [(256 + 512 * i, 256 + 512 * (i + 1)) for i in range(4)]


def moe_phase(C, xres, h2, combT, selE_sb, gate_fn, wg, wu, wd, st, nres=NRES, tiles=RES_TILES, ctx_len=CTX):
    S = C.S
    wgs = [S.sbuf(f"wg_sb{i}", [128, 8, DE], BF16, st) for i in range(2)]
    wus = [S.sbuf(f"wu_sb{i}", [128, 8, DE], BF16, st) for i in range(2)]
    wds = [S.sbuf(f"wd_sb{i}", [128, 4, D], BF16, st) for i in range(2)]
    he = [S.sbuf(f"he{i}", [128, 4, 512], BF16, st) for i in range(2)]
    sg = [S.sbuf(f"sg{i}", [128, 512], BF16, st) for i in range(2)]
    tt = [S.sbuf(f"tt{i}", [128, 512], BF16, st) for i in range(2)]
    bc = [S.sbuf(f"bc{i}", [128, 512], BF16, st) for i in range(2)]
    cnt = 0
    for ex in range(NE):
        wi = ex % 2
        S.dma("pool", lambda e, ex=ex, wi=wi: e.dma_start(out=wgs[wi][:], in_=wg[ex].rearrange("(k p) o -> p k o", p=128)),
              writes=[f"wg{wi}"])
        S.dma("pool", lambda e, ex=ex, wi=wi: e.dma_start(out=wus[wi][:], in_=wu[ex].rearrange("(k p) o -> p k o", p=128)),
              writes=[f"wu{wi}"])
        S.dma("pool", lambda e, ex=ex, wi=wi: e.dma_start(out=wds[wi][:], in_=wd[ex].rearrange("(k p) o -> p k o", p=128)),
              writes=[f"wd{wi}"])
        for (a, b) in tiles:
            n = b - a
            r = 1 if a < ctx_len else 0
            hi = cnt % 2
            cnt += 1
            S.op("pe", lambda e, ex=ex, a=a, b=b, n=n: e.matmul(C.banks[6][:, :n], selE_sb[:, ex, :], combT[:, a:b],
                                                                start=True, stop=True),
                 reads=["combT", "selE"], writes=["bank6"])
            S.op("act", lambda e, hi=hi, n=n: e.activation(out=bc[hi][:, :n], in_=C.banks[6][:, :n], func=AF.Copy),
                 reads=["bank6"], writes=[f"bc{hi}"])
            for hc in range(4):
                gb = (hc % 2) * 2
                gk, uk = f"bank{gb}", f"bank{gb + 1}"
                for k in range(8):
                    S.op("pe", lambda e, k=k, hc=hc, gb=gb, a=a, b=b, n=n, wi=wi: e.matmul(
                        C.banks[gb][:, :n], wgs[wi][:, k, hc * 128:(hc + 1) * 128], h2[:, k, a:b],
                        start=(k == 0), stop=(k == 7)), reads=[f"wg{wi}", "h2"], writes=[gk])
                for k in range(8):
                    S.op("pe", lambda e, k=k, hc=hc, gb=gb, a=a, b=b, n=n, wi=wi: e.matmul(
                        C.banks[gb + 1][:, :n], wus[wi][:, k, hc * 128:(hc + 1) * 128], h2[:, k, a:b],
                        start=(k == 0), stop=(k == 7)), reads=[f"wu{wi}", "h2"], writes=[uk])
                si = hc % 2
                S.op("act", lambda e, gb=gb, si=si, n=n: e.activation(out=sg[si][:, :n], in_=C.banks[gb][:, :n], func=AF.Silu),
                     reads=[gk], writes=[f"sg{si}"])
                S.op("dve", lambda e, gb=gb, si=si, n=n: e.tensor_tensor(out=tt[si][:, :n], in0=sg[si][:, :n],
                                                                        in1=C.banks[gb + 1][:, :n], op=ALU.mult),
                     reads=[f"sg{si}", uk], writes=[f"tt{si}"])
                S.op("pool", lambda e, si=si, hi=hi, hc=hc, n=n: e.tensor_tensor(out=he[hi][:, hc, :n], in0=tt[si][:, :n],
                                                                               in1=bc[hi][:, :n], op=ALU.mult),
                     reads=[f"tt{si}", f"bc{hi}"], writes=[f"he{hi}"])
            for oc in range(8):
                yb = 4 + (oc % 2)
                for hc in range(4):
                    S.op("pe", lambda e, oc=oc, hc=hc, yb=yb, n=n, hi=hi, wi=wi: e.matmul(
                        C.banks[yb][:, :n], wds[wi][:, hc, oc * 128:(oc + 1) * 128], he[hi][:, hc, :n],
                        start=(hc == 0), stop=(hc == 3)), reads=[f"wd{wi}", f"he{hi}"], writes=[f"bank{yb}"])
                g = gate_fn(r)
                S.op("dve", lambda e, oc=oc, yb=yb, a=a, b=b, n=n, g=g: e.scalar_tensor_tensor(
                    out=xres[:, oc, a:b], in0=C.banks[yb][:, :n], scalar=g[:, oc:oc + 1], in1=xres[:, oc, a:b],
                    op0=ALU.mult, op1=ALU.add), reads=[f"bank{yb}", f"xres{oc}"], writes=[f"xres{oc}"])


def router_phase(C, lg_all, ntile, b_r, st):
    S = C.S
    n = ntile
    def T(name, shape):
        return S.sbuf(name, shape, F32, st)
    br = T("r_br", [128, 16])
    s = T("r_s", [128, n, 16]); sel = T("r_sel", [128, n, 16])
    S.dma("sp", lambda e: e.dma_start(out=br[:], in_=b_r.partition_broadcast(128)), writes=["r_br"])
    S.op("act", lambda e: e.activation(out=s[:], in_=lg_all[:], func=AF.Sigmoid), reads=["lg_all"], writes=["r_s"])
    S.op("dve", lambda e: e.tensor_tensor(out=sel[:], in0=s[:], in1=br[:].unsqueeze(1).to_broadcast([128, n, 16]), op=ALU.add),
         reads=["r_s", "r_br"], writes=["r_sel"])
    sv = sel[:].rearrange("p t (g j) -> p t g j", j=4)
    pr = T("r_pr", [128, n, 4]); gsc = T("r_gsc", [128, n, 4])
    first = True
    for i in range(4):
        for j in range(i + 1, 4):
            S.op("dve", lambda e, i=i, j=j: e.tensor_tensor(out=pr[:], in0=sv[:, :, :, i], in1=sv[:, :, :, j], op=ALU.add),
                 reads=["r_sel"], writes=["r_pr"])
            if first:
                S.op("dve", lambda e: e.tensor_copy(out=gsc[:], in_=pr[:]), reads=["r_pr"], writes=["r_gsc"])
                first = False
            else:
                S.op("dve", lambda e: e.tensor_tensor(out=gsc[:], in0=gsc[:], in1=pr[:], op=ALU.max),
                     reads=["r_pr", "r_gsc"], writes=["r_gsc"])
    gmax = T("r_gmax", [128, n]); ing = T("r_ing", [128, n, 4])
    S.op("dve", lambda e: e.tensor_reduce(out=gmax[:], in_=gsc[:], axis=AX.X, op=ALU.max), reads=["r_gsc"], writes=["r_gmax"])
    S.op("dve", lambda e: e.tensor_tensor(out=ing[:], in0=gsc[:], in1=gmax[:].unsqueeze(2).to_broadcast([128, n, 4]), op=ALU.is_ge),
         reads=["r_gsc", "r_gmax"], writes=["r_ing"])
    cg = T("r_cg", [128, n, 4, 4]); cm = T("r_cm", [128, n, 4])
    cgv = cg[:]
    for i in range(4):
        firstj = True
        for j in range(4):
            if j == i:
                continue
            if firstj:
                S.op("dve", lambda e, i=i, j=j: e.tensor_tensor(out=cgv[:, :, :, i], in0=sv[:, :, :, j], in1=sv[:, :, :, i], op=ALU.is_gt),
                     reads=["r_sel"], writes=["r_cg"])
                firstj = False
            else:
                S.op("dve", lambda e, i=i, j=j: e.tensor_tensor(out=cm[:], in0=sv[:, :, :, j], in1=sv[:, :, :, i], op=ALU.is_gt),
                     reads=["r_sel"], writes=["r_cm"])
                S.op("dve", lambda e, i=i: e.tensor_tensor(out=cgv[:, :, :, i], in0=cgv[:, :, :, i], in1=cm[:], op=ALU.add),
                     reads=["r_cm", "r_cg"], writes=["r_cg"])
    selm = T("r_selm", [128, n, 4, 4])
    S.op("dve", lambda e: e.tensor_single_scalar(out=selm[:], in_=cg[:], scalar=1.5, op=ALU.is_lt), reads=["r_cg"], writes=["r_selm"])
    S.op("dve", lambda e: e.tensor_tensor(out=selm[:], in0=selm[:], in1=ing[:].unsqueeze(3).to_broadcast([128, n, 4, 4]), op=ALU.mult),
         reads=["r_selm", "r_ing"], writes=["r_selm"])
    comb = T("r_comb", [128, n, 16]); den = T("r_den", [128, n])
    S.op("dve", lambda e: e.tensor_tensor(out=comb[:], in0=s[:], in1=selm[:].rearrange("p t g j -> p t (g j)"), op=ALU.mult),
         reads=["r_s", "r_selm"], writes=["r_comb"])
    S.op("dve", lambda e: e.tensor_reduce(out=den[:], in_=comb[:], axis=AX.X, op=ALU.add), reads=["r_comb"], writes=["r_den"])
    S.op("dve", lambda e: e.reciprocal(out=den[:], in_=den[:]), reads=["r_den"], writes=["r_den"])
    S.op("dve", lambda e: e.tensor_tensor(out=comb[:], in0=comb[:], in1=den[:].unsqueeze(2).to_broadcast([128, n, 16]), op=ALU.mult),
         reads=["r_comb", "r_den"], writes=["r_comb"])
    return comb


def tail_phase(C, M, ybuf, w_out_d, x_src, xres_out_d, w_r, b_r, ident_d, selE_d, wg, wu, wd, st,
               final_g_d=None, nres=NRES, tiles=RES_TILES, ctx_len=CTX):
    S = C.S
    ntile = nres // 128
    xres = S.sbuf("xres", [128, 8, nres], F32, st)
    lg_all = S.sbuf("lg_all", [128, ntile, 16], F32, st)
    with contextlib.ExitStack() as st2:
        wo = S.sbuf("wo_sb", [128, 8, D], BF16, st2)
        xts = [S.sbuf(f"xt_b{i}", [128, 8, 512], F32, st2) for i in range(2)]
        S.dma("pool", lambda e: e.dma_start(out=wo[:], in_=w_out_d.rearrange("(k p) o -> p k o", p=128)), writes=["wo"])
        for ti, (a, b) in enumerate(tiles):
            n = b - a
            r = 1 if a < ctx_len else 0
            xt = xts[ti % 2]
            S.dma("sp", lambda e, a=a, b=b, n=n, xt=xt: e.dma_start(out=xt[:, :, :n], in_=x_src(a, b)), writes=[f"xtb{ti % 2}"])
            for oc in range(8):
                bk = oc % 4
                for k in range(8):
                    S.op("pe", lambda e, oc=oc, k=k, bk=bk, a=a, b=b, n=n: e.matmul(
                        C.banks[bk][:, :n], wo[:, k, oc * 128:(oc + 1) * 128], ybuf[:, k, a:b], start=(k == 0), stop=(k == 7)),
                        reads=["wo", "Y"], writes=[f"bank{bk}"])
                g = M["gate"][0][r]
                S.op("dve", lambda e, oc=oc, bk=bk, a=a, b=b, n=n, g=g, xt=xt: e.scalar_tensor_tensor(
                    out=xres[:, oc, a:b], in0=C.banks[bk][:, :n], scalar=g[:, oc:oc + 1], in1=xt[:, oc, :n],
                    op0=ALU.mult, op1=ALU.add), reads=[f"bank{bk}", f"xtb{ti % 2}"], writes=[f"xres{oc}"])
    S.barrier()
    h2 = ybuf
    with contextlib.ExitStack() as st2:
        wr = S.sbuf("wr_sb", [128, 8, 16], F32, st2)
        h2f = S.sbuf("h2f", [128, 8, 512], F32, st2)
        S.dma("sp", lambda e: e.dma_start(out=wr[:], in_=w_r.rearrange("(k p) o -> p k o", p=128)), writes=["wr"])

        def src_fn(a, b, ti):
            return xres[:, :, a:b], None

        def f32_cb(a, b, ti, when):
            if when == "pre":
                return lambda k, aa, bb: h2f[:, k, aa:bb]
            n = b - a
            for t in range(n // 128):
                tile_i = a // 128 + t
                for k in range(8):
                    S.op("pe", lambda e, k=k, t=t: e.matmul(C.banks[5][:, 0:16], h2f[:, k, t * 128:(t + 1) * 128], wr[:, k, :],
                                                            start=(k == 0), stop=(k == 7)),
                         reads=["h2f", "wr"], writes=["bank5"])
                S.op("dve", lambda e, tile_i=tile_i: e.tensor_copy(out=lg_all[:, tile_i, :], in_=C.banks[5][:, 0:16]),
                     reads=["bank5"], writes=["lg_all"])
            return None

        tmp_key = [f"xres{oc}" for oc in range(8)]

        def src_fn2(a, b, ti):
            return xres[:, :, a:b], "xres_all"
        norm_phase(C, M, 1, src_fn2, None, nres, ctx_len, h2, "h2", st2, f32_cb=f32_cb)
    S.barrier()
    ident = S.sbuf("ident", [128, 128], F32, st)
    selE = S.sbuf("selE_sb", [16, 16, 128], F32, st)
    combT = S.sbuf("combT", [16, nres], F32, st)
    with contextlib.ExitStack() as st2:
        comb = router_phase(C, lg_all, ntile, b_r, st2)
        S.dma("sp", lambda e: e.dma_start(out=ident[:], in_=ident_d), writes=["ident"])
        S.dma("sp", lambda e: e.dma_start(out=selE[:], in_=selE_d), writes=["selE"])
        for t in range(ntile):
            S.op("pe", lambda e, t=t: e.transpose(C.banks[t % 2][0:16, 0:128], comb[:, t, :], ident[:]),
                 reads=["r_comb", "ident"], writes=[f"bank{t % 2}"])
            S.op("dve", lambda e, t=t: e.tensor_copy(out=combT[:, t * 128:(t + 1) * 128], in_=C.banks[t % 2][0:16, 0:128]),
                 reads=[f"bank{t % 2}"], writes=["combT"])
    S.barrier()
    with contextlib.ExitStack() as st2:
        moe_phase(C, xres, h2, combT, selE, lambda r: M["gate"][1][r], wg, wu, wd, st2, nres=nres, tiles=tiles, ctx_len=ctx_len)
    S.barrier()
    if final_g_d is None and isinstance(xres_out_d, tuple):
        x1nat, pp = xres_out_d
        for k in range(8):
            if pp == 0:
                S.dma("sp", lambda e, k=k: e.dma_start(out=x1nat[k][:, 0:CTX], in_=xres[:, k, 0:CTX]), reads=[f"xres{k}"], writes=["x1nat"])
            S.dma("sp", lambda e, k=k: e.dma_start(out=x1nat[k][:, CTX + pp * OWN:CTX + (pp + 1) * OWN], in_=xres[:, k, CTX:]),
                  reads=[f"xres{k}"], writes=["x1nat"])
    elif final_g_d is None:
        for k in range(8):
            S.dma("sp", lambda e, k=k: e.dma_start(out=xres_out_d[k], in_=xres[:, k, :]), reads=[f"xres{k}"], is_out=True)
    else:
        with contextlib.ExitStack() as st2:
            fg = S.sbuf("fg", [128, 8], F32, st2)
            zsh = S.sbuf("zsh", [128, 8], F32, st2)
            dummy = S.sbuf("fdummy", [128, 8, 512], BF16, st2)
            fo = [S.sbuf(f"fo{i}", [128, 8, 512], F32, st2) for i in range(2)]
            S.dma("sp", lambda e: e.dma_start(out=fg[:], in_=final_g_d), writes=["fg"])
            S.op("dve", lambda e: e.memset(zsh[:], 0.0), writes=["zsh"])
            S.barrier()
            Mf = {"gs": [[fg[:], fg[:]]], "sh": [[zsh[:], zsh[:]]]}
            xo = xres_out_d.rearrange("k p t -> p k t")

            def src_fn3(a, b, ti):
                return xres[:, :, a:b], "xres_all"

            def f32_cb3(a, b, ti, when):
                if when == "pre":
                    return lambda k, aa, bb: fo[ti % 2][:, k, aa:bb]
                S.dma("sp", lambda e: e.dma_start(out=xo[:, :, a:b], in_=fo[ti % 2][:, :, :b - a]), reads=[f"fo{ti % 2}"], is_out=True)
                return None
            tmpn = {"sq": S.sbuf("f_sq", [128, 8, 512], F32, st2), "rs": S.sbuf("f_rs", [128, 512], F32, st2),
                    "t": [S.sbuf("f_t0", [128, 512], F32, st2), S.sbuf("f_t1", [128, 512], F32, st2)]}
            a = 0
            ti = 0
            while a < nres:
                b = min(a + 512, nres)
                tmpn["outkey"] = "fdummy"
                tmpn["f32key"] = f"fo{ti % 2}"
                f32o = f32_cb3(a, b, ti, "pre")
                norm_mod_tile(C, xres[:, :, a:b], "xres_all", b - a, lambda k, aa, bb: dummy[:, k, aa:bb],
                              lambda r: fg[:], lambda r: zsh[:], tmpn, 7, [(0, b - a, 0)], f32_out=f32o)
                f32_cb3(a, b, ti, "post")
                a = b
                ti += 1
    return xres


class _Stop(Exception):
    pass


def build_l0(debug=False, stop=None):
    nc = bass.Bass("TRN2", target_bir_lowering=False)
    C = Ctx(nc)
    _build_l0_body(nc, C, debug, stop, IO(nc))
    C.S.finish()
    return nc


def _build_l0_body(nc, C, debug, stop, io, write_ctx=True):
    xT = io.inp("xT", [8, 128, NTOK])
    modt = io.inp("mod", [128, 2, 6, 8])
    ng = io.inp("ng", [128, 2, 8])
    w_in = io.inp("w_in", [D, 3328])
    ropeC = io.inp("ropeC", [128, NLAT])
    ropeS = io.inp("ropeS", [128, NLAT])
    w_out = io.inp("w_out", [D, D])
    sink = io.inp("sink", [8])
    rpbT = io.inp("rpbT", [128, 7, 8, 128])
    amask_d = io.inp("amask", [128, 4, 128])
    vint_d = io.inp("vint", [128, 5, 128])
    vb_d = io.inp("vb", [128, 4, 6, 128])
    w_r = io.inp("w_r", [D, 16])
    b_r = io.inp("b_r", [16])
    if stop is None or stop == "full":
        wg = io.inp("wg", [NE, D, DE])
        wu = io.inp("wu", [NE, D, DE])
        wd = io.inp("wd", [NE, DE, D])
    else:
        wg = wu = wd = None
    ident_d = io.inp("ident", [128, 128])
    selE_d = io.inp("selE", [16, 16, 128])
    x1T = io.out("x1T", [8, 128, NRES])
    dbg = {}
    if debug:
        dbg["hx"] = io.out("d_hx", [128, 8, NTOK], BF16)
        dbg["y"] = io.out("d_y", [128, 8, NRES], BF16)

    S = C.S
    st0 = contextlib.ExitStack()
    M = load_mod(C, modt, ng, st0)
    xv = xT.rearrange("k p t -> p k t")
    hy = S.sbuf("hy", [128, 8, NTOK], BF16, st0)
    hx = hy
    ybuf = hy[:, :, 0:NRES]
    with contextlib.ExitStack() as stA:
        xts = [S.sbuf(f"xa{i}", [128, 8, 512], F32, stA) for i in range(2)]

        def src_fn(a, b, ti):
            xt = xts[ti % 2]
            S.dma("sp", lambda e: e.dma_start(out=xt[:, :, :b - a], in_=xv[:, :, a:b]), writes=[f"xa{ti % 2}"])
            return xt[:, :, :b - a], f"xa{ti % 2}"
        norm_phase(C, M, 0, src_fn, None, NTOK, CTX, hx, "hx", stA)
    S.barrier()
    if stop == "A0":
        S.kill()
    if debug:
        for k in range(8):
            S.dma("sp", lambda e, k=k: e.dma_start(out=dbg["hx"][:, k, :], in_=hx[:, k, :]), reads=["hx"], is_out=True)
        S.barrier()
    if stop == "A":
        S.kill()

    with contextlib.ExitStack() as stQ:
        QA = S.sbuf("QA", [128, 4, NRES], BF16, stQ)
        QB = S.sbuf("QB", [128, 4, NRES], BF16, stQ)
        KAB = S.sbuf("KAB", [128, 2, NTOK], BF16, stQ)
        BK = S.sbuf("BK", [128, 4, NTOK], BF16, stQ)
        VA2 = S.sbuf("VA2", [128, 22, 256], BF16, stQ)
        VB = S.sbuf("VB", [128, 22, 512], BF16, stQ)
        if True:
            with contextlib.ExitStack() as stB:
                wts = [S.sbuf(f"wt{i}", [128, 8, 512], BF16, stB) for i in range(2)]
                rc = S.sbuf("rc", [128, NLAT], F32, stB)
                rs_ = S.sbuf("rs", [128, NLAT], F32, stB)
                t1 = [S.sbuf(f"t1_{i}", [128, 512], F32, stB) for i in range(2)]
                t2 = [S.sbuf(f"t2_{i}", [128, 512], F32, stB) for i in range(2)]
                S.dma("sp", lambda e: e.dma_start(out=rc[:], in_=ropeC), writes=["rc"])
                S.dma("sp", lambda e: e.dma_start(out=rs_[:], in_=ropeS), writes=["rs"])
                wv = w_in.rearrange("(k p) o -> p k o", p=128)
                ngrp = [0]

                def load_w(c0, ncol):
                    wi = ngrp[0] % 2
                    ngrp[0] += 1
                    S.dma("pool", lambda e: e.dma_start(out=wts[wi][:, :, :ncol], in_=wv[:, :, c0:c0 + ncol]), writes=[f"wt{wi}"])
                    return wts[wi], f"wt{wi}"

                cnt = [0]

                def fm_block(wt, wkey, j, toks, out_fn, okey):
                    for (a, b, da) in toks:
                        n = b - a
                        bk = cnt[0] % 4
                        cnt[0] += 1
                        for k in range(8):
                            S.op("pe", lambda e, k=k: e.matmul(C.banks[bk][:, :n], wt[:, k, j * 128:(j + 1) * 128], hx[:, k, a:b],
                                                               start=(k == 0), stop=(k == 7)), reads=[wkey, "hx"], writes=[f"bank{bk}"])
                        S.op("act", lambda e: e.activation(out=out_fn(da, da + n), in_=C.banks[bk][:, :n], func=AF.Copy),
                             reads=[f"bank{bk}"], writes=[okey])

                def rope_block(wt, wkey, j, jp, toks, out_fn, okey):
                    for (a, b, da) in toks:
                        n = b - a
                        bk = (cnt[0] % 2) * 2
                        ti = cnt[0] % 2
                        cnt[0] += 1
                        la = a - CTX
                        for k in range(8):
                            S.op("pe", lambda e, k=k: e.matmul(C.banks[bk][:, :n], wt[:, k, j * 128:(j + 1) * 128], hx[:, k, a:b],
                                                               start=(k == 0), stop=(k == 7)), reads=[wkey, "hx"], writes=[f"bank{bk}"])
                        for k in range(8):
                            S.op("pe", lambda e, k=k: e.matmul(C.banks[bk + 1][:, :n], wt[:, k, jp * 128:(jp + 1) * 128], hx[:, k, a:b],
                                                               start=(k == 0), stop=(k == 7)), reads=[wkey, "hx"], writes=[f"bank{bk + 1}"])
                        S.op("dve", lambda e: e.tensor_tensor(out=t1[ti][:, :n], in0=C.banks[bk][:, :n], in1=rc[:, la:la + n], op=ALU.mult),
                             reads=[f"bank{bk}", "rc"], writes=[f"t1_{ti}"])
                        S.op("dve", lambda e: e.tensor_tensor(out=t2[ti][:, :n], in0=C.banks[bk + 1][:, :n], in1=rs_[:, la:la + n], op=ALU.mult),
                             reads=[f"bank{bk + 1}", "rs"], writes=[f"t2_{ti}"])
                        S.op("pool", lambda e: e.tensor_tensor(out=out_fn(da, da + n), in0=t1[ti][:, :n], in1=t2[ti][:, :n], op=ALU.add),
                             reads=[f"t1_{ti}", f"t2_{ti}"], writes=[okey])

                own_toks = [(OWN0 + 512 * i, OWN0 + 512 * (i + 1), CTX + 512 * i) for i in range(4)]
                ctx_toks = [(0, CTX, 0)]
                lat_toks = [(CTX + 512 * i, CTX + 512 * (i + 1), CTX + 512 * i) for i in range(5)]
                wA, kA = load_w(0, 512)
                wP, kP = load_w(512, 512)
                for j in range(4):
                    fm_block(wA, kA, j, ctx_toks, lambda a, b, j=j: QA[:, j, a:b], "QA")
                    for (a, b, da) in own_toks:
                        n = b - a
                        bk = (cnt[0] % 2) * 2
                        ti = cnt[0] % 2
                        cnt[0] += 1
                        la = a - CTX
                        for k in range(8):
                            S.op("pe", lambda e, k=k: e.matmul(C.banks[bk][:, :n], wA[:, k, j * 128:(j + 1) * 128], hx[:, k, a:b],
                                                               start=(k == 0), stop=(k == 7)), reads=[kA, "hx"], writes=[f"bank{bk}"])
                        for k in range(8):
                            S.op("pe", lambda e, k=k: e.matmul(C.banks[bk + 1][:, :n], wP[:, k, j * 128:(j + 1) * 128], hx[:, k, a:b],
                                                               start=(k == 0), stop=(k == 7)), reads=[kP, "hx"], writes=[f"bank{bk + 1}"])
                        S.op("dve", lambda e: e.tensor_tensor(out=t1[ti][:, :n], in0=C.banks[bk][:, :n], in1=rc[:, la:la + n], op=ALU.mult),
                             reads=[f"bank{bk}", "rc"], writes=[f"t1_{ti}"])
                        S.op("dve", lambda e: e.tensor_tensor(out=t2[ti][:, :n], in0=C.banks[bk + 1][:, :n], in1=rs_[:, la:la + n], op=ALU.mult),
                             reads=[f"bank{bk + 1}", "rs"], writes=[f"t2_{ti}"])
                        S.op("pool", lambda e, da=da, n=n: e.tensor_tensor(out=QA[:, j, da:da + n], in0=t1[ti][:, :n], in1=t2[ti][:, :n], op=ALU.add),
                             reads=[f"t1_{ti}", f"t2_{ti}"], writes=["QA"])
                wK, kK = load_w(1024, 512)
                for g in range(2):
                    fm_block(wK, kK, 2 * g, ctx_toks, lambda a, b, g=g: KAB[:, g, a:b], "KAB")
                    rope_block(wK, kK, 2 * g, 2 * g + 1, lat_toks, lambda a, b, g=g: KAB[:, g, a:b], "KAB")
                wq, kq = load_w(1536, 512)
                for j in range(4):
                    fm_block(wq, kq, j, ctx_toks + own_toks, lambda a, b, j=j: QB[:, j, a:b], "QB")
                wk_, kk_ = load_w(2048, 512)
                all_toks = ctx_toks + lat_toks
                for j in range(4):
                    fm_block(wk_, kk_, j, all_toks, lambda a, b, j=j: BK[:, j, a:b], "BK")
                wvb, kvb = load_w(2560, 512)
                wva, kva = load_w(3072, 256)
                for t in range(22):
                    bk = cnt[0] % 4
                    cnt[0] += 1
                    for k in range(8):
                        S.op("pe", lambda e, k=k: e.matmul(C.banks[bk][:, :], hx[:, k, t * 128:(t + 1) * 128], wvb[:, k, :],
                                                           start=(k == 0), stop=(k == 7)), reads=[kvb, "hx"], writes=[f"bank{bk}"])
                    S.op("act", lambda e: e.activation(out=VB[:, t, :], in_=C.banks[bk][:, :], func=AF.Copy),
                         reads=[f"bank{bk}"], writes=["VB"])
                    bk = cnt[0] % 4
                    cnt[0] += 1
                    for k in range(8):
                        S.op("pe", lambda e, k=k: e.matmul(C.banks[bk][:, :256], hx[:, k, t * 128:(t + 1) * 128], wva[:, k, :256],
                                                           start=(k == 0), stop=(k == 7)), reads=[kva, "hx"], writes=[f"bank{bk}"])
                    S.op("dve", lambda e: e.tensor_copy(out=VA2[:, t, :], in_=C.banks[bk][:, :256]),
                         reads=[f"bank{bk}"], writes=["VA2"])
            S.barrier()
        if stop == "B":
            S.kill()
        with contextlib.ExitStack() as stC:
            esk = S.sbuf("esk", [128, 8], F32, stC)
            am = S.sbuf("am", [128, 4, 128], BF16, stC)
            Tm = S.sbuf("Tm", [128, 7, 8, 128], BF16, stC)
            TV = S.sbuf("TV", [128, 5, 8, 128], BF16, stC)
            vint = S.sbuf("vint", [128, 5, 128], BF16, stC)
            vbm = S.sbuf("vbm", [128, 4, 6, 128], BF16, stC)
            pts = [S.sbuf(f"pt{i}", [128, 1024], BF16, stC) for i in range(2)]
            dn = S.sbuf("dn", [128, 1024], F32, stC)
            S.dma("sp", lambda e: e.dma_start(out=esk[:], in_=sink.partition_broadcast(128)), writes=["esk"])
            S.op("act", lambda e: e.activation(out=esk[:], in_=esk[:], func=AF.Exp), reads=["esk"], writes=["esk"])
            S.dma("pool", lambda e: e.dma_start(out=am[:], in_=amask_d), writes=["am"])
            S.dma("pool", lambda e: e.dma_start(out=vint[:], in_=vint_d), writes=["vint"])
            S.dma("pool", lambda e: e.dma_start(out=vbm[:], in_=vb_d), writes=["vbm"])
            with contextlib.ExitStack() as stT:
                stg = S.sbuf("rp_stage", [128, 8, 128], F32, stT)
                for oi in range(7):
                    S.dma("sp", lambda e, oi=oi: e.dma_start(out=stg[:], in_=rpbT[:, oi]), writes=["rp_stage"])
                    S.op("act", lambda e, oi=oi: e.activation(out=Tm[:, oi], in_=stg[:], func=AF.Exp), reads=["rp_stage"], writes=["Tm"])
                for oi in range(5):
                    S.op("dve", lambda e, oi=oi: e.tensor_tensor(out=TV[:, oi], in0=Tm[:, oi + 1],
                                                                 in1=vint[:, oi, :].unsqueeze(1).to_broadcast([128, 8, 128]), op=ALU.mult),
                         reads=["Tm", "vint"], writes=["TV"])
            S.barrier()
            if stop == "C0":
                S.kill()
            ac = [0]

            import os
            ATT_STAGE = int(os.environ.get("ATT_STAGE", "9"))
            ATT_N = int(os.environ.get("ATT_N", "999"))

            def attnA(q0, klist):
                for g in range(2):
                    if ac[0] >= ATT_N:
                        return
                    it = ac[0]
                    ac[0] += 1
                    nb, db = 4 + it % 2, 6 + it % 2
                    def emit_SA(idx):
                        tk = klist[idx][0]
                        sb = idx % 2
                        for s_ in range(4):
                            hh = (0, 2, 1, 3)[s_]
                            h = 4 * g + hh
                            c, off = h // 2, (h % 2) * 64
                            bnk = 2 * sb + s_ // 2
                            S.op("pe", lambda e, s_=s_, c=c, off=off, tk=tk, bnk=bnk: e.matmul(
                                C.banks[bnk][:, (s_ % 2) * 128:(s_ % 2 + 1) * 128], KAB[off:off + 64, g, tk * 128:(tk + 1) * 128],
                                QA[off:off + 64, c, q0:q0 + 128], start=True, stop=True),
                                reads=["KAB", "QA"], writes=[f"SA{sb}"])
                    emit_SA(0)
                    for idx, (tk, mk) in enumerate(klist):
                        sb = idx % 2
                        pt = pts[idx % 2]
                        if idx + 1 < len(klist):
                            emit_SA(idx + 1)
                        S.op("act", lambda e, sb=sb, pt=pt: e.activation(
                            out=pt[:, 0:512].rearrange("p (a b) -> p a b", a=2), in_=C.ps[:, 2 * sb:2 * sb + 2, 0:256], func=AF.Exp, scale=SCALE),
                            reads=[f"SA{sb}"], writes=[f"pt{idx % 2}"])
                        if mk is not None:
                            S.op("pool", lambda e, pt=pt, mk=mk: e.tensor_tensor(
                                out=pt[:, 0:512].rearrange("p (h q) -> p h q", h=4), in0=pt[:, 0:512].rearrange("p (h q) -> p h q", h=4),
                                in1=mk.unsqueeze(1).to_broadcast([128, 4, 128]), op=ALU.mult),
                                reads=[f"pt{idx % 2}", "am"], writes=[f"pt{idx % 2}"])
                        last = idx == len(klist) - 1
                        if ATT_STAGE < 2:
                            continue
                        S.op("pe", lambda e, pt=pt, tk=tk, idx=idx, last=last, nb=nb: e.matmul(
                            C.banks[nb][:, :], VA2[:, tk, g * 128:(g + 1) * 128], pt[:, 0:512], start=(idx == 0), stop=last),
                            reads=["VA2", f"pt{idx % 2}"], writes=[f"bank{nb}"])
                        S.op("pe", lambda e, pt=pt, idx=idx, last=last, db=db: e.matmul(
                            C.banks[db][:, :], C.ones_b[:], pt[:, 0:512], start=(idx == 0), stop=last),
                            reads=["ones_b", f"pt{idx % 2}"], writes=[f"bank{db}"])
                    if ATT_STAGE < 3:
                        continue
                    for s_ in range(4):
                        hh = s_
                        h = 4 * g + (0, 2, 1, 3)[s_]
                        S.op("dve", lambda e, hh=hh, h=h, db=db: e.tensor_scalar(
                            out=dn[:, hh * 128:(hh + 1) * 128], in0=C.banks[db][:, hh * 128:(hh + 1) * 128],
                            scalar1=esk[:, h:h + 1], scalar2=None, op0=ALU.add), reads=[f"bank{db}", "esk"], writes=["dn"])
                    S.op("dve", lambda e: e.reciprocal(out=dn[:, 0:512], in_=dn[:, 0:512]), reads=["dn"], writes=["dn"])
                    for s_ in range(4):
                        hh = s_
                        h = 4 * g + (0, 2, 1, 3)[s_]
                        c, off = h // 2, (h % 2) * 64
                        S.op("dve", lambda e, hh=hh, c=c, off=off, nb=nb: e.tensor_tensor(
                            out=ybuf[off:off + 64, c, q0:q0 + 128], in0=C.banks[nb][off:off + 64, hh * 128:(hh + 1) * 128],
                            in1=dn[off:off + 64, hh * 128:(hh + 1) * 128], op=ALU.mult),
                            reads=[f"bank{nb}", "dn"], writes=["Y"])

            for qt in range(2):
                attnA(qt * 128, [(0, None), (1, None)])
            for jl in range(16):
                kl = [(0, None), (1, None)]
                for o in (-1, 0, 1):
                    tk = 4 + jl + o
                    mk = None
                    if o == -1:
                        mk = am[:, 2, :] if jl == 0 else am[:, 0, :]
                    elif o == 1:
                        mk = am[:, 3, :] if jl == 15 else am[:, 1, :]
                    kl.append((tk, mk))
                attnA(CTX + jl * 128, kl)
            S.barrier()
            if stop == "C":
                S.kill()
            if debug:
                pass

            bc_ = [0]

            def attnB(q0, klist):
                S2 = [C.ps[:, 0:2, :].rearrange("p a b -> p (a b)"), C.ps[:, 2:4, :].rearrange("p a b -> p (a b)")]
                NUM = C.ps[:, 4:6, :].rearrange("p a b -> p (a b)")
                DEN = C.ps[:, 6:8, :].rearrange("p a b -> p (a b)")
                BST = int(os.environ.get("ATTB_STAGE", "9"))
                bc_[0] += 1
                if bc_[0] > int(os.environ.get("ATTB_N", "999")):
                    return
                def emit_SB(idx):
                    tk = klist[idx][0]
                    sb = idx % 2
                    for s_ in range(8):
                        h = s_
                        hd = HORD[s_]
                        c, off = hd // 2, (hd % 2) * 64
                        S.op("pe", lambda e, h=h, c=c, off=off, tk=tk, sb=sb: e.matmul(
                            S2[sb][:, h * 128:(h + 1) * 128], BK[off:off + 64, c, tk * 128:(tk + 1) * 128],
                            QB[off:off + 64, c, q0:q0 + 128], start=True, stop=True), reads=["BK", "QB"], writes=[f"S2_{sb}"])
                emit_SB(0)
                for idx, (tk, mks) in enumerate(klist):
                    sb = idx % 2
                    pt = pts[idx % 2]
                    sk = f"S2_{sb}"
                    if idx + 1 < len(klist):
                        emit_SB(idx + 1)
                    for hf in range(2):
                        S.op("act", lambda e, hf=hf, sb=sb, pt=pt: e.activation(
                            out=pt[:, hf * 512:(hf + 1) * 512], in_=S2[sb][:, hf * 512:(hf + 1) * 512], func=AF.Exp, scale=SCALE),
                            reads=[sk], writes=[f"pt{idx % 2}"])
                    for mk, full in (mks if BST >= 2 else []):
                        in1 = mk if full else mk.unsqueeze(1).to_broadcast([128, 8, 128])
                        S.op("pool", lambda e, pt=pt, in1=in1: e.tensor_tensor(
                            out=pt[:].rearrange("p (h q) -> p h q", h=8), in0=pt[:].rearrange("p (h q) -> p h q", h=8),
                            in1=in1, op=ALU.mult), reads=[f"pt{idx % 2}", "TV", "Tm", "vbm"], writes=[f"pt{idx % 2}"])
                    last = idx == len(klist) - 1
                    if BST < 3:
                        continue
                    for h in range(8):
                        c = HORD[h] // 2
                        S.op("pe", lambda e, h=h, c=c, pt=pt, tk=tk, idx=idx, last=last: e.matmul(
                            NUM[:, h * 128:(h + 1) * 128], VB[:, tk, c * 128:(c + 1) * 128], pt[:, h * 128:(h + 1) * 128],
                            start=(idx == 0 and h % 4 == 0), stop=(last and h % 4 == 3), skip_group_check=True),
                            reads=["VB", f"pt{idx % 2}"], writes=["NUM"])
                    for hf in range(2):
                        S.op("pe", lambda e, hf=hf, pt=pt, idx=idx, last=last: e.matmul(
                            DEN[:, hf * 512:(hf + 1) * 512], C.ones_b[:], pt[:, hf * 512:(hf + 1) * 512],
                            start=(idx == 0), stop=last), reads=["ones_b", f"pt{idx % 2}"], writes=["DEN"])
                if BST < 4:
                    return
                S.op("dve", lambda e: e.reciprocal(out=dn[:], in_=DEN), reads=["DEN"], writes=["dn"])
                for h in range(8):
                    c, off = HORD[h] // 2, (HORD[h] % 2) * 64
                    S.op("dve", lambda e, h=h, c=c, off=off: e.tensor_tensor(
                        out=ybuf[off:off + 64, 4 + c, q0:q0 + 128], in0=NUM[off:off + 64, h * 128:(h + 1) * 128],
                        in1=dn[off:off + 64, h * 128:(h + 1) * 128], op=ALU.mult), reads=["NUM", "dn"], writes=["Y"])

            for qt in range(2):
                attnB(qt * 128, [(0, []), (1, [])])
            for jl in range(16):
                kl = [(0, []), (1, [])]
                offs = b_offsets(jl)
                for oi, o in enumerate(offs):
                    tk = 4 + jl + o
                    if jl in (0, 1, 14, 15):
                        ci = (0, 1, 14, 15).index(jl)
                        mks = [(Tm[:, o + 3], True), (vbm[:, ci, oi, :], False)]
                    else:
                        mks = [(TV[:, o + 2], True)]
                    kl.append((tk, mks))
                attnB(CTX + jl * 128, kl)
            S.barrier()
    if debug:
        for k in range(8):
            S.dma("sp", lambda e, k=k: e.dma_start(out=dbg["y"][:, k, :], in_=ybuf[:, k, :]), reads=["Y"], is_out=True)
        S.barrier()
    if stop == "Y":
        S.kill()

    def x_src(a, b):
        if a < CTX:
            return xv[:, :, a:b]
        return xv[:, :, a + HALO:b + HALO]
    tail_phase(C, M, ybuf, w_out, x_src, x1T, w_r, b_r, ident_d, selE_d, wg, wu, wd, st0)
    S.barrier()
    st0.close()


NFFT = 2 * SEQ
_HC = {}


def hy_consts():
    if _HC:
        return _HC
    import ml_dtypes
    bf = ml_dtypes.bfloat16
    L = SEQ
    t = np.arange(L, dtype=np.float32)
    tn = t / np.float32(L - 1)
    bands = np.linspace(1e-4, 15, 16, dtype=np.float32)
    ang = (np.float32(2.0 * math.pi) * t[:, None] * bands[None] / np.float32(L)).astype(np.float32)
    feats = np.concatenate([tn[:, None], np.cos(ang), np.sin(ang)], axis=-1).astype(np.float32)
    _HC["featsT"] = np.ascontiguousarray(feats.T)
    deltas = np.abs(np.linspace(math.log(1e-2) / 1.5, math.log(1e-2) / 0.3, 512, dtype=np.float32))
    decay = np.exp(-tn[:, None] * deltas[None]).astype(np.float32)
    _HC["decay"] = np.ascontiguousarray(decay.reshape(32, 128, 512).transpose(1, 0, 2))
    k = np.arange(NFFT, dtype=np.float64)
    ctab = np.cos(2 * np.pi * k / NFFT).astype(np.float32)
    stab = np.sin(2 * np.pi * k / NFFT).astype(np.float32)
    a = np.arange(L, dtype=np.int64)
    idx = (a[:, None] * a[None, :]) % NFFT
    Fc = ctab[idx]
    Fs = stab[idx]
    sgn = np.where(a % 2 == 0, 1.0, -1.0).astype(np.float32)
    Fs_f = Fs.copy()
    Fs_f[:, 0] = sgn

    def blk(Mx):
        return np.ascontiguousarray(Mx.reshape(32, 128, 32, 128).transpose(2, 1, 0, 3)).astype(bf)
    _HC["FcB"] = blk(Fc)
    _HC["FsB"] = blk(Fs_f)
    _HC["FsBi"] = blk(np.ascontiguousarray(Fs_f.T))
    cv = np.ones((128, 4), np.float32)
    cv[0, 0] = 0.5
    cv[0, 1] = 0.0
    cv[:, 2] = 0.0
    cv[0, 2] = 0.5
    _HC["cv"] = cv
    return _HC


PI = math.pi


def sin_rr(C, out_ap, x, xkey, out_key, ti_, tf_, tc_, tag):
    S = C.S
    ki, kf, kc = f"rri{tag}", f"rrf{tag}", f"rrc{tag}"
    S.op("dve", lambda e: e.tensor_scalar(out=ti_, in0=x, scalar1=1.0 / (2.0 * PI), scalar2=None, op0=ALU.mult), reads=[xkey], writes=[ki])
    S.op("dve", lambda e: e.tensor_copy(out=tf_, in_=ti_), reads=[ki], writes=[kf])
    S.op("dve", lambda e: e.scalar_tensor_tensor(out=x, in0=tf_, scalar=-2.0 * PI, in1=x, op0=ALU.mult, op1=ALU.add), reads=[kf, xkey], writes=[xkey])
    S.op("dve", lambda e: e.tensor_single_scalar(out=tc_, in_=x, scalar=PI, op=ALU.is_gt), reads=[xkey], writes=[kc])
    S.op("dve", lambda e: e.scalar_tensor_tensor(out=x, in0=tc_, scalar=-2.0 * PI, in1=x, op0=ALU.mult, op1=ALU.add), reads=[kc, xkey], writes=[xkey])
    S.op("dve", lambda e: e.tensor_single_scalar(out=tc_, in_=x, scalar=-PI, op=ALU.is_lt), reads=[xkey], writes=[kc])
    S.op("dve", lambda e: e.scalar_tensor_tensor(out=x, in0=tc_, scalar=2.0 * PI, in1=x, op0=ALU.mult, op1=ALU.add), reads=[kc, xkey], writes=[xkey])
    S.op("act", lambda e: e.activation(out=out_ap, in_=x, func=AF.Sin), reads=[xkey], writes=[out_key])


def build_l1k():
    nc = bass.Bass("TRN2", target_bir_lowering=False)
    C = Ctx(nc)
    l1k_body(nc, C, IO(nc))
    C.S.finish()
    return nc


def l1k_body(nc, C, io, NCH=64):
    W4, W2 = 4 * NCH, 2 * NCH
    featsT = io.inp("featsT", [33, SEQ])
    w1 = io.inp("w1", [33, 64]); w2 = io.inp("w2", [64, 64]); w3s = io.inp("w3s", [64, W4])
    pvec = io.inp("pvec", [64, 4])
    b3s = io.inp("b3s", [W4])
    decay = io.inp("decay", [128, 32, NCH])
    FcB = io.inp("FcB", [32, 128, 32, 128], BF16)
    FsB = io.inp("FsB", [32, 128, 32, 128], BF16)
    cv_d = io.inp("cv", [128, 4])
    ktab = io.out("ktab", [32, 128, 3, W2])
    S = C.S
    st = contextlib.ExitStack()
    st2 = contextlib.ExitStack()
    ksum = S.sbuf("ksum", [128, 32, W2], BF16, st); kdif = S.sbuf("kdif", [128, 32, W2], BF16, st)
    cv = S.sbuf("cvs", [128, 4], F32, st)
    C.negpi = S.sbuf("negpi", [128, 1], F32, st2)
    S.op("dve", lambda e: e.memset(C.negpi[:], -PI), writes=["negpi"])
    ft = S.sbuf("ft", [33, SEQ], F32, st2)
    w1t = S.sbuf("w1t", [33, 64], F32, st2); w2t = S.sbuf("w2t", [64, 64], F32, st2); w3t = S.sbuf("w3t", [64, W4], F32, st2)
    pv = S.sbuf("pv", [64, 4], F32, st2); b3bc = S.sbuf("b3bc", [128, W4], F32, st2)
    dec = S.sbuf("dec", [128, 32, NCH], F32, st2)
    h1 = S.sbuf("h1", [64, SEQ], F32, st2); h2 = S.sbuf("h2", [64, SEQ], F32, st2)
    hraw = S.sbuf("hraw", [128, 32, W4], F32, st2)
    for (dst, src, key) in ((ft, featsT, "ft"), (w1t, w1, "w1t"), (w2t, w2, "w2t"), (w3t, w3s, "w3t"), (pv, pvec, "pv"),
                            (dec, decay, "dec"), (cv, cv_d, "cv")):
        S.dma("sp", lambda e, dst=dst, src=src: e.dma_start(out=dst[:], in_=src), writes=[key])
    S.dma("sp", lambda e: e.dma_start(out=b3bc[:], in_=b3s.partition_broadcast(128)), writes=["b3bc"])
    pre = [S.sbuf(f"pre{i}", [64, 512], F32, st2) for i in range(2)]
    rr_i = [S.sbuf(f"rr_i{i}", [64, 512], mybir.dt.int32, st2) for i in range(2)]
    rr_f = [S.sbuf(f"rr_f{i}", [64, 512], F32, st2) for i in range(2)]
    rr_c = [S.sbuf(f"rr_c{i}", [64, 512], F32, st2) for i in range(2)]
    for layer, (wt, wk, src, skey, dst, dkey, K_) in enumerate(((w1t, "w1t", ft, "ft", h1, "h1", 33), (w2t, "w2t", h1, "h1", h2, "h2", 64))):
        for tt in range(8):
            bk = tt % 2
            S.op("pe", lambda e, tt=tt, bk=bk: e.matmul(C.banks[bk][0:64, :], wt[0:K_, :], src[0:K_, tt * 512:(tt + 1) * 512], start=True, stop=True),
                 reads=[wk, skey], writes=[f"bank{bk}"])
            S.op("dve", lambda e, bk=bk: e.tensor_scalar(out=pre[bk][:], in0=C.banks[bk][0:64, :], scalar1=pv[:, 2 * layer:2 * layer + 1],
                                                        scalar2=pv[:, 2 * layer + 1:2 * layer + 2], op0=ALU.add, op1=ALU.mult),
                 reads=[f"bank{bk}", "pv"], writes=[f"pre{bk}"])
            sin_rr(C, dst[:, tt * 512:(tt + 1) * 512], pre[bk][:], f"pre{bk}", dkey, rr_i[bk][:], rr_f[bk][:], rr_c[bk][:], bk)
    ab = [S.sbuf(f"habs{i}", [128, W4], F32, st2) for i in range(2)]
    for ti in range(32):
        bk = ti % 2
        S.op("pe", lambda e, ti=ti, bk=bk: e.matmul(C.banks[bk][:, 0:W4], h2[0:64, ti * 128:(ti + 1) * 128], w3t[0:64, :], start=True, stop=True),
             reads=["h2", "w3t"], writes=[f"bank{bk}"])
        S.op("dve", lambda e, ti=ti, bk=bk: e.tensor_tensor(out=hraw[:, ti, :], in0=C.banks[bk][:, 0:W4], in1=b3bc[:], op=ALU.add),
             reads=[f"bank{bk}", "b3bc"], writes=["hraw"])
        S.op("dve", lambda e, ti=ti: e.tensor_tensor(out=hraw[:, ti, :].rearrange("p (a c) -> p a c", a=4), in0=hraw[:, ti, :].rearrange("p (a c) -> p a c", a=4),
                                                  in1=dec[:, ti, :].unsqueeze(1).to_broadcast([128, 4, NCH]), op=ALU.mult),
             reads=["hraw", "dec"], writes=["hraw"])
        S.op("act", lambda e, ti=ti, bk=bk: e.activation(out=ab[bk][:], in_=hraw[:, ti, :], func=AF.Abs),
             reads=["hraw"], writes=[f"habs{bk}"])
        S.op("pe", lambda e, ti=ti, bk=bk: e.matmul(C.banks[2][:, 0:W4], C.ones_f[:], ab[bk][:], start=(ti == 0), stop=(ti == 31)),
             reads=[f"habs{bk}", "ones_f"], writes=["bank2"])
    scl = S.sbuf("scl", [128, W2], F32, st2)
    S.op("dve", lambda e: e.tensor_copy(out=scl[:], in_=C.banks[2][:, 0:W2]), reads=["bank2"], writes=["scl"])
    S.op("dve", lambda e: e.tensor_tensor(out=scl[:], in0=scl[:], in1=C.banks[2][:, W2:W4], op=ALU.add), reads=["bank2", "scl"], writes=["scl"])
    S.op("dve", lambda e: e.tensor_scalar(out=scl[:], in0=scl[:], scalar1=EPS, scalar2=None, op0=ALU.add), reads=["scl"], writes=["scl"])
    S.op("dve", lambda e: e.reciprocal(out=scl[:], in_=scl[:]), reads=["scl"], writes=["scl"])
    hn = S.sbuf("hn", [128, W4], F32, st2)
    for ti in range(32):
        S.op("dve", lambda e, ti=ti: e.tensor_tensor(out=hn[:].rearrange("p (a c) -> p a c", a=2), in0=hraw[:, ti, :].rearrange("p (a c) -> p a c", a=2),
                                                  in1=scl[:].unsqueeze(1).to_broadcast([128, 2, W2]), op=ALU.mult),
             reads=["hraw", "scl"], writes=["hn"])
        if ti == 0:
            S.op("dve", lambda e: e.tensor_scalar(out=hn[:, W2:W4], in0=hn[:, W2:W4], scalar1=cv[:, 1:2], scalar2=None, op0=ALU.mult),
                 reads=["hn", "cv"], writes=["hn"])
        S.op("dve", lambda e, ti=ti: e.tensor_tensor(out=ksum[:, ti, :], in0=hn[:, 0:W2], in1=hn[:, W2:W4], op=ALU.add), reads=["hn"], writes=["ksum"])
        S.op("dve", lambda e, ti=ti: e.tensor_tensor(out=kdif[:, ti, :], in0=hn[:, 0:W2], in1=hn[:, W2:W4], op=ALU.subtract), reads=["hn"], writes=["kdif"])
    S.barrier()
    st2.close()
    fcs = [S.sbuf(f"fcb{i}", [128, 32, 128], BF16, st) for i in range(2)]
    fss = [S.sbuf(f"fsb{i}", [128, 32, 128], BF16, st) for i in range(2)]
    kt = [S.sbuf(f"kt{i}", [128, 3, W2], F32, st) for i in range(2)]
    for j in range(32):
        bi = j % 2
        S.dma("sp", lambda e, j=j, bi=bi: e.dma_start(out=fcs[bi][:], in_=FcB[j]), writes=[f"fcb{bi}"])
        S.dma("sp", lambda e, j=j, bi=bi: e.dma_start(out=fss[bi][:], in_=FsB[j]), writes=[f"fsb{bi}"])
        pb = 3 + 2 * bi
        for ti in range(32):
            S.op("pe", lambda e, ti=ti, bi=bi, pb=pb: e.matmul(C.banks[pb][:, 0:W2], fcs[bi][:, ti, :], ksum[:, ti, :], start=(ti == 0), stop=(ti == 31)),
                 reads=[f"fcb{bi}", "ksum"], writes=[f"bank{pb}"])
        for ti in range(32):
            S.op("pe", lambda e, ti=ti, bi=bi, pb=pb: e.matmul(C.banks[pb + 1][:, 0:W2], fss[bi][:, ti, :], kdif[:, ti, :], start=(ti == 0), stop=(ti == 31)),
                 reads=[f"fsb{bi}", "kdif"], writes=[f"bank{pb + 1}"])
        if j == 0:
            for ti in range(32):
                S.op("pe", lambda e, ti=ti, bi=bi: e.matmul(C.banks[7][:, 0:W2], fss[bi][:, ti, :], ksum[:, ti, :], start=(ti == 0), stop=(ti == 31)),
                     reads=[f"fsb{bi}", "ksum"], writes=["bank7"])
            S.op("dve", lambda e, bi=bi, pb=pb: e.tensor_scalar(out=kt[bi][:, 0, :], in0=C.banks[pb][:, 0:W2], scalar1=cv[:, 0:1], scalar2=None, op0=ALU.mult),
                 reads=[f"bank{pb}", "cv"], writes=[f"kt{bi}"])
            S.op("dve", lambda e, bi=bi, pb=pb: e.tensor_scalar(out=kt[bi][:, 1, :], in0=C.banks[pb + 1][:, 0:W2], scalar1=cv[:, 1:2], scalar2=None, op0=ALU.mult),
                 reads=[f"bank{pb + 1}", "cv"], writes=[f"kt{bi}"])
            S.op("dve", lambda e, bi=bi, pb=pb: e.tensor_scalar(out=kt[bi][:, 2, :], in0=C.banks[pb][:, 0:W2], scalar1=cv[:, 1:2], scalar2=None, op0=ALU.mult),
                 reads=[f"bank{pb}", "cv"], writes=[f"kt{bi}"])
            S.op("dve", lambda e, bi=bi: e.scalar_tensor_tensor(out=kt[bi][:, 2, :], in0=C.banks[7][:, 0:W2], scalar=cv[:, 2:3], in1=kt[bi][:, 2, :],
                                                               op0=ALU.mult, op1=ALU.add), reads=["bank7", "cv", f"kt{bi}"], writes=[f"kt{bi}"])
        else:
            S.op("act", lambda e, bi=bi, pb=pb: e.activation(out=kt[bi][:, 0, :], in_=C.banks[pb][:, 0:W2], func=AF.Copy), reads=[f"bank{pb}"], writes=[f"kt{bi}"])
            S.op("dve", lambda e, bi=bi, pb=pb: e.tensor_copy(out=kt[bi][:, 1, :], in_=C.banks[pb + 1][:, 0:W2]), reads=[f"bank{pb + 1}"], writes=[f"kt{bi}"])
            S.op("act", lambda e, bi=bi, pb=pb: e.activation(out=kt[bi][:, 2, :], in_=C.banks[pb][:, 0:W2], func=AF.Copy), reads=[f"bank{pb}"], writes=[f"kt{bi}"])
        S.dma("sp", lambda e, j=j, bi=bi: e.dma_start(out=ktab[j], in_=kt[bi][:]), reads=[f"kt{bi}"], writes=["ktblk"], is_out=True)
    S.barrier()
    st.close()


def l1k_inputs(inp, core):
    hc = hy_consts()
    ch = 64 * core + np.arange(64)
    cols = np.concatenate([d * 1024 + o * 512 + ch for d in range(2) for o in range(2)])
    pvec = np.stack([inp["hy_b1"][0], inp["hy_f1"][0], inp["hy_b2"][0], inp["hy_f2"][0]], axis=1).astype(np.float32)
    return {"featsT": hc["featsT"], "w1": np.ascontiguousarray(inp["hy_w1"][0]), "w2": np.ascontiguousarray(inp["hy_w2"][0]),
            "w3s": np.ascontiguousarray(inp["hy_w3"][0][:, cols]), "pvec": np.ascontiguousarray(pvec),
            "b3s": np.ascontiguousarray(inp["hy_b3"][0][cols]), "decay": np.ascontiguousarray(hc["decay"][:, :, ch]),
            "FcB": hc["FcB"], "FsB": hc["FsB"], "cv": hc["cv"]}


NT1 = CTX + SEQ


def l1_w_in_cols(half):
    P = rope_perm()
    q = np.arange(0, 512)
    qP = (q.reshape(8, 64)[:, P]).reshape(-1)
    k0 = 512 + np.arange(64)
    k1 = 576 + np.arange(64)
    v0 = 640 + np.arange(64)
    v1 = 704 + np.arange(64)
    c0 = half * 256
    hy = np.concatenate([768 + o * 512 + c0 + np.arange(256) for o in range(3)])
    return np.concatenate([q, qP, np.concatenate([k0, k0]), np.concatenate([k0[P], k0[P]]),
                           np.concatenate([k1, k1]), np.concatenate([k1[P], k1[P]]),
                           np.concatenate([v0, v0, v1, v1]), hy])


def build_l1a(stop=None):
    nc = bass.Bass("TRN2", target_bir_lowering=False)
    C = Ctx(nc)
    l1a_body(nc, C, IO(nc))
    C.S.finish()
    return nc


def l1a_body(nc, C, io, hy_halves=(None,)):
    xT = io.inp("xT", [8, 128, NT1])
    xTo = io.inp("xTo", [8, 128, OWN])
    ropeCq = io.inp("ropeCq", [128, OWN])
    ropeSq = io.inp("ropeSq", [128, OWN])
    modt = io.inp("mod", [128, 2, 6, 8])
    ng = io.inp("ng", [128, 2, 8])
    w_in = io.inp("w_in", [D, 2560])
    ropeC = io.inp("ropeC", [128, SEQ])
    ropeS = io.inp("ropeS", [128, SEQ])
    gvec_d = io.inp("gvec", [128, 4])
    bones_d = io.inp("bones", [128, 128])
    ident_d = io.inp("ident", [128, 128])
    if hy_halves == (None,):
        swT_d = io.inp("swT", [128, 6, 4])
        hb_d = io.inp("hbias", [2, 256])
        Kt = io.inp("Kt", [32, 128, 2, 3, 256])
    else:
        swT2_d = io.inp("swT2", [128, 2, 6, 4])
        hb2_d = io.inp("hbias2", [2, 2, 256])
        kt_blk = io.inp("kt_blk", None)
        Kt2 = [kt_blk[0], kt_blk[1]]
        w_hy = io.inp("w_hy", [D, 2, 768])
        w_hy_v = w_hy.rearrange("(k p) c o -> p k c o", p=128)
    FcB = io.inp("FcB", [32, 128, 32, 128], BF16)
    FsB = io.inp("FsB", [32, 128, 32, 128], BF16)
    FsBi = io.inp("FsBi", [32, 128, 32, 128], BF16)
    yatt_o = io.out("yatt", [4, 128, OWN], BF16)
    yhy_o = io.out("yhy", [128, 32, 256], BF16) if hy_halves == (None,) else io.out("yhy2", [2, 128, 32, 256], BF16)
    S = C.S
    st0 = contextlib.ExitStack()
    M = load_mod(C, modt, ng, st0)
    xv = xT.rearrange("k p t -> p k t")
    gvec = S.sbuf("gvec", [128, 4], F32, st0)
    bones = S.sbuf("bones", [128, 128], F32, st0)
    ident = S.sbuf("ident1", [128, 128], F32, st0)
    swT = S.sbuf("swT", [128, 6, 4], F32, st0)
    hbb = S.sbuf("hbb", [128, 2, 256], F32, st0)
    for dst, src, key in ((gvec, gvec_d, "gvec"), (bones, bones_d, "bones"), (ident, ident_d, "ident")):
        S.dma("sp", lambda e, dst=dst, src=src: e.dma_start(out=dst[:], in_=src), writes=[key])
    wv = w_in.rearrange("(k p) o -> p k o", p=128)
    def make_qk_block(src, srckey, rc, rs_, sq, rstd, ta, tb_):
        def qk_block(wa, wak, ja, wp, wpk, jp, gi, toks, out_fn, okey, rope):
            for (a, b, da, la) in toks:
                n = b - a
                for k in range(8):
                    S.op("pe", lambda e, k=k: e.matmul(C.banks[0][:, :n], wa[:, k, ja * 128:(ja + 1) * 128], src[:, k, a:b],
                                                       start=(k == 0), stop=(k == 7)), reads=[wak, srckey], writes=["bank0"])
                if rope:
                    for k in range(8):
                        S.op("pe", lambda e, k=k: e.matmul(C.banks[1][:, :n], wp[:, k, jp * 128:(jp + 1) * 128], src[:, k, a:b],
                                                           start=(k == 0), stop=(k == 7)), reads=[wpk, srckey], writes=["bank1"])
                S.op("act", lambda e: e.activation(out=sq[:, :n], in_=C.banks[0][:, :n], func=AF.Square), reads=["bank0"], writes=["sq1"])
                S.op("pe", lambda e: e.matmul(C.banks[2][:, :n], bones[:], sq[:, :n], start=True, stop=True), reads=["bones", "sq1"], writes=["bank2"])
                S.op("act", lambda e: e.activation(out=rstd[:, :n], in_=C.banks[2][:, :n], func=AF.Sqrt, bias=EPS, scale=1.0 / HD),
                     reads=["bank2"], writes=["rstd1"])
                S.op("dve", lambda e: e.reciprocal(out=rstd[:, :n], in_=rstd[:, :n]), reads=["rstd1"], writes=["rstd1"])
                if rope:
                    S.op("dve", lambda e: e.scalar_tensor_tensor(out=ta[:, :n], in0=C.banks[0][:, :n], scalar=gvec[:, gi:gi + 1], in1=rc[:, la:la + n],
                                                                 op0=ALU.mult, op1=ALU.mult), reads=["bank0", "gvec", "rc"], writes=["ta1"])
                    S.op("dve", lambda e: e.scalar_tensor_tensor(out=tb_[:, :n], in0=C.banks[1][:, :n], scalar=gvec[:, gi + 1:gi + 2], in1=rs_[:, la:la + n],
                                                                 op0=ALU.mult, op1=ALU.mult), reads=["bank1", "gvec", "rs"], writes=["tb1"])
                    S.op("pool", lambda e: e.tensor_tensor(out=ta[:, :n], in0=ta[:, :n], in1=tb_[:, :n], op=ALU.add), reads=["ta1", "tb1"], writes=["ta1"])
                    S.op("pool", lambda e: e.tensor_tensor(out=out_fn(da, da + n), in0=ta[:, :n], in1=rstd[:, :n], op=ALU.mult),
                         reads=["ta1", "rstd1"], writes=[okey])
                else:
                    S.op("dve", lambda e: e.scalar_tensor_tensor(out=out_fn(da, da + n), in0=C.banks[0][:, :n], scalar=gvec[:, gi:gi + 1], in1=rstd[:, :n],
                                                                 op0=ALU.mult, op1=ALU.mult), reads=["bank0", "gvec", "rstd1"], writes=[okey])
        return qk_block

    with contextlib.ExitStack() as stH:
        hx = S.sbuf("hx1", [128, 8, NT1], BF16, stH)
        with contextlib.ExitStack() as stA:
            xts = [S.sbuf(f"xa{i}", [128, 8, 512], F32, stA) for i in range(2)]

            def src_fn(a, b, ti):
                xt = xts[ti % 2]
                S.dma("sp", lambda e: e.dma_start(out=xt[:, :, :b - a], in_=xv[:, :, a:b]), writes=[f"xa{ti % 2}"])
                return xt[:, :, :b - a], f"xa{ti % 2}"
            norm_phase(C, M, 0, src_fn, None, NT1, CTX, hx, "hx", stA)
        S.barrier()
        for chalf in hy_halves:
            if chalf is None:
                hy_w = lambda grp: wv[:, :, 1792 + grp * 256:1792 + (grp + 1) * 256]
                swT_src, hb_src, Kt_c, yhy_c = swT_d, hb_d, Kt, yhy_o
            else:
                hy_w = lambda grp, chalf=chalf: w_hy_v[:, :, chalf, grp * 256:(grp + 1) * 256]
                swT_src, hb_src, Kt_c, yhy_c = swT2_d[:, chalf], hb2_d[chalf], Kt2[chalf], yhy_o[chalf]
            with contextlib.ExitStack() as stZ:
                zg = [S.sbuf(f"zg{i}", [128, 32, 256], BF16, stZ) for i in range(3)]
                S.dma("sp", lambda e: e.dma_start(out=swT[:], in_=swT_src), writes=["swT"])
                for o in range(2):
                    S.dma("sp", lambda e, o=o: e.dma_start(out=hbb[:, o, :], in_=hb_src[o].partition_broadcast(128)), writes=["hbb"])
                with contextlib.ExitStack() as stU:
                    wu_ = [S.sbuf(f"wu{i}", [128, 8, 256], BF16, stU) for i in range(2)]
                    U = S.sbuf("U", [128, SEQ + 2], F32, stU)
                    cvt = [S.sbuf(f"cv{i}", [128, 512], F32, stU) for i in range(2)]
                    S.op("dve", lambda e: e.memset(U[:, 0:1], 0.0), writes=["Upad0"])
                    S.op("dve", lambda e: e.memset(U[:, SEQ + 1:SEQ + 2], 0.0), writes=["Upad1"])
                    tcount = [0]
                    for grp in range(3):
                        wb = wu_[grp % 2]
                        S.dma("pool", lambda e, grp=grp, wb=wb: e.dma_start(out=wb[:], in_=hy_w(grp)), writes=[f"wu{grp % 2}"])
                        for cc in range(2):
                            c = grp * 2 + cc
                            for tt in range(8):
                                bk = tt % 2
                                for k in range(8):
                                    S.op("pe", lambda e, k=k, tt=tt, bk=bk, cc=cc, wb=wb: e.matmul(
                                        C.banks[bk][:, :], wb[:, k, cc * 128:(cc + 1) * 128], hx[:, k, CTX + tt * 512:CTX + (tt + 1) * 512],
                                        start=(k == 0), stop=(k == 7)), reads=[f"wu{grp % 2}", "hx"], writes=[f"bank{bk}"])
                                S.op("act", lambda e, tt=tt, bk=bk: e.activation(out=U[:, 1 + tt * 512:1 + (tt + 1) * 512], in_=C.banks[bk][:, :], func=AF.Copy),
                                     reads=[f"bank{bk}"], writes=["U"])
                            for tt in range(8):
                                cv_ = cvt[tt % 2]
                                ck = f"cv{tt % 2}"
                                a = tt * 512
                                S.op("dve", lambda e, a=a, c=c, cv_=cv_: e.tensor_scalar(out=cv_[:], in0=U[:, 1 + a:1 + a + 512], scalar1=swT[:, c, 1:2],
                                                                                       scalar2=swT[:, c, 3:4], op0=ALU.mult, op1=ALU.add),
                                     reads=["U", "swT", "Upad0", "Upad1"], writes=[ck])
                                S.op("dve", lambda e, a=a, c=c, cv_=cv_: e.scalar_tensor_tensor(out=cv_[:], in0=U[:, a:a + 512], scalar=swT[:, c, 0:1], in1=cv_[:],
                                                                                              op0=ALU.mult, op1=ALU.add), reads=["U", "swT", ck], writes=[ck])
                                S.op("dve", lambda e, a=a, c=c, cv_=cv_: e.scalar_tensor_tensor(out=cv_[:], in0=U[:, 2 + a:2 + a + 512], scalar=swT[:, c, 2:3], in1=cv_[:],
                                                                                              op0=ALU.mult, op1=ALU.add), reads=["U", "swT", ck], writes=[ck])
                                for s_ in range(4):
                                    tb = 2 + tcount[0] % 4
                                    tcount[0] += 1
                                    S.op("pe", lambda e, s_=s_, tb=tb, cv_=cv_: e.transpose(C.banks[tb][:, 0:128], cv_[:, s_ * 128:(s_ + 1) * 128], ident[:]),
                                         reads=[ck, "ident"], writes=[f"bank{tb}"])
                                    dst = zg[grp][:, tt * 4 + s_, cc * 128:(cc + 1) * 128]
                                    eng = "act" if s_ % 2 == 0 else "dve"
                                    if eng == "act":
                                        S.op("act", lambda e, tb=tb, dst=dst: e.activation(out=dst, in_=C.banks[tb][:, 0:128], func=AF.Copy), reads=[f"bank{tb}"], writes=[f"zg{grp}"])
                                    else:
                                        S.op("dve", lambda e, tb=tb, dst=dst: e.tensor_copy(out=dst, in_=C.banks[tb][:, 0:128]), reads=[f"bank{tb}"], writes=[f"zg{grp}"])
                S.barrier()
                S.barrier()
                with contextlib.ExitStack() as stF:
                    YW = S.sbuf("YW", [128, 2, 32, 256], BF16, stF)
                    fa = [S.sbuf(f"fa{i}", [128, 32, 128], BF16, stF) for i in range(2)]
                    fb = [S.sbuf(f"fb{i}", [128, 32, 128], BF16, stF) for i in range(2)]
                    kts = [S.sbuf(f"ktb{i}", [128, 3, 256], F32, stF) for i in range(2)]
                    tmp = [S.sbuf(f"hyt{i}", [128, 256], F32, stF) for i in range(4)]
                    z = zg[0]
                    for o in range(2):
                        gate = zg[1 + o]
                        for j in range(32):
                            bi = j % 2
                            S.dma("sp", lambda e, j=j, bi=bi: e.dma_start(out=fa[bi][:], in_=FcB[j]), writes=[f"fa{bi}"])
                            S.dma("sp", lambda e, j=j, bi=bi: e.dma_start(out=fb[bi][:], in_=FsB[j]), writes=[f"fb{bi}"])
                            if chalf is None:
                                S.dma("sp", lambda e, j=j, bi=bi, o=o: e.dma_start(out=kts[bi][:], in_=Kt_c[j, :, o]), writes=[f"ktb{bi}"])
                            else:
                                S.dma("sp", lambda e, j=j, bi=bi, o=o: e.dma_start(out=kts[bi][:], in_=Kt_c[j, :, :, o * 256:(o + 1) * 256]), reads=["ktblk"], writes=[f"ktb{bi}"])
                            zc, zs = 2 * bi, 2 * bi + 1
                            for ti in range(32):
                                S.op("pe", lambda e, ti=ti, bi=bi, zc=zc: e.matmul(C.banks[zc][:, 0:256], fa[bi][:, ti, :], z[:, ti, :], start=(ti == 0), stop=(ti == 31)),
                                     reads=[f"fa{bi}", "z"], writes=[f"bank{zc}"])
                            for ti in range(32):
                                S.op("pe", lambda e, ti=ti, bi=bi, zs=zs: e.matmul(C.banks[zs][:, 0:256], fb[bi][:, ti, :], z[:, ti, :], start=(ti == 0), stop=(ti == 31)),
                                     reads=[f"fb{bi}", "z"], writes=[f"bank{zs}"])
                            kt = kts[bi]
                            S.op("dve", lambda e, zc=zc, kt=kt: e.tensor_tensor(out=tmp[0][:], in0=C.banks[zc][:, 0:256], in1=kt[:, 0, :], op=ALU.mult),
                                 reads=[f"bank{zc}", f"ktb{bi}"], writes=["hyt0"])
                            S.op("dve", lambda e, zs=zs, kt=kt: e.tensor_tensor(out=tmp[1][:], in0=C.banks[zs][:, 0:256], in1=kt[:, 1, :], op=ALU.mult),
                                 reads=[f"bank{zs}", f"ktb{bi}"], writes=["hyt1"])
                            S.op("pool", lambda e, j=j: e.tensor_tensor(out=YW[:, 0, j, :], in0=tmp[0][:], in1=tmp[1][:], op=ALU.subtract),
                                 reads=["hyt0", "hyt1"], writes=["YW"])
                            S.op("dve", lambda e, zc=zc, kt=kt: e.tensor_tensor(out=tmp[2][:], in0=C.banks[zc][:, 0:256], in1=kt[:, 1, :], op=ALU.mult),
                                 reads=[f"bank{zc}", f"ktb{bi}"], writes=["hyt2"])
                            S.op("dve", lambda e, zs=zs, kt=kt: e.tensor_tensor(out=tmp[3][:], in0=C.banks[zs][:, 0:256], in1=kt[:, 2, :], op=ALU.mult),
                                 reads=[f"bank{zs}", f"ktb{bi}"], writes=["hyt3"])
                            S.op("pool", lambda e, j=j: e.tensor_tensor(out=YW[:, 1, j, :], in0=tmp[2][:], in1=tmp[3][:], op=ALU.add),
                                 reads=["hyt2", "hyt3"], writes=["YW"])
                        for ni in range(32):
                            bi = ni % 2
                            S.dma("sp", lambda e, ni=ni, bi=bi: e.dma_start(out=fa[bi][:], in_=FcB[ni]), writes=[f"fa{bi}"])
                            S.dma("sp", lambda e, ni=ni, bi=bi: e.dma_start(out=fb[bi][:], in_=FsBi[ni]), writes=[f"fb{bi}"])
                            yb_ = 4 + bi
                            for fj in range(32):
                                S.op("pe", lambda e, fj=fj, bi=bi, yb_=yb_: e.matmul(C.banks[yb_][:, 0:256], fa[bi][:, fj, :], YW[:, 0, fj, :], start=(fj == 0), stop=False),
                                     reads=[f"fa{bi}", "YW"], writes=[f"bank{yb_}"])
                            for fj in range(32):
                                S.op("pe", lambda e, fj=fj, bi=bi, yb_=yb_: e.matmul(C.banks[yb_][:, 0:256], fb[bi][:, fj, :], YW[:, 1, fj, :], start=False, stop=(fj == 31)),
                                     reads=[f"fb{bi}", "YW"], writes=[f"bank{yb_}"])
                            S.op("pool", lambda e, ni=ni, o=o: e.tensor_tensor(out=tmp[0][:], in0=z[:, ni, :], in1=hbb[:, o, :], op=ALU.mult), reads=["z", "hbb"], writes=["hyt0"])
                            S.op("dve", lambda e, yb_=yb_: e.scalar_tensor_tensor(out=tmp[1][:], in0=C.banks[yb_][:, 0:256], scalar=2.0 / NFFT, in1=tmp[0][:],
                                                                                 op0=ALU.mult, op1=ALU.add), reads=[f"bank{yb_}", "hyt0"], writes=["hyt1"])
                            S.op("pool", lambda e, ni=ni, gate=gate: e.tensor_tensor(out=z[:, ni, :], in0=gate[:, ni, :], in1=tmp[1][:], op=ALU.mult),
                                 reads=["hyt1", "zg"], writes=["z"])
                        S.barrier()
                    for q in range(4):
                        S.dma("sp", lambda e, q=q: e.dma_start(out=yhy_c[:, q * 8:(q + 1) * 8, :], in_=z[:, q * 8:(q + 1) * 8, :]), reads=["z"], is_out=True)
                S.barrier()
        with contextlib.ExitStack() as stQ:
            KAB = S.sbuf("KABc", [128, 2, NT1], BF16, stQ)
            V2 = S.sbuf("V2c", [128, 34, 256], BF16, stQ)
            with contextlib.ExitStack() as stB:
                wts = [S.sbuf(f"wt{i}", [128, 8, 256], BF16, stB) for i in range(2)]
                rc = S.sbuf("rc", [128, SEQ], BF16, stB)
                rs_ = S.sbuf("rs", [128, SEQ], BF16, stB)
                sq = S.sbuf("sq1", [128, 512], F32, stB)
                rstd = S.sbuf("rstd1", [128, 512], F32, stB)
                ta = S.sbuf("ta1", [128, 512], F32, stB)
                tb_ = S.sbuf("tb1", [128, 512], F32, stB)
                S.dma("pool", lambda e: e.dma_start(out=rc[:], in_=ropeC), writes=["rc"])
                S.dma("pool", lambda e: e.dma_start(out=rs_[:], in_=ropeS), writes=["rs"])

                qk_block = make_qk_block(hx, "hx", rc, rs_, sq, rstd, ta, tb_)

                def loadw(i, c0):
                    S.dma("pool", lambda e: e.dma_start(out=wts[i][:], in_=wv[:, :, c0:c0 + 256]), writes=[f"wt{i}"])
                    return wts[i], f"wt{i}"
                lat_toks = [(CTX + 512 * i, CTX + 512 * (i + 1), CTX + 512 * i, 512 * i) for i in range(8)]
                ctx_toks = [(0, CTX, 0, 0)]
                wk1, wk1k = loadw(0, 1024)
                wk2, wk2k = loadw(1, 1280)
                for g, (wk_, wkk) in enumerate(((wk1, wk1k), (wk2, wk2k))):
                    qk_block(wk_, wkk, 0, wk_, wkk, 1, 2, ctx_toks, lambda a, b, g=g: KAB[:, g, a:b], "KAB", False)
                    qk_block(wk_, wkk, 0, wk_, wkk, 1, 2, lat_toks, lambda a, b, g=g: KAB[:, g, a:b], "KAB", True)
                wv_, wvk = loadw(0, 1536)
                for t in range(34):
                    bk = 4 + t % 2
                    for k in range(8):
                        S.op("pe", lambda e, k=k, t=t, bk=bk: e.matmul(C.banks[bk][:, 0:256], hx[:, k, t * 128:(t + 1) * 128], wv_[:, k, :],
                                                                       start=(k == 0), stop=(k == 7)), reads=[wvk, "hx"], writes=[f"bank{bk}"])
                    S.op("act", lambda e, t=t, bk=bk: e.activation(out=V2[:, t, :], in_=C.banks[bk][:, 0:256], func=AF.Copy), reads=[f"bank{bk}"], writes=["V2"])
            S.barrier()
            Q = S.sbuf("Qc", [128, 4, OWN], BF16, stQ)
            xvo = xTo.rearrange("k p t -> p k t")
            with contextlib.ExitStack() as stO:
                hxo = S.sbuf("hxo", [128, 8, OWN], BF16, stO)
                with contextlib.ExitStack() as stA:
                    xts = [S.sbuf(f"xo{i}", [128, 8, 512], F32, stA) for i in range(1)]

                    def src_fno(a, b, ti):
                        xt = xts[0]
                        S.dma("sp", lambda e: e.dma_start(out=xt[:, :, :b - a], in_=xvo[:, :, a:b]), writes=["xo0"])
                        return xt[:, :, :b - a], "xo0"
                    norm_phase(C, M, 0, src_fno, None, OWN, 0, hxo, "hxo", stA)
                S.barrier()
                with contextlib.ExitStack() as stB:
                    wts = [S.sbuf(f"wq{i}", [128, 8, 256], BF16, stB) for i in range(2)]
                    rcq = S.sbuf("rcq", [128, OWN], BF16, stB)
                    rsq = S.sbuf("rsq", [128, OWN], BF16, stB)
                    sq = S.sbuf("sq1", [128, 512], F32, stB)
                    rstd = S.sbuf("rstd1", [128, 512], F32, stB)
                    ta = S.sbuf("ta1", [128, 512], F32, stB)
                    tb_ = S.sbuf("tb1", [128, 512], F32, stB)
                    S.dma("pool", lambda e: e.dma_start(out=rcq[:], in_=ropeCq), writes=["rc"])
                    S.dma("pool", lambda e: e.dma_start(out=rsq[:], in_=ropeSq), writes=["rs"])
                    qkb = make_qk_block(hxo, "hxo", rcq, rsq, sq, rstd, ta, tb_)
                    own_toks = [(512 * i, 512 * (i + 1), 512 * i, 512 * i) for i in range(4)]
                    for jj in range(2):
                        S.dma("pool", lambda e, jj=jj: e.dma_start(out=wts[0][:], in_=wv[:, :, jj * 256:(jj + 1) * 256]), writes=["wq0"])
                        S.dma("pool", lambda e, jj=jj: e.dma_start(out=wts[1][:], in_=wv[:, :, 512 + jj * 256:512 + (jj + 1) * 256]), writes=["wq1"])
                        for j2 in range(2):
                            j = jj * 2 + j2
                            qkb(wts[0], "wq0", j2, wts[1], "wq1", j2, 0, own_toks, lambda a, b, j=j: Q[:, j, a:b], "Q", True)
                S.barrier()
            yat = S.sbuf("yat", [128, 4, OWN], BF16, stQ)
            with contextlib.ExitStack() as stC:
                pts = [S.sbuf(f"ptc{i}", [128, 512], BF16, stC) for i in range(2)]
                dn = S.sbuf("dnc", [128, 512], F32, stC)
                it = 0
                for qg in range(4):
                    for h in range(8):
                        g = h // 4
                        c, off = h // 2, (h % 2) * 64
                        nb, db = 2 + it % 2, 4 + it % 2
                        it += 1
                        def emit_S(tk):
                            sb = tk % 2
                            S.op("pe", lambda e, tk=tk, sb=sb: e.matmul(C.banks[sb][:, :], KAB[off:off + 64, g, tk * 128:(tk + 1) * 128],
                                                                        Q[off:off + 64, c, qg * 512:(qg + 1) * 512], start=True, stop=True),
                                 reads=["KAB", "Q"], writes=[f"bank{sb}"])
                        emit_S(0)
                        for tk in range(34):
                            sb = tk % 2
                            pt = pts[sb]
                            if tk + 1 < 34:
                                emit_S(tk + 1)
                            S.op("act", lambda e, sb=sb, pt=pt: e.activation(out=pt[:], in_=C.banks[sb][:, :], func=AF.Exp, scale=SCALE),
                                 reads=[f"bank{sb}"], writes=[f"ptc{sb}"])
                            S.op("pe", lambda e, tk=tk, pt=pt: e.matmul(C.banks[nb][:, :], V2[:, tk, g * 128:(g + 1) * 128], pt[:], start=(tk == 0), stop=(tk == 33)),
                                 reads=["V2", f"ptc{sb}"], writes=[f"bank{nb}"])
                            S.op("pe", lambda e, tk=tk, pt=pt: e.matmul(C.banks[db][:, :], C.ones_b[:], pt[:], start=(tk == 0), stop=(tk == 33)),
                                 reads=["ones_b", f"ptc{sb}"], writes=[f"bank{db}"])
                        S.op("dve", lambda e: e.reciprocal(out=dn[:], in_=C.banks[db][:, :]), reads=[f"bank{db}"], writes=["dnc"])
                        S.op("dve", lambda e: e.tensor_tensor(out=yat[off:off + 64, c, qg * 512:(qg + 1) * 512], in0=C.banks[nb][off:off + 64, :],
                                                              in1=dn[off:off + 64, :], op=ALU.mult), reads=[f"bank{nb}", "dnc"], writes=["yat"])
                for c in range(4):
                    S.dma("sp", lambda e, c=c: e.dma_start(out=yatt_o[c], in_=yat[:, c, :]), reads=["yat"], is_out=True)
            S.barrier()
    S.barrier()
    st0.close()

def l1a_inputs(inp, mod, x1_full, Ktabs, core):
    hc = hy_consts()
    b, half = divmod(core, 2)
    order = np.arange(SEQ)
    xt = x1_full[b]
    xT = np.ascontiguousarray(xt)
    xTo = np.ascontiguousarray(xt[:, :, CTX + half * OWN:CTX + (half + 1) * OWN])
    rc, rs = rope_np(order)
    rcq, rsq = rope_np(half * OWN + np.arange(OWN))
    modt = np.stack([fm(mod[1, b].reshape(6, D)), fm(mod[1, 4].reshape(6, D))], axis=1)
    P = rope_perm()
    qn, kn = inp["c_qnorm"][0], inp["c_knorm"][0]
    gvec = np.stack([np.tile(qn, 2), np.tile(qn[P], 2), np.tile(kn, 2), np.tile(kn[P], 2)], axis=1).astype(np.float32)
    bones = np.zeros((128, 128), np.float32)
    bones[:64, :64] = 1.0
    bones[64:, 64:] = 1.0
    c0 = half * 256
    chs = np.concatenate([o * 512 + c0 + np.arange(256) for o in range(3)])
    sw = inp["hy_short_w"][0][:, chs]
    sb = inp["hy_short_b"][0][chs]
    swT = np.concatenate([sw, sb[None]], 0).T.reshape(6, 128, 4).transpose(1, 0, 2)
    return {"xT": xT, "xTo": xTo, "ropeCq": rcq, "ropeSq": rsq, "mod": np.ascontiguousarray(modt), "ng": fm(inp["norm_g"][1]),
            "w_in": np.ascontiguousarray(inp["w_in_odd"][0][:, l1_w_in_cols(half)]),
            "ropeC": rc, "ropeS": rs, "gvec": np.ascontiguousarray(gvec), "bones": bones, "ident": np.eye(128, dtype=np.float32),
            "swT": np.ascontiguousarray(swT, dtype=np.float32), "hbias": np.ascontiguousarray(inp["hy_bias"][0][:, c0:c0 + 256]),
            "Kt": Ktabs[half], "FcB": hc["FcB"], "FsB": hc["FsB"], "FsBi": hc["FsBi"]}, order


def assemble_ktabs(kouts):
    res = []
    for half in range(2):
        t = np.zeros((32, 128, 2, 3, 256), np.float32)
        for q in range(4):
            k = kouts[half * 4 + q].reshape(32, 128, 3, 2, 64)
            t[:, :, :, :, q * 64:(q + 1) * 64] = k.transpose(0, 1, 3, 2, 4)
        res.append(t)
    return res


def assemble_x1(l0_out):
    res = []
    for b in range(NB):
        a, c = l0_out[2 * b], l0_out[2 * b + 1]
        res.append(np.ascontiguousarray(np.concatenate([a[:, :, :CTX], a[:, :, CTX:], c[:, :, CTX:]], axis=2)))
    return res


L1B_TILES = [(512 * i, 512 * (i + 1)) for i in range(4)]


def build_l1b():
    nc = bass.Bass("TRN2", target_bir_lowering=False)
    C = Ctx(nc)
    l1b_body(nc, C, IO(nc))
    C.S.finish()
    return nc


def l1b_body(nc, C, io, fill_y=None):
    yT = io.inp("yT", [8, 128, OWN], BF16) if fill_y is None else None
    xTo = io.inp("xTo", [8, 128, OWN])
    modt = io.inp("mod", [128, 2, 6, 8])
    ng = io.inp("ng", [128, 2, 8])
    w_out = io.inp("w_out", [D, D])
    w_r = io.inp("w_r", [D, 16])
    b_r = io.inp("b_r", [16])
    wg = io.inp("wg", [NE, D, DE])
    wu = io.inp("wu", [NE, D, DE])
    wd = io.inp("wd", [NE, DE, D])
    ident_d = io.inp("ident", [128, 128])
    selE_d = io.inp("selE", [16, 16, 128])
    fg_d = io.inp("fg", [128, 8])
    outT = io.out("outT", [8, 128, OWN])
    S = C.S
    st0 = contextlib.ExitStack()
    M = load_mod(C, modt, ng, st0)
    ybuf = S.sbuf("ybuf1", [128, 8, OWN], BF16, st0)
    if fill_y is None:
        for k in range(8):
            S.dma("sp", lambda e, k=k: e.dma_start(out=ybuf[:, k, :], in_=yT[k]), writes=["Y"])
    else:
        fill_y(ybuf)
    xv = xTo.rearrange("k p t -> p k t")
    tail_phase(C, M, ybuf, w_out, lambda a, b: xv[:, :, a:b], outT, w_r, b_r, ident_d, selE_d, wg, wu, wd, st0,
               final_g_d=fg_d, nres=OWN, tiles=L1B_TILES, ctx_len=0)
    S.barrier()
    st0.close()


def l1b_inputs(inp, mod, x1_full, yatt, yhy, core):
    import ml_dtypes
    b, half = divmod(core, 2)
    yh = np.concatenate([np.asarray(yhy[2 * b + h]).transpose(1, 0, 2).reshape(SEQ, 256) for h in range(2)], axis=1)
    yh_own = yh[half * OWN:(half + 1) * OWN]
    yhT = np.ascontiguousarray(yh_own.T.reshape(4, 128, OWN))
    yT = np.ascontiguousarray(np.concatenate([np.asarray(yatt[core]), yhT], axis=0)).astype(ml_dtypes.bfloat16)
    xt = x1_full[b]
    modt = np.stack([fm(mod[1, b].reshape(6, D)), fm(mod[1, 4].reshape(6, D))], axis=1)
    selE = np.zeros((16, 16, 128), np.float32)
    for e in range(16):
        selE[e, e, :] = 1.0
    return {"yT": yT, "xTo": np.ascontiguousarray(xt[:, :, CTX + half * OWN:CTX + (half + 1) * OWN]),
            "mod": np.ascontiguousarray(modt), "ng": fm(inp["norm_g"][1]),
            "w_out": np.ascontiguousarray(inp["w_out_odd"][0]),
            "w_r": np.ascontiguousarray(inp["w_router"]), "b_r": np.ascontiguousarray(inp["b_router"]),
            "wg": np.ascontiguousarray(inp["moe_wg"][1]), "wu": np.ascontiguousarray(inp["moe_wu"][1]),
            "wd": np.ascontiguousarray(inp["moe_wd"][1]),
            "ident": np.eye(128, dtype=np.float32), "selE": selE, "fg": fm(inp["final_g"])}


def kernel_unfused(**inp):
    inp = {k: np.asarray(v) for k, v in inp.items()}
    cores = list(range(NCORES))
    mod = run_mod(inp)
    r0 = run_bass_kernel_spmd(build_l0(), [l0_inputs(inp, mod, c) for c in cores], core_ids=cores)
    x1_full = assemble_x1([r0.results[c]["x1T"] for c in cores])
    rk = run_bass_kernel_spmd(build_l1k(), [l1k_inputs(inp, c) for c in cores], core_ids=cores)
    Kt = assemble_ktabs([rk.results[c]["ktab"] for c in cores])
    ra = run_bass_kernel_spmd(build_l1a(), [l1a_inputs(inp, mod, x1_full, Kt, c)[0] for c in cores], core_ids=cores)
    yatt = [ra.results[c]["yatt"] for c in cores]
    yhy = [ra.results[c]["yhy"] for c in cores]
    rb = run_bass_kernel_spmd(build_l1b(), [l1b_inputs(inp, mod, x1_full, yatt, yhy, c) for c in cores], core_ids=cores)
    out = np.zeros((NB, SEQ, D), np.float32)
    for c in cores:
        b, half = divmod(c, 2)
        o = rb.results[c]["outT"]
        out[b, half * OWN:(half + 1) * OWN] = o.transpose(2, 0, 1).reshape(OWN, D)
    return out


def mod_phase(nc, C, cT_d, w_ada_d, b_ada_fm_d):
    S = C.S
    st = S.stack
    mods = [S.sbuf(f"modL{l}", [128, 2, 6, 8], F32, st) for l in range(2)]
    with contextlib.ExitStack() as st2:
        ct = S.sbuf("m_ct", [128, 8, 2], F32, st2)
        sg = S.sbuf("m_sg", [128, 8, 2], F32, st2)
        bfm = S.sbuf("m_b", [128, 2, 6, 8], F32, st2)
        wts = [S.sbuf(f"m_w{i}", [128, 8, 512], F32, st2) for i in range(2)]
        S.dma("sp", lambda e: e.dma_start(out=ct[:], in_=cT_d), writes=["m_ct"])
        S.dma("sp", lambda e: e.dma_start(out=bfm[:], in_=b_ada_fm_d), writes=["m_b"])
        S.op("act", lambda e: e.activation(out=sg[:], in_=ct[:], func=AF.Silu), reads=["m_ct"], writes=["m_sg"])
        it = 0
        for l in range(2):
            wv = w_ada_d[l].rearrange("(k p) o -> p k o", p=128)
            for grp in range(12):
                wi = it % 2
                it += 1
                S.dma("sp", lambda e, grp=grp, wi=wi, wv=wv: e.dma_start(out=wts[wi][:], in_=wv[:, :, grp * 512:(grp + 1) * 512]), writes=[f"m_w{wi}"])
                for oc in range(4):
                    col = grp * 4 + oc
                    j, kk = divmod(col, 8)
                    bk = col % 4
                    for k in range(8):
                        S.op("pe", lambda e, k=k, oc=oc, wi=wi, bk=bk: e.matmul(C.banks[bk][:, 0:2], wts[wi][:, k, oc * 128:(oc + 1) * 128], sg[:, k, :],
                                                                               start=(k == 0), stop=(k == 7)), reads=[f"m_w{wi}", "m_sg"], writes=[f"bank{bk}"])
                    S.op("dve", lambda e, l=l, j=j, kk=kk, bk=bk: e.tensor_scalar(out=mods[l][:, :, j, kk], in0=C.banks[bk][:, 0:2], scalar1=bfm[:, l, j, kk:kk + 1],
                                                                                 scalar2=None, op0=ALU.add), reads=[f"bank{bk}", "m_b"], writes=[f"modL{l}"])
        S.barrier()
    return mods


def build_fused():
    nc = bass.Bass("TRN2", target_bir_lowering=False)
    C = Ctx(nc)
    S = C.S

    def scr(name, shape, dt=F32):
        return nc.dram_tensor(name, list(shape), dt, kind="Internal").ap()
    E = {}
    for name, shape, dt in (
            ("cT", [128, 8, 2], F32), ("w_ada", [2, D, 6 * D], F32), ("b_ada_fm", [128, 2, 6, 8], F32),
            ("ng0", [128, 2, 8], F32), ("ng1", [128, 2, 8], F32),
            ("w_in0", [D, 3328], F32), ("w_out0", [D, D], F32), ("sink", [8], F32), ("rpbT", [128, 7, 8, 128], F32),
            ("vint", [128, 5, 128], F32), ("w_r", [D, 16], F32), ("b_r", [16], F32),
            ("wg0", [NE, D, DE], F32), ("wu0", [NE, D, DE], F32), ("wd0", [NE, DE, D], F32),
            ("wg1", [NE, D, DE], F32), ("wu1", [NE, D, DE], F32), ("wd1", [NE, DE, D], F32),
            ("ident", [128, 128], F32), ("selE", [16, 16, 128], F32),
            ("featsT", [33, SEQ], F32), ("w1", [33, 64], F32), ("w2", [64, 64], F32), ("pvec", [64, 4], F32),
            ("FcB", [32, 128, 32, 128], BF16), ("FsB", [32, 128, 32, 128], BF16), ("FsBi", [32, 128, 32, 128], BF16), ("cv", [128, 4], F32),
            ("w_in1", [D, 2560], F32), ("w_out1", [D, D], F32), ("ropeCn", [128, SEQ], F32), ("ropeSn", [128, SEQ], F32),
            ("ropeCq", [128, OWN], F32), ("ropeSq", [128, OWN], F32), ("gvec", [128, 4], F32), ("bones", [128, 128], F32),
            ("swT2", [128, 2, 6, 4], F32), ("hbias2", [2, 2, 256], F32), ("w_hy", [D, 2, 768], F32),
            ("selv", [128, 2], F32), ("fg", [128, 8], F32)):
        E[name] = din(nc, name, shape, dt)
    outT = dout(nc, "outT", [8, 128, OWN])
    x1nat = scr("x1nat", [8, 128, NT1])
    xown = scr("xown", [8, 128, OWN])
    kt_blk = [scr(f"ktblk{i}", [32, 128, 3, 512]) for i in range(2)]
    yatt_s = scr("yatt_s", [4, 128, OWN], BF16)
    yhy_s = scr("yhy_s", [2, 128, 32, 256], BF16)

    mods = mod_phase(nc, C, E["cT"], E["w_ada"], E["b_ada_fm"])
    for p in range(2):
        ov = {"mod": mods[0][:], "ng": E["ng0"], "w_in": E["w_in0"], "w_out": E["w_out0"], "sink": E["sink"], "rpbT": E["rpbT"],
              "vint": E["vint"], "w_r": E["w_r"], "b_r": E["b_r"], "wg": E["wg0"], "wu": E["wu0"], "wd": E["wd0"],
              "ident": E["ident"], "selE": E["selE"], "x1T": (x1nat, p)}
        _build_l0_body(nc, C, False, "full", IO(nc, ov, prefix=f"p{p}_"))
        S.barrier()
    for cb in range(2):
        ov = {"featsT": E["featsT"], "w1": E["w1"], "w2": E["w2"], "pvec": E["pvec"], "FcB": E["FcB"], "FsB": E["FsB"], "cv": E["cv"],
              "ktab": kt_blk[cb]}
        l1k_body2(nc, C, IO(nc, ov, prefix=f"k{cb}_"))
        S.barrier()
    with contextlib.ExitStack() as st:
        sel = S.sbuf("selv_sb", [128, 2], F32, st)
        ta = [S.sbuf(f"bl_a{i}", [128, 512], F32, st) for i in range(2)]
        tb = [S.sbuf(f"bl_b{i}", [128, 512], F32, st) for i in range(2)]
        S.dma("sp", lambda e: e.dma_start(out=sel[:], in_=E["selv"]), writes=["selv"])
        it = 0
        for k in range(8):
            for tt in range(4):
                bi = it % 2
                it += 1
                a0 = CTX + tt * 512
                S.dma("sp", lambda e, k=k, a0=a0, bi=bi: e.dma_start(out=ta[bi][:], in_=x1nat[k][:, a0:a0 + 512]), reads=["x1nat"], writes=[f"bl_a{bi}"])
                S.dma("sp", lambda e, k=k, a0=a0, bi=bi: e.dma_start(out=tb[bi][:], in_=x1nat[k][:, OWN + a0:OWN + a0 + 512]), reads=["x1nat"], writes=[f"bl_b{bi}"])
                S.op("dve", lambda e, bi=bi: e.tensor_scalar(out=ta[bi][:], in0=ta[bi][:], scalar1=sel[:, 0:1], scalar2=None, op0=ALU.mult),
                     reads=[f"bl_a{bi}", "selv"], writes=[f"bl_a{bi}"])
                S.op("dve", lambda e, bi=bi: e.scalar_tensor_tensor(out=ta[bi][:], in0=tb[bi][:], scalar=sel[:, 1:2], in1=ta[bi][:], op0=ALU.mult, op1=ALU.add),
                     reads=[f"bl_a{bi}", f"bl_b{bi}", "selv"], writes=[f"bl_a{bi}"])
                S.dma("sp", lambda e, k=k, tt=tt, bi=bi: e.dma_start(out=xown[k][:, tt * 512:(tt + 1) * 512], in_=ta[bi][:]), reads=[f"bl_a{bi}"], writes=["xown"])
        S.barrier()
    ov = {"xT": x1nat, "xTo": xown, "ropeCq": E["ropeCq"], "ropeSq": E["ropeSq"], "mod": mods[1][:], "ng": E["ng1"], "w_in": E["w_in1"],
          "ropeC": E["ropeCn"], "ropeS": E["ropeSn"], "gvec": E["gvec"], "bones": E["bones"], "ident": E["ident"],
          "swT2": E["swT2"], "hbias2": E["hbias2"], "kt_blk": kt_blk, "w_hy": E["w_hy"],
          "FcB": E["FcB"], "FsB": E["FsB"], "FsBi": E["FsBi"], "yatt": yatt_s, "yhy2": yhy_s}
    l1a_body(nc, C, IO(nc, ov), hy_halves=(0, 1))
    S.barrier()

    def fill_y(ybuf):
        for c in range(4):
            S.dma("sp", lambda e, c=c: e.dma_start(out=ybuf[:, c, :], in_=yatt_s[c]), writes=["Y"])
        with contextlib.ExitStack() as st:
            sel = S.sbuf("selv_sb2", [128, 2], F32, st)
            idn = S.sbuf("idn2", [128, 128], F32, st)
            t0 = [S.sbuf(f"fy_a{i}", [128, 256], BF16, st) for i in range(2)]
            t1 = [S.sbuf(f"fy_b{i}", [128, 256], BF16, st) for i in range(2)]
            tf = [S.sbuf(f"fy_f{i}", [128, 256], F32, st) for i in range(2)]
            S.dma("sp", lambda e: e.dma_start(out=sel[:], in_=E["selv"]), writes=["selv2"])
            S.dma("sp", lambda e: e.dma_start(out=idn[:], in_=E["ident"]), writes=["idn2"])
            it = 0
            for c in range(2):
                for i in range(16):
                    bi = it % 2
                    it += 1
                    S.dma("sp", lambda e, c=c, i=i, bi=bi: e.dma_start(out=t0[bi][:], in_=yhy_s[c][:, i, :]), writes=[f"fy_a{bi}"])
                    S.dma("sp", lambda e, c=c, i=i, bi=bi: e.dma_start(out=t1[bi][:], in_=yhy_s[c][:, 16 + i, :]), writes=[f"fy_b{bi}"])
                    S.op("dve", lambda e, bi=bi: e.tensor_scalar(out=tf[bi][:], in0=t0[bi][:], scalar1=sel[:, 0:1], scalar2=None, op0=ALU.mult),
                         reads=[f"fy_a{bi}", "selv2"], writes=[f"fy_f{bi}"])
                    S.op("dve", lambda e, bi=bi: e.scalar_tensor_tensor(out=tf[bi][:], in0=t1[bi][:], scalar=sel[:, 1:2], in1=tf[bi][:], op0=ALU.mult, op1=ALU.add),
                         reads=[f"fy_b{bi}", f"fy_f{bi}", "selv2"], writes=[f"fy_f{bi}"])
                    for cc in range(2):
                        bk = (2 * it + cc) % 4
                        S.op("pe", lambda e, bi=bi, cc=cc, bk=bk: e.transpose(C.banks[bk][:, 0:128], tf[bi][:, cc * 128:(cc + 1) * 128], idn[:]),
                             reads=[f"fy_f{bi}", "idn2"], writes=[f"bank{bk}"])
                        S.op("act", lambda e, c=c, cc=cc, i=i, bk=bk: e.activation(out=ybuf[:, 4 + 2 * c + cc, i * 128:(i + 1) * 128], in_=C.banks[bk][:, 0:128], func=AF.Copy),
                             reads=[f"bank{bk}"], writes=["Y"])
            S.barrier()
    ov = {"xTo": xown, "mod": mods[1][:], "ng": E["ng1"], "w_out": E["w_out1"], "w_r": E["w_r"], "b_r": E["b_r"],
          "wg": E["wg1"], "wu": E["wu1"], "wd": E["wd1"], "ident": E["ident"], "selE": E["selE"], "fg": E["fg"], "outT": outT}
    l1b_body(nc, C, IO(nc, ov), fill_y=fill_y)
    S.finish()
    return nc


def fused_inputs(inp, core):
    hc = hy_consts()
    b, half = divmod(core, 2)
    m = {}
    cond = np.stack([inp["c"][b], inp["c_ctx"]], axis=0)
    m["cT"] = np.ascontiguousarray(cond.T.reshape(8, 128, 2).transpose(1, 0, 2))
    m["w_ada"] = np.ascontiguousarray(inp["w_ada"])
    m["b_ada_fm"] = np.ascontiguousarray(np.stack([fm(inp["b_ada"][l].reshape(6, D)) for l in range(2)], axis=1))
    m["ng0"] = fm(inp["norm_g"][0]); m["ng1"] = fm(inp["norm_g"][1])
    m["w_in0"] = np.ascontiguousarray(inp["w_in_even"][0][:, l0_w_in_cols()])
    m["w_out0"] = np.ascontiguousarray(inp["w_out_even"][0])
    m["sink"] = np.ascontiguousarray(inp["a_sink"][0]); m["rpbT"] = rpb_gather(inp["b_rpb"][0])
    m["vint"] = np.ascontiguousarray(np.stack([b_valid(10, o) for o in range(-2, 3)], axis=1))
    m["w_r"] = np.ascontiguousarray(inp["w_router"]); m["b_r"] = np.ascontiguousarray(inp["b_router"])
    for l in range(2):
        m[f"wg{l}"] = np.ascontiguousarray(inp["moe_wg"][l]); m[f"wu{l}"] = np.ascontiguousarray(inp["moe_wu"][l])
        m[f"wd{l}"] = np.ascontiguousarray(inp["moe_wd"][l])
    m["ident"] = np.eye(128, dtype=np.float32)
    selE = np.zeros((16, 16, 128), np.float32)
    for e in range(16):
        selE[e, e, :] = 1.0
    m["selE"] = selE
    k = np.arange(128)
    tri_lo = (k[:, None] >= k[None, :]).astype(np.float32)
    tri_hi = (k[:, None] <= k[None, :]).astype(np.float32)
    z = np.zeros_like(tri_lo)
    for p in range(2):
        pos = p * OWN - HALO + np.arange(NLAT)
        ok = (pos >= 0) & (pos < SEQ)
        xl = np.zeros((NTOK, D), np.float32)
        xl[:CTX] = inp["ctx"][b]
        xl[CTX:][ok] = inp["x"][b][pos[ok]]
        m[f"p{p}_xT"] = np.ascontiguousarray(xl.T.reshape(8, 128, NTOK))
        rc, rs = rope_np(np.clip(pos, 0, SEQ - 1))
        m[f"p{p}_ropeC"], m[f"p{p}_ropeS"] = rc, rs
        m[f"p{p}_amask"] = np.ascontiguousarray(np.stack([tri_lo, tri_hi, tri_lo if p == 1 else z, tri_hi if p == 0 else z], axis=1))
        vb = np.zeros((128, 4, 6, 128), np.float32)
        for ci, jl in enumerate((0, 1, 14, 15)):
            for oi, o in enumerate(b_offsets(jl)):
                vb[:, ci, oi] = b_valid(p * 16 + jl, o)
        m[f"p{p}_vb"] = vb
    m["featsT"] = hc["featsT"]; m["w1"] = np.ascontiguousarray(inp["hy_w1"][0]); m["w2"] = np.ascontiguousarray(inp["hy_w2"][0])
    m["pvec"] = np.ascontiguousarray(np.stack([inp["hy_b1"][0], inp["hy_f1"][0], inp["hy_b2"][0], inp["hy_f2"][0]], axis=1).astype(np.float32))
    m["FcB"], m["FsB"], m["FsBi"], m["cv"] = hc["FcB"], hc["FsB"], hc["FsBi"], hc["cv"]
    for cb in range(2):
        ch = 256 * cb + np.arange(256)
        cols = np.concatenate([d * 1024 + o * 512 + ch for d in range(2) for o in range(2)])
        m[f"k{cb}_w3s"] = np.ascontiguousarray(inp["hy_w3"][0][:, cols])
        m[f"k{cb}_b3s"] = np.ascontiguousarray(inp["hy_b3"][0][cols])
        m[f"k{cb}_decay"] = np.ascontiguousarray(hc["decay"][:, :, ch])
    m["w_in1"] = np.ascontiguousarray(inp["w_in_odd"][0][:, l1_w_in_cols(0)])
    m["w_out1"] = np.ascontiguousarray(inp["w_out_odd"][0])
    m["ropeCn"], m["ropeSn"] = rope_np(np.arange(SEQ))
    m["ropeCq"], m["ropeSq"] = rope_np(half * OWN + np.arange(OWN))
    P = rope_perm()
    qn, kn = inp["c_qnorm"][0], inp["c_knorm"][0]
    m["gvec"] = np.ascontiguousarray(np.stack([np.tile(qn, 2), np.tile(qn[P], 2), np.tile(kn, 2), np.tile(kn[P], 2)], axis=1).astype(np.float32))
    bones = np.zeros((128, 128), np.float32)
    bones[:64, :64] = 1.0
    bones[64:, 64:] = 1.0
    m["bones"] = bones
    swT2 = np.zeros((128, 2, 6, 4), np.float32)
    w_hy = np.zeros((D, 2, 768), np.float32)
    for c in range(2):
        chs = np.concatenate([o * 512 + c * 256 + np.arange(256) for o in range(3)])
        sw = inp["hy_short_w"][0][:, chs]
        sb = inp["hy_short_b"][0][chs]
        swT2[:, c] = np.concatenate([sw, sb[None]], 0).T.reshape(6, 128, 4).transpose(1, 0, 2)
        w_hy[:, c] = inp["w_in_odd"][0][:, 768 + chs]
    m["swT2"] = swT2
    m["w_hy"] = w_hy
    m["hbias2"] = np.ascontiguousarray(inp["hy_bias"][0].reshape(2, 2, 256).transpose(1, 0, 2))
    selv = np.zeros((128, 2), np.float32)
    selv[:, half] = 1.0
    m["selv"] = selv
    m["fg"] = fm(inp["final_g"])
    return m


def kernel(**inp):
    inp = {k: np.asarray(v) for k, v in inp.items()}
    cores = list(range(NCORES))
    res = run_bass_kernel_spmd(build_fused(), [fused_inputs(inp, c) for c in cores], core_ids=cores)
    out = np.zeros((NB, SEQ, D), np.float32)
    for c in cores:
        b, half = divmod(c, 2)
        o = res.results[c]["outT"]
        out[b, half * OWN:(half + 1) * OWN] = o.transpose(2, 0, 1).reshape(OWN, D)
    return out


def l1k_body2(nc, C, io):
    NCH = 256
    W4, W2 = 4 * NCH, 2 * NCH
    featsT = io.inp("featsT", [33, SEQ])
    w1 = io.inp("w1", [33, 64]); w2 = io.inp("w2", [64, 64]); w3s = io.inp("w3s", [64, W4])
    pvec = io.inp("pvec", [64, 4])
    b3s = io.inp("b3s", [W4])
    decay = io.inp("decay", [128, 32, NCH])
    FcB = io.inp("FcB", [32, 128, 32, 128], BF16)
    FsB = io.inp("FsB", [32, 128, 32, 128], BF16)
    cv_d = io.inp("cv", [128, 4])
    ktab = io.out("ktab", [32, 128, 3, W2])
    S = C.S
    st = contextlib.ExitStack()
    st2 = contextlib.ExitStack()
    ksum = S.sbuf("ksum", [128, 32, W2], BF16, st); kdif = S.sbuf("kdif", [128, 32, W2], BF16, st)
    cv = S.sbuf("cvs", [128, 4], F32, st)
    ft = S.sbuf("ft", [33, SEQ], F32, st2)
    w1t = S.sbuf("w1t", [33, 64], F32, st2); w2t = S.sbuf("w2t", [64, 64], F32, st2); w3t = S.sbuf("w3t", [64, W4], F32, st2)
    pv = S.sbuf("pv", [64, 4], F32, st2); b3bc = S.sbuf("b3bc", [128, W4], F32, st2)
    dec = S.sbuf("dec", [128, 32, NCH], BF16, st2)
    h1 = S.sbuf("h1", [64, SEQ], F32, st2); h2 = S.sbuf("h2", [64, SEQ], F32, st2)
    for (dst, src, key) in ((ft, featsT, "ft"), (w1t, w1, "w1t"), (w2t, w2, "w2t"), (w3t, w3s, "w3t"), (pv, pvec, "pv"), (cv, cv_d, "cv")):
        S.dma("sp", lambda e, dst=dst, src=src: e.dma_start(out=dst[:], in_=src), writes=[key])
    S.dma("pool", lambda e: e.dma_start(out=dec[:], in_=decay), writes=["dec"])
    S.dma("sp", lambda e: e.dma_start(out=b3bc[:], in_=b3s.partition_broadcast(128)), writes=["b3bc"])
    pre = [S.sbuf(f"pre{i}", [64, 512], F32, st2) for i in range(2)]
    rr_i = [S.sbuf(f"rr_i{i}", [64, 512], mybir.dt.int32, st2) for i in range(2)]
    rr_f = [S.sbuf(f"rr_f{i}", [64, 512], F32, st2) for i in range(2)]
    rr_c = [S.sbuf(f"rr_c{i}", [64, 512], F32, st2) for i in range(2)]
    for layer, (wt, wk, src, skey, dst, dkey, K_) in enumerate(((w1t, "w1t", ft, "ft", h1, "h1", 33), (w2t, "w2t", h1, "h1", h2, "h2", 64))):
        for tt in range(8):
            bk = tt % 2
            S.op("pe", lambda e, tt=tt, bk=bk: e.matmul(C.banks[bk][0:64, :], wt[0:K_, :], src[0:K_, tt * 512:(tt + 1) * 512], start=True, stop=True),
                 reads=[wk, skey], writes=[f"bank{bk}"])
            S.op("dve", lambda e, bk=bk: e.tensor_scalar(out=pre[bk][:], in0=C.banks[bk][0:64, :], scalar1=pv[:, 2 * layer:2 * layer + 1],
                                                        scalar2=pv[:, 2 * layer + 1:2 * layer + 2], op0=ALU.add, op1=ALU.mult),
                 reads=[f"bank{bk}", "pv"], writes=[f"pre{bk}"])
            sin_rr(C, dst[:, tt * 512:(tt + 1) * 512], pre[bk][:], f"pre{bk}", dkey, rr_i[bk][:], rr_f[bk][:], rr_c[bk][:], bk)
    hts = [S.sbuf(f"hts{i}", [128, W4], F32, st2) for i in range(2)]
    ab = [S.sbuf(f"habs{i}", [128, W4], F32, st2) for i in range(2)]
    scl = S.sbuf("scl", [128, W2], F32, st2)

    def h_tile(ti, hi):
        for hf in range(2):
            S.op("pe", lambda e, hf=hf: e.matmul(C.banks[hf][:, :], h2[0:64, ti * 128:(ti + 1) * 128], w3t[0:64, hf * 512:(hf + 1) * 512], start=True, stop=True),
                 reads=["h2", "w3t"], writes=[f"bank{hf}"])
            S.op("dve", lambda e, hf=hf: e.tensor_tensor(out=hts[hi][:, hf * 512:(hf + 1) * 512], in0=C.banks[hf][:, :], in1=b3bc[:, hf * 512:(hf + 1) * 512], op=ALU.add),
                 reads=[f"bank{hf}", "b3bc"], writes=[f"hts{hi}"])
        S.op("pool", lambda e: e.tensor_tensor(out=hts[hi][:].rearrange("p (a c) -> p a c", a=4), in0=hts[hi][:].rearrange("p (a c) -> p a c", a=4),
                                              in1=dec[:, ti, :].unsqueeze(1).to_broadcast([128, 4, NCH]), op=ALU.mult),
             reads=[f"hts{hi}", "dec"], writes=[f"hts{hi}"])
    for ti in range(32):
        hi = ti % 2
        h_tile(ti, hi)
        S.op("act", lambda e, hi=hi: e.activation(out=ab[hi][:], in_=hts[hi][:], func=AF.Abs), reads=[f"hts{hi}"], writes=[f"habs{hi}"])
        for hf in range(2):
            S.op("pe", lambda e, hf=hf, hi=hi: e.matmul(C.banks[2 + hf][:, :], C.ones_f[:], ab[hi][:, hf * 512:(hf + 1) * 512], start=(ti == 0), stop=(ti == 31)),
                 reads=[f"habs{hi}", "ones_f"], writes=[f"bank{2 + hf}"])
    S.op("dve", lambda e: e.tensor_copy(out=scl[:], in_=C.banks[2][:, :]), reads=["bank2"], writes=["scl"])
    S.op("dve", lambda e: e.tensor_tensor(out=scl[:], in0=scl[:], in1=C.banks[3][:, :], op=ALU.add), reads=["bank3", "scl"], writes=["scl"])
    S.op("dve", lambda e: e.tensor_scalar(out=scl[:], in0=scl[:], scalar1=EPS, scalar2=None, op0=ALU.add), reads=["scl"], writes=["scl"])
    S.op("dve", lambda e: e.reciprocal(out=scl[:], in_=scl[:]), reads=["scl"], writes=["scl"])
    for ti in range(32):
        hi = ti % 2
        h_tile(ti, hi)
        S.op("dve", lambda e, hi=hi: e.tensor_tensor(out=hts[hi][:].rearrange("p (a c) -> p a c", a=2), in0=hts[hi][:].rearrange("p (a c) -> p a c", a=2),
                                                  in1=scl[:].unsqueeze(1).to_broadcast([128, 2, W2]), op=ALU.mult),
             reads=[f"hts{hi}", "scl"], writes=[f"hts{hi}"])
        if ti == 0:
            S.op("dve", lambda e, hi=hi: e.tensor_scalar(out=hts[hi][:, W2:W4], in0=hts[hi][:, W2:W4], scalar1=cv[:, 1:2], scalar2=None, op0=ALU.mult),
                 reads=[f"hts{hi}", "cv"], writes=[f"hts{hi}"])
        S.op("pool", lambda e, ti=ti, hi=hi: e.tensor_tensor(out=ksum[:, ti, :], in0=hts[hi][:, 0:W2], in1=hts[hi][:, W2:W4], op=ALU.add), reads=[f"hts{hi}"], writes=["ksum"])
        S.op("dve", lambda e, ti=ti, hi=hi: e.tensor_tensor(out=kdif[:, ti, :], in0=hts[hi][:, 0:W2], in1=hts[hi][:, W2:W4], op=ALU.subtract), reads=[f"hts{hi}"], writes=["kdif"])
    S.barrier()
    st2.close()
    fcs = [S.sbuf(f"fcb{i}", [128, 32, 128], BF16, st) for i in range(2)]
    fss = [S.sbuf(f"fsb{i}", [128, 32, 128], BF16, st) for i in range(2)]
    kt = [S.sbuf(f"kt{i}", [128, 3, W2], F32, st) for i in range(2)]
    for j in range(32):
        bi = j % 2
        S.dma("sp", lambda e, j=j, bi=bi: e.dma_start(out=fcs[bi][:], in_=FcB[j]), writes=[f"fcb{bi}"])
        S.dma("sp", lambda e, j=j, bi=bi: e.dma_start(out=fss[bi][:], in_=FsB[j]), writes=[f"fsb{bi}"])
        pb = 3 + 2 * bi
        for ti in range(32):
            S.op("pe", lambda e, ti=ti, bi=bi, pb=pb: e.matmul(C.banks[pb][:, :], fcs[bi][:, ti, :], ksum[:, ti, :], start=(ti == 0), stop=(ti == 31)),
                 reads=[f"fcb{bi}", "ksum"], writes=[f"bank{pb}"])
        for ti in range(32):
            S.op("pe", lambda e, ti=ti, bi=bi, pb=pb: e.matmul(C.banks[pb + 1][:, :], fss[bi][:, ti, :], kdif[:, ti, :], start=(ti == 0), stop=(ti == 31)),
                 reads=[f"fsb{bi}", "kdif"], writes=[f"bank{pb + 1}"])
        if j == 0:
            for ti in range(32):
                S.op("pe", lambda e, ti=ti, bi=bi: e.matmul(C.banks[7][:, :], fss[bi][:, ti, :], ksum[:, ti, :], start=(ti == 0), stop=(ti == 31)),
                     reads=[f"fsb{bi}", "ksum"], writes=["bank7"])
            S.op("dve", lambda e, bi=bi, pb=pb: e.tensor_scalar(out=kt[bi][:, 0, :], in0=C.banks[pb][:, :], scalar1=cv[:, 0:1], scalar2=None, op0=ALU.mult),
                 reads=[f"bank{pb}", "cv"], writes=[f"kt{bi}"])
            S.op("dve", lambda e, bi=bi, pb=pb: e.tensor_scalar(out=kt[bi][:, 1, :], in0=C.banks[pb + 1][:, :], scalar1=cv[:, 1:2], scalar2=None, op0=ALU.mult),
                 reads=[f"bank{pb + 1}", "cv"], writes=[f"kt{bi}"])
            S.op("dve", lambda e, bi=bi, pb=pb: e.tensor_scalar(out=kt[bi][:, 2, :], in0=C.banks[pb][:, :], scalar1=cv[:, 1:2], scalar2=None, op0=ALU.mult),
                 reads=[f"bank{pb}", "cv"], writes=[f"kt{bi}"])
            S.op("dve", lambda e, bi=bi: e.scalar_tensor_tensor(out=kt[bi][:, 2, :], in0=C.banks[7][:, :], scalar=cv[:, 2:3], in1=kt[bi][:, 2, :],
                                                               op0=ALU.mult, op1=ALU.add), reads=["bank7", "cv", f"kt{bi}"], writes=[f"kt{bi}"])
        else:
            S.op("act", lambda e, bi=bi, pb=pb: e.activation(out=kt[bi][:, 0, :], in_=C.banks[pb][:, :], func=AF.Copy), reads=[f"bank{pb}"], writes=[f"kt{bi}"])
            S.op("dve", lambda e, bi=bi, pb=pb: e.tensor_copy(out=kt[bi][:, 1, :], in_=C.banks[pb + 1][:, :]), reads=[f"bank{pb + 1}"], writes=[f"kt{bi}"])
            S.op("act", lambda e, bi=bi, pb=pb: e.activation(out=kt[bi][:, 2, :], in_=C.banks[pb][:, :], func=AF.Copy), reads=[f"bank{pb}"], writes=[f"kt{bi}"])
        S.dma("sp", lambda e, j=j, bi=bi: e.dma_start(out=ktab[j], in_=kt[bi][:]), reads=[f"kt{bi}"], writes=["ktblk"], is_out=True)
    S.barrier()
    st.close()
```

```python
import contextlib
import math
import numpy as np
import concourse.bass as bass
import concourse.mybir as mybir
from concourse.bass_utils import run_bass_kernel_spmd

F32 = mybir.dt.float32
BF16 = mybir.dt.bfloat16
AF = mybir.ActivationFunctionType
ALU = mybir.AluOpType
AX = mybir.AxisListType

EPOCH = 3000
N_DMA_SEMS = 24
NCORES = 8

D = 1024
SEQ = 4096
NB = 4
CTX = 256
GW = 64
HD = 64
EPS = 1e-6
SCALE = HD ** -0.5
OWN = 2048
HALO = 256
NLAT = OWN + 2 * HALO
NTOK = CTX + NLAT
OWN0 = CTX + HALO
NRES = CTX + OWN
NE = 16
DE = 512


class Sched:
    ENGS = ("pe", "act", "dve", "pool", "sp")

    def __init__(self, nc):
        self.nc = nc
        self.stack = contextlib.ExitStack()
        self.eng = {"pe": nc.tensor, "act": nc.scalar, "dve": nc.vector, "pool": nc.gpsimd, "sp": nc.sync}
        self.seq = {e: 0 for e in self.ENGS}
        self.sems = {}
        self.dma_sems = []
        self.dma_uses = []
        self.dma_rr = 0
        self.dma_q = {}
        self.dead = False
        self.waited = {e: {} for e in self.ENGS}
        self.state = {}
        self.out_deps = []
        self.uid = 0

    def sbuf(self, name, shape, dtype, stack=None):
        self.uid += 1
        return (stack or self.stack).enter_context(self.nc.sbuf_tensor(f"sb{self.uid}_{name}", list(shape), dtype))

    def psum(self, name, shape, dtype, stack=None):
        return (stack or self.stack).enter_context(self.nc.psum_tensor(name, list(shape), dtype))

    def _new_sem(self, name):
        return self.stack.enter_context(self.nc.semaphore(name))

    def _sem(self, semkey):
        if semkey[0] == "c":
            k = (semkey[1], semkey[2])
            if k not in self.sems:
                self.sems[k] = self._new_sem(f"s_{semkey[1]}_{semkey[2]}")
            return self.sems[k]
        return self.dma_sems[semkey[1]]

    def _deps(self, eng, reads, writes):
        deps = []
        for k in reads:
            st = self.state.get(k)
            if st and st[0] is not None:
                deps.append(st[0])
        for k in writes:
            st = self.state.get(k)
            if st:
                if st[0] is not None:
                    deps.append(st[0])
                deps.extend(st[1].values())
        best = {}
        for semkey, val, deng in deps:
            if deng == eng and eng == "pe":
                continue
            if self.waited[eng].get(semkey, 0) >= val:
                continue
            best[semkey] = max(best.get(semkey, 0), val)
        for sk, v in best.items():
            self.waited[eng][sk] = v
        return list(best.items())

    def _commit(self, who, dep, reads, writes):
        for k in reads:
            st = self.state.setdefault(k, [None, {}])
            st[1][(who, dep[0])] = dep
        for k in writes:
            self.state[k] = [dep, {}]

    def _emit_waits(self, eng, waits):
        e = self.eng[eng]
        for sk, v in waits:
            e.wait_ge(self._sem(sk), v)

    def kill(self):
        self.barrier()
        self.dead = True

    def op(self, eng, fn, reads=(), writes=()):
        if self.dead:
            return
        waits = self._deps(eng, reads, writes)
        self.seq[eng] += 1
        epoch, val = divmod(self.seq[eng] - 1, EPOCH)
        val += 1
        semkey = ("c", eng, epoch)
        self._emit_waits(eng, waits)
        ins = fn(self.eng[eng])
        ins.then_inc(self._sem(semkey), 1)
        self._commit(eng, (semkey, val, eng), reads, writes)

    def dma(self, q, fn, reads=(), writes=(), is_out=False):
        if self.dead:
            return None
        if q not in self.dma_q:
            base = len(self.dma_sems)
            nq = 16 if q == "sp" else 8
            for i in range(nq):
                self.dma_sems.append(self._new_sem(f"s_dma_{q}_{i}"))
                self.dma_uses.append(0)
            self.dma_q[q] = [base, nq, 0]
        base, nq, rr = self.dma_q[q]
        j = base + rr
        self.dma_q[q][2] = (rr + 1) % nq
        semkey = ("d", j)
        waits = dict(self._deps(q, reads, writes))
        prev = self.dma_uses[j] * 16
        if prev > 0 and self.waited[q].get(semkey, 0) < prev:
            self.waited[q][semkey] = prev
            waits[semkey] = max(waits.get(semkey, 0), prev)
        self.dma_uses[j] += 1
        val = self.dma_uses[j] * 16
        self._emit_waits(q, list(waits.items()))
        ins = fn(self.eng[q])
        ins.then_inc(self.dma_sems[j], 16)
        dep = (semkey, val, "dma")
        self._commit("dma", dep, reads, writes)
        if is_out:
            self.out_deps.append(dep)
        return dep

    def barrier(self):
        if self.dead:
            return
        targets = []
        for e in self.ENGS:
            if self.seq[e] > 0:
                epoch, val = divmod(self.seq[e] - 1, EPOCH)
                targets.append((("c", e, epoch), val + 1, e))
        for j, u in enumerate(self.dma_uses):
            if u > 0:
                targets.append((("d", j), u * 16, "dma"))
        for e in self.ENGS:
            waits = []
            for sk, v, de in targets:
                if de == e:
                    continue
                if self.waited[e].get(sk, 0) >= v:
                    continue
                self.waited[e][sk] = v
                waits.append((sk, v))
            self._emit_waits(e, waits)
        self.state = {}

    def finish(self):
        self.dead = False
        self.barrier()
        self.stack.close()


class IO:
    def __init__(self, nc, ov=None, prefix=""):
        self.nc, self.ov, self.prefix = nc, dict(ov or {}), prefix

    def inp(self, name, shape, dt=F32):
        if name in self.ov:
            return self.ov[name]
        return din(self.nc, self.prefix + name, shape, dt)

    def out(self, name, shape, dt=F32):
        if name in self.ov:
            return self.ov[name]
        return dout(self.nc, self.prefix + name, shape, dt)


def din(nc, name, shape, dt=F32):
    return nc.dram_tensor(name, list(shape), dt, kind="ExternalInput").ap()


def dout(nc, name, shape, dt=F32):
    return nc.dram_tensor(name, list(shape), dt, kind="ExternalOutput").ap()


class Ctx:
    def __init__(self, nc):
        self.nc = nc
        self.S = Sched(nc)
        S = self.S
        self.ps = S.psum("psall", [128, 8, 512], F32)
        self.banks = [self.ps[:, i, :] for i in range(8)]
        self.ones_f = S.sbuf("ones_f", [128, 128], F32)
        self.ones_b = S.sbuf("ones_b", [128, 128], BF16)
        S.op("dve", lambda e: e.memset(self.ones_f[:], 1.0), writes=["ones_f"])
        S.op("dve", lambda e: e.memset(self.ones_b[:], 1.0), writes=["ones_b"])
        self.k = 0

    def key(self, base):
        self.k += 1
        return f"{base}#{self.k}"


def norm_mod_tile(C, xt, xkey, ntok, out_fn, gs_fn, sh_fn, tmp, bank, ranges, f32_out=None):
    S = C.S
    sq, rs = tmp["sq"], tmp["rs"]
    kq = C.key("sq")
    S.op("act", lambda e: e.activation(out=sq[:, :, :ntok], in_=xt, func=AF.Square), reads=[xkey], writes=[kq])
    bk = f"bank{bank}"
    ps = C.banks[bank]
    for k in range(8):
        S.op("pe", lambda e, k=k: e.matmul(ps[:, :ntok], C.ones_f[:], sq[:, k, :ntok], start=(k == 0), stop=(k == 7)),
             reads=[kq, "ones_f"], writes=[bk])
    kr = C.key("rs")
    S.op("act", lambda e: e.activation(out=rs[:, :ntok], in_=ps[:, :ntok], func=AF.Sqrt, bias=EPS, scale=1.0 / D),
         reads=[bk], writes=[kr])
    kr2 = C.key("rs2")
    S.op("dve", lambda e: e.reciprocal(out=rs[:, :ntok], in_=rs[:, :ntok]), reads=[kr], writes=[kr, kr2])
    for k in range(8):
        t = tmp["t"][k % 2]
        kt = f"normt{k % 2}"
        for (a, b, r) in ranges:
            gs, sh = gs_fn(r), sh_fn(r)
            S.op("dve", lambda e, k=k, a=a, b=b, gs=gs, t=t: e.scalar_tensor_tensor(
                out=t[:, a:b], in0=xt[:, k, a:b], scalar=gs[:, k:k + 1], in1=rs[:, a:b], op0=ALU.mult, op1=ALU.mult),
                reads=[xkey, kr2], writes=[kt])
            S.op("act", lambda e, k=k, a=a, b=b, sh=sh, t=t: e.activation(
                out=out_fn(k, a, b), in_=t[:, a:b], func=AF.Identity, bias=sh[:, k:k + 1], scale=1.0),
                reads=[kt], writes=[tmp["outkey"]])
            if f32_out is not None:
                S.op("pool", lambda e, k=k, a=a, b=b, sh=sh, t=t: e.tensor_scalar(
                    out=f32_out(k, a, b), in0=t[:, a:b], scalar1=sh[:, k:k + 1], scalar2=None, op0=ALU.add),
                    reads=[kt], writes=[tmp["f32key"]])


def build_mod():
    nc = bass.Bass("TRN2", target_bir_lowering=False)
    cT = din(nc, "cT", [128, 8, 5])
    w = din(nc, "w", [D, 1536])
    b = din(nc, "b", [1536])
    o = dout(nc, "o", [5, 1536])
    C = Ctx(nc)
    S = C.S
    ct = S.sbuf("ct", [128, 8, 5], F32)
    sg = S.sbuf("sg", [128, 8, 5], F32)
    wt = S.sbuf("wt", [128, 8, 1536], F32)
    bt = S.sbuf("bt", [5, 1536], F32)
    ot = S.sbuf("ot", [5, 1536], F32)
    S.dma("sp", lambda e: e.dma_start(out=ct[:], in_=cT), writes=["ct"])
    for k in range(8):
        S.dma("sp", lambda e, k=k: e.dma_start(out=wt[:, k, :], in_=w[k * 128:(k + 1) * 128, :]), writes=[f"wt{k}"])
    S.dma("sp", lambda e: e.dma_start(out=bt[:], in_=b.partition_broadcast(5)), writes=["bt"])
    S.op("act", lambda e: e.activation(out=sg[:], in_=ct[:], func=AF.Silu), reads=["ct"], writes=["sg"])
    for j in range(3):
        ps = C.banks[j]
        for k in range(8):
            S.op("pe", lambda e, k=k, j=j, ps=ps: e.matmul(ps[0:5, :], sg[:, k, :], wt[:, k, j * 512:(j + 1) * 512],
                                                           start=(k == 0), stop=(k == 7)),
                 reads=["sg", f"wt{k}"], writes=[f"bank{j}"])
        S.op("dve", lambda e, j=j, ps=ps: e.tensor_tensor(out=ot[:, j * 512:(j + 1) * 512], in0=ps[0:5, :],
                                                        in1=bt[:, j * 512:(j + 1) * 512], op=ALU.add),
             reads=[f"bank{j}", "bt"], writes=["ot"])
    S.dma("sp", lambda e: e.dma_start(out=o, in_=ot[:]), reads=["ot"], is_out=True)
    S.finish()
    return nc


def run_mod(inp):
    cond = np.concatenate([inp["c"], inp["c_ctx"][None]], axis=0)
    cT = np.ascontiguousarray(cond.T.reshape(8, 128, 5).transpose(1, 0, 2))
    maps = []
    for i in range(NCORES):
        l, q = divmod(i, 4)
        maps.append({"cT": cT,
                     "w": np.ascontiguousarray(inp["w_ada"][l][:, q * 1536:(q + 1) * 1536]),
                     "b": np.ascontiguousarray(inp["b_ada"][l][q * 1536:(q + 1) * 1536])})
    res = run_bass_kernel_spmd(build_mod(), maps, core_ids=list(range(NCORES)))
    mod = np.zeros((2, 5, 6144), np.float32)
    for i in range(NCORES):
        l, q = divmod(i, 4)
        mod[l][:, q * 1536:(q + 1) * 1536] = res.results[i]["o"]
    return mod


def rope_np(pos):
    pos = np.asarray(pos)
    row = (pos // GW).astype(np.float32)
    col = (pos % GW).astype(np.float32)
    inv = (10000.0 ** (-np.arange(0, 32, 2, dtype=np.float32) / 32)).astype(np.float32)
    ar = row[None, :] * inv[:, None]
    ac = col[None, :] * inv[:, None]
    Ct = np.concatenate([np.cos(ar), np.cos(ar), np.cos(ac), np.cos(ac)], 0)
    St = np.concatenate([-np.sin(ar), np.sin(ar), -np.sin(ac), np.sin(ac)], 0)
    return (np.ascontiguousarray(np.concatenate([Ct, Ct], 0), dtype=np.float32),
            np.ascontiguousarray(np.concatenate([St, St], 0), dtype=np.float32))


def rope_perm():
    return np.concatenate([np.arange(16, 32), np.arange(0, 16), np.arange(48, 64), np.arange(32, 48)])


def b_offsets(jl):
    if jl in (0, 1):
        return list(range(-2, 4))
    if jl in (14, 15):
        return list(range(-3, 3))
    return list(range(-2, 3))


def b_valid(j, o):
    kt = j + o
    m = np.zeros((128, 128), np.float32)
    if kt < 0 or kt >= 32:
        return m
    k = np.arange(128)
    krow = 2 * kt + k // 64
    kcol = k % 64
    qrow = 2 * j + k // 64
    qcol = k % 64
    rs = np.clip(qrow - 4, 0, 56)
    cs = np.clip(qcol - 8, 0, 48)
    ok_r = (krow[:, None] >= rs[None, :]) & (krow[:, None] < rs[None, :] + 8)
    ok_c = (kcol[:, None] >= cs[None, :]) & (kcol[:, None] < cs[None, :] + 16)
    return (ok_r & ok_c).astype(np.float32)


HORD = [0, 2, 4, 6, 1, 3, 5, 7]


def rpb_gather(rpb):
    k = np.arange(128)
    a, kc = k // 64, k % 64
    out = np.zeros((128, 7, 8, 128), np.float32)
    for oi, o in enumerate(range(-3, 4)):
        dr = 2 * o + a[:, None] - a[None, :]
        dc = kc[:, None] - kc[None, :]
        ok = (np.abs(dr) <= 7) & (np.abs(dc) <= 15)
        g = rpb[HORD][:, np.clip(dr + 7, 0, 14), np.clip(dc + 15, 0, 30)]
        g = np.where(ok[None], g, 0.0)
        out[:, oi] = g.transpose(1, 0, 2)
    return out


def l0_w_in_cols():
    P = rope_perm()
    aq = np.arange(0, 512)
    aqP = (aq.reshape(8, 64)[:, P]).reshape(-1)
    ak0 = 512 + np.arange(64)
    ak1 = 576 + np.arange(64)
    av0 = 640 + np.arange(64)
    av1 = 704 + np.arange(64)
    cols = [aq, aqP,
            np.concatenate([ak0, ak0]), np.concatenate([ak0[P], ak0[P]]),
            np.concatenate([ak1, ak1]), np.concatenate([ak1[P], ak1[P]]),
            768 + np.arange(512), 1280 + np.arange(512), 1792 + np.arange(512),
            np.concatenate([av0, av0, av1, av1])]
    return np.concatenate(cols)


def fm(v):
    v = np.asarray(v, np.float32)
    lead = v.shape[:-1]
    r = v.reshape(lead + (8, 128))
    return np.ascontiguousarray(np.moveaxis(r, -1, 0))


def l0_inputs(inp, mod, core):
    b, half = divmod(core, 2)
    pos = half * OWN - HALO + np.arange(NLAT)
    ok = (pos >= 0) & (pos < SEQ)
    xl = np.zeros((NTOK, D), np.float32)
    xl[:CTX] = inp["ctx"][b]
    xl[CTX:][ok] = inp["x"][b][pos[ok]]
    xT = np.ascontiguousarray(xl.T.reshape(8, 128, NTOK))
    rc, rs = rope_np(np.clip(pos, 0, SEQ - 1))
    modt = np.stack([fm(mod[0, b].reshape(6, D)), fm(mod[0, 4].reshape(6, D))], axis=1)
    k = np.arange(128)
    tri_lo = (k[:, None] >= k[None, :]).astype(np.float32)
    tri_hi = (k[:, None] <= k[None, :]).astype(np.float32)
    z = np.zeros_like(tri_lo)
    amask = np.stack([tri_lo, tri_hi, tri_lo if half == 1 else z, tri_hi if half == 0 else z], axis=1)
    vint = np.stack([b_valid(10, o) for o in range(-2, 3)], axis=1)
    vb = np.zeros((128, 4, 6, 128), np.float32)
    for ci, jl in enumerate((0, 1, 14, 15)):
        for oi, o in enumerate(b_offsets(jl)):
            vb[:, ci, oi] = b_valid(half * 16 + jl, o)
    selE = np.zeros((16, 16, 128), np.float32)
    for e in range(16):
        selE[e, e, :] = 1.0
    return {
        "xT": xT, "mod": np.ascontiguousarray(modt), "ng": fm(inp["norm_g"][0]),
        "w_in": np.ascontiguousarray(inp["w_in_even"][0][:, l0_w_in_cols()]),
        "ropeC": rc, "ropeS": rs, "w_out": np.ascontiguousarray(inp["w_out_even"][0]),
        "sink": np.ascontiguousarray(inp["a_sink"][0]), "rpbT": rpb_gather(inp["b_rpb"][0]),
        "amask": np.ascontiguousarray(amask), "vint": np.ascontiguousarray(vint), "vb": vb,
        "w_r": np.ascontiguousarray(inp["w_router"]), "b_r": np.ascontiguousarray(inp["b_router"]),
        "wg": np.ascontiguousarray(inp["moe_wg"][0]), "wu": np.ascontiguousarray(inp["moe_wu"][0]),
        "wd": np.ascontiguousarray(inp["moe_wd"][0]),
        "ident": np.eye(128, dtype=np.float32), "selE": selE,
    }


def load_mod(C, modt, ng, st):
    S = C.S
    mod_sb = S.sbuf("mod_sb", [128, 2, 6, 8], F32, st)
    ng_sb = S.sbuf("ng_sb", [128, 2, 8], F32, st)
    gs_sb = S.sbuf("gs_sb", [128, 2, 2, 8], F32, st)
    S.dma("sp", lambda e: e.dma_start(out=mod_sb[:], in_=modt), writes=["mod_sb"])
    S.dma("sp", lambda e: e.dma_start(out=ng_sb[:], in_=ng), writes=["ng_sb"])
    for i in range(2):
        for r in range(2):
            S.op("dve", lambda e, i=i, r=r: e.scalar_tensor_tensor(
                out=gs_sb[:, i, r, :], in0=mod_sb[:, r, 1 + 3 * i, :], scalar=1.0, in1=ng_sb[:, i, :],
                op0=ALU.add, op1=ALU.mult), reads=["mod_sb", "ng_sb"], writes=["gs_sb"])
    M = {"gs": [[gs_sb[:, i, r, :] for r in range(2)] for i in range(2)],
         "sh": [[mod_sb[:, r, 3 * i, :] for r in range(2)] for i in range(2)],
         "gate": [[mod_sb[:, r, 2 + 3 * i, :] for r in range(2)] for i in range(2)]}
    return M


def norm_phase(C, M, i, src_fn, src_key_fn, ntok_total, ctx_len, out_t, out_key, st, f32_cb=None):
    S = C.S
    tmp = {"sq": S.sbuf("n_sq", [128, 8, 512], F32, st), "rs": S.sbuf("n_rs", [128, 512], F32, st),
           "t": [S.sbuf("n_t0", [128, 512], F32, st), S.sbuf("n_t1", [128, 512], F32, st)],
           "outkey": out_key, "f32key": "h2f"}
    a = 0
    ti = 0
    while a < ntok_total:
        b = min(a + 512, ntok_total)
        n = b - a
        xt, xkey = src_fn(a, b, ti)
        ranges = []
        if a < ctx_len:
            ranges.append((0, min(ctx_len, b) - a, 1))
            if b > ctx_len:
                ranges.append((ctx_len - a, n, 0))
        else:
            ranges.append((0, n, 0))
        f32o = None
        if f32_cb is not None:
            f32o = f32_cb(a, b, ti, "pre")
        norm_mod_tile(C, xt, xkey, n, lambda k, aa, bb, a=a: out_t[:, k, a + aa:a + bb],
                      lambda r: M["gs"][i][r], lambda r: M["sh"][i][r], tmp, 7, ranges, f32_out=f32o)
        if f32_cb is not None:
            f32_cb(a, b, ti, "post")
        a = b
        ti += 1


RES_TILES = [(0, 256)] + [(256 + 512 * i, 256 + 512 * (i + 1)) for i in range(4)]


def moe_phase(C, xres, h2, combT, selE_sb, gate_fn, wg, wu, wd, st, nres=NRES, tiles=RES_TILES, ctx_len=CTX):
    S = C.S
    wgs = [S.sbuf(f"wg_sb{i}", [128, 8, DE], BF16, st) for i in range(2)]
    wus = [S.sbuf(f"wu_sb{i}", [128, 8, DE], BF16, st) for i in range(2)]
    wds = [S.sbuf(f"wd_sb{i}", [128, 4, D], BF16, st) for i in range(2)]
    he = [S.sbuf(f"he{i}", [128, 4, 512], BF16, st) for i in range(2)]
    sg = [S.sbuf(f"sg{i}", [128, 512], BF16, st) for i in range(2)]
    tt = [S.sbuf(f"tt{i}", [128, 512], BF16, st) for i in range(2)]
    bc = [S.sbuf(f"bc{i}", [128, 512], BF16, st) for i in range(2)]
    cnt = 0
    for ex in range(NE):
        wi = ex % 2
        S.dma("pool", lambda e, ex=ex, wi=wi: e.dma_start(out=wgs[wi][:], in_=wg[ex].rearrange("(k p) o -> p k o", p=128)),
              writes=[f"wg{wi}"])
        S.dma("pool", lambda e, ex=ex, wi=wi: e.dma_start(out=wus[wi][:], in_=wu[ex].rearrange("(k p) o -> p k o", p=128)),
              writes=[f"wu{wi}"])
        S.dma("pool", lambda e, ex=ex, wi=wi: e.dma_start(out=wds[wi][:], in_=wd[ex].rearrange("(k p) o -> p k o", p=128)),
              writes=[f"wd{wi}"])
        for (a, b) in tiles:
            n = b - a
            r = 1 if a < ctx_len else 0
            hi = cnt % 2
            cnt += 1
            S.op("pe", lambda e, ex=ex, a=a, b=b, n=n: e.matmul(C.banks[6][:, :n], selE_sb[:, ex, :], combT[:, a:b],
                                                                start=True, stop=True),
                 reads=["combT", "selE"], writes=["bank6"])
            S.op("act", lambda e, hi=hi, n=n: e.activation(out=bc[hi][:, :n], in_=C.banks[6][:, :n], func=AF.Copy),
                 reads=["bank6"], writes=[f"bc{hi}"])
            for hc in range(4):
                gb = (hc % 2) * 2
                gk, uk = f"bank{gb}", f"bank{gb + 1}"
                for k in range(8):
                    S.op("pe", lambda e, k=k, hc=hc, gb=gb, a=a, b=b, n=n, wi=wi: e.matmul(
                        C.banks[gb][:, :n], wgs[wi][:, k, hc * 128:(hc + 1) * 128], h2[:, k, a:b],
                        start=(k == 0), stop=(k == 7)), reads=[f"wg{wi}", "h2"], writes=[gk])
                for k in range(8):
                    S.op("pe", lambda e, k=k, hc=hc, gb=gb, a=a, b=b, n=n, wi=wi: e.matmul(
                        C.banks[gb + 1][:, :n], wus[wi][:, k, hc * 128:(hc + 1) * 128], h2[:, k, a:b],
                        start=(k == 0), stop=(k == 7)), reads=[f"wu{wi}", "h2"], writes=[uk])
                si = hc % 2
                S.op("act", lambda e, gb=gb, si=si, n=n: e.activation(out=sg[si][:, :n], in_=C.banks[gb][:, :n], func=AF.Silu),
                     reads=[gk], writes=[f"sg{si}"])
                S.op("dve", lambda e, gb=gb, si=si, n=n: e.tensor_tensor(out=tt[si][:, :n], in0=sg[si][:, :n],
                                                                        in1=C.banks[gb + 1][:, :n], op=ALU.mult),
                     reads=[f"sg{si}", uk], writes=[f"tt{si}"])
                S.op("pool", lambda e, si=si, hi=hi, hc=hc, n=n: e.tensor_tensor(out=he[hi][:, hc, :n], in0=tt[si][:, :n],
                                                                               in1=bc[hi][:, :n], op=ALU.mult),
                     reads=[f"tt{si}", f"bc{hi}"], writes=[f"he{hi}"])
            for oc in range(8):
                yb = 4 + (oc % 2)
                for hc in range(4):
                    S.op("pe", lambda e, oc=oc, hc=hc, yb=yb, n=n, hi=hi, wi=wi: e.matmul(
                        C.banks[yb][:, :n], wds[wi][:, hc, oc * 128:(oc + 1) * 128], he[hi][:, hc, :n],
                        start=(hc == 0), stop=(hc == 3)), reads=[f"wd{wi}", f"he{hi}"], writes=[f"bank{yb}"])
                g = gate_fn(r)
                S.op("dve", lambda e, oc=oc, yb=yb, a=a, b=b, n=n, g=g: e.scalar_tensor_tensor(
                    out=xres[:, oc, a:b], in0=C.banks[yb][:, :n], scalar=g[:, oc:oc + 1], in1=xres[:, oc, a:b],
                    op0=ALU.mult, op1=ALU.add), reads=[f"bank{yb}", f"xres{oc}"], writes=[f"xres{oc}"])


def router_phase(C, lg_all, ntile, b_r, st):
    S = C.S
    n = ntile
    def T(name, shape):
        return S.sbuf(name, shape, F32, st)
    br = T("r_br", [128, 16])
    s = T("r_s", [128, n, 16]); sel = T("r_sel", [128, n, 16])
    S.dma("sp", lambda e: e.dma_start(out=br[:], in_=b_r.partition_broadcast(128)), writes=["r_br"])
    S.op("act", lambda e: e.activation(out=s[:], in_=lg_all[:], func=AF.Sigmoid), reads=["lg_all"], writes=["r_s"])
    S.op("dve", lambda e: e.tensor_tensor(out=sel[:], in0=s[:], in1=br[:].unsqueeze(1).to_broadcast([128, n, 16]), op=ALU.add),
         reads=["r_s", "r_br"], writes=["r_sel"])
    sv = sel[:].rearrange("p t (g j) -> p t g j", j=4)
    pr = T("r_pr", [128, n, 4]); gsc = T("r_gsc", [128, n, 4])
    first = True
    for i in range(4):
        for j in range(i + 1, 4):
            S.op("dve", lambda e, i=i, j=j: e.tensor_tensor(out=pr[:], in0=sv[:, :, :, i], in1=sv[:, :, :, j], op=ALU.add),
                 reads=["r_sel"], writes=["r_pr"])
            if first:
                S.op("dve", lambda e: e.tensor_copy(out=gsc[:], in_=pr[:]), reads=["r_pr"], writes=["r_gsc"])
                first = False
            else:
                S.op("dve", lambda e: e.tensor_tensor(out=gsc[:], in0=gsc[:], in1=pr[:], op=ALU.max),
                     reads=["r_pr", "r_gsc"], writes=["r_gsc"])
    gmax = T("r_gmax", [128, n]); ing = T("r_ing", [128, n, 4])
    S.op("dve", lambda e: e.tensor_reduce(out=gmax[:], in_=gsc[:], axis=AX.X, op=ALU.max), reads=["r_gsc"], writes=["r_gmax"])
    S.op("dve", lambda e: e.tensor_tensor(out=ing[:], in0=gsc[:], in1=gmax[:].unsqueeze(2).to_broadcast([128, n, 4]), op=ALU.is_ge),
         reads=["r_gsc", "r_gmax"], writes=["r_ing"])
    cg = T("r_cg", [128, n, 4, 4]); cm = T("r_cm", [128, n, 4])
    cgv = cg[:]
    for i in range(4):
        firstj = True
        for j in range(4):
            if j == i:
                continue
            if firstj:
                S.op("dve", lambda e, i=i, j=j: e.tensor_tensor(out=cgv[:, :, :, i], in0=sv[:, :, :, j], in1=sv[:, :, :, i], op=ALU.is_gt),
                     reads=["r_sel"], writes=["r_cg"])
                firstj = False
            else:
                S.op("dve", lambda e, i=i, j=j: e.tensor_tensor(out=cm[:], in0=sv[:, :, :, j], in1=sv[:, :, :, i], op=ALU.is_gt),
                     reads=["r_sel"], writes=["r_cm"])
                S.op("dve", lambda e, i=i: e.tensor_tensor(out=cgv[:, :, :, i], in0=cgv[:, :, :, i], in1=cm[:], op=ALU.add),
                     reads=["r_cm", "r_cg"], writes=["r_cg"])
    selm = T("r_selm", [128, n, 4, 4])
    S.op("dve", lambda e: e.tensor_single_scalar(out=selm[:], in_=cg[:], scalar=1.5, op=ALU.is_lt), reads=["r_cg"], writes=["r_selm"])
    S.op("dve", lambda e: e.tensor_tensor(out=selm[:], in0=selm[:], in1=ing[:].unsqueeze(3).to_broadcast([128, n, 4, 4]), op=ALU.mult),
         reads=["r_selm", "r_ing"], writes=["r_selm"])
    comb = T("r_comb", [128, n, 16]); den = T("r_den", [128, n])
    S.op("dve", lambda e: e.tensor_tensor(out=comb[:], in0=s[:], in1=selm[:].rearrange("p t g j -> p t (g j)"), op=ALU.mult),
         reads=["r_s", "r_selm"], writes=["r_comb"])
    S.op("dve", lambda e: e.tensor_reduce(out=den[:], in_=comb[:], axis=AX.X, op=ALU.add), reads=["r_comb"], writes=["r_den"])
    S.op("dve", lambda e: e.reciprocal(out=den[:], in_=den[:]), reads=["r_den"], writes=["r_den"])
    S.op("dve", lambda e: e.tensor_tensor(out=comb[:], in0=comb[:], in1=den[:].unsqueeze(2).to_broadcast([128, n, 16]), op=ALU.mult),
         reads=["r_comb", "r_den"], writes=["r_comb"])
    return comb


def tail_phase(C, M, ybuf, w_out_d, x_src, xres_out_d, w_r, b_r, ident_d, selE_d, wg, wu, wd, st,
               final_g_d=None, nres=NRES, tiles=RES_TILES, ctx_len=CTX):
    S = C.S
    ntile = nres // 128
    xres = S.sbuf("xres", [128, 8, nres], F32, st)
    lg_all = S.sbuf("lg_all", [128, ntile, 16], F32, st)
    with contextlib.ExitStack() as st2:
        wo = S.sbuf("wo_sb", [128, 8, D], BF16, st2)
        xts = [S.sbuf(f"xt_b{i}", [128, 8, 512], F32, st2) for i in range(2)]
        S.dma("pool", lambda e: e.dma_start(out=wo[:], in_=w_out_d.rearrange("(k p) o -> p k o", p=128)), writes=["wo"])
        for ti, (a, b) in enumerate(tiles):
            n = b - a
            r = 1 if a < ctx_len else 0
            xt = xts[ti % 2]
            S.dma("sp", lambda e, a=a, b=b, n=n, xt=xt: e.dma_start(out=xt[:, :, :n], in_=x_src(a, b)), writes=[f"xtb{ti % 2}"])
            for oc in range(8):
                bk = oc % 4
                for k in range(8):
                    S.op("pe", lambda e, oc=oc, k=k, bk=bk, a=a, b=b, n=n: e.matmul(
                        C.banks[bk][:, :n], wo[:, k, oc * 128:(oc + 1) * 128], ybuf[:, k, a:b], start=(k == 0), stop=(k == 7)),
                        reads=["wo", "Y"], writes=[f"bank{bk}"])
                g = M["gate"][0][r]
                S.op("dve", lambda e, oc=oc, bk=bk, a=a, b=b, n=n, g=g, xt=xt: e.scalar_tensor_tensor(
                    out=xres[:, oc, a:b], in0=C.banks[bk][:, :n], scalar=g[:, oc:oc + 1], in1=xt[:, oc, :n],
                    op0=ALU.mult, op1=ALU.add), reads=[f"bank{bk}", f"xtb{ti % 2}"], writes=[f"xres{oc}"])
    S.barrier()
    h2 = ybuf
    with contextlib.ExitStack() as st2:
        wr = S.sbuf("wr_sb", [128, 8, 16], F32, st2)
        h2f = S.sbuf("h2f", [128, 8, 512], F32, st2)
        S.dma("sp", lambda e: e.dma_start(out=wr[:], in_=w_r.rearrange("(k p) o -> p k o", p=128)), writes=["wr"])

        def src_fn(a, b, ti):
            return xres[:, :, a:b], None

        def f32_cb(a, b, ti, when):
            if when == "pre":
                return lambda k, aa, bb: h2f[:, k, aa:bb]
            n = b - a
            for t in range(n // 128):
                tile_i = a // 128 + t
                for k in range(8):
                    S.op("pe", lambda e, k=k, t=t: e.matmul(C.banks[5][:, 0:16], h2f[:, k, t * 128:(t + 1) * 128], wr[:, k, :],
                                                            start=(k == 0), stop=(k == 7)),
                         reads=["h2f", "wr"], writes=["bank5"])
                S.op("dve", lambda e, tile_i=tile_i: e.tensor_copy(out=lg_all[:, tile_i, :], in_=C.banks[5][:, 0:16]),
                     reads=["bank5"], writes=["lg_all"])
            return None

        tmp_key = [f"xres{oc}" for oc in range(8)]

        def src_fn2(a, b, ti):
            return xres[:, :, a:b], "xres_all"
        norm_phase(C, M, 1, src_fn2, None, nres, ctx_len, h2, "h2", st2, f32_cb=f32_cb)
    S.barrier()
    ident = S.sbuf("ident", [128, 128], F32, st)
    selE = S.sbuf("selE_sb", [16, 16, 128], F32, st)
    combT = S.sbuf("combT", [16, nres], F32, st)
    with contextlib.ExitStack() as st2:
        comb = router_phase(C, lg_all, ntile, b_r, st2)
        S.dma("sp", lambda e: e.dma_start(out=ident[:], in_=ident_d), writes=["ident"])
        S.dma("sp", lambda e: e.dma_start(out=selE[:], in_=selE_d), writes=["selE"])
        for t in range(ntile):
            S.op("pe", lambda e, t=t: e.transpose(C.banks[t % 2][0:16, 0:128], comb[:, t, :], ident[:]),
                 reads=["r_comb", "ident"], writes=[f"bank{t % 2}"])
            S.op("dve", lambda e, t=t: e.tensor_copy(out=combT[:, t * 128:(t + 1) * 128], in_=C.banks[t % 2][0:16, 0:128]),
                 reads=[f"bank{t % 2}"], writes=["combT"])
    S.barrier()
    with contextlib.ExitStack() as st2:
        moe_phase(C, xres, h2, combT, selE, lambda r: M["gate"][1][r], wg, wu, wd, st2, nres=nres, tiles=tiles, ctx_len=ctx_len)
    S.barrier()
    if final_g_d is None and isinstance(xres_out_d, tuple):
        x1nat, pp = xres_out_d
        for k in range(8):
            if pp == 0:
                S.dma("sp", lambda e, k=k: e.dma_start(out=x1nat[k][:, 0:CTX], in_=xres[:, k, 0:CTX]), reads=[f"xres{k}"], writes=["x1nat"])
            S.dma("sp", lambda e, k=k: e.dma_start(out=x1nat[k][:, CTX + pp * OWN:CTX + (pp + 1) * OWN], in_=xres[:, k, CTX:]),
                  reads=[f"xres{k}"], writes=["x1nat"])
    elif final_g_d is None:
        for k in range(8):
            S.dma("sp", lambda e, k=k: e.dma_start(out=xres_out_d[k], in_=xres[:, k, :]), reads=[f"xres{k}"], is_out=True)
    else:
        with contextlib.ExitStack() as st2:
            fg = S.sbuf("fg", [128, 8], F32, st2)
            zsh = S.sbuf("zsh", [128, 8], F32, st2)
            dummy = S.sbuf("fdummy", [128, 8, 512], BF16, st2)
            fo = [S.sbuf(f"fo{i}", [128, 8, 512], F32, st2) for i in range(2)]
            S.dma("sp", lambda e: e.dma_start(out=fg[:], in_=final_g_d), writes=["fg"])
            S.op("dve", lambda e: e.memset(zsh[:], 0.0), writes=["zsh"])
            S.barrier()
            Mf = {"gs": [[fg[:], fg[:]]], "sh": [[zsh[:], zsh[:]]]}
            xo = xres_out_d.rearrange("k p t -> p k t")

            def src_fn3(a, b, ti):
                return xres[:, :, a:b], "xres_all"

            def f32_cb3(a, b, ti, when):
                if when == "pre":
                    return lambda k, aa, bb: fo[ti % 2][:, k, aa:bb]
                S.dma("sp", lambda e: e.dma_start(out=xo[:, :, a:b], in_=fo[ti % 2][:, :, :b - a]), reads=[f"fo{ti % 2}"], is_out=True)
                return None
            tmpn = {"sq": S.sbuf("f_sq", [128, 8, 512], F32, st2), "rs": S.sbuf("f_rs", [128, 512], F32, st2),
                    "t": [S.sbuf("f_t0", [128, 512], F32, st2), S.sbuf("f_t1", [128, 512], F32, st2)]}
            a = 0
            ti = 0
            while a < nres:
                b = min(a + 512, nres)
                tmpn["outkey"] = "fdummy"
                tmpn["f32key"] = f"fo{ti % 2}"
                f32o = f32_cb3(a, b, ti, "pre")
                norm_mod_tile(C, xres[:, :, a:b], "xres_all", b - a, lambda k, aa, bb: dummy[:, k, aa:bb],
                              lambda r: fg[:], lambda r: zsh[:], tmpn, 7, [(0, b - a, 0)], f32_out=f32o)
                f32_cb3(a, b, ti, "post")
                a = b
                ti += 1
    return xres


class _Stop(Exception):
    pass


def build_l0(debug=False, stop=None):
    nc = bass.Bass("TRN2", target_bir_lowering=False)
    C = Ctx(nc)
    _build_l0_body(nc, C, debug, stop, IO(nc))
    C.S.finish()
    return nc


def _build_l0_body(nc, C, debug, stop, io, write_ctx=True):
    xT = io.inp("xT", [8, 128, NTOK])
    modt = io.inp("mod", [128, 2, 6, 8])
    ng = io.inp("ng", [128, 2, 8])
    w_in = io.inp("w_in", [D, 3328])
    ropeC = io.inp("ropeC", [128, NLAT])
    ropeS = io.inp("ropeS", [128, NLAT])
    w_out = io.inp("w_out", [D, D])
    sink = io.inp("sink", [8])
    rpbT = io.inp("rpbT", [128, 7, 8, 128])
    amask_d = io.inp("amask", [128, 4, 128])
    vint_d = io.inp("vint", [128, 5, 128])
    vb_d = io.inp("vb", [128, 4, 6, 128])
    w_r = io.inp("w_r", [D, 16])
    b_r = io.inp("b_r", [16])
    if stop is None or stop == "full":
        wg = io.inp("wg", [NE, D, DE])
        wu = io.inp("wu", [NE, D, DE])
        wd = io.inp("wd", [NE, DE, D])
    else:
        wg = wu = wd = None
    ident_d = io.inp("ident", [128, 128])
    selE_d = io.inp("selE", [16, 16, 128])
    x1T = io.out("x1T", [8, 128, NRES])
    dbg = {}
    if debug:
        dbg["hx"] = io.out("d_hx", [128, 8, NTOK], BF16)
        dbg["y"] = io.out("d_y", [128, 8, NRES], BF16)

    S = C.S
    st0 = contextlib.ExitStack()
    M = load_mod(C, modt, ng, st0)
    xv = xT.rearrange("k p t -> p k t")
    hy = S.sbuf("hy", [128, 8, NTOK], BF16, st0)
    hx = hy
    ybuf = hy[:, :, 0:NRES]
    with contextlib.ExitStack() as stA:
        xts = [S.sbuf(f"xa{i}", [128, 8, 512], F32, stA) for i in range(2)]

        def src_fn(a, b, ti):
            xt = xts[ti % 2]
            S.dma("sp", lambda e: e.dma_start(out=xt[:, :, :b - a], in_=xv[:, :, a:b]), writes=[f"xa{ti % 2}"])
            return xt[:, :, :b - a], f"xa{ti % 2}"
        norm_phase(C, M, 0, src_fn, None, NTOK, CTX, hx, "hx", stA)
    S.barrier()
    if stop == "A0":
        S.kill()
    if debug:
        for k in range(8):
            S.dma("sp", lambda e, k=k: e.dma_start(out=dbg["hx"][:, k, :], in_=hx[:, k, :]), reads=["hx"], is_out=True)
        S.barrier()
    if stop == "A":
        S.kill()

    with contextlib.ExitStack() as stQ:
        QA = S.sbuf("QA", [128, 4, NRES], BF16, stQ)
        QB = S.sbuf("QB", [128, 4, NRES], BF16, stQ)
        KAB = S.sbuf("KAB", [128, 2, NTOK], BF16, stQ)
        BK = S.sbuf("BK", [128, 4, NTOK], BF16, stQ)
        VA2 = S.sbuf("VA2", [128, 22, 256], BF16, stQ)
        VB = S.sbuf("VB", [128, 22, 512], BF16, stQ)
        if True:
            with contextlib.ExitStack() as stB:
                wts = [S.sbuf(f"wt{i}", [128, 8, 512], BF16, stB) for i in range(2)]
                rc = S.sbuf("rc", [128, NLAT], F32, stB)
                rs_ = S.sbuf("rs", [128, NLAT], F32, stB)
                t1 = [S.sbuf(f"t1_{i}", [128, 512], F32, stB) for i in range(2)]
                t2 = [S.sbuf(f"t2_{i}", [128, 512], F32, stB) for i in range(2)]
                S.dma("sp", lambda e: e.dma_start(out=rc[:], in_=ropeC), writes=["rc"])
                S.dma("sp", lambda e: e.dma_start(out=rs_[:], in_=ropeS), writes=["rs"])
                wv = w_in.rearrange("(k p) o -> p k o", p=128)
                ngrp = [0]

                def load_w(c0, ncol):
                    wi = ngrp[0] % 2
                    ngrp[0] += 1
                    S.dma("pool", lambda e: e.dma_start(out=wts[wi][:, :, :ncol], in_=wv[:, :, c0:c0 + ncol]), writes=[f"wt{wi}"])
                    return wts[wi], f"wt{wi}"

                cnt = [0]

                def fm_block(wt, wkey, j, toks, out_fn, okey):
                    for (a, b, da) in toks:
                        n = b - a
                        bk = cnt[0] % 4
                        cnt[0] += 1
                        for k in range(8):
                            S.op("pe", lambda e, k=k: e.matmul(C.banks[bk][:, :n], wt[:, k, j * 128:(j + 1) * 128], hx[:, k, a:b],
                                                               start=(k == 0), stop=(k == 7)), reads=[wkey, "hx"], writes=[f"bank{bk}"])
                        S.op("act", lambda e: e.activation(out=out_fn(da, da + n), in_=C.banks[bk][:, :n], func=AF.Copy),
                             reads=[f"bank{bk}"], writes=[okey])

                def rope_block(wt, wkey, j, jp, toks, out_fn, okey):
                    for (a, b, da) in toks:
                        n = b - a
                        bk = (cnt[0] % 2) * 2
                        ti = cnt[0] % 2
                        cnt[0] += 1
                        la = a - CTX
                        for k in range(8):
                            S.op("pe", lambda e, k=k: e.matmul(C.banks[bk][:, :n], wt[:, k, j * 128:(j + 1) * 128], hx[:, k, a:b],
                                                               start=(k == 0), stop=(k == 7)), reads=[wkey, "hx"], writes=[f"bank{bk}"])
                        for k in range(8):
                            S.op("pe", lambda e, k=k: e.matmul(C.banks[bk + 1][:, :n], wt[:, k, jp * 128:(jp + 1) * 128], hx[:, k, a:b],
                                                               start=(k == 0), stop=(k == 7)), reads=[wkey, "hx"], writes=[f"bank{bk + 1}"])
                        S.op("dve", lambda e: e.tensor_tensor(out=t1[ti][:, :n], in0=C.banks[bk][:, :n], in1=rc[:, la:la + n], op=ALU.mult),
                             reads=[f"bank{bk}", "rc"], writes=[f"t1_{ti}"])
                        S.op("dve", lambda e: e.tensor_tensor(out=t2[ti][:, :n], in0=C.banks[bk + 1][:, :n], in1=rs_[:, la:la + n], op=ALU.mult),
                             reads=[f"bank{bk + 1}", "rs"], writes=[f"t2_{ti}"])
                        S.op("pool", lambda e: e.tensor_tensor(out=out_fn(da, da + n), in0=t1[ti][:, :n], in1=t2[ti][:, :n], op=ALU.add),
                             reads=[f"t1_{ti}", f"t2_{ti}"], writes=[okey])

                own_toks = [(OWN0 + 512 * i, OWN0 + 512 * (i + 1), CTX + 512 * i) for i in range(4)]
                ctx_toks = [(0, CTX, 0)]
                lat_toks = [(CTX + 512 * i, CTX + 512 * (i + 1), CTX + 512 * i) for i in range(5)]
                wA, kA = load_w(0, 512)
                wP, kP = load_w(512, 512)
                for j in range(4):
                    fm_block(wA, kA, j, ctx_toks, lambda a, b, j=j: QA[:, j, a:b], "QA")
                    for (a, b, da) in own_toks:
                        n = b - a
                        bk = (cnt[0] % 2) * 2
                        ti = cnt[0] % 2
                        cnt[0] += 1
                        la = a - CTX
                        for k in range(8):
                            S.op("pe", lambda e, k=k: e.matmul(C.banks[bk][:, :n], wA[:, k, j * 128:(j + 1) * 128], hx[:, k, a:b],
                                                               start=(k == 0), stop=(k == 7)), reads=[kA, "hx"], writes=[f"bank{bk}"])
                        for k in range(8):
                            S.op("pe", lambda e, k=k: e.matmul(C.banks[bk + 1][:, :n], wP[:, k, j * 128:(j + 1) * 128], hx[:, k, a:b],
                                                               start=(k == 0), stop=(k == 7)), reads=[kP, "hx"], writes=[f"bank{bk + 1}"])
                        S.op("dve", lambda e: e.tensor_tensor(out=t1[ti][:, :n], in0=C.banks[bk][:, :n], in1=rc[:, la:la + n], op=ALU.mult),
                             reads=[f"bank{bk}", "rc"], writes=[f"t1_{ti}"])
                        S.op("dve", lambda e: e.tensor_tensor(out=t2[ti][:, :n], in0=C.banks[bk + 1][:, :n], in1=rs_[:, la:la + n], op=ALU.mult),
                             reads=[f"bank{bk + 1}", "rs"], writes=[f"t2_{ti}"])
                        S.op("pool", lambda e, da=da, n=n: e.tensor_tensor(out=QA[:, j, da:da + n], in0=t1[ti][:, :n], in1=t2[ti][:, :n], op=ALU.add),
                             reads=[f"t1_{ti}", f"t2_{ti}"], writes=["QA"])
                wK, kK = load_w(1024, 512)
                for g in range(2):
                    fm_block(wK, kK, 2 * g, ctx_toks, lambda a, b, g=g: KAB[:, g, a:b], "KAB")
                    rope_block(wK, kK, 2 * g, 2 * g + 1, lat_toks, lambda a, b, g=g: KAB[:, g, a:b], "KAB")
                wq, kq = load_w(1536, 512)
                for j in range(4):
                    fm_block(wq, kq, j, ctx_toks + own_toks, lambda a, b, j=j: QB[:, j, a:b], "QB")
                wk_, kk_ = load_w(2048, 512)
                all_toks = ctx_toks + lat_toks
                for j in range(4):
                    fm_block(wk_, kk_, j, all_toks, lambda a, b, j=j: BK[:, j, a:b], "BK")
                wvb, kvb = load_w(2560, 512)
                wva, kva = load_w(3072, 256)
                for t in range(22):
                    bk = cnt[0] % 4
                    cnt[0] += 1
                    for k in range(8):
                        S.op("pe", lambda e, k=k: e.matmul(C.banks[bk][:, :], hx[:, k, t * 128:(t + 1) * 128], wvb[:, k, :],
                                                           start=(k == 0), stop=(k == 7)), reads=[kvb, "hx"], writes=[f"bank{bk}"])
                    S.op("act", lambda e: e.activation(out=VB[:, t, :], in_=C.banks[bk][:, :], func=AF.Copy),
                         reads=[f"bank{bk}"], writes=["VB"])
                    bk = cnt[0] % 4
                    cnt[0] += 1
                    for k in range(8):
                        S.op("pe", lambda e, k=k: e.matmul(C.banks[bk][:, :256], hx[:, k, t * 128:(t + 1) * 128], wva[:, k, :256],
                                                           start=(k == 0), stop=(k == 7)), reads=[kva, "hx"], writes=[f"bank{bk}"])
                    S.op("dve", lambda e: e.tensor_copy(out=VA2[:, t, :], in_=C.banks[bk][:, :256]),
                         reads=[f"bank{bk}"], writes=["VA2"])
            S.barrier()
        if stop == "B":
            S.kill()
        with contextlib.ExitStack() as stC:
            esk = S.sbuf("esk", [128, 8], F32, stC)
            am = S.sbuf("am", [128, 4, 128], BF16, stC)
            Tm = S.sbuf("Tm", [128, 7, 8, 128], BF16, stC)
            TV = S.sbuf("TV", [128, 5, 8, 128], BF16, stC)
            vint = S.sbuf("vint", [128, 5, 128], BF16, stC)
            vbm = S.sbuf("vbm", [128, 4, 6, 128], BF16, stC)
            pts = [S.sbuf(f"pt{i}", [128, 1024], BF16, stC) for i in range(2)]
            dn = S.sbuf("dn", [128, 1024], F32, stC)
            S.dma("sp", lambda e: e.dma_start(out=esk[:], in_=sink.partition_broadcast(128)), writes=["esk"])
            S.op("act", lambda e: e.activation(out=esk[:], in_=esk[:], func=AF.Exp), reads=["esk"], writes=["esk"])
            S.dma("pool", lambda e: e.dma_start(out=am[:], in_=amask_d), writes=["am"])
            S.dma("pool", lambda e: e.dma_start(out=vint[:], in_=vint_d), writes=["vint"])
            S.dma("pool", lambda e: e.dma_start(out=vbm[:], in_=vb_d), writes=["vbm"])
            with contextlib.ExitStack() as stT:
                stg = S.sbuf("rp_stage", [128, 8, 128], F32, stT)
                for oi in range(7):
                    S.dma("sp", lambda e, oi=oi: e.dma_start(out=stg[:], in_=rpbT[:, oi]), writes=["rp_stage"])
                    S.op("act", lambda e, oi=oi: e.activation(out=Tm[:, oi], in_=stg[:], func=AF.Exp), reads=["rp_stage"], writes=["Tm"])
                for oi in range(5):
                    S.op("dve", lambda e, oi=oi: e.tensor_tensor(out=TV[:, oi], in0=Tm[:, oi + 1],
                                                                 in1=vint[:, oi, :].unsqueeze(1).to_broadcast([128, 8, 128]), op=ALU.mult),
                         reads=["Tm", "vint"], writes=["TV"])
            S.barrier()
            if stop == "C0":
                S.kill()
            ac = [0]

            import os
            ATT_STAGE = int(os.environ.get("ATT_STAGE", "9"))
            ATT_N = int(os.environ.get("ATT_N", "999"))

            def attnA(q0, klist):
                for g in range(2):
                    if ac[0] >= ATT_N:
                        return
                    it = ac[0]
                    ac[0] += 1
                    nb, db = 4 + it % 2, 6 + it % 2
                    def emit_SA(idx):
                        tk = klist[idx][0]
                        sb = idx % 2
                        for s_ in range(4):
                            hh = (0, 2, 1, 3)[s_]
                            h = 4 * g + hh
                            c, off = h // 2, (h % 2) * 64
                            bnk = 2 * sb + s_ // 2
                            S.op("pe", lambda e, s_=s_, c=c, off=off, tk=tk, bnk=bnk: e.matmul(
                                C.banks[bnk][:, (s_ % 2) * 128:(s_ % 2 + 1) * 128], KAB[off:off + 64, g, tk * 128:(tk + 1) * 128],
                                QA[off:off + 64, c, q0:q0 + 128], start=True, stop=True),
                                reads=["KAB", "QA"], writes=[f"SA{sb}"])
                    emit_SA(0)
                    for idx, (tk, mk) in enumerate(klist):
                        sb = idx % 2
                        pt = pts[idx % 2]
                        if idx + 1 < len(klist):
                            emit_SA(idx + 1)
                        S.op("act", lambda e, sb=sb, pt=pt: e.activation(
                            out=pt[:, 0:512].rearrange("p (a b) -> p a b", a=2), in_=C.ps[:, 2 * sb:2 * sb + 2, 0:256], func=AF.Exp, scale=SCALE),
                            reads=[f"SA{sb}"], writes=[f"pt{idx % 2}"])
                        if mk is not None:
                            S.op("pool", lambda e, pt=pt, mk=mk: e.tensor_tensor(
                                out=pt[:, 0:512].rearrange("p (h q) -> p h q", h=4), in0=pt[:, 0:512].rearrange("p (h q) -> p h q", h=4),
                                in1=mk.unsqueeze(1).to_broadcast([128, 4, 128]), op=ALU.mult),
                                reads=[f"pt{idx % 2}", "am"], writes=[f"pt{idx % 2}"])
                        last = idx == len(klist) - 1
                        if ATT_STAGE < 2:
                            continue
                        S.op("pe", lambda e, pt=pt, tk=tk, idx=idx, last=last, nb=nb: e.matmul(
                            C.banks[nb][:, :], VA2[:, tk, g * 128:(g + 1) * 128], pt[:, 0:512], start=(idx == 0), stop=last),
                            reads=["VA2", f"pt{idx % 2}"], writes=[f"bank{nb}"])
                        S.op("pe", lambda e, pt=pt, idx=idx, last=last, db=db: e.matmul(
                            C.banks[db][:, :], C.ones_b[:], pt[:, 0:512], start=(idx == 0), stop=last),
                            reads=["ones_b", f"pt{idx % 2}"], writes=[f"bank{db}"])
                    if ATT_STAGE < 3:
                        continue
                    for s_ in range(4):
                        hh = s_
                        h = 4 * g + (0, 2, 1, 3)[s_]
                        S.op("dve", lambda e, hh=hh, h=h, db=db: e.tensor_scalar(
                            out=dn[:, hh * 128:(hh + 1) * 128], in0=C.banks[db][:, hh * 128:(hh + 1) * 128],
                            scalar1=esk[:, h:h + 1], scalar2=None, op0=ALU.add), reads=[f"bank{db}", "esk"], writes=["dn"])
                    S.op("dve", lambda e: e.reciprocal(out=dn[:, 0:512], in_=dn[:, 0:512]), reads=["dn"], writes=["dn"])
                    for s_ in range(4):
                        hh = s_
                        h = 4 * g + (0, 2, 1, 3)[s_]
                        c, off = h // 2, (h % 2) * 64
                        S.op("dve", lambda e, hh=hh, c=c, off=off, nb=nb: e.tensor_tensor(
                            out=ybuf[off:off + 64, c, q0:q0 + 128], in0=C.banks[nb][off:off + 64, hh * 128:(hh + 1) * 128],
                            in1=dn[off:off + 64, hh * 128:(hh + 1) * 128], op=ALU.mult),
                            reads=[f"bank{nb}", "dn"], writes=["Y"])

            for qt in range(2):
                attnA(qt * 128, [(0, None), (1, None)])
            for jl in range(16):
                kl = [(0, None), (1, None)]
                for o in (-1, 0, 1):
                    tk = 4 + jl + o
                    mk = None
                    if o == -1:
                        mk = am[:, 2, :] if jl == 0 else am[:, 0, :]
                    elif o == 1:
                        mk = am[:, 3, :] if jl == 15 else am[:, 1, :]
                    kl.append((tk, mk))
                attnA(CTX + jl * 128, kl)
            S.barrier()
            if stop == "C":
                S.kill()
            if debug:
                pass

            bc_ = [0]

            def attnB(q0, klist):
                S2 = [C.ps[:, 0:2, :].rearrange("p a b -> p (a b)"), C.ps[:, 2:4, :].rearrange("p a b -> p (a b)")]
                NUM = C.ps[:, 4:6, :].rearrange("p a b -> p (a b)")
                DEN = C.ps[:, 6:8, :].rearrange("p a b -> p (a b)")
                BST = int(os.environ.get("ATTB_STAGE", "9"))
                bc_[0] += 1
                if bc_[0] > int(os.environ.get("ATTB_N", "999")):
                    return
                def emit_SB(idx):
                    tk = klist[idx][0]
                    sb = idx % 2
                    for s_ in range(8):
                        h = s_
                        hd = HORD[s_]
                        c, off = hd // 2, (hd % 2) * 64
                        S.op("pe", lambda e, h=h, c=c, off=off, tk=tk, sb=sb: e.matmul(
                            S2[sb][:, h * 128:(h + 1) * 128], BK[off:off + 64, c, tk * 128:(tk + 1) * 128],
                            QB[off:off + 64, c, q0:q0 + 128], start=True, stop=True), reads=["BK", "QB"], writes=[f"S2_{sb}"])
                emit_SB(0)
                for idx, (tk, mks) in enumerate(klist):
                    sb = idx % 2
                    pt = pts[idx % 2]
                    sk = f"S2_{sb}"
                    if idx + 1 < len(klist):
                        emit_SB(idx + 1)
                    for hf in range(2):
                        S.op("act", lambda e, hf=hf, sb=sb, pt=pt: e.activation(
                            out=pt[:, hf * 512:(hf + 1) * 512], in_=S2[sb][:, hf * 512:(hf + 1) * 512], func=AF.Exp, scale=SCALE),
                            reads=[sk], writes=[f"pt{idx % 2}"])
                    for mk, full in (mks if BST >= 2 else []):
                        in1 = mk if full else mk.unsqueeze(1).to_broadcast([128, 8, 128])
                        S.op("pool", lambda e, pt=pt, in1=in1: e.tensor_tensor(
                            out=pt[:].rearrange("p (h q) -> p h q", h=8), in0=pt[:].rearrange("p (h q) -> p h q", h=8),
                            in1=in1, op=ALU.mult), reads=[f"pt{idx % 2}", "TV", "Tm", "vbm"], writes=[f"pt{idx % 2}"])
                    last = idx == len(klist) - 1
                    if BST < 3:
                        continue
                    for h in range(8):
                        c = HORD[h] // 2
                        S.op("pe", lambda e, h=h, c=c, pt=pt, tk=tk, idx=idx, last=last: e.matmul(
                            NUM[:, h * 128:(h + 1) * 128], VB[:, tk, c * 128:(c + 1) * 128], pt[:, h * 128:(h + 1) * 128],
                            start=(idx == 0 and h % 4 == 0), stop=(last and h % 4 == 3), skip_group_check=True),
                            reads=["VB", f"pt{idx % 2}"], writes=["NUM"])
                    for hf in range(2):
                        S.op("pe", lambda e, hf=hf, pt=pt, idx=idx, last=last: e.matmul(
                            DEN[:, hf * 512:(hf + 1) * 512], C.ones_b[:], pt[:, hf * 512:(hf + 1) * 512],
                            start=(idx == 0), stop=last), reads=["ones_b", f"pt{idx % 2}"], writes=["DEN"])
                if BST < 4:
                    return
                S.op("dve", lambda e: e.reciprocal(out=dn[:], in_=DEN), reads=["DEN"], writes=["dn"])
                for h in range(8):
                    c, off = HORD[h] // 2, (HORD[h] % 2) * 64
                    S.op("dve", lambda e, h=h, c=c, off=off: e.tensor_tensor(
                        out=ybuf[off:off + 64, 4 + c, q0:q0 + 128], in0=NUM[off:off + 64, h * 128:(h + 1) * 128],
                        in1=dn[off:off + 64, h * 128:(h + 1) * 128], op=ALU.mult), reads=["NUM", "dn"], writes=["Y"])

            for qt in range(2):
                attnB(qt * 128, [(0, []), (1, [])])
            for jl in range(16):
                kl = [(0, []), (1, [])]
                offs = b_offsets(jl)
                for oi, o in enumerate(offs):
                    tk = 4 + jl + o
                    if jl in (0, 1, 14, 15):
                        ci = (0, 1, 14, 15).index(jl)
                        mks = [(Tm[:, o + 3], True), (vbm[:, ci, oi, :], False)]
                    else:
                        mks = [(TV[:, o + 2], True)]
                    kl.append((tk, mks))
                attnB(CTX + jl * 128, kl)
            S.barrier()
    if debug:
        for k in range(8):
            S.dma("sp", lambda e, k=k: e.dma_start(out=dbg["y"][:, k, :], in_=ybuf[:, k, :]), reads=["Y"], is_out=True)
        S.barrier()
    if stop == "Y":
        S.kill()

    def x_src(a, b):
        if a < CTX:
            return xv[:, :, a:b]
        return xv[:, :, a + HALO:b + HALO]
    tail_phase(C, M, ybuf, w_out, x_src, x1T, w_r, b_r, ident_d, selE_d, wg, wu, wd, st0)
    S.barrier()
    st0.close()


NFFT = 2 * SEQ
_HC = {}


def hy_consts():
    if _HC:
        return _HC
    import ml_dtypes
    bf = ml_dtypes.bfloat16
    L = SEQ
    t = np.arange(L, dtype=np.float32)
    tn = t / np.float32(L - 1)
    bands = np.linspace(1e-4, 15, 16, dtype=np.float32)
    ang = (np.float32(2.0 * math.pi) * t[:, None] * bands[None] / np.float32(L)).astype(np.float32)
    feats = np.concatenate([tn[:, None], np.cos(ang), np.sin(ang)], axis=-1).astype(np.float32)
    _HC["featsT"] = np.ascontiguousarray(feats.T)
    deltas = np.abs(np.linspace(math.log(1e-2) / 1.5, math.log(1e-2) / 0.3, 512, dtype=np.float32))
    decay = np.exp(-tn[:, None] * deltas[None]).astype(np.float32)
    _HC["decay"] = np.ascontiguousarray(decay.reshape(32, 128, 512).transpose(1, 0, 2))
    k = np.arange(NFFT, dtype=np.float64)
    ctab = np.cos(2 * np.pi * k / NFFT).astype(np.float32)
    stab = np.sin(2 * np.pi * k / NFFT).astype(np.float32)
    a = np.arange(L, dtype=np.int64)
    idx = (a[:, None] * a[None, :]) % NFFT
    Fc = ctab[idx]
    Fs = stab[idx]
    sgn = np.where(a % 2 == 0, 1.0, -1.0).astype(np.float32)
    Fs_f = Fs.copy()
    Fs_f[:, 0] = sgn

    def blk(Mx):
        return np.ascontiguousarray(Mx.reshape(32, 128, 32, 128).transpose(2, 1, 0, 3)).astype(bf)
    _HC["FcB"] = blk(Fc)
    _HC["FsB"] = blk(Fs_f)
    _HC["FsBi"] = blk(np.ascontiguousarray(Fs_f.T))
    cv = np.ones((128, 4), np.float32)
    cv[0, 0] = 0.5
    cv[0, 1] = 0.0
    cv[:, 2] = 0.0
    cv[0, 2] = 0.5
    _HC["cv"] = cv
    return _HC


PI = math.pi


def sin_rr(C, out_ap, x, xkey, out_key, ti_, tf_, tc_, tag):
    S = C.S
    ki, kf, kc = f"rri{tag}", f"rrf{tag}", f"rrc{tag}"
    S.op("dve", lambda e: e.tensor_scalar(out=ti_, in0=x, scalar1=1.0 / (2.0 * PI), scalar2=None, op0=ALU.mult), reads=[xkey], writes=[ki])
    S.op("dve", lambda e: e.tensor_copy(out=tf_, in_=ti_), reads=[ki], writes=[kf])
    S.op("dve", lambda e: e.scalar_tensor_tensor(out=x, in0=tf_, scalar=-2.0 * PI, in1=x, op0=ALU.mult, op1=ALU.add), reads=[kf, xkey], writes=[xkey])
    S.op("dve", lambda e: e.tensor_single_scalar(out=tc_, in_=x, scalar=PI, op=ALU.is_gt), reads=[xkey], writes=[kc])
    S.op("dve", lambda e: e.scalar_tensor_tensor(out=x, in0=tc_, scalar=-2.0 * PI, in1=x, op0=ALU.mult, op1=ALU.add), reads=[kc, xkey], writes=[xkey])
    S.op("dve", lambda e: e.tensor_single_scalar(out=tc_, in_=x, scalar=-PI, op=ALU.is_lt), reads=[xkey], writes=[kc])
    S.op("dve", lambda e: e.scalar_tensor_tensor(out=x, in0=tc_, scalar=2.0 * PI, in1=x, op0=ALU.mult, op1=ALU.add), reads=[kc, xkey], writes=[xkey])
    S.op("act", lambda e: e.activation(out=out_ap, in_=x, func=AF.Sin), reads=[xkey], writes=[out_key])


def build_l1k():
    nc = bass.Bass("TRN2", target_bir_lowering=False)
    C = Ctx(nc)
    l1k_body(nc, C, IO(nc))
    C.S.finish()
    return nc


def l1k_body(nc, C, io, NCH=64):
    W4, W2 = 4 * NCH, 2 * NCH
    featsT = io.inp("featsT", [33, SEQ])
    w1 = io.inp("w1", [33, 64]); w2 = io.inp("w2", [64, 64]); w3s = io.inp("w3s", [64, W4])
    pvec = io.inp("pvec", [64, 4])
    b3s = io.inp("b3s", [W4])
    decay = io.inp("decay", [128, 32, NCH])
    FcB = io.inp("FcB", [32, 128, 32, 128], BF16)
    FsB = io.inp("FsB", [32, 128, 32, 128], BF16)
    cv_d = io.inp("cv", [128, 4])
    ktab = io.out("ktab", [32, 128, 3, W2])
    S = C.S
    st = contextlib.ExitStack()
    st2 = contextlib.ExitStack()
    ksum = S.sbuf("ksum", [128, 32, W2], BF16, st); kdif = S.sbuf("kdif", [128, 32, W2], BF16, st)
    cv = S.sbuf("cvs", [128, 4], F32, st)
    C.negpi = S.sbuf("negpi", [128, 1], F32, st2)
    S.op("dve", lambda e: e.memset(C.negpi[:], -PI), writes=["negpi"])
    ft = S.sbuf("ft", [33, SEQ], F32, st2)
    w1t = S.sbuf("w1t", [33, 64], F32, st2); w2t = S.sbuf("w2t", [64, 64], F32, st2); w3t = S.sbuf("w3t", [64, W4], F32, st2)
    pv = S.sbuf("pv", [64, 4], F32, st2); b3bc = S.sbuf("b3bc", [128, W4], F32, st2)
    dec = S.sbuf("dec", [128, 32, NCH], F32, st2)
    h1 = S.sbuf("h1", [64, SEQ], F32, st2); h2 = S.sbuf("h2", [64, SEQ], F32, st2)
    hraw = S.sbuf("hraw", [128, 32, W4], F32, st2)
    for (dst, src, key) in ((ft, featsT, "ft"), (w1t, w1, "w1t"), (w2t, w2, "w2t"), (w3t, w3s, "w3t"), (pv, pvec, "pv"),
                            (dec, decay, "dec"), (cv, cv_d, "cv")):
        S.dma("sp", lambda e, dst=dst, src=src: e.dma_start(out=dst[:], in_=src), writes=[key])
    S.dma("sp", lambda e: e.dma_start(out=b3bc[:], in_=b3s.partition_broadcast(128)), writes=["b3bc"])
    pre = [S.sbuf(f"pre{i}", [64, 512], F32, st2) for i in range(2)]
    rr_i = [S.sbuf(f"rr_i{i}", [64, 512], mybir.dt.int32, st2) for i in range(2)]
    rr_f = [S.sbuf(f"rr_f{i}", [64, 512], F32, st2) for i in range(2)]
    rr_c = [S.sbuf(f"rr_c{i}", [64, 512], F32, st2) for i in range(2)]
    for layer, (wt, wk, src, skey, dst, dkey, K_) in enumerate(((w1t, "w1t", ft, "ft", h1, "h1", 33), (w2t, "w2t", h1, "h1", h2, "h2", 64))):
        for tt in range(8):
            bk = tt % 2
            S.op("pe", lambda e, tt=tt, bk=bk: e.matmul(C.banks[bk][0:64, :], wt[0:K_, :], src[0:K_, tt * 512:(tt + 1) * 512], start=True, stop=True),
                 reads=[wk, skey], writes=[f"bank{bk}"])
            S.op("dve", lambda e, bk=bk: e.tensor_scalar(out=pre[bk][:], in0=C.banks[bk][0:64, :], scalar1=pv[:, 2 * layer:2 * layer + 1],
                                                        scalar2=pv[:, 2 * layer + 1:2 * layer + 2], op0=ALU.add, op1=ALU.mult),
                 reads=[f"bank{bk}", "pv"], writes=[f"pre{bk}"])
            sin_rr(C, dst[:, tt * 512:(tt + 1) * 512], pre[bk][:], f"pre{bk}", dkey, rr_i[bk][:], rr_f[bk][:], rr_c[bk][:], bk)
    ab = [S.sbuf(f"habs{i}", [128, W4], F32, st2) for i in range(2)]
    for ti in range(32):
        bk = ti % 2
        S.op("pe", lambda e, ti=ti, bk=bk: e.matmul(C.banks[bk][:, 0:W4], h2[0:64, ti * 128:(ti + 1) * 128], w3t[0:64, :], start=True, stop=True),
             reads=["h2", "w3t"], writes=[f"bank{bk}"])
        S.op("dve", lambda e, ti=ti, bk=bk: e.tensor_tensor(out=hraw[:, ti, :], in0=C.banks[bk][:, 0:W4], in1=b3bc[:], op=ALU.add),
             reads=[f"bank{bk}", "b3bc"], writes=["hraw"])
        S.op("dve", lambda e, ti=ti: e.tensor_tensor(out=hraw[:, ti, :].rearrange("p (a c) -> p a c", a=4), in0=hraw[:, ti, :].rearrange("p (a c) -> p a c", a=4),
                                                  in1=dec[:, ti, :].unsqueeze(1).to_broadcast([128, 4, NCH]), op=ALU.mult),
             reads=["hraw", "dec"], writes=["hraw"])
        S.op("act", lambda e, ti=ti, bk=bk: e.activation(out=ab[bk][:], in_=hraw[:, ti, :], func=AF.Abs),
             reads=["hraw"], writes=[f"habs{bk}"])
        S.op("pe", lambda e, ti=ti, bk=bk: e.matmul(C.banks[2][:, 0:W4], C.ones_f[:], ab[bk][:], start=(ti == 0), stop=(ti == 31)),
             reads=[f"habs{bk}", "ones_f"], writes=["bank2"])
    scl = S.sbuf("scl", [128, W2], F32, st2)
    S.op("dve", lambda e: e.tensor_copy(out=scl[:], in_=C.banks[2][:, 0:W2]), reads=["bank2"], writes=["scl"])
    S.op("dve", lambda e: e.tensor_tensor(out=scl[:], in0=scl[:], in1=C.banks[2][:, W2:W4], op=ALU.add), reads=["bank2", "scl"], writes=["scl"])
    S.op("dve", lambda e: e.tensor_scalar(out=scl[:], in0=scl[:], scalar1=EPS, scalar2=None, op0=ALU.add), reads=["scl"], writes=["scl"])
    S.op("dve", lambda e: e.reciprocal(out=scl[:], in_=scl[:]), reads=["scl"], writes=["scl"])
    hn = S.sbuf("hn", [128, W4], F32, st2)
    for ti in range(32):
        S.op("dve", lambda e, ti=ti: e.tensor_tensor(out=hn[:].rearrange("p (a c) -> p a c", a=2), in0=hraw[:, ti, :].rearrange("p (a c) -> p a c", a=2),
                                                  in1=scl[:].unsqueeze(1).to_broadcast([128, 2, W2]), op=ALU.mult),
             reads=["hraw", "scl"], writes=["hn"])
        if ti == 0:
            S.op("dve", lambda e: e.tensor_scalar(out=hn[:, W2:W4], in0=hn[:, W2:W4], scalar1=cv[:, 1:2], scalar2=None, op0=ALU.mult),
                 reads=["hn", "cv"], writes=["hn"])
        S.op("dve", lambda e, ti=ti: e.tensor_tensor(out=ksum[:, ti, :], in0=hn[:, 0:W2], in1=hn[:, W2:W4], op=ALU.add), reads=["hn"], writes=["ksum"])
        S.op("dve", lambda e, ti=ti: e.tensor_tensor(out=kdif[:, ti, :], in0=hn[:, 0:W2], in1=hn[:, W2:W4], op=ALU.subtract), reads=["hn"], writes=["kdif"])
    S.barrier()
    st2.close()
    fcs = [S.sbuf(f"fcb{i}", [128, 32, 128], BF16, st) for i in range(2)]
    fss = [S.sbuf(f"fsb{i}", [128, 32, 128], BF16, st) for i in range(2)]
    kt = [S.sbuf(f"kt{i}", [128, 3, W2], F32, st) for i in range(2)]
    for j in range(32):
        bi = j % 2
        S.dma("sp", lambda e, j=j, bi=bi: e.dma_start(out=fcs[bi][:], in_=FcB[j]), writes=[f"fcb{bi}"])
        S.dma("sp", lambda e, j=j, bi=bi: e.dma_start(out=fss[bi][:], in_=FsB[j]), writes=[f"fsb{bi}"])
        pb = 3 + 2 * bi
        for ti in range(32):
            S.op("pe", lambda e, ti=ti, bi=bi, pb=pb: e.matmul(C.banks[pb][:, 0:W2], fcs[bi][:, ti, :], ksum[:, ti, :], start=(ti == 0), stop=(ti == 31)),
                 reads=[f"fcb{bi}", "ksum"], writes=[f"bank{pb}"])
        for ti in range(32):
            S.op("pe", lambda e, ti=ti, bi=bi, pb=pb: e.matmul(C.banks[pb + 1][:, 0:W2], fss[bi][:, ti, :], kdif[:, ti, :], start=(ti == 0), stop=(ti == 31)),
                 reads=[f"fsb{bi}", "kdif"], writes=[f"bank{pb + 1}"])
        if j == 0:
            for ti in range(32):
                S.op("pe", lambda e, ti=ti, bi=bi: e.matmul(C.banks[7][:, 0:W2], fss[bi][:, ti, :], ksum[:, ti, :], start=(ti == 0), stop=(ti == 31)),
                     reads=[f"fsb{bi}", "ksum"], writes=["bank7"])
            S.op("dve", lambda e, bi=bi, pb=pb: e.tensor_scalar(out=kt[bi][:, 0, :], in0=C.banks[pb][:, 0:W2], scalar1=cv[:, 0:1], scalar2=None, op0=ALU.mult),
                 reads=[f"bank{pb}", "cv"], writes=[f"kt{bi}"])
            S.op("dve", lambda e, bi=bi, pb=pb: e.tensor_scalar(out=kt[bi][:, 1, :], in0=C.banks[pb + 1][:, 0:W2], scalar1=cv[:, 1:2], scalar2=None, op0=ALU.mult),
                 reads=[f"bank{pb + 1}", "cv"], writes=[f"kt{bi}"])
            S.op("dve", lambda e, bi=bi, pb=pb: e.tensor_scalar(out=kt[bi][:, 2, :], in0=C.banks[pb][:, 0:W2], scalar1=cv[:, 1:2], scalar2=None, op0=ALU.mult),
                 reads=[f"bank{pb}", "cv"], writes=[f"kt{bi}"])
            S.op("dve", lambda e, bi=bi: e.scalar_tensor_tensor(out=kt[bi][:, 2, :], in0=C.banks[7][:, 0:W2], scalar=cv[:, 2:3], in1=kt[bi][:, 2, :],
                                                               op0=ALU.mult, op1=ALU.add), reads=["bank7", "cv", f"kt{bi}"], writes=[f"kt{bi}"])
        else:
            S.op("act", lambda e, bi=bi, pb=pb: e.activation(out=kt[bi][:, 0, :], in_=C.banks[pb][:, 0:W2], func=AF.Copy), reads=[f"bank{pb}"], writes=[f"kt{bi}"])
            S.op("dve", lambda e, bi=bi, pb=pb: e.tensor_copy(out=kt[bi][:, 1, :], in_=C.banks[pb + 1][:, 0:W2]), reads=[f"bank{pb + 1}"], writes=[f"kt{bi}"])
            S.op("act", lambda e, bi=bi, pb=pb: e.activation(out=kt[bi][:, 2, :], in_=C.banks[pb][:, 0:W2], func=AF.Copy), reads=[f"bank{pb}"], writes=[f"kt{bi}"])
        S.dma("sp", lambda e, j=j, bi=bi: e.dma_start(out=ktab[j], in_=kt[bi][:]), reads=[f"kt{bi}"], writes=["ktblk"], is_out=True)
    S.barrier()
    st.close()


def l1k_inputs(inp, core):
    hc = hy_consts()
    ch = 64 * core + np.arange(64)
    cols = np.concatenate([d * 1024 + o * 512 + ch for d in range(2) for o in range(2)])
    pvec = np.stack([inp["hy_b1"][0], inp["hy_f1"][0], inp["hy_b2"][0], inp["hy_f2"][0]], axis=1).astype(np.float32)
    return {"featsT": hc["featsT"], "w1": np.ascontiguousarray(inp["hy_w1"][0]), "w2": np.ascontiguousarray(inp["hy_w2"][0]),
            "w3s": np.ascontiguousarray(inp["hy_w3"][0][:, cols]), "pvec": np.ascontiguousarray(pvec),
            "b3s": np.ascontiguousarray(inp["hy_b3"][0][cols]), "decay": np.ascontiguousarray(hc["decay"][:, :, ch]),
            "FcB": hc["FcB"], "FsB": hc["FsB"], "cv": hc["cv"]}


NT1 = CTX + SEQ


def l1_w_in_cols(half):
    P = rope_perm()
    q = np.arange(0, 512)
    qP = (q.reshape(8, 64)[:, P]).reshape(-1)
    k0 = 512 + np.arange(64)
    k1 = 576 + np.arange(64)
    v0 = 640 + np.arange(64)
    v1 = 704 + np.arange(64)
    c0 = half * 256
    hy = np.concatenate([768 + o * 512 + c0 + np.arange(256) for o in range(3)])
    return np.concatenate([q, qP, np.concatenate([k0, k0]), np.concatenate([k0[P], k0[P]]),
                           np.concatenate([k1, k1]), np.concatenate([k1[P], k1[P]]),
                           np.concatenate([v0, v0, v1, v1]), hy])


def build_l1a(stop=None):
    nc = bass.Bass("TRN2", target_bir_lowering=False)
    C = Ctx(nc)
    l1a_body(nc, C, IO(nc))
    C.S.finish()
    return nc


def l1a_body(nc, C, io, hy_halves=(None,)):
    xT = io.inp("xT", [8, 128, NT1])
    xTo = io.inp("xTo", [8, 128, OWN])
    ropeCq = io.inp("ropeCq", [128, OWN])
    ropeSq = io.inp("ropeSq", [128, OWN])
    modt = io.inp("mod", [128, 2, 6, 8])
    ng = io.inp("ng", [128, 2, 8])
    w_in = io.inp("w_in", [D, 2560])
    ropeC = io.inp("ropeC", [128, SEQ])
    ropeS = io.inp("ropeS", [128, SEQ])
    gvec_d = io.inp("gvec", [128, 4])
    bones_d = io.inp("bones", [128, 128])
    ident_d = io.inp("ident", [128, 128])
    if hy_halves == (None,):
        swT_d = io.inp("swT", [128, 6, 4])
        hb_d = io.inp("hbias", [2, 256])
        Kt = io.inp("Kt", [32, 128, 2, 3, 256])
    else:
        swT2_d = io.inp("swT2", [128, 2, 6, 4])
        hb2_d = io.inp("hbias2", [2, 2, 256])
        kt_blk = io.inp("kt_blk", None)
        Kt2 = [kt_blk[0], kt_blk[1]]
        w_hy = io.inp("w_hy", [D, 2, 768])
        w_hy_v = w_hy.rearrange("(k p) c o -> p k c o", p=128)
    FcB = io.inp("FcB", [32, 128, 32, 128], BF16)
    FsB = io.inp("FsB", [32, 128, 32, 128], BF16)
    FsBi = io.inp("FsBi", [32, 128, 32, 128], BF16)
    yatt_o = io.out("yatt", [4, 128, OWN], BF16)
    yhy_o = io.out("yhy", [128, 32, 256], BF16) if hy_halves == (None,) else io.out("yhy2", [2, 128, 32, 256], BF16)
    S = C.S
    st0 = contextlib.ExitStack()
    M = load_mod(C, modt, ng, st0)
    xv = xT.rearrange("k p t -> p k t")
    gvec = S.sbuf("gvec", [128, 4], F32, st0)
    bones = S.sbuf("bones", [128, 128], F32, st0)
    ident = S.sbuf("ident1", [128, 128], F32, st0)
    swT = S.sbuf("swT", [128, 6, 4], F32, st0)
    hbb = S.sbuf("hbb", [128, 2, 256], F32, st0)
    for dst, src, key in ((gvec, gvec_d, "gvec"), (bones, bones_d, "bones"), (ident, ident_d, "ident")):
        S.dma("sp", lambda e, dst=dst, src=src: e.dma_start(out=dst[:], in_=src), writes=[key])
    wv = w_in.rearrange("(k p) o -> p k o", p=128)
    def make_qk_block(src, srckey, rc, rs_, sq, rstd, ta, tb_):
        def qk_block(wa, wak, ja, wp, wpk, jp, gi, toks, out_fn, okey, rope):
            for (a, b, da, la) in toks:
                n = b - a
                for k in range(8):
                    S.op("pe", lambda e, k=k: e.matmul(C.banks[0][:, :n], wa[:, k, ja * 128:(ja + 1) * 128], src[:, k, a:b],
                                                       start=(k == 0), stop=(k == 7)), reads=[wak, srckey], writes=["bank0"])
                if rope:
                    for k in range(8):
                        S.op("pe", lambda e, k=k: e.matmul(C.banks[1][:, :n], wp[:, k, jp * 128:(jp + 1) * 128], src[:, k, a:b],
                                                           start=(k == 0), stop=(k == 7)), reads=[wpk, srckey], writes=["bank1"])
                S.op("act", lambda e: e.activation(out=sq[:, :n], in_=C.banks[0][:, :n], func=AF.Square), reads=["bank0"], writes=["sq1"])
                S.op("pe", lambda e: e.matmul(C.banks[2][:, :n], bones[:], sq[:, :n], start=True, stop=True), reads=["bones", "sq1"], writes=["bank2"])
                S.op("act", lambda e: e.activation(out=rstd[:, :n], in_=C.banks[2][:, :n], func=AF.Sqrt, bias=EPS, scale=1.0 / HD),
                     reads=["bank2"], writes=["rstd1"])
                S.op("dve", lambda e: e.reciprocal(out=rstd[:, :n], in_=rstd[:, :n]), reads=["rstd1"], writes=["rstd1"])
                if rope:
                    S.op("dve", lambda e: e.scalar_tensor_tensor(out=ta[:, :n], in0=C.banks[0][:, :n], scalar=gvec[:, gi:gi + 1], in1=rc[:, la:la + n],
                                                                 op0=ALU.mult, op1=ALU.mult), reads=["bank0", "gvec", "rc"], writes=["ta1"])
                    S.op("dve", lambda e: e.scalar_tensor_tensor(out=tb_[:, :n], in0=C.banks[1][:, :n], scalar=gvec[:, gi + 1:gi + 2], in1=rs_[:, la:la + n],
                                                                 op0=ALU.mult, op1=ALU.mult), reads=["bank1", "gvec", "rs"], writes=["tb1"])
                    S.op("pool", lambda e: e.tensor_tensor(out=ta[:, :n], in0=ta[:, :n], in1=tb_[:, :n], op=ALU.add), reads=["ta1", "tb1"], writes=["ta1"])
                    S.op("pool", lambda e: e.tensor_tensor(out=out_fn(da, da + n), in0=ta[:, :n], in1=rstd[:, :n], op=ALU.mult),
                         reads=["ta1", "rstd1"], writes=[okey])
                else:
                    S.op("dve", lambda e: e.scalar_tensor_tensor(out=out_fn(da, da + n), in0=C.banks[0][:, :n], scalar=gvec[:, gi:gi + 1], in1=rstd[:, :n],
                                                                 op0=ALU.mult, op1=ALU.mult), reads=["bank0", "gvec", "rstd1"], writes=[okey])
        return qk_block

    with contextlib.ExitStack() as stH:
        hx = S.sbuf("hx1", [128, 8, NT1], BF16, stH)
        with contextlib.ExitStack() as stA:
            xts = [S.sbuf(f"xa{i}", [128, 8, 512], F32, stA) for i in range(2)]

            def src_fn(a, b, ti):
                xt = xts[ti % 2]
                S.dma("sp", lambda e: e.dma_start(out=xt[:, :, :b - a], in_=xv[:, :, a:b]), writes=[f"xa{ti % 2}"])
                return xt[:, :, :b - a], f"xa{ti % 2}"
            norm_phase(C, M, 0, src_fn, None, NT1, CTX, hx, "hx", stA)
        S.barrier()
        for chalf in hy_halves:
            if chalf is None:
                hy_w = lambda grp: wv[:, :, 1792 + grp * 256:1792 + (grp + 1) * 256]
                swT_src, hb_src, Kt_c, yhy_c = swT_d, hb_d, Kt, yhy_o
            else:
                hy_w = lambda grp, chalf=chalf: w_hy_v[:, :, chalf, grp * 256:(grp + 1) * 256]
                swT_src, hb_src, Kt_c, yhy_c = swT2_d[:, chalf], hb2_d[chalf], Kt2[chalf], yhy_o[chalf]
            with contextlib.ExitStack() as stZ:
                zg = [S.sbuf(f"zg{i}", [128, 32, 256], BF16, stZ) for i in range(3)]
                S.dma("sp", lambda e: e.dma_start(out=swT[:], in_=swT_src), writes=["swT"])
                for o in range(2):
                    S.dma("sp", lambda e, o=o: e.dma_start(out=hbb[:, o, :], in_=hb_src[o].partition_broadcast(128)), writes=["hbb"])
                with contextlib.ExitStack() as stU:
                    wu_ = [S.sbuf(f"wu{i}", [128, 8, 256], BF16, stU) for i in range(2)]
                    U = S.sbuf("U", [128, SEQ + 2], F32, stU)
                    cvt = [S.sbuf(f"cv{i}", [128, 512], F32, stU) for i in range(2)]
                    S.op("dve", lambda e: e.memset(U[:, 0:1], 0.0), writes=["Upad0"])
                    S.op("dve", lambda e: e.memset(U[:, SEQ + 1:SEQ + 2], 0.0), writes=["Upad1"])
                    tcount = [0]
                    for grp in range(3):
                        wb = wu_[grp % 2]
                        S.dma("pool", lambda e, grp=grp, wb=wb: e.dma_start(out=wb[:], in_=hy_w(grp)), writes=[f"wu{grp % 2}"])
                        for cc in range(2):
                            c = grp * 2 + cc
                            for tt in range(8):
                                bk = tt % 2
                                for k in range(8):
                                    S.op("pe", lambda e, k=k, tt=tt, bk=bk, cc=cc, wb=wb: e.matmul(
                                        C.banks[bk][:, :], wb[:, k, cc * 128:(cc + 1) * 128], hx[:, k, CTX + tt * 512:CTX + (tt + 1) * 512],
                                        start=(k == 0), stop=(k == 7)), reads=[f"wu{grp % 2}", "hx"], writes=[f"bank{bk}"])
                                S.op("act", lambda e, tt=tt, bk=bk: e.activation(out=U[:, 1 + tt * 512:1 + (tt + 1) * 512], in_=C.banks[bk][:, :], func=AF.Copy),
                                     reads=[f"bank{bk}"], writes=["U"])
                            for tt in range(8):
                                cv_ = cvt[tt % 2]
                                ck = f"cv{tt % 2}"
                                a = tt * 512
                                S.op("dve", lambda e, a=a, c=c, cv_=cv_: e.tensor_scalar(out=cv_[:], in0=U[:, 1 + a:1 + a + 512], scalar1=swT[:, c, 1:2],
                                                                                       scalar2=swT[:, c, 3:4], op0=ALU.mult, op1=ALU.add),
                                     reads=["U", "swT", "Upad0", "Upad1"], writes=[ck])
                                S.op("dve", lambda e, a=a, c=c, cv_=cv_: e.scalar_tensor_tensor(out=cv_[:], in0=U[:, a:a + 512], scalar=swT[:, c, 0:1], in1=cv_[:],
                                                                                              op0=ALU.mult, op1=ALU.add), reads=["U", "swT", ck], writes=[ck])
                                S.op("dve", lambda e, a=a, c=c, cv_=cv_: e.scalar_tensor_tensor(out=cv_[:], in0=U[:, 2 + a:2 + a + 512], scalar=swT[:, c, 2:3], in1=cv_[:],
                                                                                              op0=ALU.mult, op1=ALU.add), reads=["U", "swT", ck], writes=[ck])
                                for s_ in range(4):
                                    tb = 2 + tcount[0] % 4
                                    tcount[0] += 1
                                    S.op("pe", lambda e, s_=s_, tb=tb, cv_=cv_: e.transpose(C.banks[tb][:, 0:128], cv_[:, s_ * 128:(s_ + 1) * 128], ident[:]),
                                         reads=[ck, "ident"], writes=[f"bank{tb}"])
                                    dst = zg[grp][:, tt * 4 + s_, cc * 128:(cc + 1) * 128]
                                    eng = "act" if s_ % 2 == 0 else "dve"
                                    if eng == "act":
                                        S.op("act", lambda e, tb=tb, dst=dst: e.activation(out=dst, in_=C.banks[tb][:, 0:128], func=AF.Copy), reads=[f"bank{tb}"], writes=[f"zg{grp}"])
                                    else:
                                        S.op("dve", lambda e, tb=tb, dst=dst: e.tensor_copy(out=dst, in_=C.banks[tb][:, 0:128]), reads=[f"bank{tb}"], writes=[f"zg{grp}"])
                S.barrier()
                S.barrier()
                with contextlib.ExitStack() as stF:
                    YW = S.sbuf("YW", [128, 2, 32, 256], BF16, stF)
                    fa = [S.sbuf(f"fa{i}", [128, 32, 128], BF16, stF) for i in range(2)]
                    fb = [S.sbuf(f"fb{i}", [128, 32, 128], BF16, stF) for i in range(2)]
                    kts = [S.sbuf(f"ktb{i}", [128, 3, 256], F32, stF) for i in range(2)]
                    tmp = [S.sbuf(f"hyt{i}", [128, 256], F32, stF) for i in range(4)]
                    z = zg[0]
                    for o in range(2):
                        gate = zg[1 + o]
                        for j in range(32):
                            bi = j % 2
                            S.dma("sp", lambda e, j=j, bi=bi: e.dma_start(out=fa[bi][:], in_=FcB[j]), writes=[f"fa{bi}"])
                            S.dma("sp", lambda e, j=j, bi=bi: e.dma_start(out=fb[bi][:], in_=FsB[j]), writes=[f"fb{bi}"])
                            if chalf is None:
                                S.dma("sp", lambda e, j=j, bi=bi, o=o: e.dma_start(out=kts[bi][:], in_=Kt_c[j, :, o]), writes=[f"ktb{bi}"])
                            else:
                                S.dma("sp", lambda e, j=j, bi=bi, o=o: e.dma_start(out=kts[bi][:], in_=Kt_c[j, :, :, o * 256:(o + 1) * 256]), reads=["ktblk"], writes=[f"ktb{bi}"])
                            zc, zs = 2 * bi, 2 * bi + 1
                            for ti in range(32):
                                S.op("pe", lambda e, ti=ti, bi=bi, zc=zc: e.matmul(C.banks[zc][:, 0:256], fa[bi][:, ti, :], z[:, ti, :], start=(ti == 0), stop=(ti == 31)),
                                     reads=[f"fa{bi}", "z"], writes=[f"bank{zc}"])
                            for ti in range(32):
                                S.op("pe", lambda e, ti=ti, bi=bi, zs=zs: e.matmul(C.banks[zs][:, 0:256], fb[bi][:, ti, :], z[:, ti, :], start=(ti == 0), stop=(ti == 31)),
                                     reads=[f"fb{bi}", "z"], writes=[f"bank{zs}"])
                            kt = kts[bi]
                            S.op("dve", lambda e, zc=zc, kt=kt: e.tensor_tensor(out=tmp[0][:], in0=C.banks[zc][:, 0:256], in1=kt[:, 0, :], op=ALU.mult),
                                 reads=[f"bank{zc}", f"ktb{bi}"], writes=["hyt0"])
                            S.op("dve", lambda e, zs=zs, kt=kt: e.tensor_tensor(out=tmp[1][:], in0=C.banks[zs][:, 0:256], in1=kt[:, 1, :], op=ALU.mult),
                                 reads=[f"bank{zs}", f"ktb{bi}"], writes=["hyt1"])
                            S.op("pool", lambda e, j=j: e.tensor_tensor(out=YW[:, 0, j, :], in0=tmp[0][:], in1=tmp[1][:], op=ALU.subtract),
                                 reads=["hyt0", "hyt1"], writes=["YW"])
                            S.op("dve", lambda e, zc=zc, kt=kt: e.tensor_tensor(out=tmp[2][:], in0=C.banks[zc][:, 0:256], in1=kt[:, 1, :], op=ALU.mult),
                                 reads=[f"bank{zc}", f"ktb{bi}"], writes=["hyt2"])
                            S.op("dve", lambda e, zs=zs, kt=kt: e.tensor_tensor(out=tmp[3][:], in0=C.banks[zs][:, 0:256], in1=kt[:, 2, :], op=ALU.mult),
                                 reads=[f"bank{zs}", f"ktb{bi}"], writes=["hyt3"])
                            S.op("pool", lambda e, j=j: e.tensor_tensor(out=YW[:, 1, j, :], in0=tmp[2][:], in1=tmp[3][:], op=ALU.add),
                                 reads=["hyt2", "hyt3"], writes=["YW"])
                        for ni in range(32):
                            bi = ni % 2
                            S.dma("sp", lambda e, ni=ni, bi=bi: e.dma_start(out=fa[bi][:], in_=FcB[ni]), writes=[f"fa{bi}"])
                            S.dma("sp", lambda e, ni=ni, bi=bi: e.dma_start(out=fb[bi][:], in_=FsBi[ni]), writes=[f"fb{bi}"])
                            yb_ = 4 + bi
                            for fj in range(32):
                                S.op("pe", lambda e, fj=fj, bi=bi, yb_=yb_: e.matmul(C.banks[yb_][:, 0:256], fa[bi][:, fj, :], YW[:, 0, fj, :], start=(fj == 0), stop=False),
                                     reads=[f"fa{bi}", "YW"], writes=[f"bank{yb_}"])
                            for fj in range(32):
                                S.op("pe", lambda e, fj=fj, bi=bi, yb_=yb_: e.matmul(C.banks[yb_][:, 0:256], fb[bi][:, fj, :], YW[:, 1, fj, :], start=False, stop=(fj == 31)),
                                     reads=[f"fb{bi}", "YW"], writes=[f"bank{yb_}"])
                            S.op("pool", lambda e, ni=ni, o=o: e.tensor_tensor(out=tmp[0][:], in0=z[:, ni, :], in1=hbb[:, o, :], op=ALU.mult), reads=["z", "hbb"], writes=["hyt0"])
                            S.op("dve", lambda e, yb_=yb_: e.scalar_tensor_tensor(out=tmp[1][:], in0=C.banks[yb_][:, 0:256], scalar=2.0 / NFFT, in1=tmp[0][:],
                                                                                 op0=ALU.mult, op1=ALU.add), reads=[f"bank{yb_}", "hyt0"], writes=["hyt1"])
                            S.op("pool", lambda e, ni=ni, gate=gate: e.tensor_tensor(out=z[:, ni, :], in0=gate[:, ni, :], in1=tmp[1][:], op=ALU.mult),
                                 reads=["hyt1", "zg"], writes=["z"])
                        S.barrier()
                    for q in range(4):
                        S.dma("sp", lambda e, q=q: e.dma_start(out=yhy_c[:, q * 8:(q + 1) * 8, :], in_=z[:, q * 8:(q + 1) * 8, :]), reads=["z"], is_out=True)
                S.barrier()
        with contextlib.ExitStack() as stQ:
            KAB = S.sbuf("KABc", [128, 2, NT1], BF16, stQ)
            V2 = S.sbuf("V2c", [128, 34, 256], BF16, stQ)
            with contextlib.ExitStack() as stB:
                wts = [S.sbuf(f"wt{i}", [128, 8, 256], BF16, stB) for i in range(2)]
                rc = S.sbuf("rc", [128, SEQ], BF16, stB)
                rs_ = S.sbuf("rs", [128, SEQ], BF16, stB)
                sq = S.sbuf("sq1", [128, 512], F32, stB)
                rstd = S.sbuf("rstd1", [128, 512], F32, stB)
                ta = S.sbuf("ta1", [128, 512], F32, stB)
                tb_ = S.sbuf("tb1", [128, 512], F32, stB)
                S.dma("pool", lambda e: e.dma_start(out=rc[:], in_=ropeC), writes=["rc"])
                S.dma("pool", lambda e: e.dma_start(out=rs_[:], in_=ropeS), writes=["rs"])

                qk_block = make_qk_block(hx, "hx", rc, rs_, sq, rstd, ta, tb_)

                def loadw(i, c0):
                    S.dma("pool", lambda e: e.dma_start(out=wts[i][:], in_=wv[:, :, c0:c0 + 256]), writes=[f"wt{i}"])
                    return wts[i], f"wt{i}"
                lat_toks = [(CTX + 512 * i, CTX + 512 * (i + 1), CTX + 512 * i, 512 * i) for i in range(8)]
                ctx_toks = [(0, CTX, 0, 0)]
                wk1, wk1k = loadw(0, 1024)
                wk2, wk2k = loadw(1, 1280)
                for g, (wk_, wkk) in enumerate(((wk1, wk1k), (wk2, wk2k))):
                    qk_block(wk_, wkk, 0, wk_, wkk, 1, 2, ctx_toks, lambda a, b, g=g: KAB[:, g, a:b], "KAB", False)
                    qk_block(wk_, wkk, 0, wk_, wkk, 1, 2, lat_toks, lambda a, b, g=g: KAB[:, g, a:b], "KAB", True)
                wv_, wvk = loadw(0, 1536)
                for t in range(34):
                    bk = 4 + t % 2
                    for k in range(8):
                        S.op("pe", lambda e, k=k, t=t, bk=bk: e.matmul(C.banks[bk][:, 0:256], hx[:, k, t * 128:(t + 1) * 128], wv_[:, k, :],
                                                                       start=(k == 0), stop=(k == 7)), reads=[wvk, "hx"], writes=[f"bank{bk}"])
                    S.op("act", lambda e, t=t, bk=bk: e.activation(out=V2[:, t, :], in_=C.banks[bk][:, 0:256], func=AF.Copy), reads=[f"bank{bk}"], writes=["V2"])
            S.barrier()
            Q = S.sbuf("Qc", [128, 4, OWN], BF16, stQ)
            xvo = xTo.rearrange("k p t -> p k t")
            with contextlib.ExitStack() as stO:
                hxo = S.sbuf("hxo", [128, 8, OWN], BF16, stO)
                with contextlib.ExitStack() as stA:
                    xts = [S.sbuf(f"xo{i}", [128, 8, 512], F32, stA) for i in range(1)]

                    def src_fno(a, b, ti):
                        xt = xts[0]
                        S.dma("sp", lambda e: e.dma_start(out=xt[:, :, :b - a], in_=xvo[:, :, a:b]), writes=["xo0"])
                        return xt[:, :, :b - a], "xo0"
                    norm_phase(C, M, 0, src_fno, None, OWN, 0, hxo, "hxo", stA)
                S.barrier()
                with contextlib.ExitStack() as stB:
                    wts = [S.sbuf(f"wq{i}", [128, 8, 256], BF16, stB) for i in range(2)]
                    rcq = S.sbuf("rcq", [128, OWN], BF16, stB)
                    rsq = S.sbuf("rsq", [128, OWN], BF16, stB)
                    sq = S.sbuf("sq1", [128, 512], F32, stB)
                    rstd = S.sbuf("rstd1", [128, 512], F32, stB)
                    ta = S.sbuf("ta1", [128, 512], F32, stB)
                    tb_ = S.sbuf("tb1", [128, 512], F32, stB)
                    S.dma("pool", lambda e: e.dma_start(out=rcq[:], in_=ropeCq), writes=["rc"])
                    S.dma("pool", lambda e: e.dma_start(out=rsq[:], in_=ropeSq), writes=["rs"])
                    qkb = make_qk_block(hxo, "hxo", rcq, rsq, sq, rstd, ta, tb_)
                    own_toks = [(512 * i, 512 * (i + 1), 512 * i, 512 * i) for i in range(4)]
                    for jj in range(2):
                        S.dma("pool", lambda e, jj=jj: e.dma_start(out=wts[0][:], in_=wv[:, :, jj * 256:(jj + 1) * 256]), writes=["wq0"])
                        S.dma("pool", lambda e, jj=jj: e.dma_start(out=wts[1][:], in_=wv[:, :, 512 + jj * 256:512 + (jj + 1) * 256]), writes=["wq1"])
                        for j2 in range(2):
                            j = jj * 2 + j2
                            qkb(wts[0], "wq0", j2, wts[1], "wq1", j2, 0, own_toks, lambda a, b, j=j: Q[:, j, a:b], "Q", True)
                S.barrier()
            yat = S.sbuf("yat", [128, 4, OWN], BF16, stQ)
            with contextlib.ExitStack() as stC:
                pts = [S.sbuf(f"ptc{i}", [128, 512], BF16, stC) for i in range(3)]
                dn = S.sbuf("dnc", [128, 512], F32, stC)
                it = 0
                for qg in range(4):
                    for h in range(8):
                        g = h // 4
                        c, off = h // 2, (h % 2) * 64
                        nb, db = 2 + it % 2, 4 + it % 2
                        it += 1
                        SBK = (0, 1, 6)

                        def emit_S(tk):
                            sb = SBK[tk % 3]
                            S.op("pe", lambda e, tk=tk, sb=sb: e.matmul(C.banks[sb][:, :], KAB[off:off + 64, g, tk * 128:(tk + 1) * 128],
                                                                        Q[off:off + 64, c, qg * 512:(qg + 1) * 512], start=True, stop=True),
                                 reads=["KAB", "Q"], writes=[f"bank{sb}"])
                        emit_S(0)
                        emit_S(1)
                        for tk in range(34):
                            sb = SBK[tk % 3]
                            pt = pts[tk % 3]
                            if tk + 2 < 34:
                                emit_S(tk + 2)
                            S.op("act", lambda e, sb=sb, pt=pt: e.activation(out=pt[:], in_=C.banks[sb][:, :], func=AF.Exp, scale=SCALE),
                                 reads=[f"bank{sb}"], writes=[f"ptc{tk % 3}"])
                            S.op("pe", lambda e, tk=tk, pt=pt: e.matmul(C.banks[nb][:, :], V2[:, tk, g * 128:(g + 1) * 128], pt[:], start=(tk == 0), stop=(tk == 33)),
                                 reads=["V2", f"ptc{tk % 3}"], writes=[f"bank{nb}"])
                            S.op("pe", lambda e, tk=tk, pt=pt: e.matmul(C.banks[db][:, :], C.ones_b[:], pt[:], start=(tk == 0), stop=(tk == 33)),
                                 reads=["ones_b", f"ptc{tk % 3}"], writes=[f"bank{db}"])
                        S.op("dve", lambda e: e.reciprocal(out=dn[:], in_=C.banks[db][:, :]), reads=[f"bank{db}"], writes=["dnc"])
                        S.op("dve", lambda e: e.tensor_tensor(out=yat[off:off + 64, c, qg * 512:(qg + 1) * 512], in0=C.banks[nb][off:off + 64, :],
                                                              in1=dn[off:off + 64, :], op=ALU.mult), reads=[f"bank{nb}", "dnc"], writes=["yat"])
                for c in range(4):
                    S.dma("sp", lambda e, c=c: e.dma_start(out=yatt_o[c], in_=yat[:, c, :]), reads=["yat"], is_out=True)
            S.barrier()
    S.barrier()
    st0.close()

def l1a_inputs(inp, mod, x1_full, Ktabs, core):
    hc = hy_consts()
    b, half = divmod(core, 2)
    order = np.arange(SEQ)
    xt = x1_full[b]
    xT = np.ascontiguousarray(xt)
    xTo = np.ascontiguousarray(xt[:, :, CTX + half * OWN:CTX + (half + 1) * OWN])
    rc, rs = rope_np(order)
    rcq, rsq = rope_np(half * OWN + np.arange(OWN))
    modt = np.stack([fm(mod[1, b].reshape(6, D)), fm(mod[1, 4].reshape(6, D))], axis=1)
    P = rope_perm()
    qn, kn = inp["c_qnorm"][0], inp["c_knorm"][0]
    gvec = np.stack([np.tile(qn, 2), np.tile(qn[P], 2), np.tile(kn, 2), np.tile(kn[P], 2)], axis=1).astype(np.float32)
    bones = np.zeros((128, 128), np.float32)
    bones[:64, :64] = 1.0
    bones[64:, 64:] = 1.0
    c0 = half * 256
    chs = np.concatenate([o * 512 + c0 + np.arange(256) for o in range(3)])
    sw = inp["hy_short_w"][0][:, chs]
    sb = inp["hy_short_b"][0][chs]
    swT = np.concatenate([sw, sb[None]], 0).T.reshape(6, 128, 4).transpose(1, 0, 2)
    return {"xT": xT, "xTo": xTo, "ropeCq": rcq, "ropeSq": rsq, "mod": np.ascontiguousarray(modt), "ng": fm(inp["norm_g"][1]),
            "w_in": np.ascontiguousarray(inp["w_in_odd"][0][:, l1_w_in_cols(half)]),
            "ropeC": rc, "ropeS": rs, "gvec": np.ascontiguousarray(gvec), "bones": bones, "ident": np.eye(128, dtype=np.float32),
            "swT": np.ascontiguousarray(swT, dtype=np.float32), "hbias": np.ascontiguousarray(inp["hy_bias"][0][:, c0:c0 + 256]),
            "Kt": Ktabs[half], "FcB": hc["FcB"], "FsB": hc["FsB"], "FsBi": hc["FsBi"]}, order


def assemble_ktabs(kouts):
    res = []
    for half in range(2):
        t = np.zeros((32, 128, 2, 3, 256), np.float32)
        for q in range(4):
            k = kouts[half * 4 + q].reshape(32, 128, 3, 2, 64)
            t[:, :, :, :, q * 64:(q + 1) * 64] = k.transpose(0, 1, 3, 2, 4)
        res.append(t)
    return res


def assemble_x1(l0_out):
    res = []
    for b in range(NB):
        a, c = l0_out[2 * b], l0_out[2 * b + 1]
        res.append(np.ascontiguousarray(np.concatenate([a[:, :, :CTX], a[:, :, CTX:], c[:, :, CTX:]], axis=2)))
    return res


L1B_TILES = [(512 * i, 512 * (i + 1)) for i in range(4)]


def build_l1b():
    nc = bass.Bass("TRN2", target_bir_lowering=False)
    C = Ctx(nc)
    l1b_body(nc, C, IO(nc))
    C.S.finish()
    return nc


def l1b_body(nc, C, io, fill_y=None):
    yT = io.inp("yT", [8, 128, OWN], BF16) if fill_y is None else None
    xTo = io.inp("xTo", [8, 128, OWN])
    modt = io.inp("mod", [128, 2, 6, 8])
    ng = io.inp("ng", [128, 2, 8])
    w_out = io.inp("w_out", [D, D])
    w_r = io.inp("w_r", [D, 16])
    b_r = io.inp("b_r", [16])
    wg = io.inp("wg", [NE, D, DE])
    wu = io.inp("wu", [NE, D, DE])
    wd = io.inp("wd", [NE, DE, D])
    ident_d = io.inp("ident", [128, 128])
    selE_d = io.inp("selE", [16, 16, 128])
    fg_d = io.inp("fg", [128, 8])
    outT = io.out("outT", [8, 128, OWN])
    S = C.S
    st0 = contextlib.ExitStack()
    M = load_mod(C, modt, ng, st0)
    ybuf = S.sbuf("ybuf1", [128, 8, OWN], BF16, st0)
    if fill_y is None:
        for k in range(8):
            S.dma("sp", lambda e, k=k: e.dma_start(out=ybuf[:, k, :], in_=yT[k]), writes=["Y"])
    else:
        fill_y(ybuf)
    xv = xTo.rearrange("k p t -> p k t")
    tail_phase(C, M, ybuf, w_out, lambda a, b: xv[:, :, a:b], outT, w_r, b_r, ident_d, selE_d, wg, wu, wd, st0,
               final_g_d=fg_d, nres=OWN, tiles=L1B_TILES, ctx_len=0)
    S.barrier()
    st0.close()


def l1b_inputs(inp, mod, x1_full, yatt, yhy, core):
    import ml_dtypes
    b, half = divmod(core, 2)
    yh = np.concatenate([np.asarray(yhy[2 * b + h]).transpose(1, 0, 2).reshape(SEQ, 256) for h in range(2)], axis=1)
    yh_own = yh[half * OWN:(half + 1) * OWN]
    yhT = np.ascontiguousarray(yh_own.T.reshape(4, 128, OWN))
    yT = np.ascontiguousarray(np.concatenate([np.asarray(yatt[core]), yhT], axis=0)).astype(ml_dtypes.bfloat16)
    xt = x1_full[b]
    modt = np.stack([fm(mod[1, b].reshape(6, D)), fm(mod[1, 4].reshape(6, D))], axis=1)
    selE = np.zeros((16, 16, 128), np.float32)
    for e in range(16):
        selE[e, e, :] = 1.0
    return {"yT": yT, "xTo": np.ascontiguousarray(xt[:, :, CTX + half * OWN:CTX + (half + 1) * OWN]),
            "mod": np.ascontiguousarray(modt), "ng": fm(inp["norm_g"][1]),
            "w_out": np.ascontiguousarray(inp["w_out_odd"][0]),
            "w_r": np.ascontiguousarray(inp["w_router"]), "b_r": np.ascontiguousarray(inp["b_router"]),
            "wg": np.ascontiguousarray(inp["moe_wg"][1]), "wu": np.ascontiguousarray(inp["moe_wu"][1]),
            "wd": np.ascontiguousarray(inp["moe_wd"][1]),
            "ident": np.eye(128, dtype=np.float32), "selE": selE, "fg": fm(inp["final_g"])}


def kernel_unfused(**inp):
    inp = {k: np.asarray(v) for k, v in inp.items()}
    cores = list(range(NCORES))
    mod = run_mod(inp)
    r0 = run_bass_kernel_spmd(build_l0(), [l0_inputs(inp, mod, c) for c in cores], core_ids=cores)
    x1_full = assemble_x1([r0.results[c]["x1T"] for c in cores])
    rk = run_bass_kernel_spmd(build_l1k(), [l1k_inputs(inp, c) for c in cores], core_ids=cores)
    Kt = assemble_ktabs([rk.results[c]["ktab"] for c in cores])
    ra = run_bass_kernel_spmd(build_l1a(), [l1a_inputs(inp, mod, x1_full, Kt, c)[0] for c in cores], core_ids=cores)
    yatt = [ra.results[c]["yatt"] for c in cores]
    yhy = [ra.results[c]["yhy"] for c in cores]
    rb = run_bass_kernel_spmd(build_l1b(), [l1b_inputs(inp, mod, x1_full, yatt, yhy, c) for c in cores], core_ids=cores)
    out = np.zeros((NB, SEQ, D), np.float32)
    for c in cores:
        b, half = divmod(c, 2)
        o = rb.results[c]["outT"]
        out[b, half * OWN:(half + 1) * OWN] = o.transpose(2, 0, 1).reshape(OWN, D)
    return out


def mod_phase(nc, C, cT_d, w_ada_d, b_ada_fm_d):
    S = C.S
    st = S.stack
    mods = [S.sbuf(f"modL{l}", [128, 2, 6, 8], F32, st) for l in range(2)]
    with contextlib.ExitStack() as st2:
        ct = S.sbuf("m_ct", [128, 8, 2], F32, st2)
        sg = S.sbuf("m_sg", [128, 8, 2], F32, st2)
        bfm = S.sbuf("m_b", [128, 2, 6, 8], F32, st2)
        wts = [S.sbuf(f"m_w{i}", [128, 8, 512], F32, st2) for i in range(2)]
        S.dma("sp", lambda e: e.dma_start(out=ct[:], in_=cT_d), writes=["m_ct"])
        S.dma("sp", lambda e: e.dma_start(out=bfm[:], in_=b_ada_fm_d), writes=["m_b"])
        S.op("act", lambda e: e.activation(out=sg[:], in_=ct[:], func=AF.Silu), reads=["m_ct"], writes=["m_sg"])
        it = 0
        for l in range(2):
            wv = w_ada_d[l].rearrange("(k p) o -> p k o", p=128)
            for grp in range(12):
                wi = it % 2
                it += 1
                S.dma("sp", lambda e, grp=grp, wi=wi, wv=wv: e.dma_start(out=wts[wi][:], in_=wv[:, :, grp * 512:(grp + 1) * 512]), writes=[f"m_w{wi}"])
                for oc in range(4):
                    col = grp * 4 + oc
                    j, kk = divmod(col, 8)
                    bk = col % 4
                    for k in range(8):
                        S.op("pe", lambda e, k=k, oc=oc, wi=wi, bk=bk: e.matmul(C.banks[bk][:, 0:2], wts[wi][:, k, oc * 128:(oc + 1) * 128], sg[:, k, :],
                                                                               start=(k == 0), stop=(k == 7)), reads=[f"m_w{wi}", "m_sg"], writes=[f"bank{bk}"])
                    S.op("dve", lambda e, l=l, j=j, kk=kk, bk=bk: e.tensor_scalar(out=mods[l][:, :, j, kk], in0=C.banks[bk][:, 0:2], scalar1=bfm[:, l, j, kk:kk + 1],
                                                                                 scalar2=None, op0=ALU.add), reads=[f"bank{bk}", "m_b"], writes=[f"modL{l}"])
        S.barrier()
    return mods


def build_fused():
    nc = bass.Bass("TRN2", target_bir_lowering=False)
    C = Ctx(nc)
    S = C.S

    def scr(name, shape, dt=F32):
        return nc.dram_tensor(name, list(shape), dt, kind="Internal").ap()
    E = {}
    for name, shape, dt in (
            ("cT", [128, 8, 2], F32), ("w_ada", [2, D, 6 * D], F32), ("b_ada_fm", [128, 2, 6, 8], F32),
            ("ng0", [128, 2, 8], F32), ("ng1", [128, 2, 8], F32),
            ("w_in0", [D, 3328], F32), ("w_out0", [D, D], F32), ("sink", [8], F32), ("rpbT", [128, 7, 8, 128], F32),
            ("vint", [128, 5, 128], F32), ("w_r", [D, 16], F32), ("b_r", [16], F32),
            ("wg0", [NE, D, DE], F32), ("wu0", [NE, D, DE], F32), ("wd0", [NE, DE, D], F32),
            ("wg1", [NE, D, DE], F32), ("wu1", [NE, D, DE], F32), ("wd1", [NE, DE, D], F32),
            ("ident", [128, 128], F32), ("selE", [16, 16, 128], F32),
            ("featsT", [33, SEQ], F32), ("w1", [33, 64], F32), ("w2", [64, 64], F32), ("pvec", [64, 4], F32),
            ("FcB", [32, 128, 32, 128], BF16), ("FsB", [32, 128, 32, 128], BF16), ("FsBi", [32, 128, 32, 128], BF16), ("cv", [128, 4], F32),
            ("w_in1", [D, 2560], F32), ("w_out1", [D, D], F32), ("ropeCn", [128, SEQ], F32), ("ropeSn", [128, SEQ], F32),
            ("ropeCq", [128, OWN], F32), ("ropeSq", [128, OWN], F32), ("gvec", [128, 4], F32), ("bones", [128, 128], F32),
            ("swT2", [128, 2, 6, 4], F32), ("hbias2", [2, 2, 256], F32), ("w_hy", [D, 2, 768], F32),
            ("selv", [128, 2], F32), ("fg", [128, 8], F32)):
        E[name] = din(nc, name, shape, dt)
    outT = dout(nc, "outT", [8, 128, OWN])
    x1nat = scr("x1nat", [8, 128, NT1])
    xown = scr("xown", [8, 128, OWN])
    kt_blk = [scr(f"ktblk{i}", [32, 128, 3, 512]) for i in range(2)]
    yatt_s = scr("yatt_s", [4, 128, OWN], BF16)
    yhy_s = scr("yhy_s", [2, 128, 32, 256], BF16)

    mods = mod_phase(nc, C, E["cT"], E["w_ada"], E["b_ada_fm"])
    for p in range(2):
        ov = {"mod": mods[0][:], "ng": E["ng0"], "w_in": E["w_in0"], "w_out": E["w_out0"], "sink": E["sink"], "rpbT": E["rpbT"],
              "vint": E["vint"], "w_r": E["w_r"], "b_r": E["b_r"], "wg": E["wg0"], "wu": E["wu0"], "wd": E["wd0"],
              "ident": E["ident"], "selE": E["selE"], "x1T": (x1nat, p)}
        _build_l0_body(nc, C, False, "full", IO(nc, ov, prefix=f"p{p}_"))
        S.barrier()
    for cb in range(2):
        ov = {"featsT": E["featsT"], "w1": E["w1"], "w2": E["w2"], "pvec": E["pvec"], "FcB": E["FcB"], "FsB": E["FsB"], "cv": E["cv"],
              "ktab": kt_blk[cb]}
        l1k_body2(nc, C, IO(nc, ov, prefix=f"k{cb}_"))
        S.barrier()
    with contextlib.ExitStack() as st:
        sel = S.sbuf("selv_sb", [128, 2], F32, st)
        ta = [S.sbuf(f"bl_a{i}", [128, 512], F32, st) for i in range(2)]
        tb = [S.sbuf(f"bl_b{i}", [128, 512], F32, st) for i in range(2)]
        S.dma("sp", lambda e: e.dma_start(out=sel[:], in_=E["selv"]), writes=["selv"])
        it = 0
        for k in range(8):
            for tt in range(4):
                bi = it % 2
                it += 1
                a0 = CTX + tt * 512
                S.dma("sp", lambda e, k=k, a0=a0, bi=bi: e.dma_start(out=ta[bi][:], in_=x1nat[k][:, a0:a0 + 512]), reads=["x1nat"], writes=[f"bl_a{bi}"])
                S.dma("sp", lambda e, k=k, a0=a0, bi=bi: e.dma_start(out=tb[bi][:], in_=x1nat[k][:, OWN + a0:OWN + a0 + 512]), reads=["x1nat"], writes=[f"bl_b{bi}"])
                S.op("dve", lambda e, bi=bi: e.tensor_scalar(out=ta[bi][:], in0=ta[bi][:], scalar1=sel[:, 0:1], scalar2=None, op0=ALU.mult),
                     reads=[f"bl_a{bi}", "selv"], writes=[f"bl_a{bi}"])
                S.op("dve", lambda e, bi=bi: e.scalar_tensor_tensor(out=ta[bi][:], in0=tb[bi][:], scalar=sel[:, 1:2], in1=ta[bi][:], op0=ALU.mult, op1=ALU.add),
                     reads=[f"bl_a{bi}", f"bl_b{bi}", "selv"], writes=[f"bl_a{bi}"])
                S.dma("sp", lambda e, k=k, tt=tt, bi=bi: e.dma_start(out=xown[k][:, tt * 512:(tt + 1) * 512], in_=ta[bi][:]), reads=[f"bl_a{bi}"], writes=["xown"])
        S.barrier()
    ov = {"xT": x1nat, "xTo": xown, "ropeCq": E["ropeCq"], "ropeSq": E["ropeSq"], "mod": mods[1][:], "ng": E["ng1"], "w_in": E["w_in1"],
          "ropeC": E["ropeCn"], "ropeS": E["ropeSn"], "gvec": E["gvec"], "bones": E["bones"], "ident": E["ident"],
          "swT2": E["swT2"], "hbias2": E["hbias2"], "kt_blk": kt_blk, "w_hy": E["w_hy"],
          "FcB": E["FcB"], "FsB": E["FsB"], "FsBi": E["FsBi"], "yatt": yatt_s, "yhy2": yhy_s}
    l1a_body(nc, C, IO(nc, ov), hy_halves=(0, 1))
    S.barrier()

    def fill_y(ybuf):
        for c in range(4):
            S.dma("sp", lambda e, c=c: e.dma_start(out=ybuf[:, c, :], in_=yatt_s[c]), writes=["Y"])
        with contextlib.ExitStack() as st:
            sel = S.sbuf("selv_sb2", [128, 2], F32, st)
            idn = S.sbuf("idn2", [128, 128], F32, st)
            t0 = [S.sbuf(f"fy_a{i}", [128, 256], BF16, st) for i in range(2)]
            t1 = [S.sbuf(f"fy_b{i}", [128, 256], BF16, st) for i in range(2)]
            tf = [S.sbuf(f"fy_f{i}", [128, 256], F32, st) for i in range(2)]
            S.dma("sp", lambda e: e.dma_start(out=sel[:], in_=E["selv"]), writes=["selv2"])
            S.dma("sp", lambda e: e.dma_start(out=idn[:], in_=E["ident"]), writes=["idn2"])
            it = 0
            for c in range(2):
                for i in range(16):
                    bi = it % 2
                    it += 1
                    S.dma("sp", lambda e, c=c, i=i, bi=bi: e.dma_start(out=t0[bi][:], in_=yhy_s[c][:, i, :]), writes=[f"fy_a{bi}"])
                    S.dma("sp", lambda e, c=c, i=i, bi=bi: e.dma_start(out=t1[bi][:], in_=yhy_s[c][:, 16 + i, :]), writes=[f"fy_b{bi}"])
                    S.op("dve", lambda e, bi=bi: e.tensor_scalar(out=tf[bi][:], in0=t0[bi][:], scalar1=sel[:, 0:1], scalar2=None, op0=ALU.mult),
                         reads=[f"fy_a{bi}", "selv2"], writes=[f"fy_f{bi}"])
                    S.op("dve", lambda e, bi=bi: e.scalar_tensor_tensor(out=tf[bi][:], in0=t1[bi][:], scalar=sel[:, 1:2], in1=tf[bi][:], op0=ALU.mult, op1=ALU.add),
                         reads=[f"fy_b{bi}", f"fy_f{bi}", "selv2"], writes=[f"fy_f{bi}"])
                    for cc in range(2):
                        bk = (2 * it + cc) % 4
                        S.op("pe", lambda e, bi=bi, cc=cc, bk=bk: e.transpose(C.banks[bk][:, 0:128], tf[bi][:, cc * 128:(cc + 1) * 128], idn[:]),
                             reads=[f"fy_f{bi}", "idn2"], writes=[f"bank{bk}"])
                        S.op("act", lambda e, c=c, cc=cc, i=i, bk=bk: e.activation(out=ybuf[:, 4 + 2 * c + cc, i * 128:(i + 1) * 128], in_=C.banks[bk][:, 0:128], func=AF.Copy),
                             reads=[f"bank{bk}"], writes=["Y"])
            S.barrier()
    ov = {"xTo": xown, "mod": mods[1][:], "ng": E["ng1"], "w_out": E["w_out1"], "w_r": E["w_r"], "b_r": E["b_r"],
          "wg": E["wg1"], "wu": E["wu1"], "wd": E["wd1"], "ident": E["ident"], "selE": E["selE"], "fg": E["fg"], "outT": outT}
    l1b_body(nc, C, IO(nc, ov), fill_y=fill_y)
    S.finish()
    return nc


def fused_inputs(inp, core):
    hc = hy_consts()
    b, half = divmod(core, 2)
    m = {}
    cond = np.stack([inp["c"][b], inp["c_ctx"]], axis=0)
    m["cT"] = np.ascontiguousarray(cond.T.reshape(8, 128, 2).transpose(1, 0, 2))
    m["w_ada"] = np.ascontiguousarray(inp["w_ada"])
    m["b_ada_fm"] = np.ascontiguousarray(np.stack([fm(inp["b_ada"][l].reshape(6, D)) for l in range(2)], axis=1))
    m["ng0"] = fm(inp["norm_g"][0]); m["ng1"] = fm(inp["norm_g"][1])
    m["w_in0"] = np.ascontiguousarray(inp["w_in_even"][0][:, l0_w_in_cols()])
    m["w_out0"] = np.ascontiguousarray(inp["w_out_even"][0])
    m["sink"] = np.ascontiguousarray(inp["a_sink"][0]); m["rpbT"] = rpb_gather(inp["b_rpb"][0])
    m["vint"] = np.ascontiguousarray(np.stack([b_valid(10, o) for o in range(-2, 3)], axis=1))
    m["w_r"] = np.ascontiguousarray(inp["w_router"]); m["b_r"] = np.ascontiguousarray(inp["b_router"])
    for l in range(2):
        m[f"wg{l}"] = np.ascontiguousarray(inp["moe_wg"][l]); m[f"wu{l}"] = np.ascontiguousarray(inp["moe_wu"][l])
        m[f"wd{l}"] = np.ascontiguousarray(inp["moe_wd"][l])
    m["ident"] = np.eye(128, dtype=np.float32)
    selE = np.zeros((16, 16, 128), np.float32)
    for e in range(16):
        selE[e, e, :] = 1.0
    m["selE"] = selE
    k = np.arange(128)
    tri_lo = (k[:, None] >= k[None, :]).astype(np.float32)
    tri_hi = (k[:, None] <= k[None, :]).astype(np.float32)
    z = np.zeros_like(tri_lo)
    for p in range(2):
        pos = p * OWN - HALO + np.arange(NLAT)
        ok = (pos >= 0) & (pos < SEQ)
        xl = np.zeros((NTOK, D), np.float32)
        xl[:CTX] = inp["ctx"][b]
        xl[CTX:][ok] = inp["x"][b][pos[ok]]
        m[f"p{p}_xT"] = np.ascontiguousarray(xl.T.reshape(8, 128, NTOK))
        rc, rs = rope_np(np.clip(pos, 0, SEQ - 1))
        m[f"p{p}_ropeC"], m[f"p{p}_ropeS"] = rc, rs
        m[f"p{p}_amask"] = np.ascontiguousarray(np.stack([tri_lo, tri_hi, tri_lo if p == 1 else z, tri_hi if p == 0 else z], axis=1))
        vb = np.zeros((128, 4, 6, 128), np.float32)
        for ci, jl in enumerate((0, 1, 14, 15)):
            for oi, o in enumerate(b_offsets(jl)):
                vb[:, ci, oi] = b_valid(p * 16 + jl, o)
        m[f"p{p}_vb"] = vb
    m["featsT"] = hc["featsT"]; m["w1"] = np.ascontiguousarray(inp["hy_w1"][0]); m["w2"] = np.ascontiguousarray(inp["hy_w2"][0])
    m["pvec"] = np.ascontiguousarray(np.stack([inp["hy_b1"][0], inp["hy_f1"][0], inp["hy_b2"][0], inp["hy_f2"][0]], axis=1).astype(np.float32))
    m["FcB"], m["FsB"], m["FsBi"], m["cv"] = hc["FcB"], hc["FsB"], hc["FsBi"], hc["cv"]
    for cb in range(2):
        ch = 256 * cb + np.arange(256)
        cols = np.concatenate([d * 1024 + o * 512 + ch for d in range(2) for o in range(2)])
        m[f"k{cb}_w3s"] = np.ascontiguousarray(inp["hy_w3"][0][:, cols])
        m[f"k{cb}_b3s"] = np.ascontiguousarray(inp["hy_b3"][0][cols])
        m[f"k{cb}_decay"] = np.ascontiguousarray(hc["decay"][:, :, ch])
    m["w_in1"] = np.ascontiguousarray(inp["w_in_odd"][0][:, l1_w_in_cols(0)])
    m["w_out1"] = np.ascontiguousarray(inp["w_out_odd"][0])
    m["ropeCn"], m["ropeSn"] = rope_np(np.arange(SEQ))
    m["ropeCq"], m["ropeSq"] = rope_np(half * OWN + np.arange(OWN))
    P = rope_perm()
    qn, kn = inp["c_qnorm"][0], inp["c_knorm"][0]
    m["gvec"] = np.ascontiguousarray(np.stack([np.tile(qn, 2), np.tile(qn[P], 2), np.tile(kn, 2), np.tile(kn[P], 2)], axis=1).astype(np.float32))
    bones = np.zeros((128, 128), np.float32)
    bones[:64, :64] = 1.0
    bones[64:, 64:] = 1.0
    m["bones"] = bones
    swT2 = np.zeros((128, 2, 6, 4), np.float32)
    w_hy = np.zeros((D, 2, 768), np.float32)
    for c in range(2):
        chs = np.concatenate([o * 512 + c * 256 + np.arange(256) for o in range(3)])
        sw = inp["hy_short_w"][0][:, chs]
        sb = inp["hy_short_b"][0][chs]
        swT2[:, c] = np.concatenate([sw, sb[None]], 0).T.reshape(6, 128, 4).transpose(1, 0, 2)
        w_hy[:, c] = inp["w_in_odd"][0][:, 768 + chs]
    m["swT2"] = swT2
    m["w_hy"] = w_hy
    m["hbias2"] = np.ascontiguousarray(inp["hy_bias"][0].reshape(2, 2, 256).transpose(1, 0, 2))
    selv = np.zeros((128, 2), np.float32)
    selv[:, half] = 1.0
    m["selv"] = selv
    m["fg"] = fm(inp["final_g"])
    return m


def kernel(**inp):
    inp = {k: np.asarray(v) for k, v in inp.items()}
    cores = list(range(NCORES))
    res = run_bass_kernel_spmd(build_fused(), [fused_inputs(inp, c) for c in cores], core_ids=cores)
    out = np.zeros((NB, SEQ, D), np.float32)
    for c in cores:
        b, half = divmod(c, 2)
        o = res.results[c]["outT"]
        out[b, half * OWN:(half + 1) * OWN] = o.transpose(2, 0, 1).reshape(OWN, D)
    return out


def l1k_body2(nc, C, io):
    NCH = 256
    W4, W2 = 4 * NCH, 2 * NCH
    featsT = io.inp("featsT", [33, SEQ])
    w1 = io.inp("w1", [33, 64]); w2 = io.inp("w2", [64, 64]); w3s = io.inp("w3s", [64, W4])
    pvec = io.inp("pvec", [64, 4])
    b3s = io.inp("b3s", [W4])
    decay = io.inp("decay", [128, 32, NCH])
    FcB = io.inp("FcB", [32, 128, 32, 128], BF16)
    FsB = io.inp("FsB", [32, 128, 32, 128], BF16)
    cv_d = io.inp("cv", [128, 4])
    ktab = io.out("ktab", [32, 128, 3, W2])
    S = C.S
    st = contextlib.ExitStack()
    st2 = contextlib.ExitStack()
    ksum = S.sbuf("ksum", [128, 32, W2], BF16, st); kdif = S.sbuf("kdif", [128, 32, W2], BF16, st)
    cv = S.sbuf("cvs", [128, 4], F32, st)
    ft = S.sbuf("ft", [33, SEQ], F32, st2)
    w1t = S.sbuf("w1t", [33, 64], F32, st2); w2t = S.sbuf("w2t", [64, 64], F32, st2); w3t = S.sbuf("w3t", [64, W4], F32, st2)
    pv = S.sbuf("pv", [64, 4], F32, st2); b3bc = S.sbuf("b3bc", [128, W4], F32, st2)
    dec = S.sbuf("dec", [128, 32, NCH], BF16, st2)
    h1 = S.sbuf("h1", [64, SEQ], F32, st2); h2 = S.sbuf("h2", [64, SEQ], F32, st2)
    for (dst, src, key) in ((ft, featsT, "ft"), (w1t, w1, "w1t"), (w2t, w2, "w2t"), (w3t, w3s, "w3t"), (pv, pvec, "pv"), (cv, cv_d, "cv")):
        S.dma("sp", lambda e, dst=dst, src=src: e.dma_start(out=dst[:], in_=src), writes=[key])
    S.dma("pool", lambda e: e.dma_start(out=dec[:], in_=decay), writes=["dec"])
    S.dma("sp", lambda e: e.dma_start(out=b3bc[:], in_=b3s.partition_broadcast(128)), writes=["b3bc"])
    pre = [S.sbuf(f"pre{i}", [64, 512], F32, st2) for i in range(2)]
    rr_i = [S.sbuf(f"rr_i{i}", [64, 512], mybir.dt.int32, st2) for i in range(2)]
    rr_f = [S.sbuf(f"rr_f{i}", [64, 512], F32, st2) for i in range(2)]
    rr_c = [S.sbuf(f"rr_c{i}", [64, 512], F32, st2) for i in range(2)]
    for layer, (wt, wk, src, skey, dst, dkey, K_) in enumerate(((w1t, "w1t", ft, "ft", h1, "h1", 33), (w2t, "w2t", h1, "h1", h2, "h2", 64))):
        for tt in range(8):
            bk = tt % 2
            S.op("pe", lambda e, tt=tt, bk=bk: e.matmul(C.banks[bk][0:64, :], wt[0:K_, :], src[0:K_, tt * 512:(tt + 1) * 512], start=True, stop=True),
                 reads=[wk, skey], writes=[f"bank{bk}"])
            S.op("dve", lambda e, bk=bk: e.tensor_scalar(out=pre[bk][:], in0=C.banks[bk][0:64, :], scalar1=pv[:, 2 * layer:2 * layer + 1],
                                                        scalar2=pv[:, 2 * layer + 1:2 * layer + 2], op0=ALU.add, op1=ALU.mult),
                 reads=[f"bank{bk}", "pv"], writes=[f"pre{bk}"])
            sin_rr(C, dst[:, tt * 512:(tt + 1) * 512], pre[bk][:], f"pre{bk}", dkey, rr_i[bk][:], rr_f[bk][:], rr_c[bk][:], bk)
    hts = [S.sbuf(f"hts{i}", [128, W4], F32, st2) for i in range(2)]
    ab = [S.sbuf(f"habs{i}", [128, W4], F32, st2) for i in range(2)]
    scl = S.sbuf("scl", [128, W2], F32, st2)

    def h_tile(ti, hi):
        for hf in range(2):
            S.op("pe", lambda e, hf=hf: e.matmul(C.banks[hf][:, :], h2[0:64, ti * 128:(ti + 1) * 128], w3t[0:64, hf * 512:(hf + 1) * 512], start=True, stop=True),
                 reads=["h2", "w3t"], writes=[f"bank{hf}"])
            S.op("dve", lambda e, hf=hf: e.tensor_tensor(out=hts[hi][:, hf * 512:(hf + 1) * 512], in0=C.banks[hf][:, :], in1=b3bc[:, hf * 512:(hf + 1) * 512], op=ALU.add),
                 reads=[f"bank{hf}", "b3bc"], writes=[f"hts{hi}"])
        S.op("pool", lambda e: e.tensor_tensor(out=hts[hi][:].rearrange("p (a c) -> p a c", a=4), in0=hts[hi][:].rearrange("p (a c) -> p a c", a=4),
                                              in1=dec[:, ti, :].unsqueeze(1).to_broadcast([128, 4, NCH]), op=ALU.mult),
             reads=[f"hts{hi}", "dec"], writes=[f"hts{hi}"])
    for ti in range(32):
        hi = ti % 2
        h_tile(ti, hi)
        S.op("act", lambda e, hi=hi: e.activation(out=ab[hi][:], in_=hts[hi][:], func=AF.Abs), reads=[f"hts{hi}"], writes=[f"habs{hi}"])
        for hf in range(2):
            S.op("pe", lambda e, hf=hf, hi=hi: e.matmul(C.banks[2 + hf][:, :], C.ones_f[:], ab[hi][:, hf * 512:(hf + 1) * 512], start=(ti == 0), stop=(ti == 31)),
                 reads=[f"habs{hi}", "ones_f"], writes=[f"bank{2 + hf}"])
    S.op("dve", lambda e: e.tensor_copy(out=scl[:], in_=C.banks[2][:, :]), reads=["bank2"], writes=["scl"])
    S.op("dve", lambda e: e.tensor_tensor(out=scl[:], in0=scl[:], in1=C.banks[3][:, :], op=ALU.add), reads=["bank3", "scl"], writes=["scl"])
    S.op("dve", lambda e: e.tensor_scalar(out=scl[:], in0=scl[:], scalar1=EPS, scalar2=None, op0=ALU.add), reads=["scl"], writes=["scl"])
    S.op("dve", lambda e: e.reciprocal(out=scl[:], in_=scl[:]), reads=["scl"], writes=["scl"])
    for ti in range(32):
        hi = ti % 2
        h_tile(ti, hi)
        S.op("dve", lambda e, hi=hi: e.tensor_tensor(out=hts[hi][:].rearrange("p (a c) -> p a c", a=2), in0=hts[hi][:].rearrange("p (a c) -> p a c", a=2),
                                                  in1=scl[:].unsqueeze(1).to_broadcast([128, 2, W2]), op=ALU.mult),
             reads=[f"hts{hi}", "scl"], writes=[f"hts{hi}"])
        if ti == 0:
            S.op("dve", lambda e, hi=hi: e.tensor_scalar(out=hts[hi][:, W2:W4], in0=hts[hi][:, W2:W4], scalar1=cv[:, 1:2], scalar2=None, op0=ALU.mult),
                 reads=[f"hts{hi}", "cv"], writes=[f"hts{hi}"])
        S.op("pool", lambda e, ti=ti, hi=hi: e.tensor_tensor(out=ksum[:, ti, :], in0=hts[hi][:, 0:W2], in1=hts[hi][:, W2:W4], op=ALU.add), reads=[f"hts{hi}"], writes=["ksum"])
        S.op("dve", lambda e, ti=ti, hi=hi: e.tensor_tensor(out=kdif[:, ti, :], in0=hts[hi][:, 0:W2], in1=hts[hi][:, W2:W4], op=ALU.subtract), reads=[f"hts{hi}"], writes=["kdif"])
    S.barrier()
    st2.close()
    fcs = [S.sbuf(f"fcb{i}", [128, 32, 128], BF16, st) for i in range(2)]
    fss = [S.sbuf(f"fsb{i}", [128, 32, 128], BF16, st) for i in range(2)]
    kt = [S.sbuf(f"kt{i}", [128, 3, W2], F32, st) for i in range(2)]
    for j in range(32):
        bi = j % 2
        S.dma("sp", lambda e, j=j, bi=bi: e.dma_start(out=fcs[bi][:], in_=FcB[j]), writes=[f"fcb{bi}"])
        S.dma("sp", lambda e, j=j, bi=bi: e.dma_start(out=fss[bi][:], in_=FsB[j]), writes=[f"fsb{bi}"])
        pb = 3 + 2 * bi
        for ti in range(32):
            S.op("pe", lambda e, ti=ti, bi=bi, pb=pb: e.matmul(C.banks[pb][:, :], fcs[bi][:, ti, :], ksum[:, ti, :], start=(ti == 0), stop=(ti == 31)),
                 reads=[f"fcb{bi}", "ksum"], writes=[f"bank{pb}"])
        for ti in range(32):
            S.op("pe", lambda e, ti=ti, bi=bi, pb=pb: e.matmul(C.banks[pb + 1][:, :], fss[bi][:, ti, :], kdif[:, ti, :], start=(ti == 0), stop=(ti == 31)),
                 reads=[f"fsb{bi}", "kdif"], writes=[f"bank{pb + 1}"])
        if j == 0:
            for ti in range(32):
                S.op("pe", lambda e, ti=ti, bi=bi: e.matmul(C.banks[7][:, :], fss[bi][:, ti, :], ksum[:, ti, :], start=(ti == 0), stop=(ti == 31)),
                     reads=[f"fsb{bi}", "ksum"], writes=["bank7"])
            S.op("dve", lambda e, bi=bi, pb=pb: e.tensor_scalar(out=kt[bi][:, 0, :], in0=C.banks[pb][:, :], scalar1=cv[:, 0:1], scalar2=None, op0=ALU.mult),
                 reads=[f"bank{pb}", "cv"], writes=[f"kt{bi}"])
            S.op("dve", lambda e, bi=bi, pb=pb: e.tensor_scalar(out=kt[bi][:, 1, :], in0=C.banks[pb + 1][:, :], scalar1=cv[:, 1:2], scalar2=None, op0=ALU.mult),
                 reads=[f"bank{pb + 1}", "cv"], writes=[f"kt{bi}"])
            S.op("dve", lambda e, bi=bi, pb=pb: e.tensor_scalar(out=kt[bi][:, 2, :], in0=C.banks[pb][:, :], scalar1=cv[:, 1:2], scalar2=None, op0=ALU.mult),
                 reads=[f"bank{pb}", "cv"], writes=[f"kt{bi}"])
            S.op("dve", lambda e, bi=bi: e.scalar_tensor_tensor(out=kt[bi][:, 2, :], in0=C.banks[7][:, :], scalar=cv[:, 2:3], in1=kt[bi][:, 2, :],
                                                               op0=ALU.mult, op1=ALU.add), reads=["bank7", "cv", f"kt{bi}"], writes=[f"kt{bi}"])
        else:
            S.op("act", lambda e, bi=bi, pb=pb: e.activation(out=kt[bi][:, 0, :], in_=C.banks[pb][:, :], func=AF.Copy), reads=[f"bank{pb}"], writes=[f"kt{bi}"])
            S.op("dve", lambda e, bi=bi, pb=pb: e.tensor_copy(out=kt[bi][:, 1, :], in_=C.banks[pb + 1][:, :]), reads=[f"bank{pb + 1}"], writes=[f"kt{bi}"])
            S.op("act", lambda e, bi=bi, pb=pb: e.activation(out=kt[bi][:, 2, :], in_=C.banks[pb][:, :], func=AF.Copy), reads=[f"bank{pb}"], writes=[f"kt{bi}"])
        S.dma("sp", lambda e, j=j, bi=bi: e.dma_start(out=ktab[j], in_=kt[bi][:]), reads=[f"kt{bi}"], writes=["ktblk"], is_out=True)
    S.barrier()
    st.close()
```
